# Optimizing a Trainium2 kernel written in Bass

```python
import jax
import jax.numpy as jnp
from jax import lax
import numpy as np

D_MODEL = 1024
BATCH = 8
SEQ = 2048
DEPTH = 1
DEC_BATCH = 128
DEC_SEQ = 4
PAST_LEN = 2048
PAGE_SIZE = 128

HEAD_DIM = 128
N_KV_HEADS = 4
DILATED_GROUPS = ((128, 1), (512, 4), (2048, 16))
N_ATTN_GROUPS = 3
N_Q_HEADS = N_ATTN_GROUPS * N_KV_HEADS
ATTN_WIDTH = N_KV_HEADS * HEAD_DIM
MAX_WINDOW = 2048
ROPE_THETA = 500000.0
ROT_DIM = HEAD_DIM // 4
CONV_CH = D_MODEL // 2
CONV_WIDTH = 31
N_MEM = 256
MEM_HEADS = 4
MEM_HEAD_DIM = 128
MEM_WIDTH = MEM_HEADS * MEM_HEAD_DIM
N_BRANCHES = 3
N_EXPERT_GROUPS = 4
EXPERTS_PER_GROUP = 8
N_EXPERTS = N_EXPERT_GROUPS * EXPERTS_PER_GROUP
EXPERT_FF = 512
TOP_K_IN_GROUP = 2
EPS = 1e-6
IN_SPLIT = (N_Q_HEADS * HEAD_DIM, ATTN_WIDTH, ATTN_WIDTH, 2 * CONV_CH, MEM_WIDTH, N_BRANCHES * D_MODEL)
IN_COLS = N_Q_HEADS * HEAD_DIM + 2 * ATTN_WIDTH + 2 * CONV_CH + MEM_WIDTH + N_BRANCHES * D_MODEL

kernel_name = 'hybrid_dilated_conformer_memory_hmoe_step'


def rms_norm(x, g):
    xf = x.astype(jnp.float32)
    y = xf * lax.rsqrt(jnp.mean(xf * xf, axis=-1, keepdims=True) + EPS)
    return (y * g.astype(jnp.float32)).astype(x.dtype)


def layer_norm(x, g, b):
    xf = x.astype(jnp.float32)
    mu = jnp.mean(xf, axis=-1, keepdims=True)
    xc = xf - mu
    y = xc * lax.rsqrt(jnp.mean(xc * xc, axis=-1, keepdims=True) + EPS)
    return (y * g.astype(jnp.float32) + b.astype(jnp.float32)).astype(x.dtype)


def rope_partial(x, pos):
    half = ROT_DIM // 2
    inv_freq = jnp.power(jnp.float32(ROPE_THETA), -jnp.arange(half, dtype=jnp.float32) * (2.0 / ROT_DIM))
    ang = pos.astype(jnp.float32)[:, None] * inv_freq[None, :]
    cos = jnp.cos(ang)[:, None, :]
    sin = jnp.sin(ang)[:, None, :]
    xr = x[..., :ROT_DIM].astype(jnp.float32)
    x1, x2 = xr[..., :half], xr[..., half:]
    rot = jnp.concatenate([x1 * cos - x2 * sin, x2 * cos + x1 * sin], axis=-1).astype(x.dtype)
    return jnp.concatenate([rot, x[..., ROT_DIM:]], axis=-1)


def split_combined(z):
    idx = [int(i) for i in np.cumsum(IN_SPLIT)[:-1]]
    return jnp.split(z, idx, axis=-1)


def attention_qkv(z_q, z_k, z_v, pos, lw):
    B, S = z_q.shape[:2]
    q = rope_partial(rms_norm(z_q.reshape(B, S, N_Q_HEADS, HEAD_DIM), lw['q_norm_g']), pos)
    k = rope_partial(rms_norm(z_k.reshape(B, S, N_KV_HEADS, HEAD_DIM), lw['k_norm_g']), pos)
    v = z_v.reshape(B, S, N_KV_HEADS, HEAD_DIM)
    return q, k, v


def dilated_band_attention(q, k, v, window, dilation):
    B, S, H, Dh = q.shape
    n = window // dilation
    L = S // dilation
    nb = -(-L // n)
    Lp = nb * n

    def to_blocks(t):
        t = t.reshape(B, L, dilation, H, Dh).transpose(0, 2, 1, 3, 4)
        t = jnp.pad(t, ((0, 0), (0, 0), (0, Lp - L), (0, 0), (0, 0)))
        return t.reshape(B, dilation, nb, n, H, Dh)

    def with_prev(t):
        prev = jnp.pad(t[:, :, :-1], ((0, 0), (0, 0), (1, 0), (0, 0), (0, 0), (0, 0)))
        return jnp.concatenate([prev, t], axis=3)

    qb = to_blocks(q)
    kc = with_prev(to_blocks(k))
    vc = with_prev(to_blocks(v))
    s = jnp.einsum('brnqhd,brnkhd->brnhqk', qb, kc).astype(jnp.float32) * (HEAD_DIM ** -0.5)
    iq = jnp.arange(n)[:, None]
    ck = jnp.arange(2 * n)[None, :]
    diff = n + iq - ck
    blk = jnp.arange(nb)[:, None, None]
    valid = (diff >= 0)[None] & (diff <= n)[None] & ((blk > 0) | (ck >= n)[None])
    s = jnp.where(valid[:, None, :, :], s, -jnp.inf)
    lse = jax.nn.logsumexp(s, axis=-1)
    p = jnp.exp(s - lse[..., None])
    o = jnp.einsum('brnhqk,brnkhd->brnqhd', p.astype(v.dtype), vc)
    o = o.reshape(B, dilation, Lp, H, Dh)[:, :, :L].transpose(0, 2, 1, 3, 4).reshape(B, S, H, Dh)
    lse = lse.transpose(0, 1, 2, 4, 3).reshape(B, dilation, Lp, H)[:, :, :L]
    lse = lse.transpose(0, 2, 1, 3).reshape(B, S, H)
    return o, lse


def dilated_gather_attention(q, k_all, v_all, window, dilation):
    T = q.shape[1]
    L = k_all.shape[1] - T
    n = window // dilation
    idx = L + jnp.arange(T)[:, None] - dilation * jnp.arange(n + 1)[None, :]
    valid = idx >= 0
    idx = jnp.maximum(idx, 0)
    kg = jnp.take(k_all, idx, axis=1)
    vg = jnp.take(v_all, idx, axis=1)
    s = jnp.einsum('bthd,btjhd->bhtj', q, kg).astype(jnp.float32) * (HEAD_DIM ** -0.5)
    s = jnp.where(valid[None, None], s, -jnp.inf)
    lse = jax.nn.logsumexp(s, axis=-1)
    p = jnp.exp(s - lse[..., None])
    o = jnp.einsum('bhtj,btjhd->bthd', p.astype(v_all.dtype), vg)
    return o, lse.transpose(0, 2, 1)


def combine_dilated(outs, lses):
    w = jax.nn.softmax(jnp.stack(lses, axis=0), axis=0)
    return jnp.einsum('gbsh,gbshd->bshd', w.astype(outs[0].dtype), jnp.stack(outs, axis=0))


def glu(z):
    a, b = jnp.split(z, 2, axis=-1)
    return a * jax.nn.sigmoid(b)


def conformer_tail(u_ext, lw):
    c = lax.conv_general_dilated(u_ext, lw['conv_w'][:, None, :], window_strides=(1,), padding='VALID',
                                 dimension_numbers=('NWC', 'WIO', 'NWC'),
                                 feature_group_count=CONV_CH) + lw['conv_b']
    c = jax.nn.silu(layer_norm(c, lw['conv_ln_g'], lw['conv_ln_b']))
    return c @ lw['w_conv_proj']


def memory_kv(mem, lw):
    B, M, _ = mem.shape
    kv = rms_norm(mem, lw['mem_norm_g']) @ lw['w_mem_kv']
    k, v = jnp.split(kv, 2, axis=-1)
    k = rms_norm(k.reshape(B, M, MEM_HEADS, MEM_HEAD_DIM), lw['mk_norm_g'])
    return k, v.reshape(B, M, MEM_HEADS, MEM_HEAD_DIM)


def memory_branch(z_mq, mem_k, mem_v, lw):
    B, S, _ = z_mq.shape
    mq = rms_norm(z_mq.reshape(B, S, MEM_HEADS, MEM_HEAD_DIM), lw['mq_norm_g'])
    s = jnp.einsum('bshd,bmhd->bhsm', mq, mem_k).astype(jnp.float32) * (MEM_HEAD_DIM ** -0.5)
    p = jax.nn.softmax(s, axis=-1)
    o = jnp.einsum('bhsm,bmhd->bshd', p.astype(mem_v.dtype), mem_v)
    return o.reshape(B, S, MEM_WIDTH) @ lw['w_mem_proj']


def merge_branches(z_gate, a, c, m, w_out):
    g_a, g_c, g_m = jnp.split(z_gate, N_BRANCHES, axis=-1)
    z = jax.nn.sigmoid(g_a) * a + jax.nn.sigmoid(g_c) * c + jax.nn.sigmoid(g_m) * m
    return z @ w_out


def mixer_prompt(h, mem, lw):
    B, S, _ = h.shape
    pos = jnp.arange(S, dtype=jnp.int32)
    z_q, z_k, z_v, z_conv, z_mq, z_gate = split_combined(h @ lw['w_in'])
    q, k, v = attention_qkv(z_q, z_k, z_v, pos, lw)
    outs, lses = [], []
    for g, (window, dilation) in enumerate(DILATED_GROUPS):
        o, l = dilated_band_attention(q[:, :, g * N_KV_HEADS:(g + 1) * N_KV_HEADS], k, v, window, dilation)
        outs.append(o)
        lses.append(l)
    a = combine_dilated(outs, lses).reshape(B, S, ATTN_WIDTH) @ lw['w_attn_proj']
    u = glu(z_conv)
    c = conformer_tail(jnp.pad(u, ((0, 0), (CONV_WIDTH - 1, 0), (0, 0))), lw)
    mem_k, mem_v = memory_kv(mem, lw)
    m = memory_branch(z_mq, mem_k, mem_v, lw)
    out = merge_branches(z_gate, a, c, m, lw['w_out'])
    lw_len = min(MAX_WINDOW, S)
    state = (k[:, S - lw_len:], v[:, S - lw_len:], u[:, S - (CONV_WIDTH - 1):], mem_k, mem_v)
    return out, state


def mixer_sample(h, cache_k, cache_v, state_conv, mem_k, mem_v, lw):
    B, T, _ = h.shape
    pos = PAST_LEN + jnp.arange(T, dtype=jnp.int32)
    z_q, z_k, z_v, z_conv, z_mq, z_gate = split_combined(h @ lw['w_in'])
    q, k, v = attention_qkv(z_q, z_k, z_v, pos, lw)
    k_all = jnp.concatenate([cache_k, k], axis=1)
    v_all = jnp.concatenate([cache_v, v], axis=1)
    outs, lses = [], []
    for g, (window, dilation) in enumerate(DILATED_GROUPS):
        o, l = dilated_gather_attention(q[:, :, g * N_KV_HEADS:(g + 1) * N_KV_HEADS], k_all, v_all, window, dilation)
        outs.append(o)
        lses.append(l)
    a = combine_dilated(outs, lses).reshape(B, T, ATTN_WIDTH) @ lw['w_attn_proj']
    u_all = jnp.concatenate([state_conv, glu(z_conv)], axis=1)
    c = conformer_tail(u_all, lw)
    m = memory_branch(z_mq, mem_k, mem_v, lw)
    out = merge_branches(z_gate, a, c, m, lw['w_out'])
    state = (k_all[:, T:], v_all[:, T:], u_all[:, T:])
    return out, state


def hier_moe(h, lw):
    N = h.shape[0]
    lg = (h @ lw['w_router_group']).astype(jnp.float32) + lw['b_router_group'].astype(jnp.float32)
    pg = jax.nn.softmax(lg, axis=-1)
    pg_top, g_idx = lax.top_k(pg, 1)
    le = jnp.einsum('nd,gde->nge', h, lw['w_router_expert']).astype(jnp.float32) + lw['b_router_expert'].astype(jnp.float32)
    sel = jnp.broadcast_to(g_idx[:, :, None], (N, 1, EXPERTS_PER_GROUP))
    le_sel = jnp.take_along_axis(le, sel, axis=1)[:, 0]
    pe = jax.nn.softmax(le_sel, axis=-1)
    pe_top, e_idx = lax.top_k(pe, TOP_K_IN_GROUP)
    w = pg_top * pe_top / jnp.sum(pe_top, axis=-1, keepdims=True)
    expert_ids = g_idx * EXPERTS_PER_GROUP + e_idx
    gates = jnp.einsum('nk,nke->ne', w, jax.nn.one_hot(expert_ids, N_EXPERTS, dtype=jnp.float32)).astype(h.dtype)
    y = jnp.zeros_like(h)
    for e in range(N_EXPERTS):
        hid = jax.nn.silu(h @ lw['w_expert_gate'][e]) * (h @ lw['w_expert_up'][e])
        y = y + gates[:, e:e + 1] * (hid @ lw['w_expert_down'][e])
    return y


def channel_mixer(x, lw):
    B, S, D = x.shape
    h = rms_norm(x, lw['norm_ffn_g']).reshape(B * S, D)
    return x + hier_moe(h, lw).reshape(B, S, D)


def setup_inputs(seed: int = 0) -> dict:
    key = jax.random.key(seed)
    ks = iter(jax.random.split(key, 40))

    def nrm(shape, scale=1.0):
        return scale * jax.random.normal(next(ks), shape, jnp.float32)

    def gain(shape):
        return 1.0 + 0.05 * nrm(shape)

    lw_past = min(MAX_WINDOW, PAST_LEN)
    return {
        'x_prompt': nrm((BATCH, SEQ, D_MODEL)),
        'x_sample': nrm((DEC_BATCH, DEC_SEQ, D_MODEL)),
        'mem_prompt': nrm((BATCH, N_MEM, D_MODEL)),
        'cache_k': nrm((DEPTH, DEC_BATCH, lw_past, N_KV_HEADS, HEAD_DIM)),
        'cache_v': nrm((DEPTH, DEC_BATCH, lw_past, N_KV_HEADS, HEAD_DIM)),
        'state_conv': nrm((DEPTH, DEC_BATCH, CONV_WIDTH - 1, CONV_CH)),
        'cache_mem_k': nrm((DEPTH, DEC_BATCH, N_MEM, MEM_HEADS, MEM_HEAD_DIM)),
        'cache_mem_v': nrm((DEPTH, DEC_BATCH, N_MEM, MEM_HEADS, MEM_HEAD_DIM)),
        'norm_mix_g': gain((DEPTH, D_MODEL)),
        'w_in': nrm((DEPTH, D_MODEL, IN_COLS), D_MODEL ** -0.5),
        'q_norm_g': gain((DEPTH, HEAD_DIM)),
        'k_norm_g': gain((DEPTH, HEAD_DIM)),
        'conv_w': nrm((DEPTH, CONV_WIDTH, CONV_CH), CONV_WIDTH ** -0.5),
        'conv_b': nrm((DEPTH, CONV_CH), 0.02),
        'conv_ln_g': gain((DEPTH, CONV_CH)),
        'conv_ln_b': nrm((DEPTH, CONV_CH), 0.02),
        'mem_norm_g': gain((DEPTH, D_MODEL)),
        'w_mem_kv': nrm((DEPTH, D_MODEL, 2 * MEM_WIDTH), D_MODEL ** -0.5),
        'mq_norm_g': gain((DEPTH, MEM_HEAD_DIM)),
        'mk_norm_g': gain((DEPTH, MEM_HEAD_DIM)),
        'w_attn_proj': nrm((DEPTH, ATTN_WIDTH, D_MODEL), ATTN_WIDTH ** -0.5),
        'w_conv_proj': nrm((DEPTH, CONV_CH, D_MODEL), CONV_CH ** -0.5),
        'w_mem_proj': nrm((DEPTH, MEM_WIDTH, D_MODEL), MEM_WIDTH ** -0.5),
        'w_out': nrm((DEPTH, D_MODEL, D_MODEL), D_MODEL ** -0.5),
        'norm_ffn_g': gain((DEPTH, D_MODEL)),
        'w_router_group': nrm((DEPTH, D_MODEL, N_EXPERT_GROUPS), D_MODEL ** -0.5),
        'b_router_group': nrm((DEPTH, N_EXPERT_GROUPS), 0.01),
        'w_router_expert': nrm((DEPTH, N_EXPERT_GROUPS, D_MODEL, EXPERTS_PER_GROUP), D_MODEL ** -0.5),
        'b_router_expert': nrm((DEPTH, N_EXPERT_GROUPS, EXPERTS_PER_GROUP), 0.01),
        'w_expert_gate': nrm((DEPTH, N_EXPERTS, D_MODEL, EXPERT_FF), D_MODEL ** -0.5),
        'w_expert_up': nrm((DEPTH, N_EXPERTS, D_MODEL, EXPERT_FF), D_MODEL ** -0.5),
        'w_expert_down': nrm((DEPTH, N_EXPERTS, EXPERT_FF, D_MODEL), EXPERT_FF ** -0.5),
    }


def reference(x_prompt, x_sample, mem_prompt, cache_k, cache_v, state_conv, cache_mem_k, cache_mem_v,
              norm_mix_g, w_in, q_norm_g, k_norm_g, conv_w, conv_b, conv_ln_g, conv_ln_b,
              mem_norm_g, w_mem_kv, mq_norm_g, mk_norm_g, w_attn_proj, w_conv_proj, w_mem_proj, w_out,
              norm_ffn_g, w_router_group, b_router_group, w_router_expert, b_router_expert,
              w_expert_gate, w_expert_up, w_expert_down):
    y_prompt, y_sample = x_prompt, x_sample
    kp, vp, cp, mkp, mvp, ksm, vsm, csm = [], [], [], [], [], [], [], []
    for layer in range(DEPTH):
        lw = {
            'norm_mix_g': norm_mix_g[layer], 'w_in': w_in[layer],
            'q_norm_g': q_norm_g[layer], 'k_norm_g': k_norm_g[layer],
            'conv_w': conv_w[layer], 'conv_b': conv_b[layer],
            'conv_ln_g': conv_ln_g[layer], 'conv_ln_b': conv_ln_b[layer],
            'mem_norm_g': mem_norm_g[layer], 'w_mem_kv': w_mem_kv[layer],
            'mq_norm_g': mq_norm_g[layer], 'mk_norm_g': mk_norm_g[layer],
            'w_attn_proj': w_attn_proj[layer], 'w_conv_proj': w_conv_proj[layer],
            'w_mem_proj': w_mem_proj[layer], 'w_out': w_out[layer],
            'norm_ffn_g': norm_ffn_g[layer],
            'w_router_group': w_router_group[layer], 'b_router_group': b_router_group[layer],
            'w_router_expert': w_router_expert[layer], 'b_router_expert': b_router_expert[layer],
            'w_expert_gate': w_expert_gate[layer], 'w_expert_up': w_expert_up[layer],
            'w_expert_down': w_expert_down[layer],
        }
        mix_p, st_p = mixer_prompt(rms_norm(y_prompt, lw['norm_mix_g']), mem_prompt, lw)
        y_prompt = channel_mixer(y_prompt + mix_p, lw)
        mix_s, st_s = mixer_sample(rms_norm(y_sample, lw['norm_mix_g']), cache_k[layer], cache_v[layer],
                                   state_conv[layer], cache_mem_k[layer], cache_mem_v[layer], lw)
        y_sample = channel_mixer(y_sample + mix_s, lw)
        kp.append(st_p[0]); vp.append(st_p[1]); cp.append(st_p[2]); mkp.append(st_p[3]); mvp.append(st_p[4])
        ksm.append(st_s[0]); vsm.append(st_s[1]); csm.append(st_s[2])
    k_win_prompt = jnp.stack(kp, axis=0)
    v_win_prompt = jnp.stack(vp, axis=0)
    conv_prompt = jnp.stack(cp, axis=0)
    mem_k_prompt = jnp.stack(mkp, axis=0)
    mem_v_prompt = jnp.stack(mvp, axis=0)
    k_win_sample = jnp.stack(ksm, axis=0)
    v_win_sample = jnp.stack(vsm, axis=0)
    conv_sample = jnp.stack(csm, axis=0)
    return (y_prompt, y_sample, k_win_prompt, v_win_prompt, conv_prompt, mem_k_prompt, mem_v_prompt, k_win_sample, v_win_sample, conv_sample)
```

```python
import os
import numpy as np
import concourse.bass as bass
import concourse.mybir as mybir
from concourse.bass_utils import run_bass_kernel_spmd
from contextlib import ExitStack

F32 = mybir.dt.float32
BF16 = mybir.dt.bfloat16
ALU = mybir.AluOpType
AF = mybir.ActivationFunctionType
AX = mybir.AxisListType

NCORES = 8
S = 2048
DM = 1024
NSB = 16
NS = 64
NT = S + NS
EPS = 1e-6
NEG = -30000.0
SQ128 = float(np.sqrt(128.0))
ISQ128 = float(1.0 / np.sqrt(128.0))
TT = [(i * 128, 128) for i in range(16)] + [(S, NS)]
TG = [(i * 512, 512) for i in range(4)] + [(S, NS)]
NEXP = 32
STOP = int(os.environ.get("MK_STOP", "99"))


def ss_(start, count, step):
    return slice(start, start + (count - 1) * step + 1, step)


class Prog:
    ENG = ('pe', 'act', 'dve', 'pool', 'sp')

    def __init__(self):
        self.engs = {e: dict(ops=[], n=0, waited={}) for e in self.ENG}
        self.dsem = {}
        self.lastw = {}
        self.readers = {}

    def _deps(self, reads, writes):
        toks = {}

        def add(tok):
            if tok is None:
                return
            s, v = tok
            if s.startswith('d:') and not s.startswith('d:bg'):
                v = 16 * self.dsem[s[2:]]['n']
            if toks.get(s, 0) < v:
                toks[s] = v
        for k in reads:
            add(self.lastw.get(k))
        for k in writes:
            add(self.lastw.get(k))
            for s, v in self.readers.get(k, {}).items():
                add((s, v))
        return toks

    def _commit(self, tok, reads, writes):
        for k in reads:
            d = self.readers.setdefault(k, {})
            if d.get(tok[0], 0) < tok[1]:
                d[tok[0]] = tok[1]
        for k in writes:
            self.lastw[k] = tok
            self.readers[k] = {}

    def _waits(self, eng, toks):
        E = self.engs[eng]
        waits = []
        for s, v in toks.items():
            if eng == 'pe' and s == 'e:pe':
                continue
            if E['waited'].get(s, 0) >= v:
                continue
            E['waited'][s] = v
            waits.append((s, v))
        return waits

    _cap = None

    def begin_capture(self):
        self._cap = []

    def end_capture(self):
        c, self._cap = self._cap, None
        return c

    def replay(self, lists):
        idx = [0] * len(lists)
        while any(idx[i] < len(L) for i, L in enumerate(lists)):
            for i, L in enumerate(lists):
                if idx[i] < len(L):
                    kind, args = L[idx[i]]
                    idx[i] += 1
                    (self.op if kind == 'op' else self.dma)(*args)

    def op(self, eng, fn, reads=(), writes=()):
        if self._cap is not None:
            self._cap.append(('op', (eng, fn, list(reads), list(writes))))
            return
        E = self.engs[eng]
        waits = self._waits(eng, self._deps(reads, writes))
        E['n'] += 1
        tok = ('e:' + eng, E['n'])
        E['ops'].append((waits, fn, tok))
        self._commit(tok, reads, writes)

    def dma(self, queue, semname, out, in_, reads=(), writes=()):
        if self._cap is not None:
            self._cap.append(('dma', (queue, semname, out, in_, list(reads), list(writes))))
            return
        E = self.engs[queue]
        waits = self._waits(queue, self._deps(reads, writes))
        D = self.dsem.setdefault(semname, dict(n=0))
        D['n'] += 1
        tok = ('d:' + semname, 16 * D['n'])
        E['ops'].append((waits, lambda e, o=out, i=in_: e.dma_start(out=o, in_=i), tok))
        self._commit(tok, reads, writes)

    def barrier(self):
        toks = {('e:' + e): self.engs[e]['n'] for e in self.ENG if self.engs[e]['n'] > 0}
        for name, D in self.dsem.items():
            if name.startswith('bg'):
                continue
            toks['d:' + name] = 16 * D['n']
        for e in self.ENG:
            waits = self._waits(e, dict(toks))
            self.engs[e]['ops'].append((waits, None, None))
        keep_w = {k: v for k, v in self.lastw.items() if v[0].startswith('d:bg')}
        self.lastw = keep_w
        self.readers = {}

    def final_wait_all_dma(self, eng='sp'):
        E = self.engs[eng]
        waits = []
        for name, D in self.dsem.items():
            s = 'd:' + name
            v = 16 * D['n']
            if E['waited'].get(s, 0) < v:
                E['waited'][s] = v
                waits.append((s, v))
        E['ops'].append((waits, None, None))

    def emit(self, nc, stack):
        sems = {}
        for e in self.ENG:
            sems['e:' + e] = stack.enter_context(nc.semaphore('s_' + e))
        for name in self.dsem:
            sems['d:' + name] = stack.enter_context(nc.semaphore('d_' + name))
        block = stack.enter_context(nc.Block())

        def run(engname):
            def f(eng):
                for waits, fn, tok in self.engs[engname]['ops']:
                    for s, v in waits:
                        eng.wait_ge(sems[s], v)
                    if fn is None:
                        continue
                    ins = fn(eng)
                    ins.then_inc(sems[tok[0]], 16 if tok[0].startswith('d:') else 1)
            return f
        block.tensor(run('pe'))
        block.scalar(run('act'))
        block.vector(run('dve'))
        block.gpsimd(run('pool'))
        block.sync(run('sp'))


class Arena:
    def __init__(self, t, nelem_bf16):
        self.t = t
        self.n = nelem_bf16
        self.off = 0

    def alloc(self, shape, dt):
        size = 2 if dt == BF16 else 4
        ne = int(np.prod(shape))
        nb = (ne * size + 31) // 32 * 32
        assert self.off + nb // 2 <= self.n, ("arena overflow", self.off * 2, nb, self.n * 2)
        ap = self.t[:, self.off:self.off + ne * size // 2]
        self.off += nb // 2
        if dt != BF16:
            ap = ap.bitcast(dt)
        if len(shape) == 2:
            ap = ap.rearrange("p (a b) -> p a b", a=shape[0], b=shape[1])
        elif len(shape) == 3:
            ap = ap.rearrange("p (a b c) -> p a b c", a=shape[0], b=shape[1], c=shape[2])
        elif len(shape) == 4:
            ap = ap.rearrange("p (a b c d) -> p a b c d", a=shape[0], b=shape[1], c=shape[2], d=shape[3])
        return ap


def build_nc():
    nc = bass.Bass("TRN2", target_bir_lowering=False)

    def din(name, shape):
        return nc.dram_tensor(name, list(shape), F32, kind="ExternalInput").ap()

    def dout(name, shape):
        return nc.dram_tensor(name, list(shape), F32, kind="ExternalOutput").ap()

    xin = din("xin", [NT, DM])
    mem = din("mem", [256, DM])
    cache_k = din("cache_k", [NSB, 2048, 4, 128])
    cache_v = din("cache_v", [NSB, 2048, 4, 128])
    state_conv = din("state_conv", [NSB, 30, 512])
    cmk = din("cmk", [NSB, 256, 4, 128])
    cmv = din("cmv", [NSB, 256, 4, 128])
    norm_mix_g = din("norm_mix_g", [DM])
    w_in = din("w_in", [DM, 7168])
    q_norm_g = din("q_norm_g", [128])
    k_norm_g = din("k_norm_g", [128])
    conv_wT = din("conv_wT", [512, 31])
    conv_b = din("conv_b", [128, 4])
    conv_ln_g = din("conv_ln_g", [128, 4])
    conv_ln_b = din("conv_ln_b", [128, 4])
    mem_norm_g = din("mem_norm_g", [DM])
    w_mem_kv = din("w_mem_kv", [DM, 1024])
    mq_norm_g = din("mq_norm_g", [128])
    mk_norm_g = din("mk_norm_g", [128])
    w_attn_proj = din("w_attn_proj", [512, DM])
    w_conv_proj = din("w_conv_proj", [512, DM])
    w_mem_proj = din("w_mem_proj", [512, DM])
    w_out = din("w_out", [DM, DM])
    norm_ffn_g = din("norm_ffn_g", [DM])
    w_router = din("w_router", [DM, 36])
    b_router = din("b_router", [36])
    w_eg = din("w_eg", [NEXP, DM, 512])
    w_eu = din("w_eu", [NEXP, DM, 512])
    w_ed = din("w_ed", [NEXP, 512, DM])
    c_ident = din("c_ident", [128, 128])
    c_cs = din("c_cs", [17 * 128, 32])
    c_mask = din("c_mask", [128, 256])
    c_smask = din("c_smask", [128, 7 * 48])
    c_nmask = din("c_nmask", [64, 192])

    y = dout("y", [NT, DM])
    kwp = dout("kwp", [S, 4, 128])
    vwp = dout("vwp", [S, 4, 128])
    convp = dout("convp", [30, 512])
    mkp = dout("mkp", [256, 4, 128])
    mvp = dout("mvp", [256, 4, 128])
    kws = dout("kws", [NSB, 2048, 4, 128])
    vws = dout("vws", [NSB, 2048, 4, 128])
    convs = dout("convs", [NSB, 30, 512])
    DBG = bool(int(os.environ.get("MK_DBG", "0")))
    x1s = nc.dram_tensor("x1s", [NT, DM], F32, kind=("ExternalOutput" if DBG else "Internal")).ap()
    dbg = dout("dbg", [3, 128, 4 * NT]) if DBG else None

    P = Prog()
    with ExitStack() as st:
        ARENA_BYTES = 204 * 1024
        arena_t = st.enter_context(nc.sbuf_tensor("arena", [128, ARENA_BYTES // 2], BF16))
        A = Arena(arena_t, ARENA_BYTES // 2)
        psum = st.enter_context(nc.psum_tensor("psum", [128, 4096], F32))

        def bank(i, n=1):
            return psum[:, i * 512:(i + n) * 512]

        def pk(i):
            return ('pb', i)

        ident_f = A.alloc([128], F32)
        ident_b = A.alloc([128], BF16)
        ones_b = A.alloc([128], BF16)
        ones_f = A.alloc([128], F32)
        cs = A.alloc([17, 32], F32)
        maskf = A.alloc([256], F32)
        maskb = A.alloc([256], BF16)
        smaskf = A.alloc([7 * 48], F32)
        smaskb = A.alloc([7 * 48], BF16)
        nmaskf = A.alloc([192], F32)
        nmaskb = A.alloc([192], BF16)
        g_mix = A.alloc([8], F32)
        g_ffn = A.alloc([8], F32)
        g_mem = A.alloc([8], F32)
        gqk = A.alloc([4, 128], F32)
        gmq = A.alloc([4, 128], F32)
        gmk = A.alloc([4, 128], F32)
        convw = A.alloc([4, 31], F32)
        convb = A.alloc([4], F32)
        lng = A.alloc([4], F32)
        lnb = A.alloc([4], F32)
        neghalf = A.alloc([512], F32)
        wr = A.alloc([8, 36], F32)
        br = A.alloc([36], F32)
        zero_c = A.alloc([1], F32)

        P.dma('sp', 'c0', ident_f, c_ident, writes=['ident_f'])
        P.dma('sp', 'c0', cs, c_cs.rearrange("(t p) c -> p t c", p=128), writes=['cs'])
        P.dma('sp', 'c0', maskf, c_mask, writes=['maskf'])
        P.dma('sp', 'c0', smaskf, c_smask, writes=['smaskf'])
        P.dma('sp', 'c0', nmaskf[0:64, :], c_nmask, writes=['nmaskf'])
        P.dma('sp', 'c0', g_mix, norm_mix_g.rearrange("(p k) -> p k", k=8), writes=['g_mix'])
        P.dma('sp', 'c0', g_ffn, norm_ffn_g.rearrange("(p k) -> p k", k=8), writes=['g_ffn'])
        P.dma('sp', 'c0', g_mem, mem_norm_g.rearrange("(p k) -> p k", k=8), writes=['g_mem'])
        for i in range(4):
            P.dma('sp', 'c0', gqk[:, i, :], (q_norm_g if i < 3 else k_norm_g).partition_broadcast(128), writes=['gqk'])
            P.dma('sp', 'c0', gmq[:, i, :], mq_norm_g.partition_broadcast(128), writes=['gmq'])
            P.dma('sp', 'c0', gmk[:, i, :], mk_norm_g.partition_broadcast(128), writes=['gmk'])
        P.dma('sp', 'c0', convw, conv_wT.rearrange("(j p) i -> p j i", p=128), writes=['convw'])
        P.dma('sp', 'c0', convb, conv_b, writes=['convb'])
        P.dma('sp', 'c0', lng, conv_ln_g, writes=['lng'])
        P.dma('sp', 'c0', lnb, conv_ln_b, writes=['lnb'])
        P.dma('sp', 'c0', wr, w_router.rearrange("(p k) f -> p k f", k=8), writes=['wr'])
        P.dma('sp', 'c0', br, b_router.partition_broadcast(128), writes=['br'])
        P.op('dve', lambda e: e.tensor_copy(out=ident_b, in_=ident_f), ['ident_f'], ['ident_b'])
        P.op('dve', lambda e: e.tensor_copy(out=maskb, in_=maskf), ['maskf'], ['maskb'])
        P.op('dve', lambda e: e.tensor_copy(out=smaskb, in_=smaskf), ['smaskf'], ['smaskb'])
        P.op('dve', lambda e: e.tensor_copy(out=nmaskb[0:64, :], in_=nmaskf[0:64, :]), ['nmaskf'], ['nmaskb'])
        P.op('pool', lambda e: e.memset(ones_b, 1.0), [], ['ones_b'])
        P.op('pool', lambda e: e.memset(ones_f, 1.0), [], ['ones_f'])
        P.op('pool', lambda e: e.memset(neghalf, -0.5), [], ['neghalf'])
        P.op('pool', lambda e: e.memset(zero_c, 0.0), [], ['zero_c'])

        P.barrier()
        def issue_bg():
            NB_ = 2044 * 512
            for b in range(NSB):
                for (dst_, src_, nm_) in ((kws, cache_k, 'bgk'), (vws, cache_v, 'bgv')):
                    P.dma('act', nm_, dst_[b].rearrange("t h d -> (t h d)")[0:NB_].rearrange("(p n) -> p n", p=128),
                          src_[b].rearrange("t h d -> (t h d)")[2048:2048 + NB_].rearrange("(p n) -> p n", p=128), writes=[(nm_, b)])
            P.dma('act', 'bgc', convs[:, 0:26, :], state_conv[:, 4:30, :], writes=['convs_bg'])
        if STOP < 2:
            issue_bg()

        hT = A.alloc([8, NT], BF16)

        def mm(out, pairs, reads, writes):
            def f(e):
                ins = None
                n = len(pairs)
                for i, (l, r) in enumerate(pairs):
                    ins = e.matmul(out=out, lhsT=l, rhs=r, start=(i == 0), stop=(i == n - 1))
                return ins
            P.op('pe', f, reads, writes)

        def rms_rstd(src, rows, width, sqt, ss, tag, src_keys):
            P.op('pool', lambda e: e.memset(ss[0:rows, 0:1], 0.0), [], [tag + 'ss'])
            P.op('act', lambda e: e.activation(out=sqt[0:rows, 0:width], in_=src, func=AF.Square,
                                               accum_out=ss[0:rows, 0:1]),
                 src_keys + [tag + 'ss'], [tag + 'sq', tag + 'ss'])
            P.op('dve', lambda e: e.tensor_scalar(out=ss[0:rows, 0:1], in0=ss[0:rows, 0:1], scalar1=width * EPS,
                                                  scalar2=None, op0=ALU.add), [tag + 'ss'], [tag + 'ss'])
            P.op('pool', lambda e: e.tensor_tensor(out=ss[0:rows, 0:1], in0=ss[0:rows, 0:1], in1=neghalf[0:rows, 0:1],
                                                   op=ALU.pow), [tag + 'ss', 'neghalf'], [tag + 'ss'])

        def norm_transpose(xt, rows, gain, gain_key, dst_b, dst_keys, tag, xkeys, sqt, ss, xn, dst_f=None, dst_f_keys=()):
            rms_rstd(xt, rows, DM, sqt, ss, tag, xkeys)
            P.op('dve', lambda e: e.tensor_scalar(out=xn[0:rows, :], in0=xt, scalar1=ss[0:rows, 0:1], scalar2=32.0,
                                                  op0=ALU.mult, op1=ALU.mult), xkeys + [tag + 'ss'], [tag + 'xn'])
            xv = xn[0:rows, :].rearrange("t (p k) -> t k p", k=8)
            for half in range(2):
                bi = 6 + half

                def tr(e, half=half, bi=bi):
                    ins = None
                    for kk in range(4):
                        k = half * 4 + kk
                        ins = e.transpose(out=bank(bi)[:, kk * 128:kk * 128 + rows], in_=xv[:, k, :],
                                          identity=ident_f[0:rows, 0:rows])
                    return ins
                P.op('pe', tr, [tag + 'xn', 'ident_f'], [pk(bi)])
                pv = bank(bi).rearrange("p (k t) -> p k t", k=4)[:, :, 0:rows]
                gv = gain[:, half * 4:half * 4 + 4].unsqueeze(2).to_broadcast([128, 4, rows])
                P.op('dve', lambda e, pv=pv, gv=gv, half=half: e.tensor_tensor(
                    out=dst_b[:, half * 4:half * 4 + 4, :], in0=pv, in1=gv, op=ALU.mult),
                    [pk(bi), gain_key], list(dst_keys))
                if dst_f is not None:
                    P.op('act', lambda e, half=half, bi=bi: [e.activation(
                        out=dst_f[:, half * 4 + kk, 0:rows], in_=bank(bi)[:, kk * 128:kk * 128 + rows], func=AF.Copy,
                        scale=gain[:, half * 4 + kk:half * 4 + kk + 1]) for kk in range(4)][-1],
                        [pk(bi), gain_key], list(dst_f_keys))

        def headnorm(zps, zkeys, rows, nh, gain, gain_key, out_f, out_key, tag, sqt, ss, rope_cs=None, tmp=None):
            zv = zps.rearrange("t (h d) -> t h d", h=nh)
            P.op('act', lambda e: e.activation(out=sqt[0:rows, 0:nh * 128], in_=zps, func=AF.Square),
                 zkeys, [tag + 'sq'])
            P.op('dve', lambda e: e.reduce_sum(out=ss[0:rows, 0:nh],
                                               in_=sqt[0:rows, 0:nh * 128].rearrange("t (h d) -> t h d", h=nh),
                                               axis=AX.X), [tag + 'sq'], [tag + 'ss'])
            P.op('dve', lambda e: e.tensor_scalar(out=ss[0:rows, 0:nh], in0=ss[0:rows, 0:nh], scalar1=128 * EPS,
                                                  scalar2=None, op0=ALU.add), [tag + 'ss'], [tag + 'ss'])
            P.op('pool', lambda e: e.tensor_tensor(out=ss[0:rows, 0:nh], in0=ss[0:rows, 0:nh], in1=neghalf[0:rows, 0:nh],
                                                   op=ALU.pow), [tag + 'ss', 'neghalf'], [tag + 'ss'])
            rb = ss[0:rows, 0:nh].unsqueeze(2).to_broadcast([rows, nh, 128])
            P.op('dve', lambda e: e.scalar_tensor_tensor(out=out_f, in0=zv, scalar=SQ128, in1=rb,
                                                         op0=ALU.mult, op1=ALU.mult), zkeys + [tag + 'ss'], [out_key])
            P.op('dve', lambda e: e.tensor_tensor(out=out_f, in0=out_f, in1=gain[0:rows], op=ALU.mult),
                 [out_key, gain_key], [out_key])
            if rope_cs is not None:
                cosb = rope_cs[:, 0:16].unsqueeze(1).to_broadcast([rows, nh, 16])
                sinb = rope_cs[:, 16:32].unsqueeze(1).to_broadcast([rows, nh, 16])
                x1 = out_f[:, :, 0:16]
                x2 = out_f[:, :, 16:32]
                t = [tmp[0:rows, i * nh * 16:(i + 1) * nh * 16].rearrange("t (h d) -> t h d", h=nh) for i in range(4)]
                tk = tag + 'rt'
                P.op('dve', lambda e: e.tensor_tensor(out=t[0], in0=x1, in1=cosb, op=ALU.mult), [out_key, 'cs'], [tk + '0'])
                P.op('dve', lambda e: e.tensor_tensor(out=t[1], in0=x2, in1=sinb, op=ALU.mult), [out_key, 'cs'], [tk + '1'])
                P.op('dve', lambda e: e.tensor_tensor(out=t[2], in0=x2, in1=cosb, op=ALU.mult), [out_key, 'cs'], [tk + '2'])
                P.op('dve', lambda e: e.tensor_tensor(out=t[3], in0=x1, in1=sinb, op=ALU.mult), [out_key, 'cs'], [tk + '3'])
                P.op('dve', lambda e: e.tensor_tensor(out=x1, in0=t[0], in1=t[1], op=ALU.subtract),
                     [tk + '0', tk + '1'], [out_key])
                P.op('dve', lambda e: e.tensor_tensor(out=x2, in0=t[2], in1=t[3], op=ALU.add),
                     [tk + '2', tk + '3'], [out_key])

        mark0 = A.off
        xt2 = [A.alloc([DM], F32) for _ in range(2)]
        sqt = A.alloc([DM], F32)
        xn = A.alloc([DM], F32)
        ss = A.alloc([8], F32)
        for ti, (t0, rows) in enumerate(TT):
            sl = ti % 2
            P.dma('sp', 'x%d' % sl, xt2[sl][0:rows, :], xin[t0:t0 + rows, :], writes=[('xt', sl)])
            norm_transpose(xt2[sl][0:rows, :], rows, g_mix, 'g_mix', hT[:, :, t0:t0 + rows], [('hT', ti)],
                           'n1', [('xt', sl)], sqt, ss, xn)
        P.barrier()
        A.off = mark0

        cT = A.alloc([4, NT], BF16)
        mark = A.off
        if STOP >= 2:
            wconv = A.alloc([8, 1024], BF16)
            uP = A.alloc([4, 30 + S], F32)
            uS = A.alloc([4, NSB, 34], F32)
            cP = A.alloc([4, NT], F32)
            sig = [A.alloc([512], F32) for _ in range(2)]
            P.dma('pool', 'w0', wconv, w_in[:, 2560:3584].rearrange("(p k) f -> p k f", k=8), writes=['wconv'])
            P.op('pool', lambda e: e.memset(uP[:, :, 0:30], 0.0), [], ['uPpad'])
            stc = A.alloc([4, 512], F32)
            for bt in range(4):
                P.dma('sp', 'stc%d' % bt, stc[0:120, bt, :], state_conv[4 * bt:4 * bt + 4].rearrange("b i c -> (b i) c"),
                      writes=[('stc', bt)])
            for bt in range(4):
                P.op('pe', lambda e, bt=bt: [e.transpose(out=bank(0)[:, j * 128:j * 128 + 120],
                                                         in_=stc[0:120, bt, j * 128:(j + 1) * 128],
                                                         identity=ident_f[0:120, 0:120]) for j in range(4)][-1],
                     [('stc', bt), 'ident_f'], [pk(0)])
                P.op('dve', lambda e, bt=bt: e.tensor_copy(
                    out=uS[:, :, 4 * bt:4 * bt + 4, 0:30],
                    in_=bank(0).rearrange("p (j x) -> p j x", j=4)[:, :, 0:120].rearrange("p j (b i) -> p j b i", b=4)),
                    [pk(0)], [('uS', bt)])
            cnt = 0
            for gi, (t0, n) in enumerate(TG):
                for j in range(4):
                    ba, bb = 2 + (cnt % 2) * 2, 3 + (cnt % 2) * 2
                    sgi = cnt % 2
                    sg = sig[sgi]
                    cnt += 1
                    rk = ['wconv'] + [('hT', ti) for ti in range(17)]
                    mm(bank(ba)[:, 0:n], [(wconv[:, k, j * 128:(j + 1) * 128], hT[:, k, t0:t0 + n]) for k in range(8)],
                       rk, [pk(ba)])
                    mm(bank(bb)[:, 0:n], [(wconv[:, k, 512 + j * 128:512 + (j + 1) * 128], hT[:, k, t0:t0 + n]) for k in range(8)],
                       rk, [pk(bb)])
                    P.op('act', lambda e, bb=bb, n=n, sg=sg: e.activation(out=sg[:, 0:n], in_=bank(bb)[:, 0:n], func=AF.Sigmoid),
                         [pk(bb)], [('sig', sgi)])
                    if gi < 4:
                        dst = uP[:, j, 30 + t0:30 + t0 + n]
                        P.op('dve', lambda e, ba=ba, n=n, sg=sg, dst=dst: e.tensor_tensor(out=dst, in0=bank(ba)[:, 0:n], in1=sg[:, 0:n], op=ALU.mult),
                             [pk(ba), ('sig', sgi)], [('uP', j)])
                    else:
                        dst = uS[:, j, :, 30:34]
                        P.op('dve', lambda e, ba=ba, sg=sg, dst=dst: e.tensor_tensor(
                            out=dst, in0=bank(ba)[:, 0:NS].rearrange("p (b t) -> p b t", t=4),
                            in1=sg[:, 0:NS].rearrange("p (b t) -> p b t", t=4), op=ALU.mult),
                            [pk(ba), ('sig', sgi)], [('uSn', j)])
            issue_bg()
            cst = A.alloc([512], F32)
            P.op('pe', lambda e: [e.transpose(out=bank(0)[0:30, j * 128:(j + 1) * 128], in_=uP[:, j, S:S + 30],
                                              identity=ident_f) for j in range(4)][-1],
                 [('uP', j) for j in range(4)] + ['ident_f'], [pk(0)])
            P.op('dve', lambda e: e.tensor_copy(out=cst[0:30, :], in_=bank(0)[0:30, :]), [pk(0)], ['cst'])
            P.dma('sp', 'o_cp', convp, cst[0:30, :], reads=['cst'], writes=['convp'])
            unew = A.alloc([4, NS], F32)
            P.op('dve', lambda e: e.tensor_copy(out=unew.rearrange("p j (b t) -> p j b t", t=4), in_=uS[:, :, :, 30:34]),
                 [('uSn', j) for j in range(4)], ['unew'])
            cst2 = A.alloc([512], F32)
            P.op('pe', lambda e: [e.transpose(out=bank(1)[0:NS, j * 128:(j + 1) * 128], in_=unew[:, j, :],
                                              identity=ident_f) for j in range(4)][-1], ['unew', 'ident_f'], [pk(1)])
            P.op('dve', lambda e: e.tensor_copy(out=cst2[0:NS, :], in_=bank(1)[0:NS, :]), [pk(1)], ['cst2'])
            for b in range(NSB):
                P.dma('sp', 'o_cs', convs[b, 26:30, :], cst2[4 * b:4 * b + 4, :], reads=['cst2'], writes=[('convs', b)])
            for j in range(4):
                eng = 'dve'
                for (src, dst, rk, wk) in (
                        (lambda i, j=j: uP[:, j, i:i + S], cP[:, j, 0:S], [('uP', j), 'uPpad'], ('cP', j)),
                        (lambda i, j=j: uS[:, j, :, i:i + 4], cP[:, j, S:NT].rearrange("p (b t) -> p b t", t=4),
                         [('uSn', j)] + [('uS', bt) for bt in range(4)], ('cS', j))):
                    P.op(eng, lambda e, src=src, dst=dst, j=j: e.tensor_scalar(
                        out=dst, in0=src(0), scalar1=convw[:, j, 0:1], scalar2=convb[:, j:j + 1], op0=ALU.mult, op1=ALU.add),
                        rk + ['convw', 'convb'], [wk])
                    for i in range(1, 31):
                        P.op(eng, lambda e, src=src, dst=dst, j=j, i=i: e.scalar_tensor_tensor(
                            out=dst, in0=src(i), scalar=convw[:, j, i:i + 1], in1=dst, op0=ALU.mult, op1=ALU.add),
                            rk + ['convw', wk], [wk])
            sqc = [A.alloc([512], F32) for _ in range(2)]
            mean = A.alloc([512], F32)
            msq = A.alloc([512], F32)
            var = A.alloc([512], F32)
            nt_ = A.alloc([512], F32)
            tln = [A.alloc([512], F32) for _ in range(2)]
            for gi, (t0, n) in enumerate(TG):
                ck = [('cP', j) if gi < 4 else ('cS', j) for j in range(4)]
                mm(bank(0)[:, 0:n], [(ones_f, cP[:, j, t0:t0 + n]) for j in range(4)], ck + ['ones_f'], [pk(0)])
                for j in range(4):
                    P.op('act', lambda e, j=j, t0=t0, n=n: e.activation(out=sqc[j % 2][:, 0:n], in_=cP[:, j, t0:t0 + n], func=AF.Square),
                         [ck[j]], [('sqc', j % 2)])
                    P.op('pe', lambda e, j=j, n=n: e.matmul(out=bank(1)[:, 0:n], lhsT=ones_f, rhs=sqc[j % 2][:, 0:n],
                                                            start=(j == 0), stop=(j == 3)),
                         [('sqc', j % 2), 'ones_f'], [pk(1)])
                P.op('dve', lambda e, n=n: e.tensor_scalar(out=mean[:, 0:n], in0=bank(0)[:, 0:n], scalar1=1.0 / 512, scalar2=None,
                                                           op0=ALU.mult), [pk(0)], ['mean'])
                P.op('dve', lambda e, n=n: e.tensor_tensor(out=msq[:, 0:n], in0=mean[:, 0:n], in1=mean[:, 0:n], op=ALU.mult),
                     ['mean'], ['msq'])
                P.op('dve', lambda e, n=n: e.scalar_tensor_tensor(out=var[:, 0:n], in0=bank(1)[:, 0:n], scalar=1.0 / 512,
                                                                  in1=msq[:, 0:n], op0=ALU.mult, op1=ALU.subtract),
                     [pk(1), 'msq'], ['var'])
                P.op('dve', lambda e, n=n: e.tensor_scalar(out=var[:, 0:n], in0=var[:, 0:n], scalar1=EPS, scalar2=None, op0=ALU.add),
                     ['var'], ['var'])
                vi = var[:, 0:n].bitcast(mybir.dt.int32)
                ryi = msq[:, 0:n].bitcast(mybir.dt.int32)
                P.op('dve', lambda e, vi=vi, ryi=ryi: e.tensor_single_scalar(out=ryi, in_=vi, scalar=1, op=ALU.arith_shift_right),
                     ['var'], ['msq'])
                P.op('dve', lambda e, ryi=ryi: e.tensor_scalar(out=ryi, in0=ryi, scalar1=-1, scalar2=0x5f3759df, op0=ALU.mult, op1=ALU.add),
                     ['msq'], ['msq'])
                for it_ in range(3):
                    P.op('dve', lambda e, n=n: e.tensor_tensor(out=nt_[:, 0:n], in0=msq[:, 0:n], in1=msq[:, 0:n], op=ALU.mult), ['msq'], ['nt_'])
                    P.op('dve', lambda e, n=n: e.tensor_tensor(out=nt_[:, 0:n], in0=nt_[:, 0:n], in1=var[:, 0:n], op=ALU.mult), ['nt_', 'var'], ['nt_'])
                    P.op('dve', lambda e, n=n: e.tensor_scalar(out=nt_[:, 0:n], in0=nt_[:, 0:n], scalar1=-0.5, scalar2=1.5, op0=ALU.mult, op1=ALU.add),
                         ['nt_'], ['nt_'])
                    P.op('dve', lambda e, n=n: e.tensor_tensor(out=msq[:, 0:n], in0=msq[:, 0:n], in1=nt_[:, 0:n], op=ALU.mult), ['msq', 'nt_'], ['msq'])
                P.op('dve', lambda e, n=n: e.tensor_copy(out=var[:, 0:n], in_=msq[:, 0:n]), ['msq'], ['var'])
                for j in range(4):
                    tl = tln[j % 2]
                    P.op('dve', lambda e, j=j, t0=t0, n=n, tl=tl: e.tensor_tensor(out=tl[:, 0:n], in0=cP[:, j, t0:t0 + n], in1=mean[:, 0:n],
                                                                                 op=ALU.subtract), [ck[j], 'mean'], [('tln', j % 2)])
                    P.op('dve', lambda e, n=n, tl=tl: e.tensor_tensor(out=tl[:, 0:n], in0=tl[:, 0:n], in1=var[:, 0:n], op=ALU.mult),
                         [('tln', j % 2), 'var'], [('tln', j % 2)])
                    P.op('act', lambda e, j=j, t0=t0, n=n, tl=tl: e.activation(out=cT[:, j, t0:t0 + n], in_=tl[:, 0:n], func=AF.Silu,
                                                                              scale=lng[:, j:j + 1], bias=lnb[:, j:j + 1]),
                         [('tln', j % 2), 'lng', 'lnb'], [('cT', gi)])
        P.barrier()
        A.off = mark

        moT = A.alloc([4, NT], BF16)
        mark = A.off
        if STOP >= 3:
            wkv = A.alloc([8, 1024], BF16)
            wmq = A.alloc([8, 512], BF16)
            P.dma('pool', 'w0', wkv, w_mem_kv.rearrange("(p k) f -> p k f", k=8), writes=['wkv'])
            P.dma('pool', 'w1', wmq, w_in[:, 3584:4096].rearrange("(p k) f -> p k f", k=8), writes=['wmq'])
            xt2 = [A.alloc([DM], F32) for _ in range(2)]
            sqt = A.alloc([DM], F32)
            xn = A.alloc([DM], F32)
            ss = A.alloc([8], F32)
            memhT = A.alloc([8, 256], BF16)
            mkT = A.alloc([4, 256], BF16)
            mvb = A.alloc([2, 512], BF16)
            kf = [A.alloc([4, 128], F32) for _ in range(2)]
            vf = [A.alloc([512], F32) for _ in range(2)]
            kb = A.alloc([512], BF16)
            for ti in range(2):
                P.dma('sp', 'x%d' % ti, xt2[ti], mem[ti * 128:(ti + 1) * 128, :], writes=[('xt', ti)])
                norm_transpose(xt2[ti], 128, g_mem, 'g_mem', memhT[:, :, ti * 128:(ti + 1) * 128], [('memhT', ti)],
                               'n3', [('xt', ti)], sqt, ss, xn)
            for ti in range(2):
                mm(bank(0), [(memhT[:, k, ti * 128:(ti + 1) * 128], wkv[:, k, 0:512]) for k in range(8)],
                   [('memhT', ti), 'wkv'], [pk(0)])
                mm(bank(1), [(memhT[:, k, ti * 128:(ti + 1) * 128], wkv[:, k, 512:1024]) for k in range(8)],
                   [('memhT', ti), 'wkv'], [pk(1)])
                headnorm(bank(0), [pk(0)], 128, 4, gmk, 'gmk', kf[ti], ('kf', ti), 'h3', sqt, ss)
                P.dma('sp', 'o_mk', mkp[ti * 128:(ti + 1) * 128].rearrange("m h d -> m (h d)"),
                      kf[ti].rearrange("p h d -> p (h d)"), reads=[('kf', ti)], writes=[('mkp', ti)])
                P.op('act', lambda e, ti=ti: e.activation(out=vf[ti], in_=bank(1), func=AF.Copy), [pk(1)], [('vf', ti)])
                P.dma('sp', 'o_mv', mvp[ti * 128:(ti + 1) * 128].rearrange("m h d -> m (h d)"), vf[ti],
                      reads=[('vf', ti)], writes=[('mvp', ti)])
                P.op('dve', lambda e, ti=ti: e.tensor_copy(out=mvb[:, ti, :], in_=vf[ti]), [('vf', ti)], [('mvb', ti)])
                P.op('dve', lambda e, ti=ti: e.tensor_copy(out=kb, in_=kf[ti].rearrange("p h d -> p (h d)")), [('kf', ti)], ['kb'])
                pbv = bank(2).bitcast(BF16)
                P.op('pe', lambda e, pbv=pbv: [e.transpose(out=pbv[:, h * 128:(h + 1) * 128], in_=kb[:, h * 128:(h + 1) * 128],
                                                           identity=ident_b) for h in range(4)][-1], ['kb', 'ident_b'], [pk(2)])
                P.op('dve', lambda e, ti=ti, pbv=pbv: e.tensor_copy(out=mkT[:, :, ti * 128:(ti + 1) * 128],
                                                                   in_=pbv[:, 0:512].rearrange("p (h m) -> p h m", h=4)),
                     [pk(2)], [('mkT', ti)])
            zf = A.alloc([4, 128], F32)
            zb = A.alloc([512], BF16)
            mqT = A.alloc([4, 512], BF16)
            PT = [A.alloc([512], BF16) for _ in range(4)]
            rden = A.alloc([512], F32)
            for ti, (t0, rows) in enumerate(TT):
                mm(bank(0)[0:rows, :], [(hT[:, k, t0:t0 + rows], wmq[:, k, :]) for k in range(8)], ['wmq'], [pk(0)])
                headnorm(bank(0)[0:rows, :], [pk(0)], rows, 4, gmq, 'gmq', zf[0:rows], 'zf', 'h3', sqt, ss)
                P.op('act', lambda e, rows=rows: e.activation(out=zb[0:rows, :], in_=zf[0:rows].rearrange("p h d -> p (h d)"), func=AF.Copy),
                     ['zf'], ['zb'])
                pbv = bank(2).bitcast(BF16)
                P.op('pe', lambda e, pbv=pbv, rows=rows: [e.transpose(out=pbv[:, h * 128:h * 128 + rows], in_=zb[0:rows, h * 128:(h + 1) * 128],
                                                                     identity=ident_b[0:rows, 0:rows]) for h in range(4)][-1],
                     ['zb', 'ident_b'], [pk(2)])
                c0 = (ti % 4) * 128
                P.op('dve', lambda e, pbv=pbv, rows=rows, c0=c0: e.tensor_copy(
                    out=mqT[:, :, c0:c0 + rows], in_=pbv[:, 0:512].rearrange("p (h m) -> p h m", h=4)[:, :, 0:rows]),
                    [pk(2)], [('mqT', ti % 4)])
                if ti % 4 == 3 and ti < 16:
                    g0 = (ti // 4) * 512
                    for h in range(4):
                        for m in range(2):
                            mm(bank(3 + m), [(mkT[:, h, m * 128:(m + 1) * 128], mqT[:, h, :])],
                               [('mkT', 0), ('mkT', 1)] + [('mqT', q) for q in range(4)], [pk(3 + m)])
                            P.op('act', lambda e, m=m: e.activation(out=PT[m], in_=bank(3 + m), func=AF.Exp, scale=ISQ128),
                                 [pk(3 + m)], [('PT', m)])
                        mm(bank(5), [(mvb[:, m, h * 128:(h + 1) * 128], PT[m]) for m in range(2)],
                           [('mvb', 0), ('mvb', 1), ('PT', 0), ('PT', 1)], [pk(5)])
                        mm(bank(1), [(ones_b, PT[m]) for m in range(2)], ['ones_b', ('PT', 0), ('PT', 1)], [pk(1)])
                        P.op('dve', lambda e: e.reciprocal(out=rden, in_=bank(1)), [pk(1)], ['rden'])
                        P.op('dve', lambda e, h=h, g0=g0: e.tensor_tensor(out=moT[:, h, g0:g0 + 512], in0=bank(5), in1=rden, op=ALU.mult),
                             [pk(5), 'rden'], [('moT', h, g0)])
            if STOP >= 31:
                mkf = [A.alloc([2, 512], F32) for _ in range(2)]
                mvf = [A.alloc([2, 512], F32) for _ in range(2)]
                mvb2 = [A.alloc([2, 512], BF16) for _ in range(2)]
                mkTb = A.alloc([4, 256], BF16)
                PTs = A.alloc([2, 16], BF16)
                rds = A.alloc([16], F32)
                for b in range(NSB):
                    sl = b % 2
                    P.dma('sp', 'mk%d' % sl, mkf[sl], cmk[b].rearrange("(m p) h d -> p m (h d)", p=128), writes=[('mkf', sl)])
                    P.dma('sp', 'mv%d' % sl, mvf[sl], cmv[b].rearrange("(m p) h d -> p m (h d)", p=128), writes=[('mvf', sl)])
                    P.op('pool', lambda e, sl=sl: e.tensor_copy(out=mvb2[sl], in_=mvf[sl]), [('mvf', sl)], [('mvb2', sl)])
                    for m in range(2):
                        P.op('pe', lambda e, sl=sl, m=m: [e.transpose(out=bank(6 + m)[:, h * 128:(h + 1) * 128],
                                                                      in_=mkf[sl][:, m, h * 128:(h + 1) * 128], identity=ident_f)
                                                          for h in range(4)][-1], [('mkf', sl), 'ident_f'], [pk(6 + m)])
                        P.op('act' if m == 0 else 'dve', (lambda e, m=m: e.activation(
                            out=mkTb[:, :, m * 128:(m + 1) * 128], in_=bank(6 + m).rearrange("p (h k) -> p h k", h=4), func=AF.Copy))
                            if m == 0 else (lambda e, m=m: e.tensor_copy(
                                out=mkTb[:, :, m * 128:(m + 1) * 128], in_=bank(6 + m).rearrange("p (h k) -> p h k", h=4))),
                            [pk(6 + m)], [('mkTb', m)])
                    def sc(e, b=b):
                        ins = None
                        for m in range(2):
                            for h in range(4):
                                ins = e.matmul(out=bank(3)[:, m * 16 + h * 4:m * 16 + h * 4 + 4], lhsT=mkTb[:, h, m * 128:(m + 1) * 128],
                                               rhs=mqT[:, h, 4 * b:4 * b + 4], start=True, stop=True)
                        return ins
                    P.op('pe', sc, [('mkTb', 0), ('mkTb', 1), ('mqT', 0)], [pk(3)])
                    P.op('act', lambda e: e.activation(out=PTs.rearrange("p m x -> p (m x)"), in_=bank(3)[:, 0:32], func=AF.Exp, scale=ISQ128),
                         [pk(3)], ['PTs'])

                    def pv(e, sl=sl):
                        ins = None
                        for h in range(4):
                            for m in range(2):
                                ins = e.matmul(out=bank(4)[:, h * 4:h * 4 + 4], lhsT=mvb2[sl][:, m, h * 128:(h + 1) * 128],
                                               rhs=PTs[:, m, h * 4:h * 4 + 4], start=(m == 0), stop=(m == 1))
                        for m in range(2):
                            ins = e.matmul(out=bank(5)[:, 0:16], lhsT=ones_b, rhs=PTs[:, m, :], start=(m == 0), stop=(m == 1))
                        return ins
                    P.op('pe', pv, ['PTs', ('mvb2', sl), 'ones_b'], [pk(4), pk(5)])
                    P.op('dve', lambda e: e.reciprocal(out=rds, in_=bank(5)[:, 0:16]), [pk(5)], ['rds'])
                    P.op('dve', lambda e, b=b: e.tensor_tensor(out=moT[:, :, S + 4 * b:S + 4 * b + 4],
                                                               in0=bank(4)[:, 0:16].rearrange("p (h t) -> p h t", h=4),
                                                               in1=rds.rearrange("p (h t) -> p h t", h=4), op=ALU.mult),
                         [pk(4), 'rds'], [('moTs', b)])
        P.barrier()
        A.off = mark

        aoT = A.alloc([4, NT], BF16)
        knew = A.alloc([4, 128], F32)
        vnew = A.alloc([4, 128], F32)
        vnewb = A.alloc([4, 128], BF16)
        kTnew = A.alloc([4, NS], BF16)
        qs_all = A.alloc([4, NSB, 12], BF16)
        mark = A.off
        if STOP >= 4:
            wq2 = [A.alloc([8, 640], BF16) for _ in range(2)]
            sqt2 = [A.alloc([512], F32) for _ in range(2)]
            ss2 = [A.alloc([8], F32) for _ in range(2)]
            qkf = [A.alloc([4, 128], F32) for _ in range(2)]
            qkb2 = [A.alloc([512], BF16) for _ in range(2)]
            rtmp2 = [A.alloc([4 * 64], F32) for _ in range(2)]
            vfo = [A.alloc([128], F32) for _ in range(2)]
            qkT = A.alloc([4, NT], BF16)
            Vn = A.alloc([17, 128], BF16)
            Vp = [A.alloc([16, 128], BF16) for _ in range(2)]
            acc = A.alloc([S], F32)
            dacc = A.alloc([S], F32)
            PTa = [A.alloc([256], BF16) for _ in range(3)]

            def load_w(h):
                w = wq2[h % 2]
                for g in range(3):
                    P.dma('pool', 'wq%d' % (h % 2), w[:, :, g * 128:(g + 1) * 128],
                          w_in[:, g * 512 + h * 128:g * 512 + (h + 1) * 128].rearrange("(p k) f -> p k f", k=8), writes=[('wq', h % 2, g)])
                P.dma('pool', 'wq%d' % (h % 2), w[:, :, 384:512],
                      w_in[:, 1536 + h * 128:1536 + (h + 1) * 128].rearrange("(p k) f -> p k f", k=8), writes=[('wq', h % 2, 3)])
                P.dma('pool', 'wq%d' % (h % 2), w[:, :, 512:640],
                      w_in[:, 2048 + h * 128:2048 + (h + 1) * 128].rearrange("(p k) f -> p k f", k=8), writes=[('wq', h % 2, 4)])
            load_w(0)
            cnt_pt = 0
            for h in range(4):
                if h + 1 < 4:
                    load_w(h + 1)
                w = wq2[h % 2]
                wks_ = [('wq', h % 2, q_) for q_ in range(5)]
                def tile_ops(ti, t0, rows, sl, h=h, w=w, wks_=wks_):
                    b0, b1, b2 = (0, 1, 2) if sl == 0 else (3, 4, 7)
                    sqt, ss, qkb, rtmp = sqt2[sl], ss2[sl], qkb2[sl], rtmp2[sl]
                    mm(bank(b0)[0:rows, :], [(hT[:, k, t0:t0 + rows], w[:, k, 0:512]) for k in range(8)], wks_, [pk(b0)])
                    mm(bank(b1)[0:rows, 0:128], [(hT[:, k, t0:t0 + rows], w[:, k, 512:640]) for k in range(8)], wks_, [pk(b1)])
                    headnorm(bank(b0)[0:rows, :], [pk(b0)], rows, 4, gqk, 'gqk', qkf[sl][0:rows], ('qkf', sl), 'h4_%d' % sl, sqt, ss,
                             rope_cs=cs[0:rows, ti, :], tmp=rtmp)
                    if ti < 16:
                        P.dma('sp', 'o_k%d' % sl, kwp[t0:t0 + rows, h, :], qkf[sl][0:rows, 3, :], reads=[('qkf', sl)], writes=[('kwp', h, ti)])
                        P.op('act', lambda e, sl=sl, rows=rows: e.activation(out=vfo[sl][0:rows, :], in_=bank(b1)[0:rows, 0:128], func=AF.Copy),
                             [pk(b1)], [('vfo', sl)])
                        P.dma('sp', 'o_v%d' % sl, vwp[t0:t0 + rows, h, :], vfo[sl][0:rows, :], reads=[('vfo', sl)], writes=[('vwp', h, ti)])
                        P.op('dve', lambda e, sl=sl, ti=ti: e.tensor_copy(out=Vn[:, ti, :], in_=vfo[sl]), [('vfo', sl)], [('Vn', ti)])
                    else:
                        P.op('act', lambda e, h=h: e.activation(out=vnew[0:NS, h, :], in_=bank(b1)[0:NS, 0:128], func=AF.Copy),
                             [pk(b1)], [('vnew', h)])
                        P.op('dve', lambda e, h=h: e.tensor_copy(out=vnewb[0:NS, h, :], in_=vnew[0:NS, h, :]), [('vnew', h)], [('vnewb', h)])
                        P.op('dve', lambda e, h=h, sl=sl: e.tensor_copy(out=knew[0:NS, h, :], in_=qkf[sl][0:NS, 3, :]), [('qkf', sl)], [('knew', h)])
                    P.op('act', lambda e, sl=sl, rows=rows: e.activation(out=qkb[0:rows, :], in_=qkf[sl][0:rows].rearrange("p h d -> p (h d)"),
                                                                        func=AF.Copy), [('qkf', sl)], [('qkb', sl)])
                    pbv = bank(b2).bitcast(BF16)
                    P.op('pe', lambda e, pbv=pbv, rows=rows: [e.transpose(out=pbv[:, i * 128:i * 128 + rows], in_=qkb[0:rows, i * 128:(i + 1) * 128],
                                                                         identity=ident_b[0:rows, 0:rows]) for i in range(4)][-1],
                         [('qkb', sl), 'ident_b'], [pk(b2)])
                    P.op('dve', lambda e, pbv=pbv, rows=rows, t0=t0: e.tensor_copy(
                        out=qkT[:, :, t0:t0 + rows], in_=pbv[:, 0:512].rearrange("p (i m) -> p i m", i=4)[:, :, 0:rows]),
                        [pk(b2)], [('qkT', ti)])
                for tp in range(0, 17, 2):
                    caps = []
                    for ti in (tp, tp + 1):
                        if ti < 17:
                            P.begin_capture()
                            tile_ops(ti, TT[ti][0], TT[ti][1], ti % 2)
                            caps.append(P.end_capture())
                    P.replay(caps)
                P.op('dve', lambda e, h=h: e.tensor_copy(out=qs_all[:, h].rearrange("p b (g t) -> p b g t", g=3),
                                                         in_=qkT[:, 0:3, S:NT].rearrange("p g (b t) -> p b g t", t=4)),
                     [('qkT', 16)], [('qs_all', h)])
                P.op('dve', lambda e, h=h: e.tensor_copy(out=kTnew[:, h, :], in_=qkT[:, 3, S:NT]), [('qkT', 16)], [('kTnew', h)])
                for gi, dil in ((0, 4), (1, 16)):
                    for blk in range(16):
                        if dil == 4:
                            r, i = blk // 4, blk % 4
                            c0, step = i * 512 + r, 4
                        else:
                            c0, step = blk, 16
                        mm(bank(1)[:, 0:128], [(hT[:, k, ss_(c0, 128, step)], w[:, k, 512:640]) for k in range(8)], wks_, [pk(1)])
                        P.op('act', lambda e, gi=gi, blk=blk: e.activation(out=Vp[gi][:, blk, :], in_=bank(1)[:, 0:128], func=AF.Copy),
                             [pk(1)], [('Vp', gi, blk)])
                P.op('pool', lambda e: e.memset(acc, 0.0), [], ['acc'])
                P.op('pool', lambda e: e.memset(dacc, 0.0), [], ['dacc'])
                allq = [('qkT', ti) for ti in range(16)]
                for g in range(3):
                    dil = (1, 4, 16)[g]
                    nper = 4
                    for grp in range(4):
                        for s4 in range(4):
                            blk = grp * 4 + s4
                            if g == 0:
                                cq = slice(blk * 128, blk * 128 + 128)
                                ckp = slice((blk - 1) * 128, blk * 128) if blk > 0 else None
                                Vown = Vn[:, blk, :]
                                Vprev = Vn[:, blk - 1, :] if blk > 0 else None
                                vkeys = [('Vn', blk)] + ([('Vn', blk - 1)] if blk > 0 else [])
                            elif g == 1:
                                r, i = blk // 4, blk % 4
                                cq = ss_(i * 512 + r, 128, 4)
                                ckp = ss_((i - 1) * 512 + r, 128, 4) if i > 0 else None
                                Vown = Vp[0][:, blk, :]
                                Vprev = Vp[0][:, blk - 1, :] if i > 0 else None
                                vkeys = [('Vp', 0, blk)] + ([('Vp', 0, blk - 1)] if i > 0 else [])
                            else:
                                cq = ss_(blk, 128, 16)
                                ckp = None
                                Vown = Vp[1][:, blk, :]
                                Vprev = None
                                vkeys = [('Vp', 1, blk)]
                            sb_ = 3 + (cnt_pt % 2)
                            pt = PTa[cnt_pt % 3]
                            ptk = ('PTa', cnt_pt % 3)
                            cnt_pt += 1

                            def sc(e, g=g, cq=cq, ckp=ckp, sb_=sb_):
                                q = qkT[:, g, cq]
                                if ckp is not None:
                                    e.matmul(out=bank(sb_)[:, 0:256], lhsT=ident_b, rhs=maskb, start=True, stop=False)
                                    e.matmul(out=bank(sb_)[:, 0:128], lhsT=qkT[:, 3, ckp], rhs=q, start=False, stop=False)
                                else:
                                    e.matmul(out=bank(sb_)[:, 128:256], lhsT=ident_b, rhs=maskb[:, 128:256], start=True, stop=False)
                                return e.matmul(out=bank(sb_)[:, 128:256], lhsT=qkT[:, 3, cq], rhs=q, start=False, stop=True)
                            P.op('pe', sc, allq + ['ident_b', 'maskb'], [pk(sb_)])
                            lo = 0 if ckp is not None else 128
                            P.op('act', lambda e, sb_=sb_, pt=pt, lo=lo: e.activation(out=pt[:, lo:256], in_=bank(sb_)[:, lo:256],
                                                                                       func=AF.Exp, scale=ISQ128), [pk(sb_)], [ptk])

                            def pvf(e, s4=s4, pt=pt, Vown=Vown, Vprev=Vprev):
                                o = bank(5)[:, s4 * 128:(s4 + 1) * 128]
                                d = bank(6)[:, s4 * 128:(s4 + 1) * 128]
                                if Vprev is not None:
                                    e.matmul(out=o, lhsT=Vprev, rhs=pt[:, 0:128], start=True, stop=False)
                                    e.matmul(out=o, lhsT=Vown, rhs=pt[:, 128:256], start=False, stop=True)
                                    e.matmul(out=d, lhsT=ones_b, rhs=pt[:, 0:128], start=True, stop=False)
                                    return e.matmul(out=d, lhsT=ones_b, rhs=pt[:, 128:256], start=False, stop=True)
                                e.matmul(out=o, lhsT=Vown, rhs=pt[:, 128:256], start=True, stop=True)
                                return e.matmul(out=d, lhsT=ones_b, rhs=pt[:, 128:256], start=True, stop=True)
                            P.op('pe', pvf, [ptk, 'ones_b'] + vkeys, [pk(5), pk(6)])
                        if g == 0:
                            av = acc[:, grp * 512:(grp + 1) * 512]
                            dv = dacc[:, grp * 512:(grp + 1) * 512]
                            sh = None
                        elif g == 1:
                            av = acc[:, ss_(grp, 512, 4)]
                            dv = dacc[:, ss_(grp, 512, 4)]
                            sh = None
                        else:
                            av = acc.rearrange("p (u s) -> p s u", s=16)[:, grp * 4:grp * 4 + 4, :]
                            dv = dacc.rearrange("p (u s) -> p s u", s=16)[:, grp * 4:grp * 4 + 4, :]
                            sh = 4
                        o5 = bank(5) if sh is None else bank(5).rearrange("p (s u) -> p s u", s=4)
                        o6 = bank(6) if sh is None else bank(6).rearrange("p (s u) -> p s u", s=4)
                        P.op('dve', lambda e, av=av, o5=o5: e.tensor_tensor(out=av, in0=o5, in1=av, op=ALU.add), [pk(5), 'acc'], ['acc'])
                        P.op('dve', lambda e, dv=dv, o6=o6: e.tensor_tensor(out=dv, in0=o6, in1=dv, op=ALU.add), [pk(6), 'dacc'], ['dacc'])
                P.op('dve', lambda e: e.reciprocal(out=dacc, in_=dacc), ['dacc'], ['dacc'])
                P.op('dve', lambda e, h=h: e.tensor_tensor(out=aoT[:, h, 0:S], in0=acc, in1=dacc, op=ALU.mult), ['acc', 'dacc'], [('aoT', h)])
        P.barrier()
        A.off = mark

        if STOP >= 5:
            kst = [A.alloc([7, 512], F32) for _ in range(2)]
            vst = [A.alloc([7, 512], F32) for _ in range(2)]
            vsb = [A.alloc([7, 512], BF16) for _ in range(2)]
            kTb = A.alloc([4, 7, 128], BF16)
            PTn = A.alloc([4, 192], BF16)
            PTb = [A.alloc([7, 48], BF16) for _ in range(2)]
            for b in range(NSB):
                P.dma('sp', 'o_kn', kws[b, 2044:2048].rearrange("t h d -> t (h d)"), knew[4 * b:4 * b + 4].rearrange("p h d -> p (h d)"),
                      reads=[('knew', h) for h in range(4)], writes=[('kwsn', b)])
                P.dma('sp', 'o_vn', vws[b, 2044:2048].rearrange("t h d -> t (h d)"), vnew[4 * b:4 * b + 4].rearrange("p h d -> p (h d)"),
                      reads=[('vnew', h) for h in range(4)], writes=[('vwsn', b)])
            for hp in range(2):
                def scn(e, hp=hp):
                    ins = None
                    for hh in range(2):
                        h = hp * 2 + hh
                        o = bank(0 + hp)[0:NS, hh * 192:(hh + 1) * 192]
                        e.matmul(out=o, lhsT=ident_b[0:NS, 0:NS], rhs=nmaskb[0:NS, :], start=True, stop=False)
                        ins = e.matmul(out=o, lhsT=kTnew[:, h, :], rhs=qs_all[:, h].rearrange("p b x -> p (b x)"), start=False, stop=True)
                    return ins
                P.op('pe', scn, ['ident_b', 'nmaskb'] + [('kTnew', h) for h in range(4)] + [('qs_all', h) for h in range(4)], [pk(hp)])
                P.op('act', lambda e, hp=hp: e.activation(out=PTn[0:NS, hp * 2:hp * 2 + 2, :].rearrange("p a x -> p (a x)"),
                                                          in_=bank(hp)[0:NS, 0:384], func=AF.Exp, scale=ISQ128), [pk(hp)], [('PTn', hp)])
            def newpv(e):
                ins = None
                for h in range(4):
                    o = bank(4 + h // 2)[:, (h % 2) * 192:(h % 2 + 1) * 192]
                    ins = e.matmul(out=o, lhsT=vnewb[0:NS, h, :], rhs=PTn[0:NS, h, :], start=(h % 2 == 0), stop=False, skip_group_check=True)
                for h in range(4):
                    for half in range(2):
                        d = bank(6 + half)[:, 0:384].rearrange("p (b hx) -> p b hx", b=8)[:, :, h * 12:(h + 1) * 12]
                        ins = e.matmul(out=d, lhsT=ones_b[0:NS, :], rhs=PTn[0:NS, h, half * 96:(half + 1) * 96].rearrange("p (b x) -> p b x", b=8),
                                       start=(h == 0), stop=False, skip_group_check=True)
                return ins
            P.op('pe', newpv, [('PTn', 0), ('PTn', 1), 'ones_b'] + [('vnewb', h) for h in range(4)], [pk(4), pk(5), pk(6), pk(7)])
            for b in range(NSB):
                sl = b % 2
                P.dma('sp', 'ck%d' % sl, kst[sl][:, 0:4, :], cache_k[b, 1536:2048].rearrange("(j p) h d -> p j (h d)", p=128), writes=[('kst', sl, 4)])
                P.dma('sp', 'cv%d' % sl, vst[sl][:, 0:4, :], cache_v[b, 1536:2048].rearrange("(j p) h d -> p j (h d)", p=128), writes=[('vst', sl, 4)])
                for w_ in range(4):
                    P.dma('sp', 'ck%d' % sl, kst[sl][w_ * 32:(w_ + 1) * 32, 4:7, :],
                          cache_k[b, 0:1536].rearrange("(j gl s) h d -> s gl j (h d)", s=16, gl=32)[w_], writes=[('kst', sl, w_)])
                    P.dma('sp', 'cv%d' % sl, vst[sl][w_ * 32:(w_ + 1) * 32, 4:7, :],
                          cache_v[b, 0:1536].rearrange("(j gl s) h d -> s gl j (h d)", s=16, gl=32)[w_], writes=[('vst', sl, w_)])
                P.op('pool', lambda e, sl=sl: e.tensor_copy(out=vsb[sl], in_=vst[sl]), [('vst', sl, q_) for q_ in range(5)], [('vsb', sl)])
                for j in range(7):
                    tb = 2 + (j % 2)
                    P.op('pe', lambda e, sl=sl, j=j, tb=tb: [e.transpose(out=bank(tb)[:, h * 128:(h + 1) * 128],
                                                                        in_=kst[sl][:, j, h * 128:(h + 1) * 128], identity=ident_f)
                                                            for h in range(4)][-1], [('kst', sl, q_) for q_ in range(5)] + ['ident_f'], [pk(tb)])
                    if j % 2 == 0:
                        P.op('act', lambda e, j=j, tb=tb: e.activation(out=kTb[:, :, j, :], in_=bank(tb).rearrange("p (h k) -> p h k", h=4), func=AF.Copy),
                             [pk(tb)], [('kTb', j)])
                    else:
                        P.op('dve', lambda e, j=j, tb=tb: e.tensor_copy(out=kTb[:, :, j, :], in_=bank(tb).rearrange("p (h k) -> p h k", h=4)),
                             [pk(tb)], [('kTb', j)])
                sbk = b % 2

                def scs(e, b=b, sbk=sbk):
                    ins = None
                    o = bank(sbk)[:, 0:336]
                    e.matmul(out=o, lhsT=ident_b, rhs=smaskb, start=True, stop=False)
                    for j in range(7):
                        for h in range(4):
                            ins = e.matmul(out=bank(sbk)[:, j * 48 + h * 12:j * 48 + (h + 1) * 12], lhsT=kTb[:, h, j, :], rhs=qs_all[:, h, b, :],
                                           start=False, stop=(j == 6 and h == 3))
                    return ins
                P.op('pe', scs, ['ident_b', 'smaskb'] + [('kTb', j) for j in range(7)], [pk(sbk)])
                P.op('act', lambda e, sbk=sbk: e.activation(out=PTb[sbk].rearrange("p j x -> p (j x)"), in_=bank(sbk)[:, 0:336],
                                                            func=AF.Exp, scale=ISQ128), [pk(sbk)], [('PTb', sbk)])

                def pvs(e, b=b, sl=sl, sbk=sbk):
                    ins = None
                    last = (b == NSB - 1)
                    for h in range(4):
                        o = bank(4 + h // 2)[:, (h % 2) * 192 + b * 12:(h % 2) * 192 + (b + 1) * 12]
                        for j in range(7):
                            ins = e.matmul(out=o, lhsT=vsb[sl][:, j, h * 128:(h + 1) * 128], rhs=PTb[sbk][:, j, h * 12:(h + 1) * 12],
                                           start=False, stop=(last and j == 6), skip_group_check=True)
                    d = bank(6 + b // 8)[:, (b % 8) * 48:(b % 8 + 1) * 48]
                    for j in range(7):
                        ins = e.matmul(out=d, lhsT=ones_b, rhs=PTb[sbk][:, j, :], start=False, stop=(last and j == 6), skip_group_check=True)
                    return ins
                P.op('pe', pvs, [('PTb', sbk), ('vsb', sl), 'ones_b'], [pk(4), pk(5), pk(6), pk(7)])
            osum = A.alloc([4, NSB, 4], F32)
            dsum = A.alloc([4, NSB, 4], F32)
            for hp in range(2):
                ov = bank(4 + hp)[:, 0:384].rearrange("p (a b g t) -> p a b g t", a=2, b=NSB, g=3)
                P.op('dve', lambda e, hp=hp, ov=ov: e.tensor_copy(out=osum[:, hp * 2:hp * 2 + 2], in_=ov[:, :, :, 0, :]),
                     [pk(4 + hp)], [('osum', hp)])
                P.op('dve', lambda e, hp=hp, ov=ov: e.tensor_tensor(out=osum[:, hp * 2:hp * 2 + 2], in0=osum[:, hp * 2:hp * 2 + 2], in1=ov[:, :, :, 1, :], op=ALU.add),
                     [pk(4 + hp), ('osum', hp)], [('osum', hp)])
                P.op('dve', lambda e, hp=hp, ov=ov: e.tensor_tensor(out=osum[:, hp * 2:hp * 2 + 2], in0=osum[:, hp * 2:hp * 2 + 2], in1=ov[:, :, :, 2, :], op=ALU.add),
                     [pk(4 + hp), ('osum', hp)], [('osum', hp)])
            for half in range(2):
                dv_ = bank(6 + half)[:, 0:384].rearrange("p (b h g t) -> p h b g t", b=8, h=4, g=3)
                ds_ = dsum[:, :, half * 8:half * 8 + 8, :]
                P.op('dve', lambda e, dv_=dv_, ds_=ds_: e.tensor_copy(out=ds_, in_=dv_[:, :, :, 0, :]),
                     [pk(6 + half)], [('dsum', half)])
                P.op('dve', lambda e, dv_=dv_, ds_=ds_: e.tensor_tensor(out=ds_, in0=ds_, in1=dv_[:, :, :, 1, :], op=ALU.add),
                     [pk(6 + half), ('dsum', half)], [('dsum', half)])
                P.op('dve', lambda e, dv_=dv_, ds_=ds_: e.tensor_tensor(out=ds_, in0=ds_, in1=dv_[:, :, :, 2, :], op=ALU.add),
                     [pk(6 + half), ('dsum', half)], [('dsum', half)])
            P.op('dve', lambda e: e.reciprocal(out=dsum, in_=dsum), [('dsum', 0), ('dsum', 1)], ['dsr'])
            P.op('dve', lambda e: e.tensor_tensor(out=aoT[:, :, S:NT].rearrange("p h (b t) -> p h b t", t=4), in0=osum, in1=dsum, op=ALU.mult),
                 ['dsr', ('osum', 0), ('osum', 1)], ['aoTs'])
        P.barrier()
        A.off = mark

        if DBG:
            for i_, t_ in enumerate((aoT, cT, moT)):
                P.dma('pool', 'dbg', dbg[i_], t_.rearrange("p h t -> p (h t)"), writes=[('dbg', i_)])
            P.barrier()
        if STOP >= 6:
            mgT = A.alloc([8, NT], BF16)
            wo = A.alloc([8, DM], BF16)
            mark5 = A.off
            wj = [A.alloc([8, 384], BF16) for _ in range(2)]
            wpj = [A.alloc([4, 384], BF16) for _ in range(2)]
            sg3 = [A.alloc([512], F32) for _ in range(3)]
            t3 = [A.alloc([512], F32) for _ in range(2)]
            P.dma('pool', 'wo', wo, w_out.rearrange("(k p) f -> p k f", p=128), writes=['wo'])

            def load_j(j):
                sl = j % 2
                for br_ in range(3):
                    P.dma('pool', 'wj%d' % sl, wj[sl][:, :, br_ * 128:(br_ + 1) * 128],
                          w_in[:, 4096 + br_ * 1024 + j * 128:4096 + br_ * 1024 + (j + 1) * 128].rearrange("(p k) f -> p k f", k=8),
                          writes=[('wj', sl, br_)])
                for br_, wp in enumerate((w_attn_proj, w_conv_proj, w_mem_proj)):
                    P.dma('pool', 'wj%d' % sl, wpj[sl][:, :, br_ * 128:(br_ + 1) * 128],
                          wp[:, j * 128:(j + 1) * 128].rearrange("(c p) f -> p c f", p=128), writes=[('wpj', sl, br_)])
            load_j(0)
            brT = (aoT, cT, moT)
            for j in range(8):
                if j + 1 < 8:
                    load_j(j + 1)
                sl = j % 2
                for gi, (t0, n) in enumerate(TG):
                    for br_ in range(3):
                        mm(bank(br_)[:, 0:n], [(wj[sl][:, k, br_ * 128:(br_ + 1) * 128], hT[:, k, t0:t0 + n]) for k in range(8)],
                           [('wj', sl, br_)], [pk(br_)])
                        mm(bank(3 + br_)[:, 0:n], [(wpj[sl][:, c, br_ * 128:(br_ + 1) * 128], brT[br_][:, c, t0:t0 + n]) for c in range(4)],
                           [('wpj', sl, br_)], [pk(3 + br_)])
                        P.op('act', lambda e, br_=br_, n=n: e.activation(out=sg3[br_][:, 0:n], in_=bank(br_)[:, 0:n], func=AF.Sigmoid),
                             [pk(br_)], [('sg3', br_)])
                    P.op('dve', lambda e, n=n: e.tensor_tensor(out=t3[0][:, 0:n], in0=bank(3)[:, 0:n], in1=sg3[0][:, 0:n], op=ALU.mult),
                         [pk(3), ('sg3', 0)], [('t3', 0)])
                    P.op('dve', lambda e, n=n: e.tensor_tensor(out=t3[1][:, 0:n], in0=bank(4)[:, 0:n], in1=sg3[1][:, 0:n], op=ALU.mult),
                         [pk(4), ('sg3', 1)], [('t3', 1)])
                    P.op('dve', lambda e, n=n: e.tensor_tensor(out=t3[0][:, 0:n], in0=t3[0][:, 0:n], in1=t3[1][:, 0:n], op=ALU.add),
                         [('t3', 0), ('t3', 1)], [('t3', 0)])
                    P.op('dve', lambda e, n=n: e.tensor_tensor(out=t3[1][:, 0:n], in0=bank(5)[:, 0:n], in1=sg3[2][:, 0:n], op=ALU.mult),
                         [pk(5), ('sg3', 2)], [('t3', 1)])
                    P.op('dve', lambda e, n=n, j=j, t0=t0: e.tensor_tensor(out=mgT[:, j, t0:t0 + n], in0=t3[0][:, 0:n], in1=t3[1][:, 0:n], op=ALU.add),
                         [('t3', 0), ('t3', 1)], [('mgT', j, gi)])
            P.barrier()
            A.off = mark5
            xt2 = [A.alloc([DM], F32) for _ in range(2)]
            x1t = [A.alloc([DM], F32) for _ in range(2)]
            sqt = A.alloc([DM], F32)
            xn = A.alloc([DM], F32)
            ss = A.alloc([8], F32)
            for ti, (t0, rows) in enumerate(TT):
                sl = ti % 2
                P.dma('sp', 'x%d' % sl, xt2[sl][0:rows, :], xin[t0:t0 + rows, :], writes=[('xt', sl)])
                for half in range(2):
                    mm(bank(half)[0:rows, :], [(mgT[:, k, t0:t0 + rows], wo[:, k, half * 512:(half + 1) * 512]) for k in range(8)],
                       ['wo'], [pk(half)])
                    P.op('dve', lambda e, half=half, sl=sl, rows=rows: e.tensor_tensor(
                        out=x1t[sl][0:rows, half * 512:(half + 1) * 512], in0=bank(half)[0:rows, :],
                        in1=xt2[sl][0:rows, half * 512:(half + 1) * 512], op=ALU.add), [pk(half), ('xt', sl)], [('x1t', sl)])
                P.dma('sp', 'x1o%d' % sl, x1s[t0:t0 + rows, :], x1t[sl][0:rows, :], reads=[('x1t', sl)], writes=[('x1s', ti)])
                norm_transpose(x1t[sl][0:rows, :], rows, g_ffn, 'g_ffn', hT[:, :, t0:t0 + rows], [('hT', ti)],
                               'n5', [('x1t', sl)], sqt, ss, xn)
        P.barrier()
        A.off = mark0
        h2T = hT

        if STOP >= 7:
            yacc = A.alloc([17, DM], F32)
            gates = A.alloc([17, 32], F32)
            wg = [A.alloc([8, 512], BF16) for _ in range(2)]
            wu = [A.alloc([8, 512], BF16) for _ in range(2)]
            wd = [A.alloc([4, DM], BF16) for _ in range(2)]
            lgt = A.alloc([36], F32)
            rt = A.alloc([16, 8], F32)

            def load_e(ei):
                sl = ei % 2
                P.dma('pool', 'we%d' % sl, wg[sl], w_eg[ei].rearrange("(p k) f -> p k f", k=8), writes=[('wg', sl)])
                P.dma('pool', 'we%d' % sl, wu[sl], w_eu[ei].rearrange("(p k) f -> p k f", k=8), writes=[('wu', sl)])
                P.dma('pool', 'we%d' % sl, wd[sl], w_ed[ei].rearrange("(c p) f -> p c f", p=128), writes=[('wd', sl)])
            load_e(0)
            ss = A.alloc([8], F32)
            mark6 = A.off
            xn = A.alloc([DM], F32)
            sqt = A.alloc([DM], F32)
            h2f = A.alloc([8, 128], F32)
            P.dma('sp', 'ya0', yacc[:, 0:16, :], x1s[0:S, :].rearrange("(t p) d -> p t d", p=128), writes=[('yacc', ti) for ti in range(16)])
            P.dma('sp', 'ya1', yacc[0:NS, 16, :], x1s[S:NT, :], writes=[('yacc', 16)])
            for ti, (t0, rows) in enumerate(TT):
                rms_rstd(yacc[0:rows, ti, :], rows, DM, sqt, ss, 'n6', [('yacc', ti)])
                P.op('dve', lambda e, rows=rows, ti=ti: e.tensor_scalar(out=xn[0:rows, :], in0=yacc[0:rows, ti, :], scalar1=ss[0:rows, 0:1], scalar2=32.0,
                                                                         op0=ALU.mult, op1=ALU.mult), [('yacc', ti), 'n6ss'], ['n6xn'])
                xv = xn[0:rows, :].rearrange("t (p k) -> t k p", k=8)
                for half in range(2):
                    bi = 6 + half
                    P.op('pe', lambda e, half=half, bi=bi, rows=rows, xv=xv: [e.transpose(
                        out=bank(bi)[:, kk * 128:kk * 128 + rows], in_=xv[:, half * 4 + kk, :], identity=ident_f[0:rows, 0:rows])
                        for kk in range(4)][-1], ['n6xn', 'ident_f'], [pk(bi)])
                    P.op('dve', lambda e, half=half, bi=bi, rows=rows: e.tensor_tensor(
                        out=h2f[:, half * 4:half * 4 + 4, 0:rows], in0=bank(bi).rearrange("p (k t) -> p k t", k=4)[:, :, 0:rows],
                        in1=g_ffn[:, half * 4:half * 4 + 4].unsqueeze(2).to_broadcast([128, 4, rows]), op=ALU.mult),
                        [pk(bi), 'g_ffn'], [('h2f', half)])
                mm(bank(5)[0:rows, 0:36], [(h2f[:, k, 0:rows], wr[:, k, :]) for k in range(8)], [('h2f', 0), ('h2f', 1), 'wr'], [pk(5)])
                def router_tile(ti, rows):
                    R = rows
                    P.op('dve', lambda e, R=R: e.tensor_tensor(out=lgt[0:R, :], in0=bank(5)[0:R, 0:36], in1=br[0:R, :], op=ALU.add), [pk(5), 'br'], ['lgt'])
                    mx = rt[0:R, 0, 0:1]; gm = rt[0:R, 1, 0:4]; sme = rt[0:R, 0, 1:2]; pgt = rt[0:R, 0, 2:3]
                    ex4 = rt[0:R, 2, 0:4]; les = rt[0:R, 3, :]; m1 = rt[0:R, 0, 3:4]; oh1 = rt[0:R, 4, :]; le2 = rt[0:R, 5, :]
                    m2 = rt[0:R, 0, 4:5]; oh2 = rt[0:R, 6, :]; dm_ = rt[0:R, 0, 5:6]; e21 = rt[0:R, 0, 6:7]; w1 = rt[0:R, 0, 7:8]
                    w2 = rt[0:R, 7, 0:1]; g8 = rt[0:R, 8, :]; tmp8 = rt[0:R, 9, :]; den = rt[0:R, 7, 1:2]
                    K = 'rt'
                    seq = [
                        ('dve', lambda e: e.reduce_max(out=mx, in_=lgt[0:R, 0:4], axis=AX.X)),
                        ('dve', lambda e: e.tensor_scalar(out=gm, in0=lgt[0:R, 0:4], scalar1=mx, scalar2=None, op0=ALU.is_equal)),
                        ('dve', lambda e: e.tensor_scalar(out=ex4, in0=lgt[0:R, 0:4], scalar1=mx, scalar2=None, op0=ALU.subtract)),
                        ('act', lambda e: e.activation(out=ex4, in_=ex4, func=AF.Exp)),
                        ('dve', lambda e: e.reduce_sum(out=sme, in_=ex4, axis=AX.X)),
                        ('dve', lambda e: e.reciprocal(out=pgt, in_=sme)),
                        ('dve', lambda e: e.tensor_scalar(out=les, in0=lgt[0:R, 4:12], scalar1=gm[:, 0:1], scalar2=None, op0=ALU.mult)),
                    ] + [
                        ('dve', (lambda g: (lambda e: e.scalar_tensor_tensor(out=les, in0=lgt[0:R, 4 + g * 8:12 + g * 8], scalar=gm[:, g:g + 1], in1=les,
                                                                             op0=ALU.mult, op1=ALU.add)))(g)) for g in range(1, 4)
                    ] + [
                        ('dve', lambda e: e.reduce_max(out=m1, in_=les, axis=AX.X)),
                        ('dve', lambda e: e.tensor_scalar(out=oh1, in0=les, scalar1=m1, scalar2=None, op0=ALU.is_equal)),
                        ('dve', lambda e: e.scalar_tensor_tensor(out=le2, in0=oh1, scalar=-1e30, in1=les, op0=ALU.mult, op1=ALU.add)),
                        ('dve', lambda e: e.reduce_max(out=m2, in_=le2, axis=AX.X)),
                        ('dve', lambda e: e.tensor_scalar(out=oh2, in0=le2, scalar1=m2, scalar2=None, op0=ALU.is_equal)),
                        ('dve', lambda e: e.tensor_tensor(out=dm_, in0=m2, in1=m1, op=ALU.subtract)),
                        ('act', lambda e: e.activation(out=e21, in_=dm_, func=AF.Exp)),
                        ('dve', lambda e: e.tensor_scalar(out=den, in0=e21, scalar1=1.0, scalar2=None, op0=ALU.add)),
                        ('dve', lambda e: e.reciprocal(out=den, in_=den)),
                        ('dve', lambda e: e.tensor_tensor(out=w1, in0=pgt, in1=den, op=ALU.mult)),
                        ('dve', lambda e: e.tensor_tensor(out=w2, in0=w1, in1=e21, op=ALU.mult)),
                        ('dve', lambda e: e.tensor_scalar(out=g8, in0=oh1, scalar1=w1, scalar2=None, op0=ALU.mult)),
                        ('dve', lambda e: e.scalar_tensor_tensor(out=g8, in0=oh2, scalar=w2, in1=g8, op0=ALU.mult, op1=ALU.add)),
                    ] + [
                        ('dve', (lambda g, ti=ti: (lambda e: e.tensor_scalar(out=gates[0:R, ti, g * 8:(g + 1) * 8], in0=g8, scalar1=gm[:, g:g + 1], scalar2=None,
                                                                              op0=ALU.mult)))(g)) for g in range(4)
                    ]
                    for eng_, fn_ in seq:
                        P.op(eng_, fn_, ['lgt', K], [K, ('gates', ti)])

                router_tile(ti, rows)
            P.barrier()
            A.off = mark6
            hid = A.alloc([4, NT], BF16)
            sgt = [A.alloc([512], F32) for _ in range(2)]
            cnt = 0
            for ei in range(NEXP):
                if ei + 1 < NEXP:
                    load_e(ei + 1)
                sl = ei % 2
                for gi, (t0, n) in enumerate(TG):
                    for c in range(4):
                        bg, bu = (cnt % 2) * 2, (cnt % 2) * 2 + 1
                        sg = sgt[cnt % 2]
                        sgk = ('sgt', cnt % 2)
                        cnt += 1
                        mm(bank(bg)[:, 0:n], [(wg[sl][:, k, c * 128:(c + 1) * 128], h2T[:, k, t0:t0 + n]) for k in range(8)], [('wg', sl)], [pk(bg)])
                        mm(bank(bu)[:, 0:n], [(wu[sl][:, k, c * 128:(c + 1) * 128], h2T[:, k, t0:t0 + n]) for k in range(8)], [('wu', sl)], [pk(bu)])
                        P.op('act', lambda e, bg=bg, n=n, sg=sg: e.activation(out=sg[:, 0:n], in_=bank(bg)[:, 0:n], func=AF.Silu), [pk(bg)], [sgk])
                        P.op('dve', lambda e, bu=bu, n=n, sg=sg, c=c, t0=t0: e.tensor_tensor(out=hid[:, c, t0:t0 + n], in0=bank(bu)[:, 0:n], in1=sg[:, 0:n],
                                                                                         op=ALU.mult), [pk(bu), sgk], [('hid', gi, c)])
                for ti, (t0, rows) in enumerate(TT):
                    gi = min(ti // 4, 4)
                    for half in range(2):
                        bo = 4 + ((ti * 2 + half) % 4)
                        mm(bank(bo)[0:rows, :], [(hid[:, c, t0:t0 + rows], wd[sl][:, c, half * 512:(half + 1) * 512]) for c in range(4)],
                           [('wd', sl)] + [('hid', gi, c) for c in range(4)], [pk(bo)])
                        eng_ = 'dve' if (half == 0 or ti % 2 == 0) else 'pool'
                        eng_ = 'dve'
                        P.op(eng_, lambda e, bo=bo, rows=rows, ti=ti, half=half, ei=ei: e.scalar_tensor_tensor(
                            out=yacc[0:rows, ti, half * 512:(half + 1) * 512], in0=bank(bo)[0:rows, :], scalar=gates[0:rows, ti, ei:ei + 1],
                            in1=yacc[0:rows, ti, half * 512:(half + 1) * 512], op0=ALU.mult, op1=ALU.add),
                            [pk(bo), ('gates', ti), ('yacc', ti)], [('yacc', ti)])
            for ti, (t0, rows) in enumerate(TT):
                P.dma('sp', 'o_y', y[t0:t0 + rows, :], yacc[0:rows, ti, :], reads=[('yacc', ti)], writes=[('y', ti)])
        P.final_wait_all_dma('sp')
        P.emit(nc, st)
    return nc


def _consts():
    half = 16
    inv_freq = np.power(np.float32(500000.0), -np.arange(half, dtype=np.float32) * np.float32(2.0 / 32)).astype(np.float32)
    pos = np.zeros(17 * 128, np.float32)
    pos[:S] = np.arange(S)
    pos[S:S + NS] = 2048 + (np.arange(NS) % 4)
    ang = pos[:, None].astype(np.float32) * inv_freq[None, :]
    cs = np.concatenate([np.cos(ang), np.sin(ang)], axis=1).astype(np.float32)
    kp = np.arange(128)[:, None]
    qf = np.arange(128)[None, :]
    mask = np.full((128, 256), NEG, np.float32)
    mask[:, 0:128][kp >= qf] = 0.0
    mask[:, 128:256][kp <= qf] = 0.0
    sm = np.full((128, 7, 3, 4), NEG, np.float32)
    for j in range(7):
        for p in range(128):
            if j < 4:
                R = 1536 + 128 * j + p
                for t in range(4):
                    if R >= 1920 + t:
                        sm[p, j, 0, t] = 0.0
                    if R % 4 == t:
                        sm[p, j, 1, t] = 0.0
                    if R % 16 == t:
                        sm[p, j, 2, t] = 0.0
            else:
                w = p // 32
                sm[p, j, 2, w] = 0.0
    smask = np.repeat(sm.reshape(128, 7, 1, 12), 4, axis=2).reshape(128, 7 * 48)
    nm = np.full((64, 16, 3, 4), NEG, np.float32)
    for b in range(16):
        for tp in range(4):
            for t in range(4):
                if tp <= t:
                    nm[b * 4 + tp, b, 0, t] = 0.0
                if tp == t:
                    nm[b * 4 + tp, b, 1, t] = 0.0
                    nm[b * 4 + tp, b, 2, t] = 0.0
    return dict(c_ident=np.eye(128, dtype=np.float32), c_cs=cs, c_mask=mask, c_smask=np.ascontiguousarray(smask),
                c_nmask=np.ascontiguousarray(nm.reshape(64, 192)))


_NC = None


def kernel(x_prompt, x_sample, mem_prompt, cache_k, cache_v, state_conv, cache_mem_k, cache_mem_v,
           norm_mix_g, w_in, q_norm_g, k_norm_g, conv_w, conv_b, conv_ln_g, conv_ln_b,
           mem_norm_g, w_mem_kv, mq_norm_g, mk_norm_g, w_attn_proj, w_conv_proj, w_mem_proj, w_out,
           norm_ffn_g, w_router_group, b_router_group, w_router_expert, b_router_expert,
           w_expert_gate, w_expert_up, w_expert_down):
    global _NC
    f = lambda a: np.ascontiguousarray(np.asarray(a, dtype=np.float32))
    x_prompt, x_sample, mem_prompt = f(x_prompt), f(x_sample), f(mem_prompt)
    cache_k, cache_v, state_conv = f(cache_k), f(cache_v), f(state_conv)
    cache_mem_k, cache_mem_v = f(cache_mem_k), f(cache_mem_v)
    wre = np.transpose(f(w_router_expert)[0], (1, 0, 2)).reshape(DM, 32)
    w_router = np.ascontiguousarray(np.concatenate([f(w_router_group)[0], wre], axis=1))
    b_router = np.ascontiguousarray(np.concatenate([f(b_router_group)[0], f(b_router_expert)[0].reshape(32)]))
    shared = dict(
        norm_mix_g=f(norm_mix_g)[0], w_in=f(w_in)[0], q_norm_g=f(q_norm_g)[0], k_norm_g=f(k_norm_g)[0],
        conv_wT=np.ascontiguousarray(f(conv_w)[0].T), conv_b=np.ascontiguousarray(f(conv_b)[0].reshape(4, 128).T), conv_ln_g=np.ascontiguousarray(f(conv_ln_g)[0].reshape(4, 128).T),
        conv_ln_b=np.ascontiguousarray(f(conv_ln_b)[0].reshape(4, 128).T),
        mem_norm_g=f(mem_norm_g)[0], w_mem_kv=f(w_mem_kv)[0], mq_norm_g=f(mq_norm_g)[0], mk_norm_g=f(mk_norm_g)[0],
        w_attn_proj=f(w_attn_proj)[0], w_conv_proj=f(w_conv_proj)[0], w_mem_proj=f(w_mem_proj)[0], w_out=f(w_out)[0],
        norm_ffn_g=f(norm_ffn_g)[0], w_router=w_router, b_router=b_router,
        w_eg=f(w_expert_gate)[0], w_eu=f(w_expert_up)[0], w_ed=f(w_expert_down)[0])
    shared.update(_consts())
    in_maps = []
    for c in range(NCORES):
        m = dict(shared)
        bs = slice(c * NSB, (c + 1) * NSB)
        m["xin"] = np.ascontiguousarray(np.concatenate([x_prompt[c], x_sample[bs].reshape(NS, DM)], axis=0))
        m["mem"] = mem_prompt[c]
        m["cache_k"] = cache_k[0, bs]
        m["cache_v"] = cache_v[0, bs]
        m["state_conv"] = state_conv[0, bs]
        m["cmk"] = cache_mem_k[0, bs]
        m["cmv"] = cache_mem_v[0, bs]
        in_maps.append(m)
    if os.environ.get("MK_ONLY_MAPS"):
        return in_maps
    if _NC is None:
        _NC = build_nc()
    res = run_bass_kernel_spmd(_NC, in_maps, core_ids=list(range(NCORES)))
    R = res.results
    y_prompt = np.stack([R[c]["y"][:S] for c in range(NCORES)], 0)
    y_sample = np.concatenate([R[c]["y"][S:].reshape(NSB, 4, DM) for c in range(NCORES)], 0)
    st = lambda k: np.stack([R[c][k] for c in range(NCORES)], 0)[None]
    ct = lambda k: np.concatenate([R[c][k] for c in range(NCORES)], 0)[None]
    return (y_prompt, y_sample, st("kwp"), st("vwp"), st("convp"), st("mkp"), st("mvp"), ct("kws"), ct("vws"), ct("convs"))
```

```python
import os
import numpy as np
import concourse.bass as bass
import concourse.mybir as mybir
from concourse.bass_utils import run_bass_kernel_spmd
from contextlib import ExitStack

F32 = mybir.dt.float32
BF16 = mybir.dt.bfloat16
ALU = mybir.AluOpType
AF = mybir.ActivationFunctionType
AX = mybir.AxisListType

NCORES = 8
S = 2048
DM = 1024
NSB = 16
NS = 64
NT = S + NS
EPS = 1e-6
NEG = -30000.0
SQ128 = float(np.sqrt(128.0))
ISQ128 = float(1.0 / np.sqrt(128.0))
TT = [(i * 128, 128) for i in range(16)] + [(S, NS)]
TG = [(i * 512, 512) for i in range(4)] + [(S, NS)]
NEXP = 32
STOP = int(os.environ.get("MK_STOP", "99"))


def ss_(start, count, step):
    return slice(start, start + (count - 1) * step + 1, step)


class Prog:
    ENG = ('pe', 'act', 'dve', 'pool', 'sp')

    def __init__(self):
        self.engs = {e: dict(ops=[], n=0, waited={}) for e in self.ENG}
        self.dsem = {}
        self.lastw = {}
        self.readers = {}

    def _deps(self, reads, writes):
        toks = {}

        def add(tok):
            if tok is None:
                return
            s, v = tok
            if s.startswith('d:') and not s.startswith('d:bg'):
                v = 16 * self.dsem[s[2:]]['n']
            if toks.get(s, 0) < v:
                toks[s] = v
        for k in reads:
            add(self.lastw.get(k))
        for k in writes:
            add(self.lastw.get(k))
            for s, v in self.readers.get(k, {}).items():
                add((s, v))
        return toks

    def _commit(self, tok, reads, writes):
        for k in reads:
            d = self.readers.setdefault(k, {})
            if d.get(tok[0], 0) < tok[1]:
                d[tok[0]] = tok[1]
        for k in writes:
            self.lastw[k] = tok
            self.readers[k] = {}

    def _waits(self, eng, toks):
        E = self.engs[eng]
        waits = []
        for s, v in toks.items():
            if eng == 'pe' and s == 'e:pe':
                continue
            if E['waited'].get(s, 0) >= v:
                continue
            E['waited'][s] = v
            waits.append((s, v))
        return waits

    _cap = None

    def begin_capture(self):
        self._cap = []

    def end_capture(self):
        c, self._cap = self._cap, None
        return c

    def replay(self, lists):
        idx = [0] * len(lists)
        while any(idx[i] < len(L) for i, L in enumerate(lists)):
            for i, L in enumerate(lists):
                if idx[i] < len(L):
                    kind, args = L[idx[i]]
                    idx[i] += 1
                    (self.op if kind == 'op' else self.dma)(*args)

    def op(self, eng, fn, reads=(), writes=()):
        if self._cap is not None:
            self._cap.append(('op', (eng, fn, list(reads), list(writes))))
            return
        E = self.engs[eng]
        waits = self._waits(eng, self._deps(reads, writes))
        E['n'] += 1
        tok = ('e:' + eng, E['n'])
        E['ops'].append((waits, fn, tok))
        self._commit(tok, reads, writes)

    def dma(self, queue, semname, out, in_, reads=(), writes=()):
        if self._cap is not None:
            self._cap.append(('dma', (queue, semname, out, in_, list(reads), list(writes))))
            return
        E = self.engs[queue]
        waits = self._waits(queue, self._deps(reads, writes))
        D = self.dsem.setdefault(semname, dict(n=0))
        D['n'] += 1
        tok = ('d:' + semname, 16 * D['n'])
        E['ops'].append((waits, lambda e, o=out, i=in_: e.dma_start(out=o, in_=i), tok))
        self._commit(tok, reads, writes)

    def barrier(self):
        toks = {('e:' + e): self.engs[e]['n'] for e in self.ENG if self.engs[e]['n'] > 0}
        for name, D in self.dsem.items():
            if name.startswith('bg'):
                continue
            toks['d:' + name] = 16 * D['n']
        for e in self.ENG:
            waits = self._waits(e, dict(toks))
            self.engs[e]['ops'].append((waits, None, None))
        keep_w = {k: v for k, v in self.lastw.items() if v[0].startswith('d:bg')}
        self.lastw = keep_w
        self.readers = {}

    def final_wait_all_dma(self, eng='sp'):
        E = self.engs[eng]
        waits = []
        for name, D in self.dsem.items():
            s = 'd:' + name
            v = 16 * D['n']
            if E['waited'].get(s, 0) < v:
                E['waited'][s] = v
                waits.append((s, v))
        E['ops'].append((waits, None, None))

    def emit(self, nc, stack):
        sems = {}
        for e in self.ENG:
            sems['e:' + e] = stack.enter_context(nc.semaphore('s_' + e))
        for name in self.dsem:
            sems['d:' + name] = stack.enter_context(nc.semaphore('d_' + name))
        block = stack.enter_context(nc.Block())

        def run(engname):
            def f(eng):
                for waits, fn, tok in self.engs[engname]['ops']:
                    for s, v in waits:
                        eng.wait_ge(sems[s], v)
                    if fn is None:
                        continue
                    ins = fn(eng)
                    ins.then_inc(sems[tok[0]], 16 if tok[0].startswith('d:') else 1)
            return f
        block.tensor(run('pe'))
        block.scalar(run('act'))
        block.vector(run('dve'))
        block.gpsimd(run('pool'))
        block.sync(run('sp'))


class Arena:
    def __init__(self, t, nelem_bf16):
        self.t = t
        self.n = nelem_bf16
        self.off = 0

    def alloc(self, shape, dt):
        size = 2 if dt == BF16 else 4
        ne = int(np.prod(shape))
        nb = (ne * size + 31) // 32 * 32
        assert self.off + nb // 2 <= self.n, ("arena overflow", self.off * 2, nb, self.n * 2)
        ap = self.t[:, self.off:self.off + ne * size // 2]
        self.off += nb // 2
        if dt != BF16:
            ap = ap.bitcast(dt)
        if len(shape) == 2:
            ap = ap.rearrange("p (a b) -> p a b", a=shape[0], b=shape[1])
        elif len(shape) == 3:
            ap = ap.rearrange("p (a b c) -> p a b c", a=shape[0], b=shape[1], c=shape[2])
        elif len(shape) == 4:
            ap = ap.rearrange("p (a b c d) -> p a b c d", a=shape[0], b=shape[1], c=shape[2], d=shape[3])
        return ap


def build_nc():
    nc = bass.Bass("TRN2", target_bir_lowering=False)

    def din(name, shape):
        return nc.dram_tensor(name, list(shape), F32, kind="ExternalInput").ap()

    def dout(name, shape):
        return nc.dram_tensor(name, list(shape), F32, kind="ExternalOutput").ap()

    xin = din("xin", [NT, DM])
    mem = din("mem", [256, DM])
    cache_k = din("cache_k", [NSB, 2048, 4, 128])
    cache_v = din("cache_v", [NSB, 2048, 4, 128])
    state_conv = din("state_conv", [NSB, 30, 512])
    cmk = din("cmk", [NSB, 256, 4, 128])
    cmv = din("cmv", [NSB, 256, 4, 128])
    norm_mix_g = din("norm_mix_g", [DM])
    w_in = din("w_in", [DM, 7168])
    q_norm_g = din("q_norm_g", [128])
    k_norm_g = din("k_norm_g", [128])
    conv_wT = din("conv_wT", [512, 31])
    conv_b = din("conv_b", [128, 4])
    conv_ln_g = din("conv_ln_g", [128, 4])
    conv_ln_b = din("conv_ln_b", [128, 4])
    mem_norm_g = din("mem_norm_g", [DM])
    w_mem_kv = din("w_mem_kv", [DM, 1024])
    mq_norm_g = din("mq_norm_g", [128])
    mk_norm_g = din("mk_norm_g", [128])
    w_attn_proj = din("w_attn_proj", [512, DM])
    w_conv_proj = din("w_conv_proj", [512, DM])
    w_mem_proj = din("w_mem_proj", [512, DM])
    w_out = din("w_out", [DM, DM])
    norm_ffn_g = din("norm_ffn_g", [DM])
    w_router = din("w_router", [DM, 36])
    b_router = din("b_router", [36])
    w_eg = din("w_eg", [NEXP, DM, 512])
    w_eu = din("w_eu", [NEXP, DM, 512])
    w_ed = din("w_ed", [NEXP, 512, DM])
    c_ident = din("c_ident", [128, 128])
    c_cs = din("c_cs", [17 * 128, 32])
    c_mask = din("c_mask", [128, 256])
    c_smask = din("c_smask", [128, 7 * 48])
    c_nmask = din("c_nmask", [64, 192])

    y = dout("y", [NT, DM])
    kwp = dout("kwp", [S, 4, 128])
    vwp = dout("vwp", [S, 4, 128])
    convp = dout("convp", [30, 512])
    mkp = dout("mkp", [256, 4, 128])
    mvp = dout("mvp", [256, 4, 128])
    kws = dout("kws", [NSB, 2048, 4, 128])
    vws = dout("vws", [NSB, 2048, 4, 128])
    convs = dout("convs", [NSB, 30, 512])
    DBG = bool(int(os.environ.get("MK_DBG", "0")))
    x1s = nc.dram_tensor("x1s", [NT, DM], F32, kind=("ExternalOutput" if DBG else "Internal")).ap()
    dbg = dout("dbg", [3, 128, 4 * NT]) if DBG else None

    P = Prog()
    with ExitStack() as st:
        ARENA_BYTES = 204 * 1024
        arena_t = st.enter_context(nc.sbuf_tensor("arena", [128, ARENA_BYTES // 2], BF16))
        A = Arena(arena_t, ARENA_BYTES // 2)
        psum = st.enter_context(nc.psum_tensor("psum", [128, 4096], F32))

        def bank(i, n=1):
            return psum[:, i * 512:(i + n) * 512]

        def pk(i):
            return ('pb', i)

        ident_f = A.alloc([128], F32)
        ident_b = A.alloc([128], BF16)
        ones_b = A.alloc([128], BF16)
        ones_f = A.alloc([128], F32)
        cs = A.alloc([17, 32], F32)
        maskf = A.alloc([256], F32)
        maskb = A.alloc([256], BF16)
        smaskf = A.alloc([7 * 48], F32)
        smaskb = A.alloc([7 * 48], BF16)
        nmaskf = A.alloc([192], F32)
        nmaskb = A.alloc([192], BF16)
        g_mix = A.alloc([8], F32)
        g_ffn = A.alloc([8], F32)
        g_mem = A.alloc([8], F32)
        gqk = A.alloc([4, 128], F32)
        gmq = A.alloc([4, 128], F32)
        gmk = A.alloc([4, 128], F32)
        convw = A.alloc([4, 31], F32)
        convb = A.alloc([4], F32)
        lng = A.alloc([4], F32)
        lnb = A.alloc([4], F32)
        neghalf = A.alloc([512], F32)
        wr = A.alloc([8, 36], F32)
        br = A.alloc([36], F32)
        zero_c = A.alloc([1], F32)

        P.dma('sp', 'c0', ident_f, c_ident, writes=['ident_f'])
        P.dma('sp', 'c0', cs, c_cs.rearrange("(t p) c -> p t c", p=128), writes=['cs'])
        P.dma('sp', 'c0', maskf, c_mask, writes=['maskf'])
        P.dma('sp', 'c0', smaskf, c_smask, writes=['smaskf'])
        P.dma('sp', 'c0', nmaskf[0:64, :], c_nmask, writes=['nmaskf'])
        P.dma('sp', 'c0', g_mix, norm_mix_g.rearrange("(p k) -> p k", k=8), writes=['g_mix'])
        P.dma('sp', 'c0', g_ffn, norm_ffn_g.rearrange("(p k) -> p k", k=8), writes=['g_ffn'])
        P.dma('sp', 'c0', g_mem, mem_norm_g.rearrange("(p k) -> p k", k=8), writes=['g_mem'])
        for i in range(4):
            P.dma('sp', 'c0', gqk[:, i, :], (q_norm_g if i < 3 else k_norm_g).partition_broadcast(128), writes=['gqk'])
            P.dma('sp', 'c0', gmq[:, i, :], mq_norm_g.partition_broadcast(128), writes=['gmq'])
            P.dma('sp', 'c0', gmk[:, i, :], mk_norm_g.partition_broadcast(128), writes=['gmk'])
        P.dma('sp', 'c0', convw, conv_wT.rearrange("(j p) i -> p j i", p=128), writes=['convw'])
        P.dma('sp', 'c0', convb, conv_b, writes=['convb'])
        P.dma('sp', 'c0', lng, conv_ln_g, writes=['lng'])
        P.dma('sp', 'c0', lnb, conv_ln_b, writes=['lnb'])
        P.dma('sp', 'c0', wr, w_router.rearrange("(p k) f -> p k f", k=8), writes=['wr'])
        P.dma('sp', 'c0', br, b_router.partition_broadcast(128), writes=['br'])
        P.op('dve', lambda e: e.tensor_copy(out=ident_b, in_=ident_f), ['ident_f'], ['ident_b'])
        P.op('dve', lambda e: e.tensor_copy(out=maskb, in_=maskf), ['maskf'], ['maskb'])
        P.op('dve', lambda e: e.tensor_copy(out=smaskb, in_=smaskf), ['smaskf'], ['smaskb'])
        P.op('dve', lambda e: e.tensor_copy(out=nmaskb[0:64, :], in_=nmaskf[0:64, :]), ['nmaskf'], ['nmaskb'])
        P.op('pool', lambda e: e.memset(ones_b, 1.0), [], ['ones_b'])
        P.op('pool', lambda e: e.memset(ones_f, 1.0), [], ['ones_f'])
        P.op('pool', lambda e: e.memset(neghalf, -0.5), [], ['neghalf'])
        P.op('pool', lambda e: e.memset(zero_c, 0.0), [], ['zero_c'])

        P.barrier()
        def issue_bg():
            NB_ = 2044 * 512
            for b in range(NSB):
                for (dst_, src_, nm_) in ((kws, cache_k, 'bgk'), (vws, cache_v, 'bgv')):
                    P.dma('act', nm_, dst_[b].rearrange("t h d -> (t h d)")[0:NB_].rearrange("(p n) -> p n", p=128),
                          src_[b].rearrange("t h d -> (t h d)")[2048:2048 + NB_].rearrange("(p n) -> p n", p=128), writes=[(nm_, b)])
            P.dma('act', 'bgc', convs[:, 0:26, :], state_conv[:, 4:30, :], writes=['convs_bg'])
        if STOP < 2:
            issue_bg()

        hT = A.alloc([8, NT], BF16)

        def mm(out, pairs, reads, writes):
            def f(e):
                ins = None
                n = len(pairs)
                for i, (l, r) in enumerate(pairs):
                    ins = e.matmul(out=out, lhsT=l, rhs=r, start=(i == 0), stop=(i == n - 1))
                return ins
            P.op('pe', f, reads, writes)

        def rms_rstd(src, rows, width, sqt, ss, tag, src_keys):
            P.op('pool', lambda e: e.memset(ss[0:rows, 0:1], 0.0), [], [tag + 'ss'])
            P.op('act', lambda e: e.activation(out=sqt[0:rows, 0:width], in_=src, func=AF.Square,
                                               accum_out=ss[0:rows, 0:1]),
                 src_keys + [tag + 'ss'], [tag + 'sq', tag + 'ss'])
            P.op('dve', lambda e: e.tensor_scalar(out=ss[0:rows, 0:1], in0=ss[0:rows, 0:1], scalar1=width * EPS,
                                                  scalar2=None, op0=ALU.add), [tag + 'ss'], [tag + 'ss'])
            P.op('pool', lambda e: e.tensor_tensor(out=ss[0:rows, 0:1], in0=ss[0:rows, 0:1], in1=neghalf[0:rows, 0:1],
                                                   op=ALU.pow), [tag + 'ss', 'neghalf'], [tag + 'ss'])

        def norm_transpose(xt, rows, gain, gain_key, dst_b, dst_keys, tag, xkeys, sqt, ss, xn, dst_f=None, dst_f_keys=()):
            rms_rstd(xt, rows, DM, sqt, ss, tag, xkeys)
            P.op('dve', lambda e: e.tensor_scalar(out=xn[0:rows, :], in0=xt, scalar1=ss[0:rows, 0:1], scalar2=32.0,
                                                  op0=ALU.mult, op1=ALU.mult), xkeys + [tag + 'ss'], [tag + 'xn'])
            xv = xn[0:rows, :].rearrange("t (p k) -> t k p", k=8)
            for half in range(2):
                bi = 6 + half

                def tr(e, half=half, bi=bi):
                    ins = None
                    for kk in range(4):
                        k = half * 4 + kk
                        ins = e.transpose(out=bank(bi)[:, kk * 128:kk * 128 + rows], in_=xv[:, k, :],
                                          identity=ident_f[0:rows, 0:rows])
                    return ins
                P.op('pe', tr, [tag + 'xn', 'ident_f'], [pk(bi)])
                pv = bank(bi).rearrange("p (k t) -> p k t", k=4)[:, :, 0:rows]
                gv = gain[:, half * 4:half * 4 + 4].unsqueeze(2).to_broadcast([128, 4, rows])
                P.op('dve', lambda e, pv=pv, gv=gv, half=half: e.tensor_tensor(
                    out=dst_b[:, half * 4:half * 4 + 4, :], in0=pv, in1=gv, op=ALU.mult),
                    [pk(bi), gain_key], list(dst_keys))
                if dst_f is not None:
                    P.op('act', lambda e, half=half, bi=bi: [e.activation(
                        out=dst_f[:, half * 4 + kk, 0:rows], in_=bank(bi)[:, kk * 128:kk * 128 + rows], func=AF.Copy,
                        scale=gain[:, half * 4 + kk:half * 4 + kk + 1]) for kk in range(4)][-1],
                        [pk(bi), gain_key], list(dst_f_keys))

        def headnorm(zps, zkeys, rows, nh, gain, gain_key, out_f, out_key, tag, sqt, ss, rope_cs=None, tmp=None):
            zv = zps.rearrange("t (h d) -> t h d", h=nh)
            P.op('act', lambda e: e.activation(out=sqt[0:rows, 0:nh * 128], in_=zps, func=AF.Square),
                 zkeys, [tag + 'sq'])
            P.op('dve', lambda e: e.reduce_sum(out=ss[0:rows, 0:nh],
                                               in_=sqt[0:rows, 0:nh * 128].rearrange("t (h d) -> t h d", h=nh),
                                               axis=AX.X), [tag + 'sq'], [tag + 'ss'])
            P.op('dve', lambda e: e.tensor_scalar(out=ss[0:rows, 0:nh], in0=ss[0:rows, 0:nh], scalar1=128 * EPS,
                                                  scalar2=None, op0=ALU.add), [tag + 'ss'], [tag + 'ss'])
            P.op('pool', lambda e: e.tensor_tensor(out=ss[0:rows, 0:nh], in0=ss[0:rows, 0:nh], in1=neghalf[0:rows, 0:nh],
                                                   op=ALU.pow), [tag + 'ss', 'neghalf'], [tag + 'ss'])
            rb = ss[0:rows, 0:nh].unsqueeze(2).to_broadcast([rows, nh, 128])
            P.op('dve', lambda e: e.scalar_tensor_tensor(out=out_f, in0=zv, scalar=SQ128, in1=rb,
                                                         op0=ALU.mult, op1=ALU.mult), zkeys + [tag + 'ss'], [out_key])
            P.op('dve', lambda e: e.tensor_tensor(out=out_f, in0=out_f, in1=gain[0:rows], op=ALU.mult),
                 [out_key, gain_key], [out_key])
            if rope_cs is not None:
                cosb = rope_cs[:, 0:16].unsqueeze(1).to_broadcast([rows, nh, 16])
                sinb = rope_cs[:, 16:32].unsqueeze(1).to_broadcast([rows, nh, 16])
                x1 = out_f[:, :, 0:16]
                x2 = out_f[:, :, 16:32]
                t = [tmp[0:rows, i * nh * 16:(i + 1) * nh * 16].rearrange("t (h d) -> t h d", h=nh) for i in range(4)]
                tk = tag + 'rt'
                P.op('dve', lambda e: e.tensor_tensor(out=t[0], in0=x1, in1=cosb, op=ALU.mult), [out_key, 'cs'], [tk + '0'])
                P.op('dve', lambda e: e.tensor_tensor(out=t[1], in0=x2, in1=sinb, op=ALU.mult), [out_key, 'cs'], [tk + '1'])
                P.op('dve', lambda e: e.tensor_tensor(out=t[2], in0=x2, in1=cosb, op=ALU.mult), [out_key, 'cs'], [tk + '2'])
                P.op('dve', lambda e: e.tensor_tensor(out=t[3], in0=x1, in1=sinb, op=ALU.mult), [out_key, 'cs'], [tk + '3'])
                P.op('dve', lambda e: e.tensor_tensor(out=x1, in0=t[0], in1=t[1], op=ALU.subtract),
                     [tk + '0', tk + '1'], [out_key])
                P.op('dve', lambda e: e.tensor_tensor(out=x2, in0=t[2], in1=t[3], op=ALU.add),
                     [tk + '2', tk + '3'], [out_key])

        mark0 = A.off
        xt2 = [A.alloc([DM], F32) for _ in range(2)]
        sqt = A.alloc([DM], F32)
        xn = A.alloc([DM], F32)
        ss = A.alloc([8], F32)
        for ti, (t0, rows) in enumerate(TT):
            sl = ti % 2
            P.dma('sp', 'x%d' % sl, xt2[sl][0:rows, :], xin[t0:t0 + rows, :], writes=[('xt', sl)])
            norm_transpose(xt2[sl][0:rows, :], rows, g_mix, 'g_mix', hT[:, :, t0:t0 + rows], [('hT', ti)],
                           'n1', [('xt', sl)], sqt, ss, xn)
        P.barrier()
        A.off = mark0

        cT = A.alloc([4, NT], BF16)
        mark = A.off
        if STOP >= 2:
            wconv = A.alloc([8, 1024], BF16)
            uP = A.alloc([4, 30 + S], F32)
            uS = A.alloc([4, NSB, 34], F32)
            cP = A.alloc([4, NT], F32)
            sig = [A.alloc([512], F32) for _ in range(2)]
            P.dma('pool', 'w0', wconv, w_in[:, 2560:3584].rearrange("(p k) f -> p k f", k=8), writes=['wconv'])
            P.op('pool', lambda e: e.memset(uP[:, :, 0:30], 0.0), [], ['uPpad'])
            stc = A.alloc([4, 512], F32)
            for bt in range(4):
                P.dma('sp', 'stc%d' % bt, stc[0:120, bt, :], state_conv[4 * bt:4 * bt + 4].rearrange("b i c -> (b i) c"),
                      writes=[('stc', bt)])
            for bt in range(4):
                P.op('pe', lambda e, bt=bt: [e.transpose(out=bank(0)[:, j * 128:j * 128 + 120],
                                                         in_=stc[0:120, bt, j * 128:(j + 1) * 128],
                                                         identity=ident_f[0:120, 0:120]) for j in range(4)][-1],
                     [('stc', bt), 'ident_f'], [pk(0)])
                P.op('dve', lambda e, bt=bt: e.tensor_copy(
                    out=uS[:, :, 4 * bt:4 * bt + 4, 0:30],
                    in_=bank(0).rearrange("p (j x) -> p j x", j=4)[:, :, 0:120].rearrange("p j (b i) -> p j b i", b=4)),
                    [pk(0)], [('uS', bt)])
            cnt = 0
            for gi, (t0, n) in enumerate(TG):
                for j in range(4):
                    ba, bb = 2 + (cnt % 2) * 2, 3 + (cnt % 2) * 2
                    sgi = cnt % 2
                    sg = sig[sgi]
                    cnt += 1
                    rk = ['wconv'] + [('hT', ti) for ti in range(17)]
                    mm(bank(ba)[:, 0:n], [(wconv[:, k, j * 128:(j + 1) * 128], hT[:, k, t0:t0 + n]) for k in range(8)],
                       rk, [pk(ba)])
                    mm(bank(bb)[:, 0:n], [(wconv[:, k, 512 + j * 128:512 + (j + 1) * 128], hT[:, k, t0:t0 + n]) for k in range(8)],
                       rk, [pk(bb)])
                    P.op('act', lambda e, bb=bb, n=n, sg=sg: e.activation(out=sg[:, 0:n], in_=bank(bb)[:, 0:n], func=AF.Sigmoid),
                         [pk(bb)], [('sig', sgi)])
                    if gi < 4:
                        dst = uP[:, j, 30 + t0:30 + t0 + n]
                        P.op('dve', lambda e, ba=ba, n=n, sg=sg, dst=dst: e.tensor_tensor(out=dst, in0=bank(ba)[:, 0:n], in1=sg[:, 0:n], op=ALU.mult),
                             [pk(ba), ('sig', sgi)], [('uP', j)])
                    else:
                        dst = uS[:, j, :, 30:34]
                        P.op('dve', lambda e, ba=ba, sg=sg, dst=dst: e.tensor_tensor(
                            out=dst, in0=bank(ba)[:, 0:NS].rearrange("p (b t) -> p b t", t=4),
                            in1=sg[:, 0:NS].rearrange("p (b t) -> p b t", t=4), op=ALU.mult),
                            [pk(ba), ('sig', sgi)], [('uSn', j)])
            issue_bg()
            cst = A.alloc([512], F32)
            P.op('pe', lambda e: [e.transpose(out=bank(0)[0:30, j * 128:(j + 1) * 128], in_=uP[:, j, S:S + 30],
                                              identity=ident_f) for j in range(4)][-1],
                 [('uP', j) for j in range(4)] + ['ident_f'], [pk(0)])
            P.op('dve', lambda e: e.tensor_copy(out=cst[0:30, :], in_=bank(0)[0:30, :]), [pk(0)], ['cst'])
            P.dma('sp', 'o_cp', convp, cst[0:30, :], reads=['cst'], writes=['convp'])
            unew = A.alloc([4, NS], F32)
            P.op('dve', lambda e: e.tensor_copy(out=unew.rearrange("p j (b t) -> p j b t", t=4), in_=uS[:, :, :, 30:34]),
                 [('uSn', j) for j in range(4)], ['unew'])
            cst2 = A.alloc([512], F32)
            P.op('pe', lambda e: [e.transpose(out=bank(1)[0:NS, j * 128:(j + 1) * 128], in_=unew[:, j, :],
                                              identity=ident_f) for j in range(4)][-1], ['unew', 'ident_f'], [pk(1)])
            P.op('dve', lambda e: e.tensor_copy(out=cst2[0:NS, :], in_=bank(1)[0:NS, :]), [pk(1)], ['cst2'])
            for b in range(NSB):
                P.dma('sp', 'o_cs', convs[b, 26:30, :], cst2[4 * b:4 * b + 4, :], reads=['cst2'], writes=[('convs', b)])
            for j in range(4):
                eng = 'dve'
                for (src, dst, rk, wk) in (
                        (lambda i, j=j: uP[:, j, i:i + S], cP[:, j, 0:S], [('uP', j), 'uPpad'], ('cP', j)),
                        (lambda i, j=j: uS[:, j, :, i:i + 4], cP[:, j, S:NT].rearrange("p (b t) -> p b t", t=4),
                         [('uSn', j)] + [('uS', bt) for bt in range(4)], ('cS', j))):
                    P.op(eng, lambda e, src=src, dst=dst, j=j: e.tensor_scalar(
                        out=dst, in0=src(0), scalar1=convw[:, j, 0:1], scalar2=convb[:, j:j + 1], op0=ALU.mult, op1=ALU.add),
                        rk + ['convw', 'convb'], [wk])
                    for i in range(1, 31):
                        P.op(eng, lambda e, src=src, dst=dst, j=j, i=i: e.scalar_tensor_tensor(
                            out=dst, in0=src(i), scalar=convw[:, j, i:i + 1], in1=dst, op0=ALU.mult, op1=ALU.add),
                            rk + ['convw', wk], [wk])
            sqc = [A.alloc([512], F32) for _ in range(2)]
            mean = A.alloc([512], F32)
            msq = A.alloc([512], F32)
            var = A.alloc([512], F32)
            nt_ = A.alloc([512], F32)
            tln = [A.alloc([512], F32) for _ in range(2)]
            for gi, (t0, n) in enumerate(TG):
                ck = [('cP', j) if gi < 4 else ('cS', j) for j in range(4)]
                mm(bank(0)[:, 0:n], [(ones_f, cP[:, j, t0:t0 + n]) for j in range(4)], ck + ['ones_f'], [pk(0)])
                for j in range(4):
                    P.op('act', lambda e, j=j, t0=t0, n=n: e.activation(out=sqc[j % 2][:, 0:n], in_=cP[:, j, t0:t0 + n], func=AF.Square),
                         [ck[j]], [('sqc', j % 2)])
                    P.op('pe', lambda e, j=j, n=n: e.matmul(out=bank(1)[:, 0:n], lhsT=ones_f, rhs=sqc[j % 2][:, 0:n],
                                                            start=(j == 0), stop=(j == 3)),
                         [('sqc', j % 2), 'ones_f'], [pk(1)])
                P.op('dve', lambda e, n=n: e.tensor_scalar(out=mean[:, 0:n], in0=bank(0)[:, 0:n], scalar1=1.0 / 512, scalar2=None,
                                                           op0=ALU.mult), [pk(0)], ['mean'])
                P.op('dve', lambda e, n=n: e.tensor_tensor(out=msq[:, 0:n], in0=mean[:, 0:n], in1=mean[:, 0:n], op=ALU.mult),
                     ['mean'], ['msq'])
                P.op('dve', lambda e, n=n: e.scalar_tensor_tensor(out=var[:, 0:n], in0=bank(1)[:, 0:n], scalar=1.0 / 512,
                                                                  in1=msq[:, 0:n], op0=ALU.mult, op1=ALU.subtract),
                     [pk(1), 'msq'], ['var'])
                P.op('dve', lambda e, n=n: e.tensor_scalar(out=var[:, 0:n], in0=var[:, 0:n], scalar1=EPS, scalar2=None, op0=ALU.add),
                     ['var'], ['var'])
                vi = var[:, 0:n].bitcast(mybir.dt.int32)
                ryi = msq[:, 0:n].bitcast(mybir.dt.int32)
                P.op('dve', lambda e, vi=vi, ryi=ryi: e.tensor_single_scalar(out=ryi, in_=vi, scalar=1, op=ALU.arith_shift_right),
                     ['var'], ['msq'])
                P.op('dve', lambda e, ryi=ryi: e.tensor_scalar(out=ryi, in0=ryi, scalar1=-1, scalar2=0x5f3759df, op0=ALU.mult, op1=ALU.add),
                     ['msq'], ['msq'])
                for it_ in range(3):
                    P.op('dve', lambda e, n=n: e.tensor_tensor(out=nt_[:, 0:n], in0=msq[:, 0:n], in1=msq[:, 0:n], op=ALU.mult), ['msq'], ['nt_'])
                    P.op('dve', lambda e, n=n: e.tensor_tensor(out=nt_[:, 0:n], in0=nt_[:, 0:n], in1=var[:, 0:n], op=ALU.mult), ['nt_', 'var'], ['nt_'])
                    P.op('dve', lambda e, n=n: e.tensor_scalar(out=nt_[:, 0:n], in0=nt_[:, 0:n], scalar1=-0.5, scalar2=1.5, op0=ALU.mult, op1=ALU.add),
                         ['nt_'], ['nt_'])
                    P.op('dve', lambda e, n=n: e.tensor_tensor(out=msq[:, 0:n], in0=msq[:, 0:n], in1=nt_[:, 0:n], op=ALU.mult), ['msq', 'nt_'], ['msq'])
                P.op('dve', lambda e, n=n: e.tensor_copy(out=var[:, 0:n], in_=msq[:, 0:n]), ['msq'], ['var'])
                for j in range(4):
                    tl = tln[j % 2]
                    P.op('dve', lambda e, j=j, t0=t0, n=n, tl=tl: e.tensor_tensor(out=tl[:, 0:n], in0=cP[:, j, t0:t0 + n], in1=mean[:, 0:n],
                                                                                 op=ALU.subtract), [ck[j], 'mean'], [('tln', j % 2)])
                    P.op('dve', lambda e, n=n, tl=tl: e.tensor_tensor(out=tl[:, 0:n], in0=tl[:, 0:n], in1=var[:, 0:n], op=ALU.mult),
                         [('tln', j % 2), 'var'], [('tln', j % 2)])
                    P.op('act', lambda e, j=j, t0=t0, n=n, tl=tl: e.activation(out=cT[:, j, t0:t0 + n], in_=tl[:, 0:n], func=AF.Silu,
                                                                              scale=lng[:, j:j + 1], bias=lnb[:, j:j + 1]),
                         [('tln', j % 2), 'lng', 'lnb'], [('cT', gi)])
        P.barrier()
        A.off = mark

        moT = A.alloc([4, NT], BF16)
        mark = A.off
        if STOP >= 3:
            wkv = A.alloc([8, 1024], BF16)
            wmq = A.alloc([8, 512], BF16)
            P.dma('pool', 'w0', wkv, w_mem_kv.rearrange("(p k) f -> p k f", k=8), writes=['wkv'])
            P.dma('pool', 'w1', wmq, w_in[:, 3584:4096].rearrange("(p k) f -> p k f", k=8), writes=['wmq'])
            xt2 = [A.alloc([DM], F32) for _ in range(2)]
            sqt = A.alloc([DM], F32)
            xn = A.alloc([DM], F32)
            ss = A.alloc([8], F32)
            memhT = A.alloc([8, 256], BF16)
            mkT = A.alloc([4, 256], BF16)
            mvb = A.alloc([2, 512], BF16)
            kf = [A.alloc([4, 128], F32) for _ in range(2)]
            vf = [A.alloc([512], F32) for _ in range(2)]
            kb = A.alloc([512], BF16)
            for ti in range(2):
                P.dma('sp', 'x%d' % ti, xt2[ti], mem[ti * 128:(ti + 1) * 128, :], writes=[('xt', ti)])
                norm_transpose(xt2[ti], 128, g_mem, 'g_mem', memhT[:, :, ti * 128:(ti + 1) * 128], [('memhT', ti)],
                               'n3', [('xt', ti)], sqt, ss, xn)
            for ti in range(2):
                mm(bank(0), [(memhT[:, k, ti * 128:(ti + 1) * 128], wkv[:, k, 0:512]) for k in range(8)],
                   [('memhT', ti), 'wkv'], [pk(0)])
                mm(bank(1), [(memhT[:, k, ti * 128:(ti + 1) * 128], wkv[:, k, 512:1024]) for k in range(8)],
                   [('memhT', ti), 'wkv'], [pk(1)])
                headnorm(bank(0), [pk(0)], 128, 4, gmk, 'gmk', kf[ti], ('kf', ti), 'h3', sqt, ss)
                P.dma('sp', 'o_mk', mkp[ti * 128:(ti + 1) * 128].rearrange("m h d -> m (h d)"),
                      kf[ti].rearrange("p h d -> p (h d)"), reads=[('kf', ti)], writes=[('mkp', ti)])
                P.op('act', lambda e, ti=ti: e.activation(out=vf[ti], in_=bank(1), func=AF.Copy), [pk(1)], [('vf', ti)])
                P.dma('sp', 'o_mv', mvp[ti * 128:(ti + 1) * 128].rearrange("m h d -> m (h d)"), vf[ti],
                      reads=[('vf', ti)], writes=[('mvp', ti)])
                P.op('dve', lambda e, ti=ti: e.tensor_copy(out=mvb[:, ti, :], in_=vf[ti]), [('vf', ti)], [('mvb', ti)])
                P.op('dve', lambda e, ti=ti: e.tensor_copy(out=kb, in_=kf[ti].rearrange("p h d -> p (h d)")), [('kf', ti)], ['kb'])
                pbv = bank(2).bitcast(BF16)
                P.op('pe', lambda e, pbv=pbv: [e.transpose(out=pbv[:, h * 128:(h + 1) * 128], in_=kb[:, h * 128:(h + 1) * 128],
                                                           identity=ident_b) for h in range(4)][-1], ['kb', 'ident_b'], [pk(2)])
                P.op('dve', lambda e, ti=ti, pbv=pbv: e.tensor_copy(out=mkT[:, :, ti * 128:(ti + 1) * 128],
                                                                   in_=pbv[:, 0:512].rearrange("p (h m) -> p h m", h=4)),
                     [pk(2)], [('mkT', ti)])
            zf = A.alloc([4, 128], F32)
            zb = A.alloc([512], BF16)
            mqT = A.alloc([4, 512], BF16)
            PT = [A.alloc([512], BF16) for _ in range(4)]
            rden = A.alloc([512], F32)
            for ti, (t0, rows) in enumerate(TT):
                mm(bank(0)[0:rows, :], [(hT[:, k, t0:t0 + rows], wmq[:, k, :]) for k in range(8)], ['wmq'], [pk(0)])
                headnorm(bank(0)[0:rows, :], [pk(0)], rows, 4, gmq, 'gmq', zf[0:rows], 'zf', 'h3', sqt, ss)
                P.op('act', lambda e, rows=rows: e.activation(out=zb[0:rows, :], in_=zf[0:rows].rearrange("p h d -> p (h d)"), func=AF.Copy),
                     ['zf'], ['zb'])
                pbv = bank(2).bitcast(BF16)
                P.op('pe', lambda e, pbv=pbv, rows=rows: [e.transpose(out=pbv[:, h * 128:h * 128 + rows], in_=zb[0:rows, h * 128:(h + 1) * 128],
                                                                     identity=ident_b[0:rows, 0:rows]) for h in range(4)][-1],
                     ['zb', 'ident_b'], [pk(2)])
                c0 = (ti % 4) * 128
                P.op('dve', lambda e, pbv=pbv, rows=rows, c0=c0: e.tensor_copy(
                    out=mqT[:, :, c0:c0 + rows], in_=pbv[:, 0:512].rearrange("p (h m) -> p h m", h=4)[:, :, 0:rows]),
                    [pk(2)], [('mqT', ti % 4)])
                if ti % 4 == 3 and ti < 16:
                    g0 = (ti // 4) * 512
                    for h in range(4):
                        for m in range(2):
                            mm(bank(3 + m), [(mkT[:, h, m * 128:(m + 1) * 128], mqT[:, h, :])],
                               [('mkT', 0), ('mkT', 1)] + [('mqT', q) for q in range(4)], [pk(3 + m)])
                            P.op('act', lambda e, m=m: e.activation(out=PT[m], in_=bank(3 + m), func=AF.Exp, scale=ISQ128),
                                 [pk(3 + m)], [('PT', m)])
                        mm(bank(5), [(mvb[:, m, h * 128:(h + 1) * 128], PT[m]) for m in range(2)],
                           [('mvb', 0), ('mvb', 1), ('PT', 0), ('PT', 1)], [pk(5)])
                        mm(bank(1), [(ones_b, PT[m]) for m in range(2)], ['ones_b', ('PT', 0), ('PT', 1)], [pk(1)])
                        P.op('dve', lambda e: e.reciprocal(out=rden, in_=bank(1)), [pk(1)], ['rden'])
                        P.op('dve', lambda e, h=h, g0=g0: e.tensor_tensor(out=moT[:, h, g0:g0 + 512], in0=bank(5), in1=rden, op=ALU.mult),
                             [pk(5), 'rden'], [('moT', h, g0)])
            if STOP >= 31:
                mkf = [A.alloc([2, 512], F32) for _ in range(2)]
                mvf = [A.alloc([2, 512], F32) for _ in range(2)]
                mvb2 = [A.alloc([2, 512], BF16) for _ in range(2)]
                mkTb = A.alloc([4, 256], BF16)
                PTs = A.alloc([2, 16], BF16)
                rds = A.alloc([16], F32)
                for b in range(NSB):
                    sl = b % 2
                    P.dma('sp', 'mk%d' % sl, mkf[sl], cmk[b].rearrange("(m p) h d -> p m (h d)", p=128), writes=[('mkf', sl)])
                    P.dma('sp', 'mv%d' % sl, mvf[sl], cmv[b].rearrange("(m p) h d -> p m (h d)", p=128), writes=[('mvf', sl)])
                    P.op('pool', lambda e, sl=sl: e.tensor_copy(out=mvb2[sl], in_=mvf[sl]), [('mvf', sl)], [('mvb2', sl)])
                    for m in range(2):
                        P.op('pe', lambda e, sl=sl, m=m: [e.transpose(out=bank(6 + m)[:, h * 128:(h + 1) * 128],
                                                                      in_=mkf[sl][:, m, h * 128:(h + 1) * 128], identity=ident_f)
                                                          for h in range(4)][-1], [('mkf', sl), 'ident_f'], [pk(6 + m)])
                        P.op('act' if m == 0 else 'dve', (lambda e, m=m: e.activation(
                            out=mkTb[:, :, m * 128:(m + 1) * 128], in_=bank(6 + m).rearrange("p (h k) -> p h k", h=4), func=AF.Copy))
                            if m == 0 else (lambda e, m=m: e.tensor_copy(
                                out=mkTb[:, :, m * 128:(m + 1) * 128], in_=bank(6 + m).rearrange("p (h k) -> p h k", h=4))),
                            [pk(6 + m)], [('mkTb', m)])
                    def sc(e, b=b):
                        ins = None
                        for m in range(2):
                            for h in range(4):
                                ins = e.matmul(out=bank(3)[:, m * 16 + h * 4:m * 16 + h * 4 + 4], lhsT=mkTb[:, h, m * 128:(m + 1) * 128],
                                               rhs=mqT[:, h, 4 * b:4 * b + 4], start=True, stop=True)
                        return ins
                    P.op('pe', sc, [('mkTb', 0), ('mkTb', 1), ('mqT', 0)], [pk(3)])
                    P.op('act', lambda e: e.activation(out=PTs.rearrange("p m x -> p (m x)"), in_=bank(3)[:, 0:32], func=AF.Exp, scale=ISQ128),
                         [pk(3)], ['PTs'])

                    def pv(e, sl=sl):
                        ins = None
                        for h in range(4):
                            for m in range(2):
                                ins = e.matmul(out=bank(4)[:, h * 4:h * 4 + 4], lhsT=mvb2[sl][:, m, h * 128:(h + 1) * 128],
                                               rhs=PTs[:, m, h * 4:h * 4 + 4], start=(m == 0), stop=(m == 1))
                        for m in range(2):
                            ins = e.matmul(out=bank(5)[:, 0:16], lhsT=ones_b, rhs=PTs[:, m, :], start=(m == 0), stop=(m == 1))
                        return ins
                    P.op('pe', pv, ['PTs', ('mvb2', sl), 'ones_b'], [pk(4), pk(5)])
                    P.op('dve', lambda e: e.reciprocal(out=rds, in_=bank(5)[:, 0:16]), [pk(5)], ['rds'])
                    P.op('dve', lambda e, b=b: e.tensor_tensor(out=moT[:, :, S + 4 * b:S + 4 * b + 4],
                                                               in0=bank(4)[:, 0:16].rearrange("p (h t) -> p h t", h=4),
                                                               in1=rds.rearrange("p (h t) -> p h t", h=4), op=ALU.mult),
                         [pk(4), 'rds'], [('moTs', b)])
        P.barrier()
        A.off = mark

        aoT = A.alloc([4, NT], BF16)
        knew = A.alloc([4, 128], F32)
        vnew = A.alloc([4, 128], F32)
        vnewb = A.alloc([4, 128], BF16)
        kTnew = A.alloc([4, NS], BF16)
        qs_all = A.alloc([4, NSB, 12], BF16)
        mark = A.off
        if STOP >= 4:
            wq2 = [A.alloc([8, 640], BF16) for _ in range(2)]
            sqt2 = [A.alloc([512], F32) for _ in range(2)]
            ss2 = [A.alloc([8], F32) for _ in range(2)]
            qkf = [A.alloc([4, 128], F32) for _ in range(2)]
            qkb2 = [A.alloc([512], BF16) for _ in range(2)]
            rtmp2 = [A.alloc([4 * 64], F32) for _ in range(2)]
            vfo = [A.alloc([128], F32) for _ in range(2)]
            qkT = A.alloc([4, NT], BF16)
            Vn = A.alloc([17, 128], BF16)
            Vp = [A.alloc([16, 128], BF16) for _ in range(2)]
            acc = A.alloc([S], F32)
            dacc = A.alloc([S], F32)
            PTa = [A.alloc([256], BF16) for _ in range(3)]

            def load_w(h):
                w = wq2[h % 2]
                for g in range(3):
                    P.dma('pool', 'wq%d' % (h % 2), w[:, :, g * 128:(g + 1) * 128],
                          w_in[:, g * 512 + h * 128:g * 512 + (h + 1) * 128].rearrange("(p k) f -> p k f", k=8), writes=[('wq', h % 2, g)])
                P.dma('pool', 'wq%d' % (h % 2), w[:, :, 384:512],
                      w_in[:, 1536 + h * 128:1536 + (h + 1) * 128].rearrange("(p k) f -> p k f", k=8), writes=[('wq', h % 2, 3)])
                P.dma('pool', 'wq%d' % (h % 2), w[:, :, 512:640],
                      w_in[:, 2048 + h * 128:2048 + (h + 1) * 128].rearrange("(p k) f -> p k f", k=8), writes=[('wq', h % 2, 4)])
            load_w(0)
            cnt_pt = 0
            for h in range(4):
                if h + 1 < 4:
                    load_w(h + 1)
                w = wq2[h % 2]
                wks_ = [('wq', h % 2, q_) for q_ in range(5)]
                def tile_ops(ti, t0, rows, sl, h=h, w=w, wks_=wks_):
                    b0, b1, b2 = (0, 1, 2) if sl == 0 else (3, 4, 7)
                    sqt, ss, qkb, rtmp = sqt2[sl], ss2[sl], qkb2[sl], rtmp2[sl]
                    mm(bank(b0)[0:rows, :], [(hT[:, k, t0:t0 + rows], w[:, k, 0:512]) for k in range(8)], wks_, [pk(b0)])
                    mm(bank(b1)[0:rows, 0:128], [(hT[:, k, t0:t0 + rows], w[:, k, 512:640]) for k in range(8)], wks_, [pk(b1)])
                    headnorm(bank(b0)[0:rows, :], [pk(b0)], rows, 4, gqk, 'gqk', qkf[sl][0:rows], ('qkf', sl), 'h4_%d' % sl, sqt, ss,
                             rope_cs=cs[0:rows, ti, :], tmp=rtmp)
                    if ti < 16:
                        P.dma('sp', 'o_k%d' % sl, kwp[t0:t0 + rows, h, :], qkf[sl][0:rows, 3, :], reads=[('qkf', sl)], writes=[('kwp', h, ti)])
                        P.op('act', lambda e, sl=sl, rows=rows: e.activation(out=vfo[sl][0:rows, :], in_=bank(b1)[0:rows, 0:128], func=AF.Copy),
                             [pk(b1)], [('vfo', sl)])
                        P.dma('sp', 'o_v%d' % sl, vwp[t0:t0 + rows, h, :], vfo[sl][0:rows, :], reads=[('vfo', sl)], writes=[('vwp', h, ti)])
                        P.op('dve', lambda e, sl=sl, ti=ti: e.tensor_copy(out=Vn[:, ti, :], in_=vfo[sl]), [('vfo', sl)], [('Vn', ti)])
                    else:
                        P.op('act', lambda e, h=h: e.activation(out=vnew[0:NS, h, :], in_=bank(b1)[0:NS, 0:128], func=AF.Copy),
                             [pk(b1)], [('vnew', h)])
                        P.op('dve', lambda e, h=h: e.tensor_copy(out=vnewb[0:NS, h, :], in_=vnew[0:NS, h, :]), [('vnew', h)], [('vnewb', h)])
                        P.op('dve', lambda e, h=h, sl=sl: e.tensor_copy(out=knew[0:NS, h, :], in_=qkf[sl][0:NS, 3, :]), [('qkf', sl)], [('knew', h)])
                    P.op('act', lambda e, sl=sl, rows=rows: e.activation(out=qkb[0:rows, :], in_=qkf[sl][0:rows].rearrange("p h d -> p (h d)"),
                                                                        func=AF.Copy), [('qkf', sl)], [('qkb', sl)])
                    pbv = bank(b2).bitcast(BF16)
                    P.op('pe', lambda e, pbv=pbv, rows=rows: [e.transpose(out=pbv[:, i * 128:i * 128 + rows], in_=qkb[0:rows, i * 128:(i + 1) * 128],
                                                                         identity=ident_b[0:rows, 0:rows]) for i in range(4)][-1],
                         [('qkb', sl), 'ident_b'], [pk(b2)])
                    P.op('dve', lambda e, pbv=pbv, rows=rows, t0=t0: e.tensor_copy(
                        out=qkT[:, :, t0:t0 + rows], in_=pbv[:, 0:512].rearrange("p (i m) -> p i m", i=4)[:, :, 0:rows]),
                        [pk(b2)], [('qkT', ti)])
                for tp in range(0, 17, 2):
                    caps = []
                    for ti in (tp, tp + 1):
                        if ti < 17:
                            P.begin_capture()
                            tile_ops(ti, TT[ti][0], TT[ti][1], ti % 2)
                            caps.append(P.end_capture())
                    P.replay(caps)
                P.op('dve', lambda e, h=h: e.tensor_copy(out=qs_all[:, h].rearrange("p b (g t) -> p b g t", g=3),
                                                         in_=qkT[:, 0:3, S:NT].rearrange("p g (b t) -> p b g t", t=4)),
                     [('qkT', 16)], [('qs_all', h)])
                P.op('dve', lambda e, h=h: e.tensor_copy(out=kTnew[:, h, :], in_=qkT[:, 3, S:NT]), [('qkT', 16)], [('kTnew', h)])
                for gi, dil in ((0, 4), (1, 16)):
                    for blk in range(16):
                        if dil == 4:
                            r, i = blk // 4, blk % 4
                            c0, step = i * 512 + r, 4
                        else:
                            c0, step = blk, 16
                        mm(bank(1)[:, 0:128], [(hT[:, k, ss_(c0, 128, step)], w[:, k, 512:640]) for k in range(8)], wks_, [pk(1)])
                        P.op('act', lambda e, gi=gi, blk=blk: e.activation(out=Vp[gi][:, blk, :], in_=bank(1)[:, 0:128], func=AF.Copy),
                             [pk(1)], [('Vp', gi, blk)])
                P.op('pool', lambda e: e.memset(acc, 0.0), [], ['acc'])
                P.op('pool', lambda e: e.memset(dacc, 0.0), [], ['dacc'])
                allq = [('qkT', ti) for ti in range(16)]
                for g in range(3):
                    dil = (1, 4, 16)[g]
                    nper = 4
                    for grp in range(4):
                        for s4 in range(4):
                            blk = grp * 4 + s4
                            if g == 0:
                                cq = slice(blk * 128, blk * 128 + 128)
                                ckp = slice((blk - 1) * 128, blk * 128) if blk > 0 else None
                                Vown = Vn[:, blk, :]
                                Vprev = Vn[:, blk - 1, :] if blk > 0 else None
                                vkeys = [('Vn', blk)] + ([('Vn', blk - 1)] if blk > 0 else [])
                            elif g == 1:
                                r, i = blk // 4, blk % 4
                                cq = ss_(i * 512 + r, 128, 4)
                                ckp = ss_((i - 1) * 512 + r, 128, 4) if i > 0 else None
                                Vown = Vp[0][:, blk, :]
                                Vprev = Vp[0][:, blk - 1, :] if i > 0 else None
                                vkeys = [('Vp', 0, blk)] + ([('Vp', 0, blk - 1)] if i > 0 else [])
                            else:
                                cq = ss_(blk, 128, 16)
                                ckp = None
                                Vown = Vp[1][:, blk, :]
                                Vprev = None
                                vkeys = [('Vp', 1, blk)]
                            sb_ = 3 + (cnt_pt % 2)
                            pt = PTa[cnt_pt % 3]
                            ptk = ('PTa', cnt_pt % 3)
                            cnt_pt += 1

                            def sc(e, g=g, cq=cq, ckp=ckp, sb_=sb_):
                                q = qkT[:, g, cq]
                                if ckp is not None:
                                    e.matmul(out=bank(sb_)[:, 0:256], lhsT=ident_b, rhs=maskb, start=True, stop=False)
                                    e.matmul(out=bank(sb_)[:, 0:128], lhsT=qkT[:, 3, ckp], rhs=q, start=False, stop=False)
                                else:
                                    e.matmul(out=bank(sb_)[:, 128:256], lhsT=ident_b, rhs=maskb[:, 128:256], start=True, stop=False)
                                return e.matmul(out=bank(sb_)[:, 128:256], lhsT=qkT[:, 3, cq], rhs=q, start=False, stop=True)
                            P.op('pe', sc, allq + ['ident_b', 'maskb'], [pk(sb_)])
                            lo = 0 if ckp is not None else 128
                            P.op('act', lambda e, sb_=sb_, pt=pt, lo=lo: e.activation(out=pt[:, lo:256], in_=bank(sb_)[:, lo:256],
                                                                                       func=AF.Exp, scale=ISQ128), [pk(sb_)], [ptk])

                            def pvf(e, s4=s4, pt=pt, Vown=Vown, Vprev=Vprev):
                                o = bank(5)[:, s4 * 128:(s4 + 1) * 128]
                                d = bank(6)[:, s4 * 128:(s4 + 1) * 128]
                                if Vprev is not None:
                                    e.matmul(out=o, lhsT=Vprev, rhs=pt[:, 0:128], start=True, stop=False)
                                    e.matmul(out=o, lhsT=Vown, rhs=pt[:, 128:256], start=False, stop=True)
                                    e.matmul(out=d, lhsT=ones_b, rhs=pt[:, 0:128], start=True, stop=False)
                                    return e.matmul(out=d, lhsT=ones_b, rhs=pt[:, 128:256], start=False, stop=True)
                                e.matmul(out=o, lhsT=Vown, rhs=pt[:, 128:256], start=True, stop=True)
                                return e.matmul(out=d, lhsT=ones_b, rhs=pt[:, 128:256], start=True, stop=True)
                            P.op('pe', pvf, [ptk, 'ones_b'] + vkeys, [pk(5), pk(6)])
                        if g == 0:
                            av = acc[:, grp * 512:(grp + 1) * 512]
                            dv = dacc[:, grp * 512:(grp + 1) * 512]
                            sh = None
                        elif g == 1:
                            av = acc[:, ss_(grp, 512, 4)]
                            dv = dacc[:, ss_(grp, 512, 4)]
                            sh = None
                        else:
                            av = acc.rearrange("p (u s) -> p s u", s=16)[:, grp * 4:grp * 4 + 4, :]
                            dv = dacc.rearrange("p (u s) -> p s u", s=16)[:, grp * 4:grp * 4 + 4, :]
                            sh = 4
                        o5 = bank(5) if sh is None else bank(5).rearrange("p (s u) -> p s u", s=4)
                        o6 = bank(6) if sh is None else bank(6).rearrange("p (s u) -> p s u", s=4)
                        P.op('dve', lambda e, av=av, o5=o5: e.tensor_tensor(out=av, in0=o5, in1=av, op=ALU.add), [pk(5), 'acc'], ['acc'])
                        P.op('dve', lambda e, dv=dv, o6=o6: e.tensor_tensor(out=dv, in0=o6, in1=dv, op=ALU.add), [pk(6), 'dacc'], ['dacc'])
                P.op('dve', lambda e: e.reciprocal(out=dacc, in_=dacc), ['dacc'], ['dacc'])
                P.op('dve', lambda e, h=h: e.tensor_tensor(out=aoT[:, h, 0:S], in0=acc, in1=dacc, op=ALU.mult), ['acc', 'dacc'], [('aoT', h)])
        P.barrier()
        A.off = mark

        if STOP >= 5:
            kst = [A.alloc([7, 512], F32) for _ in range(2)]
            vst = [A.alloc([7, 512], F32) for _ in range(2)]
            vsb = [A.alloc([7, 512], BF16) for _ in range(2)]
            kTb = A.alloc([4, 7, 128], BF16)
            PTn = A.alloc([4, 192], BF16)
            PTb = [A.alloc([7, 48], BF16) for _ in range(2)]
            for b in range(NSB):
                P.dma('sp', 'o_kn', kws[b, 2044:2048].rearrange("t h d -> t (h d)"), knew[4 * b:4 * b + 4].rearrange("p h d -> p (h d)"),
                      reads=[('knew', h) for h in range(4)], writes=[('kwsn', b)])
                P.dma('sp', 'o_vn', vws[b, 2044:2048].rearrange("t h d -> t (h d)"), vnew[4 * b:4 * b + 4].rearrange("p h d -> p (h d)"),
                      reads=[('vnew', h) for h in range(4)], writes=[('vwsn', b)])
            for hp in range(2):
                def scn(e, hp=hp):
                    ins = None
                    for hh in range(2):
                        h = hp * 2 + hh
                        o = bank(0 + hp)[0:NS, hh * 192:(hh + 1) * 192]
                        e.matmul(out=o, lhsT=ident_b[0:NS, 0:NS], rhs=nmaskb[0:NS, :], start=True, stop=False)
                        ins = e.matmul(out=o, lhsT=kTnew[:, h, :], rhs=qs_all[:, h].rearrange("p b x -> p (b x)"), start=False, stop=True)
                    return ins
                P.op('pe', scn, ['ident_b', 'nmaskb'] + [('kTnew', h) for h in range(4)] + [('qs_all', h) for h in range(4)], [pk(hp)])
                P.op('act', lambda e, hp=hp: e.activation(out=PTn[0:NS, hp * 2:hp * 2 + 2, :].rearrange("p a x -> p (a x)"),
                                                          in_=bank(hp)[0:NS, 0:384], func=AF.Exp, scale=ISQ128), [pk(hp)], [('PTn', hp)])
            def newpv(e):
                ins = None
                for h in range(4):
                    o = bank(4 + h // 2)[:, (h % 2) * 192:(h % 2 + 1) * 192]
                    ins = e.matmul(out=o, lhsT=vnewb[0:NS, h, :], rhs=PTn[0:NS, h, :], start=(h % 2 == 0), stop=False, skip_group_check=True)
                for h in range(4):
                    for half in range(2):
                        d = bank(6 + half)[:, 0:384].rearrange("p (b hx) -> p b hx", b=8)[:, :, h * 12:(h + 1) * 12]
                        ins = e.matmul(out=d, lhsT=ones_b[0:NS, :], rhs=PTn[0:NS, h, half * 96:(half + 1) * 96].rearrange("p (b x) -> p b x", b=8),
                                       start=(h == 0), stop=False, skip_group_check=True)
                return ins
            P.op('pe', newpv, [('PTn', 0), ('PTn', 1), 'ones_b'] + [('vnewb', h) for h in range(4)], [pk(4), pk(5), pk(6), pk(7)])
            for b in range(NSB):
                sl = b % 2
                P.dma('sp', 'ck%d' % sl, kst[sl][:, 0:4, :], cache_k[b, 1536:2048].rearrange("(j p) h d -> p j (h d)", p=128), writes=[('kst', sl, 4)])
                P.dma('sp', 'cv%d' % sl, vst[sl][:, 0:4, :], cache_v[b, 1536:2048].rearrange("(j p) h d -> p j (h d)", p=128), writes=[('vst', sl, 4)])
                for w_ in range(4):
                    P.dma('sp', 'ck%d' % sl, kst[sl][w_ * 32:(w_ + 1) * 32, 4:7, :],
                          cache_k[b, 0:1536].rearrange("(j gl s) h d -> s gl j (h d)", s=16, gl=32)[w_], writes=[('kst', sl, w_)])
                    P.dma('sp', 'cv%d' % sl, vst[sl][w_ * 32:(w_ + 1) * 32, 4:7, :],
                          cache_v[b, 0:1536].rearrange("(j gl s) h d -> s gl j (h d)", s=16, gl=32)[w_], writes=[('vst', sl, w_)])
                P.op('pool', lambda e, sl=sl: e.tensor_copy(out=vsb[sl], in_=vst[sl]), [('vst', sl, q_) for q_ in range(5)], [('vsb', sl)])
                for j in range(7):
                    tb = 2 + (j % 2)
                    P.op('pe', lambda e, sl=sl, j=j, tb=tb: [e.transpose(out=bank(tb)[:, h * 128:(h + 1) * 128],
                                                                        in_=kst[sl][:, j, h * 128:(h + 1) * 128], identity=ident_f)
                                                            for h in range(4)][-1], [('kst', sl, q_) for q_ in range(5)] + ['ident_f'], [pk(tb)])
                    if j % 2 == 0:
                        P.op('act', lambda e, j=j, tb=tb: e.activation(out=kTb[:, :, j, :], in_=bank(tb).rearrange("p (h k) -> p h k", h=4), func=AF.Copy),
                             [pk(tb)], [('kTb', j)])
                    else:
                        P.op('dve', lambda e, j=j, tb=tb: e.tensor_copy(out=kTb[:, :, j, :], in_=bank(tb).rearrange("p (h k) -> p h k", h=4)),
                             [pk(tb)], [('kTb', j)])
                sbk = b % 2

                def scs(e, b=b, sbk=sbk):
                    ins = None
                    o = bank(sbk)[:, 0:336]
                    e.matmul(out=o, lhsT=ident_b, rhs=smaskb, start=True, stop=False)
                    for j in range(7):
                        for h in range(4):
                            ins = e.matmul(out=bank(sbk)[:, j * 48 + h * 12:j * 48 + (h + 1) * 12], lhsT=kTb[:, h, j, :], rhs=qs_all[:, h, b, :],
                                           start=False, stop=(j == 6 and h == 3))
                    return ins
                P.op('pe', scs, ['ident_b', 'smaskb'] + [('kTb', j) for j in range(7)], [pk(sbk)])
                P.op('act', lambda e, sbk=sbk: e.activation(out=PTb[sbk].rearrange("p j x -> p (j x)"), in_=bank(sbk)[:, 0:336],
                                                            func=AF.Exp, scale=ISQ128), [pk(sbk)], [('PTb', sbk)])

                def pvs(e, b=b, sl=sl, sbk=sbk):
                    ins = None
                    last = (b == NSB - 1)
                    for h in range(4):
                        o = bank(4 + h // 2)[:, (h % 2) * 192 + b * 12:(h % 2) * 192 + (b + 1) * 12]
                        for j in range(7):
                            ins = e.matmul(out=o, lhsT=vsb[sl][:, j, h * 128:(h + 1) * 128], rhs=PTb[sbk][:, j, h * 12:(h + 1) * 12],
                                           start=False, stop=(last and j == 6), skip_group_check=True)
                    d = bank(6 + b // 8)[:, (b % 8) * 48:(b % 8 + 1) * 48]
                    for j in range(7):
                        ins = e.matmul(out=d, lhsT=ones_b, rhs=PTb[sbk][:, j, :], start=False, stop=(last and j == 6), skip_group_check=True)
                    return ins
                P.op('pe', pvs, [('PTb', sbk), ('vsb', sl), 'ones_b'], [pk(4), pk(5), pk(6), pk(7)])
            osum = A.alloc([4, NSB, 4], F32)
            dsum = A.alloc([4, NSB, 4], F32)
            for hp in range(2):
                ov = bank(4 + hp)[:, 0:384].rearrange("p (a b g t) -> p a b g t", a=2, b=NSB, g=3)
                P.op('dve', lambda e, hp=hp, ov=ov: e.tensor_copy(out=osum[:, hp * 2:hp * 2 + 2], in_=ov[:, :, :, 0, :]),
                     [pk(4 + hp)], [('osum', hp)])
                P.op('dve', lambda e, hp=hp, ov=ov: e.tensor_tensor(out=osum[:, hp * 2:hp * 2 + 2], in0=osum[:, hp * 2:hp * 2 + 2], in1=ov[:, :, :, 1, :], op=ALU.add),
                     [pk(4 + hp), ('osum', hp)], [('osum', hp)])
                P.op('dve', lambda e, hp=hp, ov=ov: e.tensor_tensor(out=osum[:, hp * 2:hp * 2 + 2], in0=osum[:, hp * 2:hp * 2 + 2], in1=ov[:, :, :, 2, :], op=ALU.add),
                     [pk(4 + hp), ('osum', hp)], [('osum', hp)])
            for half in range(2):
                dv_ = bank(6 + half)[:, 0:384].rearrange("p (b h g t) -> p h b g t", b=8, h=4, g=3)
                ds_ = dsum[:, :, half * 8:half * 8 + 8, :]
                P.op('dve', lambda e, dv_=dv_, ds_=ds_: e.tensor_copy(out=ds_, in_=dv_[:, :, :, 0, :]),
                     [pk(6 + half)], [('dsum', half)])
                P.op('dve', lambda e, dv_=dv_, ds_=ds_: e.tensor_tensor(out=ds_, in0=ds_, in1=dv_[:, :, :, 1, :], op=ALU.add),
                     [pk(6 + half), ('dsum', half)], [('dsum', half)])
                P.op('dve', lambda e, dv_=dv_, ds_=ds_: e.tensor_tensor(out=ds_, in0=ds_, in1=dv_[:, :, :, 2, :], op=ALU.add),
                     [pk(6 + half), ('dsum', half)], [('dsum', half)])
            P.op('dve', lambda e: e.reciprocal(out=dsum, in_=dsum), [('dsum', 0), ('dsum', 1)], ['dsr'])
            P.op('dve', lambda e: e.tensor_tensor(out=aoT[:, :, S:NT].rearrange("p h (b t) -> p h b t", t=4), in0=osum, in1=dsum, op=ALU.mult),
                 ['dsr', ('osum', 0), ('osum', 1)], ['aoTs'])
        P.barrier()
        A.off = mark

        if DBG:
            for i_, t_ in enumerate((aoT, cT, moT)):
                P.dma('pool', 'dbg', dbg[i_], t_.rearrange("p h t -> p (h t)"), writes=[('dbg', i_)])
            P.barrier()
        if STOP >= 6:
            mgT = A.alloc([8, NT], BF16)
            wo = A.alloc([8, DM], BF16)
            mark5 = A.off
            wj = [A.alloc([8, 384], BF16) for _ in range(2)]
            wpj = [A.alloc([4, 384], BF16) for _ in range(2)]
            sg3 = [A.alloc([512], F32) for _ in range(3)]
            t3 = [A.alloc([512], F32) for _ in range(2)]
            P.dma('pool', 'wo', wo, w_out.rearrange("(k p) f -> p k f", p=128), writes=['wo'])

            def load_j(j):
                sl = j % 2
                for br_ in range(3):
                    P.dma('pool', 'wj%d' % sl, wj[sl][:, :, br_ * 128:(br_ + 1) * 128],
                          w_in[:, 4096 + br_ * 1024 + j * 128:4096 + br_ * 1024 + (j + 1) * 128].rearrange("(p k) f -> p k f", k=8),
                          writes=[('wj', sl, br_)])
                for br_, wp in enumerate((w_attn_proj, w_conv_proj, w_mem_proj)):
                    P.dma('pool', 'wj%d' % sl, wpj[sl][:, :, br_ * 128:(br_ + 1) * 128],
                          wp[:, j * 128:(j + 1) * 128].rearrange("(c p) f -> p c f", p=128), writes=[('wpj', sl, br_)])
            load_j(0)
            brT = (aoT, cT, moT)
            for j in range(8):
                if j + 1 < 8:
                    load_j(j + 1)
                sl = j % 2
                for gi, (t0, n) in enumerate(TG):
                    for br_ in range(3):
                        mm(bank(br_)[:, 0:n], [(wj[sl][:, k, br_ * 128:(br_ + 1) * 128], hT[:, k, t0:t0 + n]) for k in range(8)],
                           [('wj', sl, br_)], [pk(br_)])
                        mm(bank(3 + br_)[:, 0:n], [(wpj[sl][:, c, br_ * 128:(br_ + 1) * 128], brT[br_][:, c, t0:t0 + n]) for c in range(4)],
                           [('wpj', sl, br_)], [pk(3 + br_)])
                        P.op('act', lambda e, br_=br_, n=n: e.activation(out=sg3[br_][:, 0:n], in_=bank(br_)[:, 0:n], func=AF.Sigmoid),
                             [pk(br_)], [('sg3', br_)])
                    P.op('dve', lambda e, n=n: e.tensor_tensor(out=t3[0][:, 0:n], in0=bank(3)[:, 0:n], in1=sg3[0][:, 0:n], op=ALU.mult),
                         [pk(3), ('sg3', 0)], [('t3', 0)])
                    P.op('dve', lambda e, n=n: e.tensor_tensor(out=t3[1][:, 0:n], in0=bank(4)[:, 0:n], in1=sg3[1][:, 0:n], op=ALU.mult),
                         [pk(4), ('sg3', 1)], [('t3', 1)])
                    P.op('dve', lambda e, n=n: e.tensor_tensor(out=t3[0][:, 0:n], in0=t3[0][:, 0:n], in1=t3[1][:, 0:n], op=ALU.add),
                         [('t3', 0), ('t3', 1)], [('t3', 0)])
                    P.op('dve', lambda e, n=n: e.tensor_tensor(out=t3[1][:, 0:n], in0=bank(5)[:, 0:n], in1=sg3[2][:, 0:n], op=ALU.mult),
                         [pk(5), ('sg3', 2)], [('t3', 1)])
                    P.op('dve', lambda e, n=n, j=j, t0=t0: e.tensor_tensor(out=mgT[:, j, t0:t0 + n], in0=t3[0][:, 0:n], in1=t3[1][:, 0:n], op=ALU.add),
                         [('t3', 0), ('t3', 1)], [('mgT', j, gi)])
            P.barrier()
            A.off = mark5
            xt2 = [A.alloc([DM], F32) for _ in range(2)]
            x1t = [A.alloc([DM], F32) for _ in range(2)]
            sqt = A.alloc([DM], F32)
            xn = A.alloc([DM], F32)
            ss = A.alloc([8], F32)
            for ti, (t0, rows) in enumerate(TT):
                sl = ti % 2
                P.dma('sp', 'x%d' % sl, xt2[sl][0:rows, :], xin[t0:t0 + rows, :], writes=[('xt', sl)])
                for half in range(2):
                    mm(bank(half)[0:rows, :], [(mgT[:, k, t0:t0 + rows], wo[:, k, half * 512:(half + 1) * 512]) for k in range(8)],
                       ['wo'], [pk(half)])
                    P.op('dve', lambda e, half=half, sl=sl, rows=rows: e.tensor_tensor(
                        out=x1t[sl][0:rows, half * 512:(half + 1) * 512], in0=bank(half)[0:rows, :],
                        in1=xt2[sl][0:rows, half * 512:(half + 1) * 512], op=ALU.add), [pk(half), ('xt', sl)], [('x1t', sl)])
                P.dma('sp', 'x1o%d' % sl, x1s[t0:t0 + rows, :], x1t[sl][0:rows, :], reads=[('x1t', sl)], writes=[('x1s', ti)])
                norm_transpose(x1t[sl][0:rows, :], rows, g_ffn, 'g_ffn', hT[:, :, t0:t0 + rows], [('hT', ti)],
                               'n5', [('x1t', sl)], sqt, ss, xn)
        P.barrier()
        A.off = mark0
        h2T = hT

        if STOP >= 7:
            yacc = A.alloc([17, DM], F32)
            gates = A.alloc([17, 32], F32)
            wg = [A.alloc([8, 512], BF16) for _ in range(2)]
            wu = [A.alloc([8, 512], BF16) for _ in range(2)]
            wd = [A.alloc([4, DM], BF16) for _ in range(2)]
            lgt = A.alloc([36], F32)
            rt = A.alloc([16, 8], F32)

            def load_e(ei):
                sl = ei % 2
                P.dma('pool', 'we%d' % sl, wg[sl], w_eg[ei].rearrange("(p k) f -> p k f", k=8), writes=[('wg', sl)])
                P.dma('pool', 'we%d' % sl, wu[sl], w_eu[ei].rearrange("(p k) f -> p k f", k=8), writes=[('wu', sl)])
                P.dma('pool', 'we%d' % sl, wd[sl], w_ed[ei].rearrange("(c p) f -> p c f", p=128), writes=[('wd', sl)])
            load_e(0)
            ssR = [A.alloc([8], F32) for _ in range(2)]
            lgtR = [lgt, A.alloc([36], F32)]
            rtR = [rt, A.alloc([16, 8], F32)]
            mark6 = A.off
            xnR = [A.alloc([DM], F32) for _ in range(2)]
            sqtR = [A.alloc([DM], F32) for _ in range(2)]
            h2fR = [A.alloc([8, 128], F32) for _ in range(2)]
            P.dma('sp', 'ya0', yacc[:, 0:16, :], x1s[0:S, :].rearrange("(t p) d -> p t d", p=128), writes=[('yacc', ti) for ti in range(16)])
            P.dma('sp', 'ya1', yacc[0:NS, 16, :], x1s[S:NT, :], writes=[('yacc', 16)])
            def rtile(ti, t0, rows, sl):
                xn_, sqt_, ss_, h2f_, lgt_, rt_ = xnR[sl], sqtR[sl], ssR[sl], h2fR[sl], lgtR[sl], rtR[sl]
                b5 = 5 if sl == 0 else 2
                rms_rstd(yacc[0:rows, ti, :], rows, DM, sqt_, ss_, 'n6_%d' % sl, [('yacc', ti)])
                P.op('dve', lambda e, rows=rows, ti=ti: e.tensor_scalar(out=xn_[0:rows, :], in0=yacc[0:rows, ti, :], scalar1=ss_[0:rows, 0:1], scalar2=32.0,
                                                                         op0=ALU.mult, op1=ALU.mult), [('yacc', ti), 'n6_%dss' % sl], [('n6xn', sl)])
                xv = xn_[0:rows, :].rearrange("t (p k) -> t k p", k=8)
                for half in range(2):
                    bi = (6 + half) if sl == 0 else (3 + half)
                    P.op('pe', lambda e, half=half, bi=bi, rows=rows, xv=xv: [e.transpose(
                        out=bank(bi)[:, kk * 128:kk * 128 + rows], in_=xv[:, half * 4 + kk, :], identity=ident_f[0:rows, 0:rows])
                        for kk in range(4)][-1], [('n6xn', sl), 'ident_f'], [pk(bi)])
                    P.op('dve', lambda e, half=half, bi=bi, rows=rows: e.tensor_tensor(
                        out=h2f_[:, half * 4:half * 4 + 4, 0:rows], in0=bank(bi).rearrange("p (k t) -> p k t", k=4)[:, :, 0:rows],
                        in1=g_ffn[:, half * 4:half * 4 + 4].unsqueeze(2).to_broadcast([128, 4, rows]), op=ALU.mult),
                        [pk(bi), 'g_ffn'], [('h2f', sl, half)])
                mm(bank(b5)[0:rows, 0:36], [(h2f_[:, k, 0:rows], wr[:, k, :]) for k in range(8)], [('h2f', sl, 0), ('h2f', sl, 1), 'wr'], [pk(b5)])
                def router_tile(ti, rows):
                    R = rows
                    P.op('dve', lambda e, R=R: e.tensor_tensor(out=lgt_[0:R, :], in0=bank(b5)[0:R, 0:36], in1=br[0:R, :], op=ALU.add), [pk(b5), 'br'], [('lgt', sl)])
                    mx = rt_[0:R, 0, 0:1]; gm = rt_[0:R, 1, 0:4]; sme = rt_[0:R, 0, 1:2]; pgt = rt_[0:R, 0, 2:3]
                    ex4 = rt_[0:R, 2, 0:4]; les = rt_[0:R, 3, :]; m1 = rt_[0:R, 0, 3:4]; oh1 = rt_[0:R, 4, :]; le2 = rt_[0:R, 5, :]
                    m2 = rt_[0:R, 0, 4:5]; oh2 = rt_[0:R, 6, :]; dm_ = rt_[0:R, 0, 5:6]; e21 = rt_[0:R, 0, 6:7]; w1 = rt_[0:R, 0, 7:8]
                    w2 = rt_[0:R, 7, 0:1]; g8 = rt_[0:R, 8, :]; tmp8 = rt_[0:R, 9, :]; den = rt_[0:R, 7, 1:2]
                    K = ('rt', sl)
                    seq = [
                        ('dve', lambda e: e.reduce_max(out=mx, in_=lgt_[0:R, 0:4], axis=AX.X)),
                        ('dve', lambda e: e.tensor_scalar(out=gm, in0=lgt_[0:R, 0:4], scalar1=mx, scalar2=None, op0=ALU.is_equal)),
                        ('dve', lambda e: e.tensor_scalar(out=ex4, in0=lgt_[0:R, 0:4], scalar1=mx, scalar2=None, op0=ALU.subtract)),
                        ('act', lambda e: e.activation(out=ex4, in_=ex4, func=AF.Exp)),
                        ('dve', lambda e: e.reduce_sum(out=sme, in_=ex4, axis=AX.X)),
                        ('dve', lambda e: e.reciprocal(out=pgt, in_=sme)),
                        ('dve', lambda e: e.tensor_scalar(out=les, in0=lgt_[0:R, 4:12], scalar1=gm[:, 0:1], scalar2=None, op0=ALU.mult)),
                    ] + [
                        ('dve', (lambda g: (lambda e: e.scalar_tensor_tensor(out=les, in0=lgt_[0:R, 4 + g * 8:12 + g * 8], scalar=gm[:, g:g + 1], in1=les,
                                                                             op0=ALU.mult, op1=ALU.add)))(g)) for g in range(1, 4)
                    ] + [
                        ('dve', lambda e: e.reduce_max(out=m1, in_=les, axis=AX.X)),
                        ('dve', lambda e: e.tensor_scalar(out=oh1, in0=les, scalar1=m1, scalar2=None, op0=ALU.is_equal)),
                        ('dve', lambda e: e.scalar_tensor_tensor(out=le2, in0=oh1, scalar=-1e30, in1=les, op0=ALU.mult, op1=ALU.add)),
                        ('dve', lambda e: e.reduce_max(out=m2, in_=le2, axis=AX.X)),
                        ('dve', lambda e: e.tensor_scalar(out=oh2, in0=le2, scalar1=m2, scalar2=None, op0=ALU.is_equal)),
                        ('dve', lambda e: e.tensor_tensor(out=dm_, in0=m2, in1=m1, op=ALU.subtract)),
                        ('act', lambda e: e.activation(out=e21, in_=dm_, func=AF.Exp)),
                        ('dve', lambda e: e.tensor_scalar(out=den, in0=e21, scalar1=1.0, scalar2=None, op0=ALU.add)),
                        ('dve', lambda e: e.reciprocal(out=den, in_=den)),
                        ('dve', lambda e: e.tensor_tensor(out=w1, in0=pgt, in1=den, op=ALU.mult)),
                        ('dve', lambda e: e.tensor_tensor(out=w2, in0=w1, in1=e21, op=ALU.mult)),
                        ('dve', lambda e: e.tensor_scalar(out=g8, in0=oh1, scalar1=w1, scalar2=None, op0=ALU.mult)),
                        ('dve', lambda e: e.scalar_tensor_tensor(out=g8, in0=oh2, scalar=w2, in1=g8, op0=ALU.mult, op1=ALU.add)),
                    ] + [
                        ('dve', (lambda g, ti=ti: (lambda e: e.tensor_scalar(out=gates[0:R, ti, g * 8:(g + 1) * 8], in0=g8, scalar1=gm[:, g:g + 1], scalar2=None,
                                                                              op0=ALU.mult)))(g)) for g in range(4)
                    ]
                    for eng_, fn_ in seq:
                        P.op(eng_, fn_, [('lgt', sl), K], [K, ('gates', ti)])

                router_tile(ti, rows)
            for tp in range(0, 17, 2):
                caps = []
                for ti in (tp, tp + 1):
                    if ti < 17:
                        P.begin_capture()
                        rtile(ti, TT[ti][0], TT[ti][1], ti % 2)
                        caps.append(P.end_capture())
                P.replay(caps)
            P.barrier()
            A.off = mark6
            hid = A.alloc([4, NT], BF16)
            sgt = [A.alloc([512], F32) for _ in range(2)]
            cnt = 0
            for ei in range(NEXP):
                if ei + 1 < NEXP:
                    load_e(ei + 1)
                sl = ei % 2
                for gi, (t0, n) in enumerate(TG):
                    for c in range(4):
                        bg, bu = (cnt % 2) * 2, (cnt % 2) * 2 + 1
                        sg = sgt[cnt % 2]
                        sgk = ('sgt', cnt % 2)
                        cnt += 1
                        mm(bank(bg)[:, 0:n], [(wg[sl][:, k, c * 128:(c + 1) * 128], h2T[:, k, t0:t0 + n]) for k in range(8)], [('wg', sl)], [pk(bg)])
                        mm(bank(bu)[:, 0:n], [(wu[sl][:, k, c * 128:(c + 1) * 128], h2T[:, k, t0:t0 + n]) for k in range(8)], [('wu', sl)], [pk(bu)])
                        P.op('act', lambda e, bg=bg, n=n, sg=sg: e.activation(out=sg[:, 0:n], in_=bank(bg)[:, 0:n], func=AF.Silu), [pk(bg)], [sgk])
                        P.op('dve', lambda e, bu=bu, n=n, sg=sg, c=c, t0=t0: e.tensor_tensor(out=hid[:, c, t0:t0 + n], in0=bank(bu)[:, 0:n], in1=sg[:, 0:n],
                                                                                         op=ALU.mult), [pk(bu), sgk], [('hid', gi, c)])
                for ti, (t0, rows) in enumerate(TT):
                    gi = min(ti // 4, 4)
                    for half in range(2):
                        bo = 4 + ((ti * 2 + half) % 4)
                        mm(bank(bo)[0:rows, :], [(hid[:, c, t0:t0 + rows], wd[sl][:, c, half * 512:(half + 1) * 512]) for c in range(4)],
                           [('wd', sl)] + [('hid', gi, c) for c in range(4)], [pk(bo)])
                        eng_ = 'dve' if (half == 0 or ti % 2 == 0) else 'pool'
                        eng_ = 'dve'
                        P.op(eng_, lambda e, bo=bo, rows=rows, ti=ti, half=half, ei=ei: e.scalar_tensor_tensor(
                            out=yacc[0:rows, ti, half * 512:(half + 1) * 512], in0=bank(bo)[0:rows, :], scalar=gates[0:rows, ti, ei:ei + 1],
                            in1=yacc[0:rows, ti, half * 512:(half + 1) * 512], op0=ALU.mult, op1=ALU.add),
                            [pk(bo), ('gates', ti), ('yacc', ti)], [('yacc', ti)])
            for ti, (t0, rows) in enumerate(TT):
                P.dma('sp', 'o_y', y[t0:t0 + rows, :], yacc[0:rows, ti, :], reads=[('yacc', ti)], writes=[('y', ti)])
        P.final_wait_all_dma('sp')
        P.emit(nc, st)
    return nc


def _consts():
    half = 16
    inv_freq = np.power(np.float32(500000.0), -np.arange(half, dtype=np.float32) * np.float32(2.0 / 32)).astype(np.float32)
    pos = np.zeros(17 * 128, np.float32)
    pos[:S] = np.arange(S)
    pos[S:S + NS] = 2048 + (np.arange(NS) % 4)
    ang = pos[:, None].astype(np.float32) * inv_freq[None, :]
    cs = np.concatenate([np.cos(ang), np.sin(ang)], axis=1).astype(np.float32)
    kp = np.arange(128)[:, None]
    qf = np.arange(128)[None, :]
    mask = np.full((128, 256), NEG, np.float32)
    mask[:, 0:128][kp >= qf] = 0.0
    mask[:, 128:256][kp <= qf] = 0.0
    sm = np.full((128, 7, 3, 4), NEG, np.float32)
    for j in range(7):
        for p in range(128):
            if j < 4:
                R = 1536 + 128 * j + p
                for t in range(4):
                    if R >= 1920 + t:
                        sm[p, j, 0, t] = 0.0
                    if R % 4 == t:
                        sm[p, j, 1, t] = 0.0
                    if R % 16 == t:
                        sm[p, j, 2, t] = 0.0
            else:
                w = p // 32
                sm[p, j, 2, w] = 0.0
    smask = np.repeat(sm.reshape(128, 7, 1, 12), 4, axis=2).reshape(128, 7 * 48)
    nm = np.full((64, 16, 3, 4), NEG, np.float32)
    for b in range(16):
        for tp in range(4):
            for t in range(4):
                if tp <= t:
                    nm[b * 4 + tp, b, 0, t] = 0.0
                if tp == t:
                    nm[b * 4 + tp, b, 1, t] = 0.0
                    nm[b * 4 + tp, b, 2, t] = 0.0
    return dict(c_ident=np.eye(128, dtype=np.float32), c_cs=cs, c_mask=mask, c_smask=np.ascontiguousarray(smask),
                c_nmask=np.ascontiguousarray(nm.reshape(64, 192)))


_NC = None


def kernel(x_prompt, x_sample, mem_prompt, cache_k, cache_v, state_conv, cache_mem_k, cache_mem_v,
           norm_mix_g, w_in, q_norm_g, k_norm_g, conv_w, conv_b, conv_ln_g, conv_ln_b,
           mem_norm_g, w_mem_kv, mq_norm_g, mk_norm_g, w_attn_proj, w_conv_proj, w_mem_proj, w_out,
           norm_ffn_g, w_router_group, b_router_group, w_router_expert, b_router_expert,
           w_expert_gate, w_expert_up, w_expert_down):
    global _NC
    f = lambda a: np.ascontiguousarray(np.asarray(a, dtype=np.float32))
    x_prompt, x_sample, mem_prompt = f(x_prompt), f(x_sample), f(mem_prompt)
    cache_k, cache_v, state_conv = f(cache_k), f(cache_v), f(state_conv)
    cache_mem_k, cache_mem_v = f(cache_mem_k), f(cache_mem_v)
    wre = np.transpose(f(w_router_expert)[0], (1, 0, 2)).reshape(DM, 32)
    w_router = np.ascontiguousarray(np.concatenate([f(w_router_group)[0], wre], axis=1))
    b_router = np.ascontiguousarray(np.concatenate([f(b_router_group)[0], f(b_router_expert)[0].reshape(32)]))
    shared = dict(
        norm_mix_g=f(norm_mix_g)[0], w_in=f(w_in)[0], q_norm_g=f(q_norm_g)[0], k_norm_g=f(k_norm_g)[0],
        conv_wT=np.ascontiguousarray(f(conv_w)[0].T), conv_b=np.ascontiguousarray(f(conv_b)[0].reshape(4, 128).T), conv_ln_g=np.ascontiguousarray(f(conv_ln_g)[0].reshape(4, 128).T),
        conv_ln_b=np.ascontiguousarray(f(conv_ln_b)[0].reshape(4, 128).T),
        mem_norm_g=f(mem_norm_g)[0], w_mem_kv=f(w_mem_kv)[0], mq_norm_g=f(mq_norm_g)[0], mk_norm_g=f(mk_norm_g)[0],
        w_attn_proj=f(w_attn_proj)[0], w_conv_proj=f(w_conv_proj)[0], w_mem_proj=f(w_mem_proj)[0], w_out=f(w_out)[0],
        norm_ffn_g=f(norm_ffn_g)[0], w_router=w_router, b_router=b_router,
        w_eg=f(w_expert_gate)[0], w_eu=f(w_expert_up)[0], w_ed=f(w_expert_down)[0])
    shared.update(_consts())
    in_maps = []
    for c in range(NCORES):
        m = dict(shared)
        bs = slice(c * NSB, (c + 1) * NSB)
        m["xin"] = np.ascontiguousarray(np.concatenate([x_prompt[c], x_sample[bs].reshape(NS, DM)], axis=0))
        m["mem"] = mem_prompt[c]
        m["cache_k"] = cache_k[0, bs]
        m["cache_v"] = cache_v[0, bs]
        m["state_conv"] = state_conv[0, bs]
        m["cmk"] = cache_mem_k[0, bs]
        m["cmv"] = cache_mem_v[0, bs]
        in_maps.append(m)
    if os.environ.get("MK_ONLY_MAPS"):
        return in_maps
    if _NC is None:
        _NC = build_nc()
    res = run_bass_kernel_spmd(_NC, in_maps, core_ids=list(range(NCORES)))
    R = res.results
    y_prompt = np.stack([R[c]["y"][:S] for c in range(NCORES)], 0)
    y_sample = np.concatenate([R[c]["y"][S:].reshape(NSB, 4, DM) for c in range(NCORES)], 0)
    st = lambda k: np.stack([R[c][k] for c in range(NCORES)], 0)[None]
    ct = lambda k: np.concatenate([R[c][k] for c in range(NCORES)], 0)[None]
    return (y_prompt, y_sample, st("kwp"), st("vwp"), st("convp"), st("mkp"), st("mvp"), ct("kws"), ct("vws"), ct("convs"))
```

```python
import os
import numpy as np
import concourse.bass as bass
import concourse.mybir as mybir
from concourse.bass_utils import run_bass_kernel_spmd
from contextlib import ExitStack

F32 = mybir.dt.float32
BF16 = mybir.dt.bfloat16
ALU = mybir.AluOpType
AF = mybir.ActivationFunctionType
AX = mybir.AxisListType

NCORES = 8
S = 2048
DM = 1024
NSB = 16
NS = 64
NT = S + NS
EPS = 1e-6
NEG = -30000.0
SQ128 = float(np.sqrt(128.0))
ISQ128 = float(1.0 / np.sqrt(128.0))
TT = [(i * 128, 128) for i in range(16)] + [(S, NS)]
TG = [(i * 512, 512) for i in range(4)] + [(S, NS)]
NEXP = 32
STOP = int(os.environ.get("MK_STOP", "99"))


def ss_(start, count, step):
    return slice(start, start + (count - 1) * step + 1, step)


class Prog:
    ENG = ('pe', 'act', 'dve', 'pool', 'sp')

    def __init__(self):
        self.engs = {e: dict(ops=[], n=0, waited={}) for e in self.ENG}
        self.dsem = {}
        self.lastw = {}
        self.readers = {}

    def _deps(self, reads, writes):
        toks = {}

        def add(tok):
            if tok is None:
                return
            s, v = tok
            if s.startswith('d:') and not s.startswith('d:bg'):
                v = 16 * self.dsem[s[2:]]['n']
            if toks.get(s, 0) < v:
                toks[s] = v
        for k in reads:
            add(self.lastw.get(k))
        for k in writes:
            add(self.lastw.get(k))
            for s, v in self.readers.get(k, {}).items():
                add((s, v))
        return toks

    def _commit(self, tok, reads, writes):
        for k in reads:
            d = self.readers.setdefault(k, {})
            if d.get(tok[0], 0) < tok[1]:
                d[tok[0]] = tok[1]
        for k in writes:
            self.lastw[k] = tok
            self.readers[k] = {}

    def _waits(self, eng, toks):
        E = self.engs[eng]
        waits = []
        for s, v in toks.items():
            if eng == 'pe' and s == 'e:pe':
                continue
            if E['waited'].get(s, 0) >= v:
                continue
            E['waited'][s] = v
            waits.append((s, v))
        return waits

    _cap = None

    def begin_capture(self):
        self._cap = []

    def end_capture(self):
        c, self._cap = self._cap, None
        return c

    def replay(self, lists):
        idx = [0] * len(lists)
        while any(idx[i] < len(L) for i, L in enumerate(lists)):
            for i, L in enumerate(lists):
                if idx[i] < len(L):
                    kind, args = L[idx[i]]
                    idx[i] += 1
                    (self.op if kind == 'op' else self.dma)(*args)

    def op(self, eng, fn, reads=(), writes=()):
        if self._cap is not None:
            self._cap.append(('op', (eng, fn, list(reads), list(writes))))
            return
        E = self.engs[eng]
        waits = self._waits(eng, self._deps(reads, writes))
        E['n'] += 1
        tok = ('e:' + eng, E['n'])
        E['ops'].append((waits, fn, tok))
        self._commit(tok, reads, writes)

    def dma(self, queue, semname, out, in_, reads=(), writes=()):
        if self._cap is not None:
            self._cap.append(('dma', (queue, semname, out, in_, list(reads), list(writes))))
            return
        E = self.engs[queue]
        waits = self._waits(queue, self._deps(reads, writes))
        D = self.dsem.setdefault(semname, dict(n=0))
        D['n'] += 1
        tok = ('d:' + semname, 16 * D['n'])
        E['ops'].append((waits, lambda e, o=out, i=in_: e.dma_start(out=o, in_=i), tok))
        self._commit(tok, reads, writes)

    def barrier(self):
        toks = {('e:' + e): self.engs[e]['n'] for e in self.ENG if self.engs[e]['n'] > 0}
        for name, D in self.dsem.items():
            if name.startswith('bg'):
                continue
            toks['d:' + name] = 16 * D['n']
        for e in self.ENG:
            waits = self._waits(e, dict(toks))
            self.engs[e]['ops'].append((waits, None, None))
        keep_w = {k: v for k, v in self.lastw.items() if v[0].startswith('d:bg')}
        self.lastw = keep_w
        self.readers = {}

    def final_wait_all_dma(self, eng='sp'):
        E = self.engs[eng]
        waits = []
        for name, D in self.dsem.items():
            s = 'd:' + name
            v = 16 * D['n']
            if E['waited'].get(s, 0) < v:
                E['waited'][s] = v
                waits.append((s, v))
        E['ops'].append((waits, None, None))

    def emit(self, nc, stack):
        sems = {}
        for e in self.ENG:
            sems['e:' + e] = stack.enter_context(nc.semaphore('s_' + e))
        for name in self.dsem:
            sems['d:' + name] = stack.enter_context(nc.semaphore('d_' + name))
        block = stack.enter_context(nc.Block())

        def run(engname):
            def f(eng):
                for waits, fn, tok in self.engs[engname]['ops']:
                    for s, v in waits:
                        eng.wait_ge(sems[s], v)
                    if fn is None:
                        continue
                    ins = fn(eng)
                    ins.then_inc(sems[tok[0]], 16 if tok[0].startswith('d:') else 1)
            return f
        block.tensor(run('pe'))
        block.scalar(run('act'))
        block.vector(run('dve'))
        block.gpsimd(run('pool'))
        block.sync(run('sp'))


class Arena:
    def __init__(self, t, nelem_bf16):
        self.t = t
        self.n = nelem_bf16
        self.off = 0

    def alloc(self, shape, dt):
        size = 2 if dt == BF16 else 4
        ne = int(np.prod(shape))
        nb = (ne * size + 31) // 32 * 32
        assert self.off + nb // 2 <= self.n, ("arena overflow", self.off * 2, nb, self.n * 2)
        ap = self.t[:, self.off:self.off + ne * size // 2]
        self.off += nb // 2
        if dt != BF16:
            ap = ap.bitcast(dt)
        if len(shape) == 2:
            ap = ap.rearrange("p (a b) -> p a b", a=shape[0], b=shape[1])
        elif len(shape) == 3:
            ap = ap.rearrange("p (a b c) -> p a b c", a=shape[0], b=shape[1], c=shape[2])
        elif len(shape) == 4:
            ap = ap.rearrange("p (a b c d) -> p a b c d", a=shape[0], b=shape[1], c=shape[2], d=shape[3])
        return ap


def build_nc():
    nc = bass.Bass("TRN2", target_bir_lowering=False)

    def din(name, shape):
        return nc.dram_tensor(name, list(shape), F32, kind="ExternalInput").ap()

    def dout(name, shape):
        return nc.dram_tensor(name, list(shape), F32, kind="ExternalOutput").ap()

    xin = din("xin", [NT, DM])
    mem = din("mem", [256, DM])
    cache_k = din("cache_k", [NSB, 2048, 4, 128])
    cache_v = din("cache_v", [NSB, 2048, 4, 128])
    state_conv = din("state_conv", [NSB, 30, 512])
    cmk = din("cmk", [NSB, 256, 4, 128])
    cmv = din("cmv", [NSB, 256, 4, 128])
    norm_mix_g = din("norm_mix_g", [DM])
    w_in = din("w_in", [DM, 7168])
    q_norm_g = din("q_norm_g", [128])
    k_norm_g = din("k_norm_g", [128])
    conv_wT = din("conv_wT", [512, 31])
    conv_b = din("conv_b", [128, 4])
    conv_ln_g = din("conv_ln_g", [128, 4])
    conv_ln_b = din("conv_ln_b", [128, 4])
    mem_norm_g = din("mem_norm_g", [DM])
    w_mem_kv = din("w_mem_kv", [DM, 1024])
    mq_norm_g = din("mq_norm_g", [128])
    mk_norm_g = din("mk_norm_g", [128])
    w_attn_proj = din("w_attn_proj", [512, DM])
    w_conv_proj = din("w_conv_proj", [512, DM])
    w_mem_proj = din("w_mem_proj", [512, DM])
    w_out = din("w_out", [DM, DM])
    norm_ffn_g = din("norm_ffn_g", [DM])
    w_router = din("w_router", [DM, 36])
    b_router = din("b_router", [36])
    w_eg = din("w_eg", [NEXP, DM, 512])
    w_eu = din("w_eu", [NEXP, DM, 512])
    w_ed = din("w_ed", [NEXP, 512, DM])
    c_ident = din("c_ident", [128, 128])
    c_cs = din("c_cs", [17 * 128, 32])
    c_mask = din("c_mask", [128, 256])
    c_smask = din("c_smask", [128, 7 * 48])
    c_nmask = din("c_nmask", [64, 192])

    y = dout("y", [NT, DM])
    kwp = dout("kwp", [S, 4, 128])
    vwp = dout("vwp", [S, 4, 128])
    convp = dout("convp", [30, 512])
    mkp = dout("mkp", [256, 4, 128])
    mvp = dout("mvp", [256, 4, 128])
    kws = dout("kws", [NSB, 2048, 4, 128])
    vws = dout("vws", [NSB, 2048, 4, 128])
    convs = dout("convs", [NSB, 30, 512])
    DBG = bool(int(os.environ.get("MK_DBG", "0")))
    x1s = nc.dram_tensor("x1s", [NT, DM], F32, kind=("ExternalOutput" if DBG else "Internal")).ap()
    dbg = dout("dbg", [3, 128, 4 * NT]) if DBG else None

    P = Prog()
    with ExitStack() as st:
        ARENA_BYTES = 204 * 1024
        arena_t = st.enter_context(nc.sbuf_tensor("arena", [128, ARENA_BYTES // 2], BF16))
        A = Arena(arena_t, ARENA_BYTES // 2)
        psum = st.enter_context(nc.psum_tensor("psum", [128, 4096], F32))

        def bank(i, n=1):
            return psum[:, i * 512:(i + n) * 512]

        def pk(i):
            return ('pb', i)

        ident_f = A.alloc([128], F32)
        ident_b = A.alloc([128], BF16)
        ones_b = A.alloc([128], BF16)
        ones_f = A.alloc([128], F32)
        cs = A.alloc([17, 32], F32)
        maskf = A.alloc([256], F32)
        maskb = A.alloc([256], BF16)
        smaskf = A.alloc([7 * 48], F32)
        smaskb = A.alloc([7 * 48], BF16)
        nmaskf = A.alloc([192], F32)
        nmaskb = A.alloc([192], BF16)
        g_mix = A.alloc([8], F32)
        g_ffn = A.alloc([8], F32)
        g_mem = A.alloc([8], F32)
        gqk = A.alloc([4, 128], F32)
        gmq = A.alloc([4, 128], F32)
        gmk = A.alloc([4, 128], F32)
        convw = A.alloc([4, 31], F32)
        convb = A.alloc([4], F32)
        lng = A.alloc([4], F32)
        lnb = A.alloc([4], F32)
        neghalf = A.alloc([512], F32)
        wr = A.alloc([8, 36], F32)
        br = A.alloc([36], F32)
        zero_c = A.alloc([1], F32)

        P.dma('sp', 'c0', ident_f, c_ident, writes=['ident_f'])
        P.dma('sp', 'c0', cs, c_cs.rearrange("(t p) c -> p t c", p=128), writes=['cs'])
        P.dma('sp', 'c0', maskf, c_mask, writes=['maskf'])
        P.dma('sp', 'c0', smaskf, c_smask, writes=['smaskf'])
        P.dma('sp', 'c0', nmaskf[0:64, :], c_nmask, writes=['nmaskf'])
        P.dma('sp', 'c0', g_mix, norm_mix_g.rearrange("(p k) -> p k", k=8), writes=['g_mix'])
        P.dma('sp', 'c0', g_ffn, norm_ffn_g.rearrange("(p k) -> p k", k=8), writes=['g_ffn'])
        P.dma('sp', 'c0', g_mem, mem_norm_g.rearrange("(p k) -> p k", k=8), writes=['g_mem'])
        for i in range(4):
            P.dma('sp', 'c0', gqk[:, i, :], (q_norm_g if i < 3 else k_norm_g).partition_broadcast(128), writes=['gqk'])
            P.dma('sp', 'c0', gmq[:, i, :], mq_norm_g.partition_broadcast(128), writes=['gmq'])
            P.dma('sp', 'c0', gmk[:, i, :], mk_norm_g.partition_broadcast(128), writes=['gmk'])
        P.dma('sp', 'c0', convw, conv_wT.rearrange("(j p) i -> p j i", p=128), writes=['convw'])
        P.dma('sp', 'c0', convb, conv_b, writes=['convb'])
        P.dma('sp', 'c0', lng, conv_ln_g, writes=['lng'])
        P.dma('sp', 'c0', lnb, conv_ln_b, writes=['lnb'])
        P.dma('sp', 'c0', wr, w_router.rearrange("(p k) f -> p k f", k=8), writes=['wr'])
        P.dma('sp', 'c0', br, b_router.partition_broadcast(128), writes=['br'])
        P.op('dve', lambda e: e.tensor_copy(out=ident_b, in_=ident_f), ['ident_f'], ['ident_b'])
        P.op('dve', lambda e: e.tensor_copy(out=maskb, in_=maskf), ['maskf'], ['maskb'])
        P.op('dve', lambda e: e.tensor_copy(out=smaskb, in_=smaskf), ['smaskf'], ['smaskb'])
        P.op('dve', lambda e: e.tensor_copy(out=nmaskb[0:64, :], in_=nmaskf[0:64, :]), ['nmaskf'], ['nmaskb'])
        P.op('pool', lambda e: e.memset(ones_b, 1.0), [], ['ones_b'])
        P.op('pool', lambda e: e.memset(ones_f, 1.0), [], ['ones_f'])
        P.op('pool', lambda e: e.memset(neghalf, -0.5), [], ['neghalf'])
        P.op('pool', lambda e: e.memset(zero_c, 0.0), [], ['zero_c'])

        P.barrier()
        def issue_bg():
            NB_ = 2044 * 512
            for b in range(NSB):
                for (dst_, src_, nm_) in ((kws, cache_k, 'bgk'), (vws, cache_v, 'bgv')):
                    P.dma('act', nm_, dst_[b].rearrange("t h d -> (t h d)")[0:NB_].rearrange("(p n) -> p n", p=128),
                          src_[b].rearrange("t h d -> (t h d)")[2048:2048 + NB_].rearrange("(p n) -> p n", p=128), writes=[(nm_, b)])
            P.dma('act', 'bgc', convs[:, 0:26, :], state_conv[:, 4:30, :], writes=['convs_bg'])
        if STOP < 2:
            issue_bg()

        hT = A.alloc([8, NT], BF16)

        def mm(out, pairs, reads, writes):
            def f(e):
                ins = None
                n = len(pairs)
                for i, (l, r) in enumerate(pairs):
                    ins = e.matmul(out=out, lhsT=l, rhs=r, start=(i == 0), stop=(i == n - 1))
                return ins
            P.op('pe', f, reads, writes)

        def rms_rstd(src, rows, width, sqt, ss, tag, src_keys):
            P.op('pool', lambda e: e.memset(ss[0:rows, 0:1], 0.0), [], [tag + 'ss'])
            P.op('act', lambda e: e.activation(out=sqt[0:rows, 0:width], in_=src, func=AF.Square,
                                               accum_out=ss[0:rows, 0:1]),
                 src_keys + [tag + 'ss'], [tag + 'sq', tag + 'ss'])
            P.op('dve', lambda e: e.tensor_scalar(out=ss[0:rows, 0:1], in0=ss[0:rows, 0:1], scalar1=width * EPS,
                                                  scalar2=None, op0=ALU.add), [tag + 'ss'], [tag + 'ss'])
            P.op('pool', lambda e: e.tensor_tensor(out=ss[0:rows, 0:1], in0=ss[0:rows, 0:1], in1=neghalf[0:rows, 0:1],
                                                   op=ALU.pow), [tag + 'ss', 'neghalf'], [tag + 'ss'])

        def norm_transpose(xt, rows, gain, gain_key, dst_b, dst_keys, tag, xkeys, sqt, ss, xn, dst_f=None, dst_f_keys=(), bb=6):
            rms_rstd(xt, rows, DM, sqt, ss, tag, xkeys)
            P.op('dve', lambda e: e.tensor_scalar(out=xn[0:rows, :], in0=xt, scalar1=ss[0:rows, 0:1], scalar2=32.0,
                                                  op0=ALU.mult, op1=ALU.mult), xkeys + [tag + 'ss'], [tag + 'xn'])
            xv = xn[0:rows, :].rearrange("t (p k) -> t k p", k=8)
            for half in range(2):
                bi = bb + half

                def tr(e, half=half, bi=bi):
                    ins = None
                    for kk in range(4):
                        k = half * 4 + kk
                        ins = e.transpose(out=bank(bi)[:, kk * 128:kk * 128 + rows], in_=xv[:, k, :],
                                          identity=ident_f[0:rows, 0:rows])
                    return ins
                P.op('pe', tr, [tag + 'xn', 'ident_f'], [pk(bi)])
                pv = bank(bi).rearrange("p (k t) -> p k t", k=4)[:, :, 0:rows]
                gv = gain[:, half * 4:half * 4 + 4].unsqueeze(2).to_broadcast([128, 4, rows])
                P.op('dve', lambda e, pv=pv, gv=gv, half=half: e.tensor_tensor(
                    out=dst_b[:, half * 4:half * 4 + 4, :], in0=pv, in1=gv, op=ALU.mult),
                    [pk(bi), gain_key], list(dst_keys))
                if dst_f is not None:
                    P.op('act', lambda e, half=half, bi=bi: [e.activation(
                        out=dst_f[:, half * 4 + kk, 0:rows], in_=bank(bi)[:, kk * 128:kk * 128 + rows], func=AF.Copy,
                        scale=gain[:, half * 4 + kk:half * 4 + kk + 1]) for kk in range(4)][-1],
                        [pk(bi), gain_key], list(dst_f_keys))

        def headnorm(zps, zkeys, rows, nh, gain, gain_key, out_f, out_key, tag, sqt, ss, rope_cs=None, tmp=None):
            zv = zps.rearrange("t (h d) -> t h d", h=nh)
            P.op('act', lambda e: e.activation(out=sqt[0:rows, 0:nh * 128], in_=zps, func=AF.Square),
                 zkeys, [tag + 'sq'])
            P.op('dve', lambda e: e.reduce_sum(out=ss[0:rows, 0:nh],
                                               in_=sqt[0:rows, 0:nh * 128].rearrange("t (h d) -> t h d", h=nh),
                                               axis=AX.X), [tag + 'sq'], [tag + 'ss'])
            P.op('dve', lambda e: e.tensor_scalar(out=ss[0:rows, 0:nh], in0=ss[0:rows, 0:nh], scalar1=128 * EPS,
                                                  scalar2=None, op0=ALU.add), [tag + 'ss'], [tag + 'ss'])
            P.op('pool', lambda e: e.tensor_tensor(out=ss[0:rows, 0:nh], in0=ss[0:rows, 0:nh], in1=neghalf[0:rows, 0:nh],
                                                   op=ALU.pow), [tag + 'ss', 'neghalf'], [tag + 'ss'])
            rb = ss[0:rows, 0:nh].unsqueeze(2).to_broadcast([rows, nh, 128])
            P.op('dve', lambda e: e.scalar_tensor_tensor(out=out_f, in0=zv, scalar=SQ128, in1=rb,
                                                         op0=ALU.mult, op1=ALU.mult), zkeys + [tag + 'ss'], [out_key])
            P.op('dve', lambda e: e.tensor_tensor(out=out_f, in0=out_f, in1=gain[0:rows], op=ALU.mult),
                 [out_key, gain_key], [out_key])
            if rope_cs is not None:
                cosb = rope_cs[:, 0:16].unsqueeze(1).to_broadcast([rows, nh, 16])
                sinb = rope_cs[:, 16:32].unsqueeze(1).to_broadcast([rows, nh, 16])
                x1 = out_f[:, :, 0:16]
                x2 = out_f[:, :, 16:32]
                t = [tmp[0:rows, i * nh * 16:(i + 1) * nh * 16].rearrange("t (h d) -> t h d", h=nh) for i in range(4)]
                tk = tag + 'rt'
                P.op('dve', lambda e: e.tensor_tensor(out=t[0], in0=x1, in1=cosb, op=ALU.mult), [out_key, 'cs'], [tk + '0'])
                P.op('dve', lambda e: e.tensor_tensor(out=t[1], in0=x2, in1=sinb, op=ALU.mult), [out_key, 'cs'], [tk + '1'])
                P.op('dve', lambda e: e.tensor_tensor(out=t[2], in0=x2, in1=cosb, op=ALU.mult), [out_key, 'cs'], [tk + '2'])
                P.op('dve', lambda e: e.tensor_tensor(out=t[3], in0=x1, in1=sinb, op=ALU.mult), [out_key, 'cs'], [tk + '3'])
                P.op('dve', lambda e: e.tensor_tensor(out=x1, in0=t[0], in1=t[1], op=ALU.subtract),
                     [tk + '0', tk + '1'], [out_key])
                P.op('dve', lambda e: e.tensor_tensor(out=x2, in0=t[2], in1=t[3], op=ALU.add),
                     [tk + '2', tk + '3'], [out_key])

        mark0 = A.off
        xt2 = [A.alloc([DM], F32) for _ in range(2)]
        sqt1 = [A.alloc([DM], F32) for _ in range(2)]
        xn1 = [A.alloc([DM], F32) for _ in range(2)]
        ss1 = [A.alloc([8], F32) for _ in range(2)]

        def t1(ti, t0, rows, sl):
            P.dma('sp', 'x%d' % sl, xt2[sl][0:rows, :], xin[t0:t0 + rows, :], writes=[('xt', sl)])
            norm_transpose(xt2[sl][0:rows, :], rows, g_mix, 'g_mix', hT[:, :, t0:t0 + rows], [('hT', ti)],
                           'n1_%d' % sl, [('xt', sl)], sqt1[sl], ss1[sl], xn1[sl], bb=(6 if sl == 0 else 4))
        for tp in range(0, 17, 2):
            caps = []
            for ti in (tp, tp + 1):
                if ti < 17:
                    P.begin_capture()
                    t1(ti, TT[ti][0], TT[ti][1], ti % 2)
                    caps.append(P.end_capture())
            P.replay(caps)
        P.barrier()
        A.off = mark0

        cT = A.alloc([4, NT], BF16)
        mark = A.off
        if STOP >= 2:
            wconv = A.alloc([8, 1024], BF16)
            uP = A.alloc([4, 30 + S], F32)
            uS = A.alloc([4, NSB, 34], F32)
            cP = A.alloc([4, NT], F32)
            sig = [A.alloc([512], F32) for _ in range(2)]
            P.dma('pool', 'w0', wconv, w_in[:, 2560:3584].rearrange("(p k) f -> p k f", k=8), writes=['wconv'])
            P.op('pool', lambda e: e.memset(uP[:, :, 0:30], 0.0), [], ['uPpad'])
            stc = A.alloc([4, 512], F32)
            for bt in range(4):
                P.dma('sp', 'stc%d' % bt, stc[0:120, bt, :], state_conv[4 * bt:4 * bt + 4].rearrange("b i c -> (b i) c"),
                      writes=[('stc', bt)])
            for bt in range(4):
                P.op('pe', lambda e, bt=bt: [e.transpose(out=bank(0)[:, j * 128:j * 128 + 120],
                                                         in_=stc[0:120, bt, j * 128:(j + 1) * 128],
                                                         identity=ident_f[0:120, 0:120]) for j in range(4)][-1],
                     [('stc', bt), 'ident_f'], [pk(0)])
                P.op('dve', lambda e, bt=bt: e.tensor_copy(
                    out=uS[:, :, 4 * bt:4 * bt + 4, 0:30],
                    in_=bank(0).rearrange("p (j x) -> p j x", j=4)[:, :, 0:120].rearrange("p j (b i) -> p j b i", b=4)),
                    [pk(0)], [('uS', bt)])
            cnt = 0
            for gi, (t0, n) in enumerate(TG):
                for j in range(4):
                    ba, bb = 2 + (cnt % 2) * 2, 3 + (cnt % 2) * 2
                    sgi = cnt % 2
                    sg = sig[sgi]
                    cnt += 1
                    rk = ['wconv'] + [('hT', ti) for ti in range(17)]
                    mm(bank(ba)[:, 0:n], [(wconv[:, k, j * 128:(j + 1) * 128], hT[:, k, t0:t0 + n]) for k in range(8)],
                       rk, [pk(ba)])
                    mm(bank(bb)[:, 0:n], [(wconv[:, k, 512 + j * 128:512 + (j + 1) * 128], hT[:, k, t0:t0 + n]) for k in range(8)],
                       rk, [pk(bb)])
                    P.op('act', lambda e, bb=bb, n=n, sg=sg: e.activation(out=sg[:, 0:n], in_=bank(bb)[:, 0:n], func=AF.Sigmoid),
                         [pk(bb)], [('sig', sgi)])
                    if gi < 4:
                        dst = uP[:, j, 30 + t0:30 + t0 + n]
                        P.op('dve', lambda e, ba=ba, n=n, sg=sg, dst=dst: e.tensor_tensor(out=dst, in0=bank(ba)[:, 0:n], in1=sg[:, 0:n], op=ALU.mult),
                             [pk(ba), ('sig', sgi)], [('uP', j)])
                    else:
                        dst = uS[:, j, :, 30:34]
                        P.op('dve', lambda e, ba=ba, sg=sg, dst=dst: e.tensor_tensor(
                            out=dst, in0=bank(ba)[:, 0:NS].rearrange("p (b t) -> p b t", t=4),
                            in1=sg[:, 0:NS].rearrange("p (b t) -> p b t", t=4), op=ALU.mult),
                            [pk(ba), ('sig', sgi)], [('uSn', j)])
            issue_bg()
            cst = A.alloc([512], F32)
            P.op('pe', lambda e: [e.transpose(out=bank(0)[0:30, j * 128:(j + 1) * 128], in_=uP[:, j, S:S + 30],
                                              identity=ident_f) for j in range(4)][-1],
                 [('uP', j) for j in range(4)] + ['ident_f'], [pk(0)])
            P.op('dve', lambda e: e.tensor_copy(out=cst[0:30, :], in_=bank(0)[0:30, :]), [pk(0)], ['cst'])
            P.dma('sp', 'o_cp', convp, cst[0:30, :], reads=['cst'], writes=['convp'])
            unew = A.alloc([4, NS], F32)
            P.op('dve', lambda e: e.tensor_copy(out=unew.rearrange("p j (b t) -> p j b t", t=4), in_=uS[:, :, :, 30:34]),
                 [('uSn', j) for j in range(4)], ['unew'])
            cst2 = A.alloc([512], F32)
            P.op('pe', lambda e: [e.transpose(out=bank(1)[0:NS, j * 128:(j + 1) * 128], in_=unew[:, j, :],
                                              identity=ident_f) for j in range(4)][-1], ['unew', 'ident_f'], [pk(1)])
            P.op('dve', lambda e: e.tensor_copy(out=cst2[0:NS, :], in_=bank(1)[0:NS, :]), [pk(1)], ['cst2'])
            for b in range(NSB):
                P.dma('sp', 'o_cs', convs[b, 26:30, :], cst2[4 * b:4 * b + 4, :], reads=['cst2'], writes=[('convs', b)])
            for j in range(4):
                eng = 'dve'
                for (src, dst, rk, wk) in (
                        (lambda i, j=j: uP[:, j, i:i + S], cP[:, j, 0:S], [('uP', j), 'uPpad'], ('cP', j)),
                        (lambda i, j=j: uS[:, j, :, i:i + 4], cP[:, j, S:NT].rearrange("p (b t) -> p b t", t=4),
                         [('uSn', j)] + [('uS', bt) for bt in range(4)], ('cS', j))):
                    P.op(eng, lambda e, src=src, dst=dst, j=j: e.tensor_scalar(
                        out=dst, in0=src(0), scalar1=convw[:, j, 0:1], scalar2=convb[:, j:j + 1], op0=ALU.mult, op1=ALU.add),
                        rk + ['convw', 'convb'], [wk])
                    for i in range(1, 31):
                        P.op(eng, lambda e, src=src, dst=dst, j=j, i=i: e.scalar_tensor_tensor(
                            out=dst, in0=src(i), scalar=convw[:, j, i:i + 1], in1=dst, op0=ALU.mult, op1=ALU.add),
                            rk + ['convw', wk], [wk])
            sqc = [A.alloc([512], F32) for _ in range(2)]
            mean = A.alloc([512], F32)
            msq = A.alloc([512], F32)
            var = A.alloc([512], F32)
            nt_ = A.alloc([512], F32)
            tln = [A.alloc([512], F32) for _ in range(2)]
            for gi, (t0, n) in enumerate(TG):
                ck = [('cP', j) if gi < 4 else ('cS', j) for j in range(4)]
                mm(bank(0)[:, 0:n], [(ones_f, cP[:, j, t0:t0 + n]) for j in range(4)], ck + ['ones_f'], [pk(0)])
                for j in range(4):
                    P.op('act', lambda e, j=j, t0=t0, n=n: e.activation(out=sqc[j % 2][:, 0:n], in_=cP[:, j, t0:t0 + n], func=AF.Square),
                         [ck[j]], [('sqc', j % 2)])
                    P.op('pe', lambda e, j=j, n=n: e.matmul(out=bank(1)[:, 0:n], lhsT=ones_f, rhs=sqc[j % 2][:, 0:n],
                                                            start=(j == 0), stop=(j == 3)),
                         [('sqc', j % 2), 'ones_f'], [pk(1)])
                P.op('dve', lambda e, n=n: e.tensor_scalar(out=mean[:, 0:n], in0=bank(0)[:, 0:n], scalar1=1.0 / 512, scalar2=None,
                                                           op0=ALU.mult), [pk(0)], ['mean'])
                P.op('dve', lambda e, n=n: e.tensor_tensor(out=msq[:, 0:n], in0=mean[:, 0:n], in1=mean[:, 0:n], op=ALU.mult),
                     ['mean'], ['msq'])
                P.op('dve', lambda e, n=n: e.scalar_tensor_tensor(out=var[:, 0:n], in0=bank(1)[:, 0:n], scalar=1.0 / 512,
                                                                  in1=msq[:, 0:n], op0=ALU.mult, op1=ALU.subtract),
                     [pk(1), 'msq'], ['var'])
                P.op('dve', lambda e, n=n: e.tensor_scalar(out=var[:, 0:n], in0=var[:, 0:n], scalar1=EPS, scalar2=None, op0=ALU.add),
                     ['var'], ['var'])
                vi = var[:, 0:n].bitcast(mybir.dt.int32)
                ryi = msq[:, 0:n].bitcast(mybir.dt.int32)
                P.op('dve', lambda e, vi=vi, ryi=ryi: e.tensor_single_scalar(out=ryi, in_=vi, scalar=1, op=ALU.arith_shift_right),
                     ['var'], ['msq'])
                P.op('dve', lambda e, ryi=ryi: e.tensor_scalar(out=ryi, in0=ryi, scalar1=-1, scalar2=0x5f3759df, op0=ALU.mult, op1=ALU.add),
                     ['msq'], ['msq'])
                for it_ in range(3):
                    P.op('dve', lambda e, n=n: e.tensor_tensor(out=nt_[:, 0:n], in0=msq[:, 0:n], in1=msq[:, 0:n], op=ALU.mult), ['msq'], ['nt_'])
                    P.op('dve', lambda e, n=n: e.tensor_tensor(out=nt_[:, 0:n], in0=nt_[:, 0:n], in1=var[:, 0:n], op=ALU.mult), ['nt_', 'var'], ['nt_'])
                    P.op('dve', lambda e, n=n: e.tensor_scalar(out=nt_[:, 0:n], in0=nt_[:, 0:n], scalar1=-0.5, scalar2=1.5, op0=ALU.mult, op1=ALU.add),
                         ['nt_'], ['nt_'])
                    P.op('dve', lambda e, n=n: e.tensor_tensor(out=msq[:, 0:n], in0=msq[:, 0:n], in1=nt_[:, 0:n], op=ALU.mult), ['msq', 'nt_'], ['msq'])
                P.op('dve', lambda e, n=n: e.tensor_copy(out=var[:, 0:n], in_=msq[:, 0:n]), ['msq'], ['var'])
                for j in range(4):
                    tl = tln[j % 2]
                    P.op('dve', lambda e, j=j, t0=t0, n=n, tl=tl: e.tensor_tensor(out=tl[:, 0:n], in0=cP[:, j, t0:t0 + n], in1=mean[:, 0:n],
                                                                                 op=ALU.subtract), [ck[j], 'mean'], [('tln', j % 2)])
                    P.op('dve', lambda e, n=n, tl=tl: e.tensor_tensor(out=tl[:, 0:n], in0=tl[:, 0:n], in1=var[:, 0:n], op=ALU.mult),
                         [('tln', j % 2), 'var'], [('tln', j % 2)])
                    P.op('act', lambda e, j=j, t0=t0, n=n, tl=tl: e.activation(out=cT[:, j, t0:t0 + n], in_=tl[:, 0:n], func=AF.Silu,
                                                                              scale=lng[:, j:j + 1], bias=lnb[:, j:j + 1]),
                         [('tln', j % 2), 'lng', 'lnb'], [('cT', gi)])
        P.barrier()
        A.off = mark

        moT = A.alloc([4, NT], BF16)
        mark = A.off
        if STOP >= 3:
            wkv = A.alloc([8, 1024], BF16)
            wmq = A.alloc([8, 512], BF16)
            P.dma('pool', 'w0', wkv, w_mem_kv.rearrange("(p k) f -> p k f", k=8), writes=['wkv'])
            P.dma('pool', 'w1', wmq, w_in[:, 3584:4096].rearrange("(p k) f -> p k f", k=8), writes=['wmq'])
            xt2 = [A.alloc([DM], F32) for _ in range(2)]
            sqt = A.alloc([DM], F32)
            xn = A.alloc([DM], F32)
            ss = A.alloc([8], F32)
            memhT = A.alloc([8, 256], BF16)
            mkT = A.alloc([4, 256], BF16)
            mvb = A.alloc([2, 512], BF16)
            kf = [A.alloc([4, 128], F32) for _ in range(2)]
            vf = [A.alloc([512], F32) for _ in range(2)]
            kb = A.alloc([512], BF16)
            for ti in range(2):
                P.dma('sp', 'x%d' % ti, xt2[ti], mem[ti * 128:(ti + 1) * 128, :], writes=[('xt', ti)])
                norm_transpose(xt2[ti], 128, g_mem, 'g_mem', memhT[:, :, ti * 128:(ti + 1) * 128], [('memhT', ti)],
                               'n3', [('xt', ti)], sqt, ss, xn)
            for ti in range(2):
                mm(bank(0), [(memhT[:, k, ti * 128:(ti + 1) * 128], wkv[:, k, 0:512]) for k in range(8)],
                   [('memhT', ti), 'wkv'], [pk(0)])
                mm(bank(1), [(memhT[:, k, ti * 128:(ti + 1) * 128], wkv[:, k, 512:1024]) for k in range(8)],
                   [('memhT', ti), 'wkv'], [pk(1)])
                headnorm(bank(0), [pk(0)], 128, 4, gmk, 'gmk', kf[ti], ('kf', ti), 'h3', sqt, ss)
                P.dma('sp', 'o_mk', mkp[ti * 128:(ti + 1) * 128].rearrange("m h d -> m (h d)"),
                      kf[ti].rearrange("p h d -> p (h d)"), reads=[('kf', ti)], writes=[('mkp', ti)])
                P.op('act', lambda e, ti=ti: e.activation(out=vf[ti], in_=bank(1), func=AF.Copy), [pk(1)], [('vf', ti)])
                P.dma('sp', 'o_mv', mvp[ti * 128:(ti + 1) * 128].rearrange("m h d -> m (h d)"), vf[ti],
                      reads=[('vf', ti)], writes=[('mvp', ti)])
                P.op('dve', lambda e, ti=ti: e.tensor_copy(out=mvb[:, ti, :], in_=vf[ti]), [('vf', ti)], [('mvb', ti)])
                P.op('dve', lambda e, ti=ti: e.tensor_copy(out=kb, in_=kf[ti].rearrange("p h d -> p (h d)")), [('kf', ti)], ['kb'])
                pbv = bank(2).bitcast(BF16)
                P.op('pe', lambda e, pbv=pbv: [e.transpose(out=pbv[:, h * 128:(h + 1) * 128], in_=kb[:, h * 128:(h + 1) * 128],
                                                           identity=ident_b) for h in range(4)][-1], ['kb', 'ident_b'], [pk(2)])
                P.op('dve', lambda e, ti=ti, pbv=pbv: e.tensor_copy(out=mkT[:, :, ti * 128:(ti + 1) * 128],
                                                                   in_=pbv[:, 0:512].rearrange("p (h m) -> p h m", h=4)),
                     [pk(2)], [('mkT', ti)])
            zf = A.alloc([4, 128], F32)
            zb = A.alloc([512], BF16)
            mqT = A.alloc([4, 512], BF16)
            PT = [A.alloc([512], BF16) for _ in range(4)]
            rden = A.alloc([512], F32)
            for ti, (t0, rows) in enumerate(TT):
                mm(bank(0)[0:rows, :], [(hT[:, k, t0:t0 + rows], wmq[:, k, :]) for k in range(8)], ['wmq'], [pk(0)])
                headnorm(bank(0)[0:rows, :], [pk(0)], rows, 4, gmq, 'gmq', zf[0:rows], 'zf', 'h3', sqt, ss)
                P.op('act', lambda e, rows=rows: e.activation(out=zb[0:rows, :], in_=zf[0:rows].rearrange("p h d -> p (h d)"), func=AF.Copy),
                     ['zf'], ['zb'])
                pbv = bank(2).bitcast(BF16)
                P.op('pe', lambda e, pbv=pbv, rows=rows: [e.transpose(out=pbv[:, h * 128:h * 128 + rows], in_=zb[0:rows, h * 128:(h + 1) * 128],
                                                                     identity=ident_b[0:rows, 0:rows]) for h in range(4)][-1],
                     ['zb', 'ident_b'], [pk(2)])
                c0 = (ti % 4) * 128
                P.op('dve', lambda e, pbv=pbv, rows=rows, c0=c0: e.tensor_copy(
                    out=mqT[:, :, c0:c0 + rows], in_=pbv[:, 0:512].rearrange("p (h m) -> p h m", h=4)[:, :, 0:rows]),
                    [pk(2)], [('mqT', ti % 4)])
                if ti % 4 == 3 and ti < 16:
                    g0 = (ti // 4) * 512
                    for h in range(4):
                        for m in range(2):
                            mm(bank(3 + m), [(mkT[:, h, m * 128:(m + 1) * 128], mqT[:, h, :])],
                               [('mkT', 0), ('mkT', 1)] + [('mqT', q) for q in range(4)], [pk(3 + m)])
                            P.op('act', lambda e, m=m: e.activation(out=PT[m], in_=bank(3 + m), func=AF.Exp, scale=ISQ128),
                                 [pk(3 + m)], [('PT', m)])
                        mm(bank(5), [(mvb[:, m, h * 128:(h + 1) * 128], PT[m]) for m in range(2)],
                           [('mvb', 0), ('mvb', 1), ('PT', 0), ('PT', 1)], [pk(5)])
                        mm(bank(1), [(ones_b, PT[m]) for m in range(2)], ['ones_b', ('PT', 0), ('PT', 1)], [pk(1)])
                        P.op('dve', lambda e: e.reciprocal(out=rden, in_=bank(1)), [pk(1)], ['rden'])
                        P.op('dve', lambda e, h=h, g0=g0: e.tensor_tensor(out=moT[:, h, g0:g0 + 512], in0=bank(5), in1=rden, op=ALU.mult),
                             [pk(5), 'rden'], [('moT', h, g0)])
            if STOP >= 31:
                mkf = [A.alloc([2, 512], F32) for _ in range(2)]
                mvf = [A.alloc([2, 512], F32) for _ in range(2)]
                mvb2 = [A.alloc([2, 512], BF16) for _ in range(2)]
                mkTb = A.alloc([4, 256], BF16)
                PTs = A.alloc([2, 16], BF16)
                rds = A.alloc([16], F32)
                for b in range(NSB):
                    sl = b % 2
                    P.dma('sp', 'mk%d' % sl, mkf[sl], cmk[b].rearrange("(m p) h d -> p m (h d)", p=128), writes=[('mkf', sl)])
                    P.dma('sp', 'mv%d' % sl, mvf[sl], cmv[b].rearrange("(m p) h d -> p m (h d)", p=128), writes=[('mvf', sl)])
                    P.op('pool', lambda e, sl=sl: e.tensor_copy(out=mvb2[sl], in_=mvf[sl]), [('mvf', sl)], [('mvb2', sl)])
                    for m in range(2):
                        P.op('pe', lambda e, sl=sl, m=m: [e.transpose(out=bank(6 + m)[:, h * 128:(h + 1) * 128],
                                                                      in_=mkf[sl][:, m, h * 128:(h + 1) * 128], identity=ident_f)
                                                          for h in range(4)][-1], [('mkf', sl), 'ident_f'], [pk(6 + m)])
                        P.op('act' if m == 0 else 'dve', (lambda e, m=m: e.activation(
                            out=mkTb[:, :, m * 128:(m + 1) * 128], in_=bank(6 + m).rearrange("p (h k) -> p h k", h=4), func=AF.Copy))
                            if m == 0 else (lambda e, m=m: e.tensor_copy(
                                out=mkTb[:, :, m * 128:(m + 1) * 128], in_=bank(6 + m).rearrange("p (h k) -> p h k", h=4))),
                            [pk(6 + m)], [('mkTb', m)])
                    def sc(e, b=b):
                        ins = None
                        for m in range(2):
                            for h in range(4):
                                ins = e.matmul(out=bank(3)[:, m * 16 + h * 4:m * 16 + h * 4 + 4], lhsT=mkTb[:, h, m * 128:(m + 1) * 128],
                                               rhs=mqT[:, h, 4 * b:4 * b + 4], start=True, stop=True)
                        return ins
                    P.op('pe', sc, [('mkTb', 0), ('mkTb', 1), ('mqT', 0)], [pk(3)])
                    P.op('act', lambda e: e.activation(out=PTs.rearrange("p m x -> p (m x)"), in_=bank(3)[:, 0:32], func=AF.Exp, scale=ISQ128),
                         [pk(3)], ['PTs'])

                    def pv(e, sl=sl):
                        ins = None
                        for h in range(4):
                            for m in range(2):
                                ins = e.matmul(out=bank(4)[:, h * 4:h * 4 + 4], lhsT=mvb2[sl][:, m, h * 128:(h + 1) * 128],
                                               rhs=PTs[:, m, h * 4:h * 4 + 4], start=(m == 0), stop=(m == 1))
                        for m in range(2):
                            ins = e.matmul(out=bank(5)[:, 0:16], lhsT=ones_b, rhs=PTs[:, m, :], start=(m == 0), stop=(m == 1))
                        return ins
                    P.op('pe', pv, ['PTs', ('mvb2', sl), 'ones_b'], [pk(4), pk(5)])
                    P.op('dve', lambda e: e.reciprocal(out=rds, in_=bank(5)[:, 0:16]), [pk(5)], ['rds'])
                    P.op('dve', lambda e, b=b: e.tensor_tensor(out=moT[:, :, S + 4 * b:S + 4 * b + 4],
                                                               in0=bank(4)[:, 0:16].rearrange("p (h t) -> p h t", h=4),
                                                               in1=rds.rearrange("p (h t) -> p h t", h=4), op=ALU.mult),
                         [pk(4), 'rds'], [('moTs', b)])
        P.barrier()
        A.off = mark

        aoT = A.alloc([4, NT], BF16)
        knew = A.alloc([4, 128], F32)
        vnew = A.alloc([4, 128], F32)
        vnewb = A.alloc([4, 128], BF16)
        kTnew = A.alloc([4, NS], BF16)
        qs_all = A.alloc([4, NSB, 12], BF16)
        mark = A.off
        if STOP >= 4:
            wq2 = [A.alloc([8, 640], BF16) for _ in range(2)]
            sqt2 = [A.alloc([512], F32) for _ in range(2)]
            ss2 = [A.alloc([8], F32) for _ in range(2)]
            qkf = [A.alloc([4, 128], F32) for _ in range(2)]
            qkb2 = [A.alloc([512], BF16) for _ in range(2)]
            rtmp2 = [A.alloc([4 * 64], F32) for _ in range(2)]
            vfo = [A.alloc([128], F32) for _ in range(2)]
            qkT = A.alloc([4, NT], BF16)
            Vn = A.alloc([17, 128], BF16)
            Vp = [A.alloc([16, 128], BF16) for _ in range(2)]
            acc = A.alloc([S], F32)
            dacc = A.alloc([S], F32)
            PTa = [A.alloc([256], BF16) for _ in range(3)]

            def load_w(h):
                w = wq2[h % 2]
                for g in range(3):
                    P.dma('pool', 'wq%d' % (h % 2), w[:, :, g * 128:(g + 1) * 128],
                          w_in[:, g * 512 + h * 128:g * 512 + (h + 1) * 128].rearrange("(p k) f -> p k f", k=8), writes=[('wq', h % 2, g)])
                P.dma('pool', 'wq%d' % (h % 2), w[:, :, 384:512],
                      w_in[:, 1536 + h * 128:1536 + (h + 1) * 128].rearrange("(p k) f -> p k f", k=8), writes=[('wq', h % 2, 3)])
                P.dma('pool', 'wq%d' % (h % 2), w[:, :, 512:640],
                      w_in[:, 2048 + h * 128:2048 + (h + 1) * 128].rearrange("(p k) f -> p k f", k=8), writes=[('wq', h % 2, 4)])
            load_w(0)
            cnt_pt = 0
            for h in range(4):
                if h + 1 < 4:
                    load_w(h + 1)
                w = wq2[h % 2]
                wks_ = [('wq', h % 2, q_) for q_ in range(5)]
                def tile_ops(ti, t0, rows, sl, h=h, w=w, wks_=wks_):
                    b0, b1, b2 = (0, 1, 2) if sl == 0 else (3, 4, 7)
                    sqt, ss, qkb, rtmp = sqt2[sl], ss2[sl], qkb2[sl], rtmp2[sl]
                    mm(bank(b0)[0:rows, :], [(hT[:, k, t0:t0 + rows], w[:, k, 0:512]) for k in range(8)], wks_, [pk(b0)])
                    mm(bank(b1)[0:rows, 0:128], [(hT[:, k, t0:t0 + rows], w[:, k, 512:640]) for k in range(8)], wks_, [pk(b1)])
                    headnorm(bank(b0)[0:rows, :], [pk(b0)], rows, 4, gqk, 'gqk', qkf[sl][0:rows], ('qkf', sl), 'h4_%d' % sl, sqt, ss,
                             rope_cs=cs[0:rows, ti, :], tmp=rtmp)
                    if ti < 16:
                        P.dma('sp', 'o_k%d' % sl, kwp[t0:t0 + rows, h, :], qkf[sl][0:rows, 3, :], reads=[('qkf', sl)], writes=[('kwp', h, ti)])
                        P.op('act', lambda e, sl=sl, rows=rows: e.activation(out=vfo[sl][0:rows, :], in_=bank(b1)[0:rows, 0:128], func=AF.Copy),
                             [pk(b1)], [('vfo', sl)])
                        P.dma('sp', 'o_v%d' % sl, vwp[t0:t0 + rows, h, :], vfo[sl][0:rows, :], reads=[('vfo', sl)], writes=[('vwp', h, ti)])
                        P.op('dve', lambda e, sl=sl, ti=ti: e.tensor_copy(out=Vn[:, ti, :], in_=vfo[sl]), [('vfo', sl)], [('Vn', ti)])
                    else:
                        P.op('act', lambda e, h=h: e.activation(out=vnew[0:NS, h, :], in_=bank(b1)[0:NS, 0:128], func=AF.Copy),
                             [pk(b1)], [('vnew', h)])
                        P.op('dve', lambda e, h=h: e.tensor_copy(out=vnewb[0:NS, h, :], in_=vnew[0:NS, h, :]), [('vnew', h)], [('vnewb', h)])
                        P.op('dve', lambda e, h=h, sl=sl: e.tensor_copy(out=knew[0:NS, h, :], in_=qkf[sl][0:NS, 3, :]), [('qkf', sl)], [('knew', h)])
                    P.op('act', lambda e, sl=sl, rows=rows: e.activation(out=qkb[0:rows, :], in_=qkf[sl][0:rows].rearrange("p h d -> p (h d)"),
                                                                        func=AF.Copy), [('qkf', sl)], [('qkb', sl)])
                    pbv = bank(b2).bitcast(BF16)
                    P.op('pe', lambda e, pbv=pbv, rows=rows: [e.transpose(out=pbv[:, i * 128:i * 128 + rows], in_=qkb[0:rows, i * 128:(i + 1) * 128],
                                                                         identity=ident_b[0:rows, 0:rows]) for i in range(4)][-1],
                         [('qkb', sl), 'ident_b'], [pk(b2)])
                    P.op('dve', lambda e, pbv=pbv, rows=rows, t0=t0: e.tensor_copy(
                        out=qkT[:, :, t0:t0 + rows], in_=pbv[:, 0:512].rearrange("p (i m) -> p i m", i=4)[:, :, 0:rows]),
                        [pk(b2)], [('qkT', ti)])
                for tp in range(0, 17, 2):
                    caps = []
                    for ti in (tp, tp + 1):
                        if ti < 17:
                            P.begin_capture()
                            tile_ops(ti, TT[ti][0], TT[ti][1], ti % 2)
                            caps.append(P.end_capture())
                    P.replay(caps)
                P.op('dve', lambda e, h=h: e.tensor_copy(out=qs_all[:, h].rearrange("p b (g t) -> p b g t", g=3),
                                                         in_=qkT[:, 0:3, S:NT].rearrange("p g (b t) -> p b g t", t=4)),
                     [('qkT', 16)], [('qs_all', h)])
                P.op('dve', lambda e, h=h: e.tensor_copy(out=kTnew[:, h, :], in_=qkT[:, 3, S:NT]), [('qkT', 16)], [('kTnew', h)])
                for gi, dil in ((0, 4), (1, 16)):
                    for blk in range(16):
                        if dil == 4:
                            r, i = blk // 4, blk % 4
                            c0, step = i * 512 + r, 4
                        else:
                            c0, step = blk, 16
                        mm(bank(1)[:, 0:128], [(hT[:, k, ss_(c0, 128, step)], w[:, k, 512:640]) for k in range(8)], wks_, [pk(1)])
                        P.op('act', lambda e, gi=gi, blk=blk: e.activation(out=Vp[gi][:, blk, :], in_=bank(1)[:, 0:128], func=AF.Copy),
                             [pk(1)], [('Vp', gi, blk)])
                P.op('pool', lambda e: e.memset(acc, 0.0), [], ['acc'])
                P.op('pool', lambda e: e.memset(dacc, 0.0), [], ['dacc'])
                allq = [('qkT', ti) for ti in range(16)]
                for g in range(3):
                    dil = (1, 4, 16)[g]
                    nper = 4
                    for grp in range(4):
                        for s4 in range(4):
                            blk = grp * 4 + s4
                            if g == 0:
                                cq = slice(blk * 128, blk * 128 + 128)
                                ckp = slice((blk - 1) * 128, blk * 128) if blk > 0 else None
                                Vown = Vn[:, blk, :]
                                Vprev = Vn[:, blk - 1, :] if blk > 0 else None
                                vkeys = [('Vn', blk)] + ([('Vn', blk - 1)] if blk > 0 else [])
                            elif g == 1:
                                r, i = blk // 4, blk % 4
                                cq = ss_(i * 512 + r, 128, 4)
                                ckp = ss_((i - 1) * 512 + r, 128, 4) if i > 0 else None
                                Vown = Vp[0][:, blk, :]
                                Vprev = Vp[0][:, blk - 1, :] if i > 0 else None
                                vkeys = [('Vp', 0, blk)] + ([('Vp', 0, blk - 1)] if i > 0 else [])
                            else:
                                cq = ss_(blk, 128, 16)
                                ckp = None
                                Vown = Vp[1][:, blk, :]
                                Vprev = None
                                vkeys = [('Vp', 1, blk)]
                            sb_ = 3 + (cnt_pt % 2)
                            pt = PTa[cnt_pt % 3]
                            ptk = ('PTa', cnt_pt % 3)
                            cnt_pt += 1

                            def sc(e, g=g, cq=cq, ckp=ckp, sb_=sb_):
                                q = qkT[:, g, cq]
                                if ckp is not None:
                                    e.matmul(out=bank(sb_)[:, 0:256], lhsT=ident_b, rhs=maskb, start=True, stop=False)
                                    e.matmul(out=bank(sb_)[:, 0:128], lhsT=qkT[:, 3, ckp], rhs=q, start=False, stop=False)
                                else:
                                    e.matmul(out=bank(sb_)[:, 128:256], lhsT=ident_b, rhs=maskb[:, 128:256], start=True, stop=False)
                                return e.matmul(out=bank(sb_)[:, 128:256], lhsT=qkT[:, 3, cq], rhs=q, start=False, stop=True)
                            P.op('pe', sc, allq + ['ident_b', 'maskb'], [pk(sb_)])
                            lo = 0 if ckp is not None else 128
                            P.op('act', lambda e, sb_=sb_, pt=pt, lo=lo: e.activation(out=pt[:, lo:256], in_=bank(sb_)[:, lo:256],
                                                                                       func=AF.Exp, scale=ISQ128), [pk(sb_)], [ptk])

                            def pvf(e, s4=s4, pt=pt, Vown=Vown, Vprev=Vprev):
                                o = bank(5)[:, s4 * 128:(s4 + 1) * 128]
                                d = bank(6)[:, s4 * 128:(s4 + 1) * 128]
                                if Vprev is not None:
                                    e.matmul(out=o, lhsT=Vprev, rhs=pt[:, 0:128], start=True, stop=False)
                                    e.matmul(out=o, lhsT=Vown, rhs=pt[:, 128:256], start=False, stop=True)
                                    e.matmul(out=d, lhsT=ones_b, rhs=pt[:, 0:128], start=True, stop=False)
                                    return e.matmul(out=d, lhsT=ones_b, rhs=pt[:, 128:256], start=False, stop=True)
                                e.matmul(out=o, lhsT=Vown, rhs=pt[:, 128:256], start=True, stop=True)
                                return e.matmul(out=d, lhsT=ones_b, rhs=pt[:, 128:256], start=True, stop=True)
                            P.op('pe', pvf, [ptk, 'ones_b'] + vkeys, [pk(5), pk(6)])
                        if g == 0:
                            av = acc[:, grp * 512:(grp + 1) * 512]
                            dv = dacc[:, grp * 512:(grp + 1) * 512]
                            sh = None
                        elif g == 1:
                            av = acc[:, ss_(grp, 512, 4)]
                            dv = dacc[:, ss_(grp, 512, 4)]
                            sh = None
                        else:
                            av = acc.rearrange("p (u s) -> p s u", s=16)[:, grp * 4:grp * 4 + 4, :]
                            dv = dacc.rearrange("p (u s) -> p s u", s=16)[:, grp * 4:grp * 4 + 4, :]
                            sh = 4
                        o5 = bank(5) if sh is None else bank(5).rearrange("p (s u) -> p s u", s=4)
                        o6 = bank(6) if sh is None else bank(6).rearrange("p (s u) -> p s u", s=4)
                        P.op('dve', lambda e, av=av, o5=o5: e.tensor_tensor(out=av, in0=o5, in1=av, op=ALU.add), [pk(5), 'acc'], ['acc'])
                        P.op('dve', lambda e, dv=dv, o6=o6: e.tensor_tensor(out=dv, in0=o6, in1=dv, op=ALU.add), [pk(6), 'dacc'], ['dacc'])
                P.op('dve', lambda e: e.reciprocal(out=dacc, in_=dacc), ['dacc'], ['dacc'])
                P.op('dve', lambda e, h=h: e.tensor_tensor(out=aoT[:, h, 0:S], in0=acc, in1=dacc, op=ALU.mult), ['acc', 'dacc'], [('aoT', h)])
        P.barrier()
        A.off = mark

        if STOP >= 5:
            kst = [A.alloc([7, 512], F32) for _ in range(2)]
            vst = [A.alloc([7, 512], F32) for _ in range(2)]
            vsb = [A.alloc([7, 512], BF16) for _ in range(2)]
            kTb = A.alloc([4, 7, 128], BF16)
            PTn = A.alloc([4, 192], BF16)
            PTb = [A.alloc([7, 48], BF16) for _ in range(2)]
            for b in range(NSB):
                P.dma('sp', 'o_kn', kws[b, 2044:2048].rearrange("t h d -> t (h d)"), knew[4 * b:4 * b + 4].rearrange("p h d -> p (h d)"),
                      reads=[('knew', h) for h in range(4)], writes=[('kwsn', b)])
                P.dma('sp', 'o_vn', vws[b, 2044:2048].rearrange("t h d -> t (h d)"), vnew[4 * b:4 * b + 4].rearrange("p h d -> p (h d)"),
                      reads=[('vnew', h) for h in range(4)], writes=[('vwsn', b)])
            for hp in range(2):
                def scn(e, hp=hp):
                    ins = None
                    for hh in range(2):
                        h = hp * 2 + hh
                        o = bank(0 + hp)[0:NS, hh * 192:(hh + 1) * 192]
                        e.matmul(out=o, lhsT=ident_b[0:NS, 0:NS], rhs=nmaskb[0:NS, :], start=True, stop=False)
                        ins = e.matmul(out=o, lhsT=kTnew[:, h, :], rhs=qs_all[:, h].rearrange("p b x -> p (b x)"), start=False, stop=True)
                    return ins
                P.op('pe', scn, ['ident_b', 'nmaskb'] + [('kTnew', h) for h in range(4)] + [('qs_all', h) for h in range(4)], [pk(hp)])
                P.op('act', lambda e, hp=hp: e.activation(out=PTn[0:NS, hp * 2:hp * 2 + 2, :].rearrange("p a x -> p (a x)"),
                                                          in_=bank(hp)[0:NS, 0:384], func=AF.Exp, scale=ISQ128), [pk(hp)], [('PTn', hp)])
            def newpv(e):
                ins = None
                for h in range(4):
                    o = bank(4 + h // 2)[:, (h % 2) * 192:(h % 2 + 1) * 192]
                    ins = e.matmul(out=o, lhsT=vnewb[0:NS, h, :], rhs=PTn[0:NS, h, :], start=(h % 2 == 0), stop=False, skip_group_check=True)
                for h in range(4):
                    for half in range(2):
                        d = bank(6 + half)[:, 0:384].rearrange("p (b hx) -> p b hx", b=8)[:, :, h * 12:(h + 1) * 12]
                        ins = e.matmul(out=d, lhsT=ones_b[0:NS, :], rhs=PTn[0:NS, h, half * 96:(half + 1) * 96].rearrange("p (b x) -> p b x", b=8),
                                       start=(h == 0), stop=False, skip_group_check=True)
                return ins
            P.op('pe', newpv, [('PTn', 0), ('PTn', 1), 'ones_b'] + [('vnewb', h) for h in range(4)], [pk(4), pk(5), pk(6), pk(7)])
            for b in range(NSB):
                sl = b % 2
                P.dma('sp', 'ck%d' % sl, kst[sl][:, 0:4, :], cache_k[b, 1536:2048].rearrange("(j p) h d -> p j (h d)", p=128), writes=[('kst', sl, 4)])
                P.dma('sp', 'cv%d' % sl, vst[sl][:, 0:4, :], cache_v[b, 1536:2048].rearrange("(j p) h d -> p j (h d)", p=128), writes=[('vst', sl, 4)])
                for w_ in range(4):
                    P.dma('sp', 'ck%d' % sl, kst[sl][w_ * 32:(w_ + 1) * 32, 4:7, :],
                          cache_k[b, 0:1536].rearrange("(j gl s) h d -> s gl j (h d)", s=16, gl=32)[w_], writes=[('kst', sl, w_)])
                    P.dma('sp', 'cv%d' % sl, vst[sl][w_ * 32:(w_ + 1) * 32, 4:7, :],
                          cache_v[b, 0:1536].rearrange("(j gl s) h d -> s gl j (h d)", s=16, gl=32)[w_], writes=[('vst', sl, w_)])
                P.op('pool', lambda e, sl=sl: e.tensor_copy(out=vsb[sl], in_=vst[sl]), [('vst', sl, q_) for q_ in range(5)], [('vsb', sl)])
                for j in range(7):
                    tb = 2 + (j % 2)
                    P.op('pe', lambda e, sl=sl, j=j, tb=tb: [e.transpose(out=bank(tb)[:, h * 128:(h + 1) * 128],
                                                                        in_=kst[sl][:, j, h * 128:(h + 1) * 128], identity=ident_f)
                                                            for h in range(4)][-1], [('kst', sl, q_) for q_ in range(5)] + ['ident_f'], [pk(tb)])
                    if j % 2 == 0:
                        P.op('act', lambda e, j=j, tb=tb: e.activation(out=kTb[:, :, j, :], in_=bank(tb).rearrange("p (h k) -> p h k", h=4), func=AF.Copy),
                             [pk(tb)], [('kTb', j)])
                    else:
                        P.op('dve', lambda e, j=j, tb=tb: e.tensor_copy(out=kTb[:, :, j, :], in_=bank(tb).rearrange("p (h k) -> p h k", h=4)),
                             [pk(tb)], [('kTb', j)])
                sbk = b % 2

                def scs(e, b=b, sbk=sbk):
                    ins = None
                    o = bank(sbk)[:, 0:336]
                    e.matmul(out=o, lhsT=ident_b, rhs=smaskb, start=True, stop=False)
                    for j in range(7):
                        for h in range(4):
                            ins = e.matmul(out=bank(sbk)[:, j * 48 + h * 12:j * 48 + (h + 1) * 12], lhsT=kTb[:, h, j, :], rhs=qs_all[:, h, b, :],
                                           start=False, stop=(j == 6 and h == 3))
                    return ins
                P.op('pe', scs, ['ident_b', 'smaskb'] + [('kTb', j) for j in range(7)], [pk(sbk)])
                P.op('act', lambda e, sbk=sbk: e.activation(out=PTb[sbk].rearrange("p j x -> p (j x)"), in_=bank(sbk)[:, 0:336],
                                                            func=AF.Exp, scale=ISQ128), [pk(sbk)], [('PTb', sbk)])

                def pvs(e, b=b, sl=sl, sbk=sbk):
                    ins = None
                    last = (b == NSB - 1)
                    for h in range(4):
                        o = bank(4 + h // 2)[:, (h % 2) * 192 + b * 12:(h % 2) * 192 + (b + 1) * 12]
                        for j in range(7):
                            ins = e.matmul(out=o, lhsT=vsb[sl][:, j, h * 128:(h + 1) * 128], rhs=PTb[sbk][:, j, h * 12:(h + 1) * 12],
                                           start=False, stop=(last and j == 6), skip_group_check=True)
                    d = bank(6 + b // 8)[:, (b % 8) * 48:(b % 8 + 1) * 48]
                    for j in range(7):
                        ins = e.matmul(out=d, lhsT=ones_b, rhs=PTb[sbk][:, j, :], start=False, stop=(last and j == 6), skip_group_check=True)
                    return ins
                P.op('pe', pvs, [('PTb', sbk), ('vsb', sl), 'ones_b'], [pk(4), pk(5), pk(6), pk(7)])
            osum = A.alloc([4, NSB, 4], F32)
            dsum = A.alloc([4, NSB, 4], F32)
            for hp in range(2):
                ov = bank(4 + hp)[:, 0:384].rearrange("p (a b g t) -> p a b g t", a=2, b=NSB, g=3)
                P.op('dve', lambda e, hp=hp, ov=ov: e.tensor_copy(out=osum[:, hp * 2:hp * 2 + 2], in_=ov[:, :, :, 0, :]),
                     [pk(4 + hp)], [('osum', hp)])
                P.op('dve', lambda e, hp=hp, ov=ov: e.tensor_tensor(out=osum[:, hp * 2:hp * 2 + 2], in0=osum[:, hp * 2:hp * 2 + 2], in1=ov[:, :, :, 1, :], op=ALU.add),
                     [pk(4 + hp), ('osum', hp)], [('osum', hp)])
                P.op('dve', lambda e, hp=hp, ov=ov: e.tensor_tensor(out=osum[:, hp * 2:hp * 2 + 2], in0=osum[:, hp * 2:hp * 2 + 2], in1=ov[:, :, :, 2, :], op=ALU.add),
                     [pk(4 + hp), ('osum', hp)], [('osum', hp)])
            for half in range(2):
                dv_ = bank(6 + half)[:, 0:384].rearrange("p (b h g t) -> p h b g t", b=8, h=4, g=3)
                ds_ = dsum[:, :, half * 8:half * 8 + 8, :]
                P.op('dve', lambda e, dv_=dv_, ds_=ds_: e.tensor_copy(out=ds_, in_=dv_[:, :, :, 0, :]),
                     [pk(6 + half)], [('dsum', half)])
                P.op('dve', lambda e, dv_=dv_, ds_=ds_: e.tensor_tensor(out=ds_, in0=ds_, in1=dv_[:, :, :, 1, :], op=ALU.add),
                     [pk(6 + half), ('dsum', half)], [('dsum', half)])
                P.op('dve', lambda e, dv_=dv_, ds_=ds_: e.tensor_tensor(out=ds_, in0=ds_, in1=dv_[:, :, :, 2, :], op=ALU.add),
                     [pk(6 + half), ('dsum', half)], [('dsum', half)])
            P.op('dve', lambda e: e.reciprocal(out=dsum, in_=dsum), [('dsum', 0), ('dsum', 1)], ['dsr'])
            P.op('dve', lambda e: e.tensor_tensor(out=aoT[:, :, S:NT].rearrange("p h (b t) -> p h b t", t=4), in0=osum, in1=dsum, op=ALU.mult),
                 ['dsr', ('osum', 0), ('osum', 1)], ['aoTs'])
        P.barrier()
        A.off = mark

        if DBG:
            for i_, t_ in enumerate((aoT, cT, moT)):
                P.dma('pool', 'dbg', dbg[i_], t_.rearrange("p h t -> p (h t)"), writes=[('dbg', i_)])
            P.barrier()
        if STOP >= 6:
            mgT = A.alloc([8, NT], BF16)
            wo = A.alloc([8, DM], BF16)
            mark5 = A.off
            wj = [A.alloc([8, 384], BF16) for _ in range(2)]
            wpj = [A.alloc([4, 384], BF16) for _ in range(2)]
            sg3 = [A.alloc([512], F32) for _ in range(3)]
            t3 = [A.alloc([512], F32) for _ in range(2)]
            P.dma('pool', 'wo', wo, w_out.rearrange("(k p) f -> p k f", p=128), writes=['wo'])

            def load_j(j):
                sl = j % 2
                for br_ in range(3):
                    P.dma('pool', 'wj%d' % sl, wj[sl][:, :, br_ * 128:(br_ + 1) * 128],
                          w_in[:, 4096 + br_ * 1024 + j * 128:4096 + br_ * 1024 + (j + 1) * 128].rearrange("(p k) f -> p k f", k=8),
                          writes=[('wj', sl, br_)])
                for br_, wp in enumerate((w_attn_proj, w_conv_proj, w_mem_proj)):
                    P.dma('pool', 'wj%d' % sl, wpj[sl][:, :, br_ * 128:(br_ + 1) * 128],
                          wp[:, j * 128:(j + 1) * 128].rearrange("(c p) f -> p c f", p=128), writes=[('wpj', sl, br_)])
            load_j(0)
            brT = (aoT, cT, moT)
            for j in range(8):
                if j + 1 < 8:
                    load_j(j + 1)
                sl = j % 2
                for gi, (t0, n) in enumerate(TG):
                    for br_ in range(3):
                        mm(bank(br_)[:, 0:n], [(wj[sl][:, k, br_ * 128:(br_ + 1) * 128], hT[:, k, t0:t0 + n]) for k in range(8)],
                           [('wj', sl, br_)], [pk(br_)])
                        mm(bank(3 + br_)[:, 0:n], [(wpj[sl][:, c, br_ * 128:(br_ + 1) * 128], brT[br_][:, c, t0:t0 + n]) for c in range(4)],
                           [('wpj', sl, br_)], [pk(3 + br_)])
                        P.op('act', lambda e, br_=br_, n=n: e.activation(out=sg3[br_][:, 0:n], in_=bank(br_)[:, 0:n], func=AF.Sigmoid),
                             [pk(br_)], [('sg3', br_)])
                    P.op('dve', lambda e, n=n: e.tensor_tensor(out=t3[0][:, 0:n], in0=bank(3)[:, 0:n], in1=sg3[0][:, 0:n], op=ALU.mult),
                         [pk(3), ('sg3', 0)], [('t3', 0)])
                    P.op('dve', lambda e, n=n: e.tensor_tensor(out=t3[1][:, 0:n], in0=bank(4)[:, 0:n], in1=sg3[1][:, 0:n], op=ALU.mult),
                         [pk(4), ('sg3', 1)], [('t3', 1)])
                    P.op('dve', lambda e, n=n: e.tensor_tensor(out=t3[0][:, 0:n], in0=t3[0][:, 0:n], in1=t3[1][:, 0:n], op=ALU.add),
                         [('t3', 0), ('t3', 1)], [('t3', 0)])
                    P.op('dve', lambda e, n=n: e.tensor_tensor(out=t3[1][:, 0:n], in0=bank(5)[:, 0:n], in1=sg3[2][:, 0:n], op=ALU.mult),
                         [pk(5), ('sg3', 2)], [('t3', 1)])
                    P.op('dve', lambda e, n=n, j=j, t0=t0: e.tensor_tensor(out=mgT[:, j, t0:t0 + n], in0=t3[0][:, 0:n], in1=t3[1][:, 0:n], op=ALU.add),
                         [('t3', 0), ('t3', 1)], [('mgT', j, gi)])
            P.barrier()
            A.off = mark5
            xt2 = [A.alloc([DM], F32) for _ in range(2)]
            x1t = [A.alloc([DM], F32) for _ in range(2)]
            sqt5 = [A.alloc([DM], F32) for _ in range(2)]
            xn5 = [A.alloc([DM], F32) for _ in range(2)]
            ss5 = [A.alloc([8], F32) for _ in range(2)]

            def t5(ti, t0, rows, sl):
                P.dma('sp', 'x%d' % sl, xt2[sl][0:rows, :], xin[t0:t0 + rows, :], writes=[('xt', sl)])
                for half in range(2):
                    bw = half + 2 * sl
                    mm(bank(bw)[0:rows, :], [(mgT[:, k, t0:t0 + rows], wo[:, k, half * 512:(half + 1) * 512]) for k in range(8)],
                       ['wo'], [pk(bw)])
                    P.op('dve', lambda e, half=half, sl=sl, rows=rows, bw=bw: e.tensor_tensor(
                        out=x1t[sl][0:rows, half * 512:(half + 1) * 512], in0=bank(bw)[0:rows, :],
                        in1=xt2[sl][0:rows, half * 512:(half + 1) * 512], op=ALU.add), [pk(bw), ('xt', sl)], [('x1t', sl, half)])
                P.dma('sp', 'x1o%d' % sl, x1s[t0:t0 + rows, :], x1t[sl][0:rows, :], reads=[('x1t', sl, 0), ('x1t', sl, 1)], writes=[('x1s', ti)])
                norm_transpose(x1t[sl][0:rows, :], rows, g_ffn, 'g_ffn', hT[:, :, t0:t0 + rows], [('hT', ti)],
                               'n5_%d' % sl, [('x1t', sl, 0), ('x1t', sl, 1)], sqt5[sl], ss5[sl], xn5[sl], bb=(6 if sl == 0 else 4))
            for tp in range(0, 17, 2):
                caps = []
                for ti in (tp, tp + 1):
                    if ti < 17:
                        P.begin_capture()
                        t5(ti, TT[ti][0], TT[ti][1], ti % 2)
                        caps.append(P.end_capture())
                P.replay(caps)
        P.barrier()
        A.off = mark0
        h2T = hT

        if STOP >= 7:
            yacc = A.alloc([17, DM], F32)
            gates = A.alloc([17, 32], F32)
            wg = [A.alloc([8, 512], BF16) for _ in range(2)]
            wu = [A.alloc([8, 512], BF16) for _ in range(2)]
            wd = [A.alloc([4, DM], BF16) for _ in range(2)]
            lgt = A.alloc([36], F32)
            rt = A.alloc([16, 8], F32)

            def load_e(ei):
                sl = ei % 2
                P.dma('pool', 'we%d' % sl, wg[sl], w_eg[ei].rearrange("(p k) f -> p k f", k=8), writes=[('wg', sl)])
                P.dma('pool', 'we%d' % sl, wu[sl], w_eu[ei].rearrange("(p k) f -> p k f", k=8), writes=[('wu', sl)])
                P.dma('pool', 'we%d' % sl, wd[sl], w_ed[ei].rearrange("(c p) f -> p c f", p=128), writes=[('wd', sl)])
            load_e(0)
            ssR = [A.alloc([8], F32) for _ in range(2)]
            lgtR = [lgt, A.alloc([36], F32)]
            rtR = [rt, A.alloc([16, 8], F32)]
            mark6 = A.off
            xnR = [A.alloc([DM], F32) for _ in range(2)]
            sqtR = [A.alloc([DM], F32) for _ in range(2)]
            h2fR = [A.alloc([8, 128], F32) for _ in range(2)]
            P.dma('sp', 'ya0', yacc[:, 0:16, :], x1s[0:S, :].rearrange("(t p) d -> p t d", p=128), writes=[('yacc', ti) for ti in range(16)])
            P.dma('sp', 'ya1', yacc[0:NS, 16, :], x1s[S:NT, :], writes=[('yacc', 16)])
            def rtile(ti, t0, rows, sl):
                xn_, sqt_, ss_, h2f_, lgt_, rt_ = xnR[sl], sqtR[sl], ssR[sl], h2fR[sl], lgtR[sl], rtR[sl]
                b5 = 5 if sl == 0 else 2
                rms_rstd(yacc[0:rows, ti, :], rows, DM, sqt_, ss_, 'n6_%d' % sl, [('yacc', ti)])
                P.op('dve', lambda e, rows=rows, ti=ti: e.tensor_scalar(out=xn_[0:rows, :], in0=yacc[0:rows, ti, :], scalar1=ss_[0:rows, 0:1], scalar2=32.0,
                                                                         op0=ALU.mult, op1=ALU.mult), [('yacc', ti), 'n6_%dss' % sl], [('n6xn', sl)])
                xv = xn_[0:rows, :].rearrange("t (p k) -> t k p", k=8)
                for half in range(2):
                    bi = (6 + half) if sl == 0 else (3 + half)
                    P.op('pe', lambda e, half=half, bi=bi, rows=rows, xv=xv: [e.transpose(
                        out=bank(bi)[:, kk * 128:kk * 128 + rows], in_=xv[:, half * 4 + kk, :], identity=ident_f[0:rows, 0:rows])
                        for kk in range(4)][-1], [('n6xn', sl), 'ident_f'], [pk(bi)])
                    P.op('dve', lambda e, half=half, bi=bi, rows=rows: e.tensor_tensor(
                        out=h2f_[:, half * 4:half * 4 + 4, 0:rows], in0=bank(bi).rearrange("p (k t) -> p k t", k=4)[:, :, 0:rows],
                        in1=g_ffn[:, half * 4:half * 4 + 4].unsqueeze(2).to_broadcast([128, 4, rows]), op=ALU.mult),
                        [pk(bi), 'g_ffn'], [('h2f', sl, half)])
                mm(bank(b5)[0:rows, 0:36], [(h2f_[:, k, 0:rows], wr[:, k, :]) for k in range(8)], [('h2f', sl, 0), ('h2f', sl, 1), 'wr'], [pk(b5)])
                def router_tile(ti, rows):
                    R = rows
                    P.op('dve', lambda e, R=R: e.tensor_tensor(out=lgt_[0:R, :], in0=bank(b5)[0:R, 0:36], in1=br[0:R, :], op=ALU.add), [pk(b5), 'br'], [('lgt', sl)])
                    mx = rt_[0:R, 0, 0:1]; gm = rt_[0:R, 1, 0:4]; sme = rt_[0:R, 0, 1:2]; pgt = rt_[0:R, 0, 2:3]
                    ex4 = rt_[0:R, 2, 0:4]; les = rt_[0:R, 3, :]; m1 = rt_[0:R, 0, 3:4]; oh1 = rt_[0:R, 4, :]; le2 = rt_[0:R, 5, :]
                    m2 = rt_[0:R, 0, 4:5]; oh2 = rt_[0:R, 6, :]; dm_ = rt_[0:R, 0, 5:6]; e21 = rt_[0:R, 0, 6:7]; w1 = rt_[0:R, 0, 7:8]
                    w2 = rt_[0:R, 7, 0:1]; g8 = rt_[0:R, 8, :]; tmp8 = rt_[0:R, 9, :]; den = rt_[0:R, 7, 1:2]
                    K = ('rt', sl)
                    seq = [
                        ('dve', lambda e: e.reduce_max(out=mx, in_=lgt_[0:R, 0:4], axis=AX.X)),
                        ('dve', lambda e: e.tensor_scalar(out=gm, in0=lgt_[0:R, 0:4], scalar1=mx, scalar2=None, op0=ALU.is_equal)),
                        ('dve', lambda e: e.tensor_scalar(out=ex4, in0=lgt_[0:R, 0:4], scalar1=mx, scalar2=None, op0=ALU.subtract)),
                        ('act', lambda e: e.activation(out=ex4, in_=ex4, func=AF.Exp)),
                        ('dve', lambda e: e.reduce_sum(out=sme, in_=ex4, axis=AX.X)),
                        ('dve', lambda e: e.reciprocal(out=pgt, in_=sme)),
                        ('dve', lambda e: e.tensor_scalar(out=les, in0=lgt_[0:R, 4:12], scalar1=gm[:, 0:1], scalar2=None, op0=ALU.mult)),
                    ] + [
                        ('dve', (lambda g: (lambda e: e.scalar_tensor_tensor(out=les, in0=lgt_[0:R, 4 + g * 8:12 + g * 8], scalar=gm[:, g:g + 1], in1=les,
                                                                             op0=ALU.mult, op1=ALU.add)))(g)) for g in range(1, 4)
                    ] + [
                        ('dve', lambda e: e.reduce_max(out=m1, in_=les, axis=AX.X)),
                        ('dve', lambda e: e.tensor_scalar(out=oh1, in0=les, scalar1=m1, scalar2=None, op0=ALU.is_equal)),
                        ('dve', lambda e: e.scalar_tensor_tensor(out=le2, in0=oh1, scalar=-1e30, in1=les, op0=ALU.mult, op1=ALU.add)),
                        ('dve', lambda e: e.reduce_max(out=m2, in_=le2, axis=AX.X)),
                        ('dve', lambda e: e.tensor_scalar(out=oh2, in0=le2, scalar1=m2, scalar2=None, op0=ALU.is_equal)),
                        ('dve', lambda e: e.tensor_tensor(out=dm_, in0=m2, in1=m1, op=ALU.subtract)),
                        ('act', lambda e: e.activation(out=e21, in_=dm_, func=AF.Exp)),
                        ('dve', lambda e: e.tensor_scalar(out=den, in0=e21, scalar1=1.0, scalar2=None, op0=ALU.add)),
                        ('dve', lambda e: e.reciprocal(out=den, in_=den)),
                        ('dve', lambda e: e.tensor_tensor(out=w1, in0=pgt, in1=den, op=ALU.mult)),
                        ('dve', lambda e: e.tensor_tensor(out=w2, in0=w1, in1=e21, op=ALU.mult)),
                        ('dve', lambda e: e.tensor_scalar(out=g8, in0=oh1, scalar1=w1, scalar2=None, op0=ALU.mult)),
                        ('dve', lambda e: e.scalar_tensor_tensor(out=g8, in0=oh2, scalar=w2, in1=g8, op0=ALU.mult, op1=ALU.add)),
                    ] + [
                        ('dve', (lambda g, ti=ti: (lambda e: e.tensor_scalar(out=gates[0:R, ti, g * 8:(g + 1) * 8], in0=g8, scalar1=gm[:, g:g + 1], scalar2=None,
                                                                              op0=ALU.mult)))(g)) for g in range(4)
                    ]
                    for eng_, fn_ in seq:
                        P.op(eng_, fn_, [('lgt', sl), K], [K, ('gates', ti)])

                router_tile(ti, rows)
            for tp in range(0, 17, 2):
                caps = []
                for ti in (tp, tp + 1):
                    if ti < 17:
                        P.begin_capture()
                        rtile(ti, TT[ti][0], TT[ti][1], ti % 2)
                        caps.append(P.end_capture())
                P.replay(caps)
            P.barrier()
            A.off = mark6
            hid = A.alloc([4, NT], BF16)
            sgt = [A.alloc([512], F32) for _ in range(2)]
            cnt = 0
            for ei in range(NEXP):
                if ei + 1 < NEXP:
                    load_e(ei + 1)
                sl = ei % 2
                for gi, (t0, n) in enumerate(TG):
                    for c in range(4):
                        bg, bu = (cnt % 2) * 2, (cnt % 2) * 2 + 1
                        sg = sgt[cnt % 2]
                        sgk = ('sgt', cnt % 2)
                        cnt += 1
                        mm(bank(bg)[:, 0:n], [(wg[sl][:, k, c * 128:(c + 1) * 128], h2T[:, k, t0:t0 + n]) for k in range(8)], [('wg', sl)], [pk(bg)])
                        mm(bank(bu)[:, 0:n], [(wu[sl][:, k, c * 128:(c + 1) * 128], h2T[:, k, t0:t0 + n]) for k in range(8)], [('wu', sl)], [pk(bu)])
                        P.op('act', lambda e, bg=bg, n=n, sg=sg: e.activation(out=sg[:, 0:n], in_=bank(bg)[:, 0:n], func=AF.Silu), [pk(bg)], [sgk])
                        P.op('dve', lambda e, bu=bu, n=n, sg=sg, c=c, t0=t0: e.tensor_tensor(out=hid[:, c, t0:t0 + n], in0=bank(bu)[:, 0:n], in1=sg[:, 0:n],
                                                                                         op=ALU.mult), [pk(bu), sgk], [('hid', gi, c)])
                for ti, (t0, rows) in enumerate(TT):
                    gi = min(ti // 4, 4)
                    for half in range(2):
                        bo = 4 + ((ti * 2 + half) % 4)
                        mm(bank(bo)[0:rows, :], [(hid[:, c, t0:t0 + rows], wd[sl][:, c, half * 512:(half + 1) * 512]) for c in range(4)],
                           [('wd', sl)] + [('hid', gi, c) for c in range(4)], [pk(bo)])
                        eng_ = 'dve' if (half == 0 or ti % 2 == 0) else 'pool'
                        eng_ = 'dve'
                        P.op(eng_, lambda e, bo=bo, rows=rows, ti=ti, half=half, ei=ei: e.scalar_tensor_tensor(
                            out=yacc[0:rows, ti, half * 512:(half + 1) * 512], in0=bank(bo)[0:rows, :], scalar=gates[0:rows, ti, ei:ei + 1],
                            in1=yacc[0:rows, ti, half * 512:(half + 1) * 512], op0=ALU.mult, op1=ALU.add),
                            [pk(bo), ('gates', ti), ('yacc', ti)], [('yacc', ti)])
            for ti, (t0, rows) in enumerate(TT):
                P.dma('sp', 'o_y', y[t0:t0 + rows, :], yacc[0:rows, ti, :], reads=[('yacc', ti)], writes=[('y', ti)])
        P.final_wait_all_dma('sp')
        P.emit(nc, st)
    return nc


def _consts():
    half = 16
    inv_freq = np.power(np.float32(500000.0), -np.arange(half, dtype=np.float32) * np.float32(2.0 / 32)).astype(np.float32)
    pos = np.zeros(17 * 128, np.float32)
    pos[:S] = np.arange(S)
    pos[S:S + NS] = 2048 + (np.arange(NS) % 4)
    ang = pos[:, None].astype(np.float32) * inv_freq[None, :]
    cs = np.concatenate([np.cos(ang), np.sin(ang)], axis=1).astype(np.float32)
    kp = np.arange(128)[:, None]
    qf = np.arange(128)[None, :]
    mask = np.full((128, 256), NEG, np.float32)
    mask[:, 0:128][kp >= qf] = 0.0
    mask[:, 128:256][kp <= qf] = 0.0
    sm = np.full((128, 7, 3, 4), NEG, np.float32)
    for j in range(7):
        for p in range(128):
            if j < 4:
                R = 1536 + 128 * j + p
                for t in range(4):
                    if R >= 1920 + t:
                        sm[p, j, 0, t] = 0.0
                    if R % 4 == t:
                        sm[p, j, 1, t] = 0.0
                    if R % 16 == t:
                        sm[p, j, 2, t] = 0.0
            else:
                w = p // 32
                sm[p, j, 2, w] = 0.0
    smask = np.repeat(sm.reshape(128, 7, 1, 12), 4, axis=2).reshape(128, 7 * 48)
    nm = np.full((64, 16, 3, 4), NEG, np.float32)
    for b in range(16):
        for tp in range(4):
            for t in range(4):
                if tp <= t:
                    nm[b * 4 + tp, b, 0, t] = 0.0
                if tp == t:
                    nm[b * 4 + tp, b, 1, t] = 0.0
                    nm[b * 4 + tp, b, 2, t] = 0.0
    return dict(c_ident=np.eye(128, dtype=np.float32), c_cs=cs, c_mask=mask, c_smask=np.ascontiguousarray(smask),
                c_nmask=np.ascontiguousarray(nm.reshape(64, 192)))


_NC = None


def kernel(x_prompt, x_sample, mem_prompt, cache_k, cache_v, state_conv, cache_mem_k, cache_mem_v,
           norm_mix_g, w_in, q_norm_g, k_norm_g, conv_w, conv_b, conv_ln_g, conv_ln_b,
           mem_norm_g, w_mem_kv, mq_norm_g, mk_norm_g, w_attn_proj, w_conv_proj, w_mem_proj, w_out,
           norm_ffn_g, w_router_group, b_router_group, w_router_expert, b_router_expert,
           w_expert_gate, w_expert_up, w_expert_down):
    global _NC
    f = lambda a: np.ascontiguousarray(np.asarray(a, dtype=np.float32))
    x_prompt, x_sample, mem_prompt = f(x_prompt), f(x_sample), f(mem_prompt)
    cache_k, cache_v, state_conv = f(cache_k), f(cache_v), f(state_conv)
    cache_mem_k, cache_mem_v = f(cache_mem_k), f(cache_mem_v)
    wre = np.transpose(f(w_router_expert)[0], (1, 0, 2)).reshape(DM, 32)
    w_router = np.ascontiguousarray(np.concatenate([f(w_router_group)[0], wre], axis=1))
    b_router = np.ascontiguousarray(np.concatenate([f(b_router_group)[0], f(b_router_expert)[0].reshape(32)]))
    shared = dict(
        norm_mix_g=f(norm_mix_g)[0], w_in=f(w_in)[0], q_norm_g=f(q_norm_g)[0], k_norm_g=f(k_norm_g)[0],
        conv_wT=np.ascontiguousarray(f(conv_w)[0].T), conv_b=np.ascontiguousarray(f(conv_b)[0].reshape(4, 128).T), conv_ln_g=np.ascontiguousarray(f(conv_ln_g)[0].reshape(4, 128).T),
        conv_ln_b=np.ascontiguousarray(f(conv_ln_b)[0].reshape(4, 128).T),
        mem_norm_g=f(mem_norm_g)[0], w_mem_kv=f(w_mem_kv)[0], mq_norm_g=f(mq_norm_g)[0], mk_norm_g=f(mk_norm_g)[0],
        w_attn_proj=f(w_attn_proj)[0], w_conv_proj=f(w_conv_proj)[0], w_mem_proj=f(w_mem_proj)[0], w_out=f(w_out)[0],
        norm_ffn_g=f(norm_ffn_g)[0], w_router=w_router, b_router=b_router,
        w_eg=f(w_expert_gate)[0], w_eu=f(w_expert_up)[0], w_ed=f(w_expert_down)[0])
    shared.update(_consts())
    in_maps = []
    for c in range(NCORES):
        m = dict(shared)
        bs = slice(c * NSB, (c + 1) * NSB)
        m["xin"] = np.ascontiguousarray(np.concatenate([x_prompt[c], x_sample[bs].reshape(NS, DM)], axis=0))
        m["mem"] = mem_prompt[c]
        m["cache_k"] = cache_k[0, bs]
        m["cache_v"] = cache_v[0, bs]
        m["state_conv"] = state_conv[0, bs]
        m["cmk"] = cache_mem_k[0, bs]
        m["cmv"] = cache_mem_v[0, bs]
        in_maps.append(m)
    if os.environ.get("MK_ONLY_MAPS"):
        return in_maps
    if _NC is None:
        _NC = build_nc()
    res = run_bass_kernel_spmd(_NC, in_maps, core_ids=list(range(NCORES)))
    R = res.results
    y_prompt = np.stack([R[c]["y"][:S] for c in range(NCORES)], 0)
    y_sample = np.concatenate([R[c]["y"][S:].reshape(NSB, 4, DM) for c in range(NCORES)], 0)
    st = lambda k: np.stack([R[c][k] for c in range(NCORES)], 0)[None]
    ct = lambda k: np.concatenate([R[c][k] for c in range(NCORES)], 0)[None]
    return (y_prompt, y_sample, st("kwp"), st("vwp"), st("convp"), st("mkp"), st("mvp"), ct("kws"), ct("vws"), ct("convs"))
```

```python
import os
import numpy as np
import concourse.bass as bass
import concourse.mybir as mybir
from concourse.bass_utils import run_bass_kernel_spmd
from contextlib import ExitStack

F32 = mybir.dt.float32
BF16 = mybir.dt.bfloat16
ALU = mybir.AluOpType
AF = mybir.ActivationFunctionType
AX = mybir.AxisListType

NCORES = 8
S = 2048
DM = 1024
NSB = 16
NS = 64
NT = S + NS
EPS = 1e-6
NEG = -30000.0
SQ128 = float(np.sqrt(128.0))
ISQ128 = float(1.0 / np.sqrt(128.0))
TT = [(i * 128, 128) for i in range(16)] + [(S, NS)]
TG = [(i * 512, 512) for i in range(4)] + [(S, NS)]
NEXP = 32
STOP = int(os.environ.get("MK_STOP", "99"))


def ss_(start, count, step):
    return slice(start, start + (count - 1) * step + 1, step)


class Prog:
    ENG = ('pe', 'act', 'dve', 'pool', 'sp')

    def __init__(self):
        self.engs = {e: dict(ops=[], n=0, waited={}) for e in self.ENG}
        self.dsem = {}
        self.lastw = {}
        self.readers = {}

    def _deps(self, reads, writes):
        toks = {}

        def add(tok):
            if tok is None:
                return
            s, v = tok
            if s.startswith('d:') and not s.startswith('d:bg'):
                v = 16 * self.dsem[s[2:]]['n']
            if toks.get(s, 0) < v:
                toks[s] = v
        for k in reads:
            add(self.lastw.get(k))
        for k in writes:
            add(self.lastw.get(k))
            for s, v in self.readers.get(k, {}).items():
                add((s, v))
        return toks

    def _commit(self, tok, reads, writes):
        for k in reads:
            d = self.readers.setdefault(k, {})
            if d.get(tok[0], 0) < tok[1]:
                d[tok[0]] = tok[1]
        for k in writes:
            self.lastw[k] = tok
            self.readers[k] = {}

    def _waits(self, eng, toks):
        E = self.engs[eng]
        waits = []
        for s, v in toks.items():
            if eng == 'pe' and s == 'e:pe':
                continue
            if E['waited'].get(s, 0) >= v:
                continue
            E['waited'][s] = v
            waits.append((s, v))
        return waits

    _cap = None

    def begin_capture(self):
        self._cap = []

    def end_capture(self):
        c, self._cap = self._cap, None
        return c

    def replay(self, lists):
        idx = [0] * len(lists)
        while any(idx[i] < len(L) for i, L in enumerate(lists)):
            for i, L in enumerate(lists):
                if idx[i] < len(L):
                    kind, args = L[idx[i]]
                    idx[i] += 1
                    (self.op if kind == 'op' else self.dma)(*args)

    def op(self, eng, fn, reads=(), writes=()):
        if self._cap is not None:
            self._cap.append(('op', (eng, fn, list(reads), list(writes))))
            return
        E = self.engs[eng]
        waits = self._waits(eng, self._deps(reads, writes))
        E['n'] += 1
        tok = ('e:' + eng, E['n'])
        E['ops'].append((waits, fn, tok))
        self._commit(tok, reads, writes)

    def dma(self, queue, semname, out, in_, reads=(), writes=()):
        if self._cap is not None:
            self._cap.append(('dma', (queue, semname, out, in_, list(reads), list(writes))))
            return
        E = self.engs[queue]
        waits = self._waits(queue, self._deps(reads, writes))
        D = self.dsem.setdefault(semname, dict(n=0))
        D['n'] += 1
        tok = ('d:' + semname, 16 * D['n'])
        E['ops'].append((waits, lambda e, o=out, i=in_: e.dma_start(out=o, in_=i), tok))
        self._commit(tok, reads, writes)

    def barrier(self):
        toks = {('e:' + e): self.engs[e]['n'] for e in self.ENG if self.engs[e]['n'] > 0}
        for name, D in self.dsem.items():
            if name.startswith('bg'):
                continue
            toks['d:' + name] = 16 * D['n']
        for e in self.ENG:
            waits = self._waits(e, dict(toks))
            self.engs[e]['ops'].append((waits, None, None))
        keep_w = {k: v for k, v in self.lastw.items() if v[0].startswith('d:bg')}
        self.lastw = keep_w
        self.readers = {}

    def final_wait_all_dma(self, eng='sp'):
        E = self.engs[eng]
        waits = []
        for name, D in self.dsem.items():
            s = 'd:' + name
            v = 16 * D['n']
            if E['waited'].get(s, 0) < v:
                E['waited'][s] = v
                waits.append((s, v))
        E['ops'].append((waits, None, None))

    def emit(self, nc, stack):
        sems = {}
        for e in self.ENG:
            sems['e:' + e] = stack.enter_context(nc.semaphore('s_' + e))
        for name in self.dsem:
            sems['d:' + name] = stack.enter_context(nc.semaphore('d_' + name))
        block = stack.enter_context(nc.Block())

        def run(engname):
            def f(eng):
                for waits, fn, tok in self.engs[engname]['ops']:
                    for s, v in waits:
                        eng.wait_ge(sems[s], v)
                    if fn is None:
                        continue
                    ins = fn(eng)
                    ins.then_inc(sems[tok[0]], 16 if tok[0].startswith('d:') else 1)
            return f
        block.tensor(run('pe'))
        block.scalar(run('act'))
        block.vector(run('dve'))
        block.gpsimd(run('pool'))
        block.sync(run('sp'))


class Arena:
    def __init__(self, t, nelem_bf16):
        self.t = t
        self.n = nelem_bf16
        self.off = 0

    def alloc(self, shape, dt):
        size = 2 if dt == BF16 else 4
        ne = int(np.prod(shape))
        nb = (ne * size + 31) // 32 * 32
        assert self.off + nb // 2 <= self.n, ("arena overflow", self.off * 2, nb, self.n * 2)
        ap = self.t[:, self.off:self.off + ne * size // 2]
        self.off += nb // 2
        if dt != BF16:
            ap = ap.bitcast(dt)
        if len(shape) == 2:
            ap = ap.rearrange("p (a b) -> p a b", a=shape[0], b=shape[1])
        elif len(shape) == 3:
            ap = ap.rearrange("p (a b c) -> p a b c", a=shape[0], b=shape[1], c=shape[2])
        elif len(shape) == 4:
            ap = ap.rearrange("p (a b c d) -> p a b c d", a=shape[0], b=shape[1], c=shape[2], d=shape[3])
        return ap


def build_nc():
    nc = bass.Bass("TRN2", target_bir_lowering=False)

    def din(name, shape):
        return nc.dram_tensor(name, list(shape), F32, kind="ExternalInput").ap()

    def dout(name, shape):
        return nc.dram_tensor(name, list(shape), F32, kind="ExternalOutput").ap()

    xin = din("xin", [NT, DM])
    mem = din("mem", [256, DM])
    cache_k = din("cache_k", [NSB, 2048, 4, 128])
    cache_v = din("cache_v", [NSB, 2048, 4, 128])
    state_conv = din("state_conv", [NSB, 30, 512])
    cmk = din("cmk", [NSB, 256, 4, 128])
    cmv = din("cmv", [NSB, 256, 4, 128])
    norm_mix_g = din("norm_mix_g", [DM])
    w_in = din("w_in", [DM, 7168])
    q_norm_g = din("q_norm_g", [128])
    k_norm_g = din("k_norm_g", [128])
    conv_wT = din("conv_wT", [512, 31])
    conv_b = din("conv_b", [128, 4])
    conv_ln_g = din("conv_ln_g", [128, 4])
    conv_ln_b = din("conv_ln_b", [128, 4])
    mem_norm_g = din("mem_norm_g", [DM])
    w_mem_kv = din("w_mem_kv", [DM, 1024])
    mq_norm_g = din("mq_norm_g", [128])
    mk_norm_g = din("mk_norm_g", [128])
    w_attn_proj = din("w_attn_proj", [512, DM])
    w_conv_proj = din("w_conv_proj", [512, DM])
    w_mem_proj = din("w_mem_proj", [512, DM])
    w_out = din("w_out", [DM, DM])
    norm_ffn_g = din("norm_ffn_g", [DM])
    w_router = din("w_router", [DM, 36])
    b_router = din("b_router", [36])
    w_eg = din("w_eg", [NEXP, DM, 512])
    w_eu = din("w_eu", [NEXP, DM, 512])
    w_ed = din("w_ed", [NEXP, 512, DM])
    c_ident = din("c_ident", [128, 128])
    c_cs = din("c_cs", [17 * 128, 32])
    c_mask = din("c_mask", [128, 256])
    c_smask = din("c_smask", [128, 7 * 48])
    c_nmask = din("c_nmask", [64, 192])

    y = dout("y", [NT, DM])
    kwp = dout("kwp", [S, 4, 128])
    vwp = dout("vwp", [S, 4, 128])
    convp = dout("convp", [30, 512])
    mkp = dout("mkp", [256, 4, 128])
    mvp = dout("mvp", [256, 4, 128])
    kws = dout("kws", [NSB, 2048, 4, 128])
    vws = dout("vws", [NSB, 2048, 4, 128])
    convs = dout("convs", [NSB, 30, 512])
    DBG = bool(int(os.environ.get("MK_DBG", "0")))
    x1s = nc.dram_tensor("x1s", [NT, DM], F32, kind=("ExternalOutput" if DBG else "Internal")).ap()
    dbg = dout("dbg", [3, 128, 4 * NT]) if DBG else None

    P = Prog()
    with ExitStack() as st:
        ARENA_BYTES = 204 * 1024
        arena_t = st.enter_context(nc.sbuf_tensor("arena", [128, ARENA_BYTES // 2], BF16))
        A = Arena(arena_t, ARENA_BYTES // 2)
        psum = st.enter_context(nc.psum_tensor("psum", [128, 4096], F32))

        def bank(i, n=1):
            return psum[:, i * 512:(i + n) * 512]

        def pk(i):
            return ('pb', i)

        ident_f = A.alloc([128], F32)
        ident_b = A.alloc([128], BF16)
        ones_b = A.alloc([128], BF16)
        ones_f = A.alloc([128], F32)
        cs = A.alloc([17, 32], F32)
        maskf = A.alloc([256], F32)
        maskb = A.alloc([256], BF16)
        smaskf = A.alloc([7 * 48], F32)
        smaskb = A.alloc([7 * 48], BF16)
        nmaskf = A.alloc([192], F32)
        nmaskb = A.alloc([192], BF16)
        g_mix = A.alloc([8], F32)
        g_ffn = A.alloc([8], F32)
        g_mem = A.alloc([8], F32)
        gqk = A.alloc([4, 128], F32)
        gmq = A.alloc([4, 128], F32)
        gmk = A.alloc([4, 128], F32)
        convw = A.alloc([4, 31], F32)
        convb = A.alloc([4], F32)
        lng = A.alloc([4], F32)
        lnb = A.alloc([4], F32)
        neghalf = A.alloc([512], F32)
        wr = A.alloc([8, 36], F32)
        br = A.alloc([36], F32)
        zero_c = A.alloc([1], F32)

        P.dma('sp', 'c0', ident_f, c_ident, writes=['ident_f'])
        P.dma('sp', 'c0', cs, c_cs.rearrange("(t p) c -> p t c", p=128), writes=['cs'])
        P.dma('sp', 'c0', maskf, c_mask, writes=['maskf'])
        P.dma('sp', 'c0', smaskf, c_smask, writes=['smaskf'])
        P.dma('sp', 'c0', nmaskf[0:64, :], c_nmask, writes=['nmaskf'])
        P.dma('sp', 'c0', g_mix, norm_mix_g.rearrange("(p k) -> p k", k=8), writes=['g_mix'])
        P.dma('sp', 'c0', g_ffn, norm_ffn_g.rearrange("(p k) -> p k", k=8), writes=['g_ffn'])
        P.dma('sp', 'c0', g_mem, mem_norm_g.rearrange("(p k) -> p k", k=8), writes=['g_mem'])
        for i in range(4):
            P.dma('sp', 'c0', gqk[:, i, :], (q_norm_g if i < 3 else k_norm_g).partition_broadcast(128), writes=['gqk'])
            P.dma('sp', 'c0', gmq[:, i, :], mq_norm_g.partition_broadcast(128), writes=['gmq'])
            P.dma('sp', 'c0', gmk[:, i, :], mk_norm_g.partition_broadcast(128), writes=['gmk'])
        P.dma('sp', 'c0', convw, conv_wT.rearrange("(j p) i -> p j i", p=128), writes=['convw'])
        P.dma('sp', 'c0', convb, conv_b, writes=['convb'])
        P.dma('sp', 'c0', lng, conv_ln_g, writes=['lng'])
        P.dma('sp', 'c0', lnb, conv_ln_b, writes=['lnb'])
        P.dma('sp', 'c0', wr, w_router.rearrange("(p k) f -> p k f", k=8), writes=['wr'])
        P.dma('sp', 'c0', br, b_router.partition_broadcast(128), writes=['br'])
        P.op('dve', lambda e: e.tensor_copy(out=ident_b, in_=ident_f), ['ident_f'], ['ident_b'])
        P.op('dve', lambda e: e.tensor_copy(out=maskb, in_=maskf), ['maskf'], ['maskb'])
        P.op('dve', lambda e: e.tensor_copy(out=smaskb, in_=smaskf), ['smaskf'], ['smaskb'])
        P.op('dve', lambda e: e.tensor_copy(out=nmaskb[0:64, :], in_=nmaskf[0:64, :]), ['nmaskf'], ['nmaskb'])
        P.op('pool', lambda e: e.memset(ones_b, 1.0), [], ['ones_b'])
        P.op('pool', lambda e: e.memset(ones_f, 1.0), [], ['ones_f'])
        P.op('pool', lambda e: e.memset(neghalf, -0.5), [], ['neghalf'])
        P.op('pool', lambda e: e.memset(zero_c, 0.0), [], ['zero_c'])

        P.barrier()
        def issue_bg():
            NB_ = 2044 * 512
            for b in range(NSB):
                for (dst_, src_, nm_) in ((kws, cache_k, 'bgk'), (vws, cache_v, 'bgv')):
                    P.dma('act', nm_, dst_[b].rearrange("t h d -> (t h d)")[0:NB_].rearrange("(p n) -> p n", p=128),
                          src_[b].rearrange("t h d -> (t h d)")[2048:2048 + NB_].rearrange("(p n) -> p n", p=128), writes=[(nm_, b)])
            P.dma('act', 'bgc', convs[:, 0:26, :], state_conv[:, 4:30, :], writes=['convs_bg'])
        if STOP < 2:
            issue_bg()

        hT = A.alloc([8, NT], BF16)

        def mm(out, pairs, reads, writes):
            def f(e):
                ins = None
                n = len(pairs)
                for i, (l, r) in enumerate(pairs):
                    ins = e.matmul(out=out, lhsT=l, rhs=r, start=(i == 0), stop=(i == n - 1))
                return ins
            P.op('pe', f, reads, writes)

        def rms_rstd(src, rows, width, sqt, ss, tag, src_keys):
            P.op('pool', lambda e: e.memset(ss[0:rows, 0:1], 0.0), [], [tag + 'ss'])
            P.op('act', lambda e: e.activation(out=sqt[0:rows, 0:width], in_=src, func=AF.Square,
                                               accum_out=ss[0:rows, 0:1]),
                 src_keys + [tag + 'ss'], [tag + 'sq', tag + 'ss'])
            P.op('dve', lambda e: e.tensor_scalar(out=ss[0:rows, 0:1], in0=ss[0:rows, 0:1], scalar1=width * EPS,
                                                  scalar2=None, op0=ALU.add), [tag + 'ss'], [tag + 'ss'])
            P.op('pool', lambda e: e.tensor_tensor(out=ss[0:rows, 0:1], in0=ss[0:rows, 0:1], in1=neghalf[0:rows, 0:1],
                                                   op=ALU.pow), [tag + 'ss', 'neghalf'], [tag + 'ss'])

        def norm_transpose(xt, rows, gain, gain_key, dst_b, dst_keys, tag, xkeys, sqt, ss, xn, dst_f=None, dst_f_keys=(), bb=6):
            rms_rstd(xt, rows, DM, sqt, ss, tag, xkeys)
            P.op('dve', lambda e: e.tensor_scalar(out=xn[0:rows, :], in0=xt, scalar1=ss[0:rows, 0:1], scalar2=32.0,
                                                  op0=ALU.mult, op1=ALU.mult), xkeys + [tag + 'ss'], [tag + 'xn'])
            xv = xn[0:rows, :].rearrange("t (p k) -> t k p", k=8)
            for half in range(2):
                bi = bb + half

                def tr(e, half=half, bi=bi):
                    ins = None
                    for kk in range(4):
                        k = half * 4 + kk
                        ins = e.transpose(out=bank(bi)[:, kk * 128:kk * 128 + rows], in_=xv[:, k, :],
                                          identity=ident_f[0:rows, 0:rows])
                    return ins
                P.op('pe', tr, [tag + 'xn', 'ident_f'], [pk(bi)])
                pv = bank(bi).rearrange("p (k t) -> p k t", k=4)[:, :, 0:rows]
                gv = gain[:, half * 4:half * 4 + 4].unsqueeze(2).to_broadcast([128, 4, rows])
                P.op('dve', lambda e, pv=pv, gv=gv, half=half: e.tensor_tensor(
                    out=dst_b[:, half * 4:half * 4 + 4, :], in0=pv, in1=gv, op=ALU.mult),
                    [pk(bi), gain_key], list(dst_keys))
                if dst_f is not None:
                    P.op('act', lambda e, half=half, bi=bi: [e.activation(
                        out=dst_f[:, half * 4 + kk, 0:rows], in_=bank(bi)[:, kk * 128:kk * 128 + rows], func=AF.Copy,
                        scale=gain[:, half * 4 + kk:half * 4 + kk + 1]) for kk in range(4)][-1],
                        [pk(bi), gain_key], list(dst_f_keys))

        def headnorm(zps, zkeys, rows, nh, gain, gain_key, out_f, out_key, tag, sqt, ss, rope_cs=None, tmp=None):
            zv = zps.rearrange("t (h d) -> t h d", h=nh)
            P.op('act', lambda e: e.activation(out=sqt[0:rows, 0:nh * 128], in_=zps, func=AF.Square),
                 zkeys, [tag + 'sq'])
            P.op('dve', lambda e: e.reduce_sum(out=ss[0:rows, 0:nh],
                                               in_=sqt[0:rows, 0:nh * 128].rearrange("t (h d) -> t h d", h=nh),
                                               axis=AX.X), [tag + 'sq'], [tag + 'ss'])
            P.op('dve', lambda e: e.tensor_scalar(out=ss[0:rows, 0:nh], in0=ss[0:rows, 0:nh], scalar1=128 * EPS,
                                                  scalar2=None, op0=ALU.add), [tag + 'ss'], [tag + 'ss'])
            P.op('pool', lambda e: e.tensor_tensor(out=ss[0:rows, 0:nh], in0=ss[0:rows, 0:nh], in1=neghalf[0:rows, 0:nh],
                                                   op=ALU.pow), [tag + 'ss', 'neghalf'], [tag + 'ss'])
            rb = ss[0:rows, 0:nh].unsqueeze(2).to_broadcast([rows, nh, 128])
            P.op('dve', lambda e: e.scalar_tensor_tensor(out=out_f, in0=zv, scalar=SQ128, in1=rb,
                                                         op0=ALU.mult, op1=ALU.mult), zkeys + [tag + 'ss'], [out_key])
            P.op('dve', lambda e: e.tensor_tensor(out=out_f, in0=out_f, in1=gain[0:rows], op=ALU.mult),
                 [out_key, gain_key], [out_key])
            if rope_cs is not None:
                cosb = rope_cs[:, 0:16].unsqueeze(1).to_broadcast([rows, nh, 16])
                sinb = rope_cs[:, 16:32].unsqueeze(1).to_broadcast([rows, nh, 16])
                x1 = out_f[:, :, 0:16]
                x2 = out_f[:, :, 16:32]
                t = [tmp[0:rows, i * nh * 16:(i + 1) * nh * 16].rearrange("t (h d) -> t h d", h=nh) for i in range(4)]
                tk = tag + 'rt'
                P.op('dve', lambda e: e.tensor_tensor(out=t[0], in0=x1, in1=cosb, op=ALU.mult), [out_key, 'cs'], [tk + '0'])
                P.op('dve', lambda e: e.tensor_tensor(out=t[1], in0=x2, in1=sinb, op=ALU.mult), [out_key, 'cs'], [tk + '1'])
                P.op('dve', lambda e: e.tensor_tensor(out=t[2], in0=x2, in1=cosb, op=ALU.mult), [out_key, 'cs'], [tk + '2'])
                P.op('dve', lambda e: e.tensor_tensor(out=t[3], in0=x1, in1=sinb, op=ALU.mult), [out_key, 'cs'], [tk + '3'])
                P.op('dve', lambda e: e.tensor_tensor(out=x1, in0=t[0], in1=t[1], op=ALU.subtract),
                     [tk + '0', tk + '1'], [out_key])
                P.op('dve', lambda e: e.tensor_tensor(out=x2, in0=t[2], in1=t[3], op=ALU.add),
                     [tk + '2', tk + '3'], [out_key])

        mark0 = A.off
        xt2 = [A.alloc([DM], F32) for _ in range(2)]
        sqt1 = [A.alloc([DM], F32) for _ in range(2)]
        xn1 = [A.alloc([DM], F32) for _ in range(2)]
        ss1 = [A.alloc([8], F32) for _ in range(2)]

        def t1(ti, t0, rows, sl):
            P.dma('sp', 'x%d' % sl, xt2[sl][0:rows, :], xin[t0:t0 + rows, :], writes=[('xt', sl)])
            norm_transpose(xt2[sl][0:rows, :], rows, g_mix, 'g_mix', hT[:, :, t0:t0 + rows], [('hT', ti)],
                           'n1_%d' % sl, [('xt', sl)], sqt1[sl], ss1[sl], xn1[sl], bb=(6 if sl == 0 else 4))
        for tp in range(0, 17, 2):
            caps = []
            for ti in (tp, tp + 1):
                if ti < 17:
                    P.begin_capture()
                    t1(ti, TT[ti][0], TT[ti][1], ti % 2)
                    caps.append(P.end_capture())
            P.replay(caps)
        P.barrier()
        A.off = mark0

        cT = A.alloc([4, NT], BF16)
        mark = A.off
        if STOP >= 2:
            wconv = A.alloc([8, 1024], BF16)
            uP = A.alloc([4, 30 + S], F32)
            uS = A.alloc([4, NSB, 34], F32)
            cP = A.alloc([4, NT], F32)
            sig = [A.alloc([512], F32) for _ in range(2)]
            P.dma('pool', 'w0', wconv, w_in[:, 2560:3584].rearrange("(p k) f -> p k f", k=8), writes=['wconv'])
            P.op('pool', lambda e: e.memset(uP[:, :, 0:30], 0.0), [], ['uPpad'])
            stc = A.alloc([4, 512], F32)
            for bt in range(4):
                P.dma('sp', 'stc%d' % bt, stc[0:120, bt, :], state_conv[4 * bt:4 * bt + 4].rearrange("b i c -> (b i) c"),
                      writes=[('stc', bt)])
            for bt in range(4):
                P.op('pe', lambda e, bt=bt: [e.transpose(out=bank(0)[:, j * 128:j * 128 + 120],
                                                         in_=stc[0:120, bt, j * 128:(j + 1) * 128],
                                                         identity=ident_f[0:120, 0:120]) for j in range(4)][-1],
                     [('stc', bt), 'ident_f'], [pk(0)])
                P.op('dve', lambda e, bt=bt: e.tensor_copy(
                    out=uS[:, :, 4 * bt:4 * bt + 4, 0:30],
                    in_=bank(0).rearrange("p (j x) -> p j x", j=4)[:, :, 0:120].rearrange("p j (b i) -> p j b i", b=4)),
                    [pk(0)], [('uS', bt)])
            cnt = 0
            for gi, (t0, n) in enumerate(TG):
                for j in range(4):
                    ba, bb = 2 + (cnt % 2) * 2, 3 + (cnt % 2) * 2
                    sgi = cnt % 2
                    sg = sig[sgi]
                    cnt += 1
                    rk = ['wconv'] + [('hT', ti) for ti in range(17)]
                    mm(bank(ba)[:, 0:n], [(wconv[:, k, j * 128:(j + 1) * 128], hT[:, k, t0:t0 + n]) for k in range(8)],
                       rk, [pk(ba)])
                    mm(bank(bb)[:, 0:n], [(wconv[:, k, 512 + j * 128:512 + (j + 1) * 128], hT[:, k, t0:t0 + n]) for k in range(8)],
                       rk, [pk(bb)])
                    P.op('act', lambda e, bb=bb, n=n, sg=sg: e.activation(out=sg[:, 0:n], in_=bank(bb)[:, 0:n], func=AF.Sigmoid),
                         [pk(bb)], [('sig', sgi)])
                    if gi < 4:
                        dst = uP[:, j, 30 + t0:30 + t0 + n]
                        P.op('dve', lambda e, ba=ba, n=n, sg=sg, dst=dst: e.tensor_tensor(out=dst, in0=bank(ba)[:, 0:n], in1=sg[:, 0:n], op=ALU.mult),
                             [pk(ba), ('sig', sgi)], [('uP', j)])
                    else:
                        dst = uS[:, j, :, 30:34]
                        P.op('dve', lambda e, ba=ba, sg=sg, dst=dst: e.tensor_tensor(
                            out=dst, in0=bank(ba)[:, 0:NS].rearrange("p (b t) -> p b t", t=4),
                            in1=sg[:, 0:NS].rearrange("p (b t) -> p b t", t=4), op=ALU.mult),
                            [pk(ba), ('sig', sgi)], [('uSn', j)])
            issue_bg()
            cst = A.alloc([512], F32)
            P.op('pe', lambda e: [e.transpose(out=bank(0)[0:30, j * 128:(j + 1) * 128], in_=uP[:, j, S:S + 30],
                                              identity=ident_f) for j in range(4)][-1],
                 [('uP', j) for j in range(4)] + ['ident_f'], [pk(0)])
            P.op('dve', lambda e: e.tensor_copy(out=cst[0:30, :], in_=bank(0)[0:30, :]), [pk(0)], ['cst'])
            P.dma('sp', 'o_cp', convp, cst[0:30, :], reads=['cst'], writes=['convp'])
            unew = A.alloc([4, NS], F32)
            P.op('dve', lambda e: e.tensor_copy(out=unew.rearrange("p j (b t) -> p j b t", t=4), in_=uS[:, :, :, 30:34]),
                 [('uSn', j) for j in range(4)], ['unew'])
            cst2 = A.alloc([512], F32)
            P.op('pe', lambda e: [e.transpose(out=bank(1)[0:NS, j * 128:(j + 1) * 128], in_=unew[:, j, :],
                                              identity=ident_f) for j in range(4)][-1], ['unew', 'ident_f'], [pk(1)])
            P.op('dve', lambda e: e.tensor_copy(out=cst2[0:NS, :], in_=bank(1)[0:NS, :]), [pk(1)], ['cst2'])
            for b in range(NSB):
                P.dma('sp', 'o_cs', convs[b, 26:30, :], cst2[4 * b:4 * b + 4, :], reads=['cst2'], writes=[('convs', b)])
            for j in range(4):
                eng = 'dve'
                for (src, dst, rk, wk) in (
                        (lambda i, j=j: uP[:, j, i:i + S], cP[:, j, 0:S], [('uP', j), 'uPpad'], ('cP', j)),
                        (lambda i, j=j: uS[:, j, :, i:i + 4], cP[:, j, S:NT].rearrange("p (b t) -> p b t", t=4),
                         [('uSn', j)] + [('uS', bt) for bt in range(4)], ('cS', j))):
                    P.op(eng, lambda e, src=src, dst=dst, j=j: e.tensor_scalar(
                        out=dst, in0=src(0), scalar1=convw[:, j, 0:1], scalar2=convb[:, j:j + 1], op0=ALU.mult, op1=ALU.add),
                        rk + ['convw', 'convb'], [wk])
                    for i in range(1, 31):
                        P.op(eng, lambda e, src=src, dst=dst, j=j, i=i: e.scalar_tensor_tensor(
                            out=dst, in0=src(i), scalar=convw[:, j, i:i + 1], in1=dst, op0=ALU.mult, op1=ALU.add),
                            rk + ['convw', wk], [wk])
            sqc = [A.alloc([512], F32) for _ in range(2)]
            mean = A.alloc([512], F32)
            msq = A.alloc([512], F32)
            var = A.alloc([512], F32)
            nt_ = A.alloc([512], F32)
            tln = [A.alloc([512], F32) for _ in range(2)]
            for gi, (t0, n) in enumerate(TG):
                ck = [('cP', j) if gi < 4 else ('cS', j) for j in range(4)]
                mm(bank(0)[:, 0:n], [(ones_f, cP[:, j, t0:t0 + n]) for j in range(4)], ck + ['ones_f'], [pk(0)])
                for j in range(4):
                    P.op('act', lambda e, j=j, t0=t0, n=n: e.activation(out=sqc[j % 2][:, 0:n], in_=cP[:, j, t0:t0 + n], func=AF.Square),
                         [ck[j]], [('sqc', j % 2)])
                    P.op('pe', lambda e, j=j, n=n: e.matmul(out=bank(1)[:, 0:n], lhsT=ones_f, rhs=sqc[j % 2][:, 0:n],
                                                            start=(j == 0), stop=(j == 3)),
                         [('sqc', j % 2), 'ones_f'], [pk(1)])
                P.op('dve', lambda e, n=n: e.tensor_scalar(out=mean[:, 0:n], in0=bank(0)[:, 0:n], scalar1=1.0 / 512, scalar2=None,
                                                           op0=ALU.mult), [pk(0)], ['mean'])
                P.op('dve', lambda e, n=n: e.tensor_tensor(out=msq[:, 0:n], in0=mean[:, 0:n], in1=mean[:, 0:n], op=ALU.mult),
                     ['mean'], ['msq'])
                P.op('dve', lambda e, n=n: e.scalar_tensor_tensor(out=var[:, 0:n], in0=bank(1)[:, 0:n], scalar=1.0 / 512,
                                                                  in1=msq[:, 0:n], op0=ALU.mult, op1=ALU.subtract),
                     [pk(1), 'msq'], ['var'])
                P.op('dve', lambda e, n=n: e.tensor_scalar(out=var[:, 0:n], in0=var[:, 0:n], scalar1=EPS, scalar2=None, op0=ALU.add),
                     ['var'], ['var'])
                vi = var[:, 0:n].bitcast(mybir.dt.int32)
                ryi = msq[:, 0:n].bitcast(mybir.dt.int32)
                P.op('dve', lambda e, vi=vi, ryi=ryi: e.tensor_single_scalar(out=ryi, in_=vi, scalar=1, op=ALU.arith_shift_right),
                     ['var'], ['msq'])
                P.op('dve', lambda e, ryi=ryi: e.tensor_scalar(out=ryi, in0=ryi, scalar1=-1, scalar2=0x5f3759df, op0=ALU.mult, op1=ALU.add),
                     ['msq'], ['msq'])
                for it_ in range(3):
                    P.op('dve', lambda e, n=n: e.tensor_tensor(out=nt_[:, 0:n], in0=msq[:, 0:n], in1=msq[:, 0:n], op=ALU.mult), ['msq'], ['nt_'])
                    P.op('dve', lambda e, n=n: e.tensor_tensor(out=nt_[:, 0:n], in0=nt_[:, 0:n], in1=var[:, 0:n], op=ALU.mult), ['nt_', 'var'], ['nt_'])
                    P.op('dve', lambda e, n=n: e.tensor_scalar(out=nt_[:, 0:n], in0=nt_[:, 0:n], scalar1=-0.5, scalar2=1.5, op0=ALU.mult, op1=ALU.add),
                         ['nt_'], ['nt_'])
                    P.op('dve', lambda e, n=n: e.tensor_tensor(out=msq[:, 0:n], in0=msq[:, 0:n], in1=nt_[:, 0:n], op=ALU.mult), ['msq', 'nt_'], ['msq'])
                P.op('dve', lambda e, n=n: e.tensor_copy(out=var[:, 0:n], in_=msq[:, 0:n]), ['msq'], ['var'])
                for j in range(4):
                    tl = tln[j % 2]
                    P.op('dve', lambda e, j=j, t0=t0, n=n, tl=tl: e.tensor_tensor(out=tl[:, 0:n], in0=cP[:, j, t0:t0 + n], in1=mean[:, 0:n],
                                                                                 op=ALU.subtract), [ck[j], 'mean'], [('tln', j % 2)])
                    P.op('dve', lambda e, n=n, tl=tl: e.tensor_tensor(out=tl[:, 0:n], in0=tl[:, 0:n], in1=var[:, 0:n], op=ALU.mult),
                         [('tln', j % 2), 'var'], [('tln', j % 2)])
                    P.op('act', lambda e, j=j, t0=t0, n=n, tl=tl: e.activation(out=cT[:, j, t0:t0 + n], in_=tl[:, 0:n], func=AF.Silu,
                                                                              scale=lng[:, j:j + 1], bias=lnb[:, j:j + 1]),
                         [('tln', j % 2), 'lng', 'lnb'], [('cT', gi)])
        P.barrier()
        A.off = mark

        moT = A.alloc([4, NT], BF16)
        mark = A.off
        if STOP >= 3:
            wkv = A.alloc([8, 1024], BF16)
            wmq = A.alloc([8, 512], BF16)
            P.dma('pool', 'w0', wkv, w_mem_kv.rearrange("(p k) f -> p k f", k=8), writes=['wkv'])
            P.dma('pool', 'w1', wmq, w_in[:, 3584:4096].rearrange("(p k) f -> p k f", k=8), writes=['wmq'])
            xt2 = [A.alloc([DM], F32) for _ in range(2)]
            sqt = A.alloc([DM], F32)
            xn = A.alloc([DM], F32)
            ss = A.alloc([8], F32)
            memhT = A.alloc([8, 256], BF16)
            mkT = A.alloc([4, 256], BF16)
            mvb = A.alloc([2, 512], BF16)
            kf = [A.alloc([4, 128], F32) for _ in range(2)]
            vf = [A.alloc([512], F32) for _ in range(2)]
            kb = A.alloc([512], BF16)
            for ti in range(2):
                P.dma('sp', 'x%d' % ti, xt2[ti], mem[ti * 128:(ti + 1) * 128, :], writes=[('xt', ti)])
                norm_transpose(xt2[ti], 128, g_mem, 'g_mem', memhT[:, :, ti * 128:(ti + 1) * 128], [('memhT', ti)],
                               'n3', [('xt', ti)], sqt, ss, xn)
            for ti in range(2):
                mm(bank(0), [(memhT[:, k, ti * 128:(ti + 1) * 128], wkv[:, k, 0:512]) for k in range(8)],
                   [('memhT', ti), 'wkv'], [pk(0)])
                mm(bank(1), [(memhT[:, k, ti * 128:(ti + 1) * 128], wkv[:, k, 512:1024]) for k in range(8)],
                   [('memhT', ti), 'wkv'], [pk(1)])
                headnorm(bank(0), [pk(0)], 128, 4, gmk, 'gmk', kf[ti], ('kf', ti), 'h3', sqt, ss)
                P.dma('sp', 'o_mk', mkp[ti * 128:(ti + 1) * 128].rearrange("m h d -> m (h d)"),
                      kf[ti].rearrange("p h d -> p (h d)"), reads=[('kf', ti)], writes=[('mkp', ti)])
                P.op('act', lambda e, ti=ti: e.activation(out=vf[ti], in_=bank(1), func=AF.Copy), [pk(1)], [('vf', ti)])
                P.dma('sp', 'o_mv', mvp[ti * 128:(ti + 1) * 128].rearrange("m h d -> m (h d)"), vf[ti],
                      reads=[('vf', ti)], writes=[('mvp', ti)])
                P.op('dve', lambda e, ti=ti: e.tensor_copy(out=mvb[:, ti, :], in_=vf[ti]), [('vf', ti)], [('mvb', ti)])
                P.op('dve', lambda e, ti=ti: e.tensor_copy(out=kb, in_=kf[ti].rearrange("p h d -> p (h d)")), [('kf', ti)], ['kb'])
                pbv = bank(2).bitcast(BF16)
                P.op('pe', lambda e, pbv=pbv: [e.transpose(out=pbv[:, h * 128:(h + 1) * 128], in_=kb[:, h * 128:(h + 1) * 128],
                                                           identity=ident_b) for h in range(4)][-1], ['kb', 'ident_b'], [pk(2)])
                P.op('dve', lambda e, ti=ti, pbv=pbv: e.tensor_copy(out=mkT[:, :, ti * 128:(ti + 1) * 128],
                                                                   in_=pbv[:, 0:512].rearrange("p (h m) -> p h m", h=4)),
                     [pk(2)], [('mkT', ti)])
            zf = A.alloc([4, 128], F32)
            zb = A.alloc([512], BF16)
            mqT = A.alloc([4, 512], BF16)
            PT = [A.alloc([512], BF16) for _ in range(4)]
            rden = A.alloc([512], F32)
            zfQ = [zf, A.alloc([4, 128], F32)]
            zbQ = [zb, A.alloc([512], BF16)]
            sqtQ = [sqt, A.alloc([DM], F32)]
            ssQ = [ss, A.alloc([8], F32)]

            def t3(ti, t0, rows, sl):
                bz, bt = (0, 2) if sl == 0 else (6, 7)
                zf_, zb_, sqt_, ss_ = zfQ[sl], zbQ[sl], sqtQ[sl], ssQ[sl]
                mm(bank(bz)[0:rows, :], [(hT[:, k, t0:t0 + rows], wmq[:, k, :]) for k in range(8)], ['wmq'], [pk(bz)])
                headnorm(bank(bz)[0:rows, :], [pk(bz)], rows, 4, gmq, 'gmq', zf_[0:rows], ('zf', sl), 'h3q%d' % sl, sqt_, ss_)
                P.op('act', lambda e, rows=rows: e.activation(out=zb_[0:rows, :], in_=zf_[0:rows].rearrange("p h d -> p (h d)"), func=AF.Copy),
                     [('zf', sl)], [('zb', sl)])
                pbv = bank(bt).bitcast(BF16)
                P.op('pe', lambda e, pbv=pbv, rows=rows: [e.transpose(out=pbv[:, h * 128:h * 128 + rows], in_=zb_[0:rows, h * 128:(h + 1) * 128],
                                                                     identity=ident_b[0:rows, 0:rows]) for h in range(4)][-1],
                     [('zb', sl), 'ident_b'], [pk(bt)])
                c0 = (ti % 4) * 128
                P.op('dve', lambda e, pbv=pbv, rows=rows, c0=c0: e.tensor_copy(
                    out=mqT[:, :, c0:c0 + rows], in_=pbv[:, 0:512].rearrange("p (h m) -> p h m", h=4)[:, :, 0:rows]),
                    [pk(bt)], [('mqT', ti % 4)])
                if ti % 4 == 3 and ti < 16:
                    g0 = (ti // 4) * 512
                    for h in range(4):
                        for m in range(2):
                            mm(bank(3 + m), [(mkT[:, h, m * 128:(m + 1) * 128], mqT[:, h, :])],
                               [('mkT', 0), ('mkT', 1)] + [('mqT', q) for q in range(4)], [pk(3 + m)])
                            P.op('act', lambda e, m=m: e.activation(out=PT[m], in_=bank(3 + m), func=AF.Exp, scale=ISQ128),
                                 [pk(3 + m)], [('PT', m)])
                        mm(bank(5), [(mvb[:, m, h * 128:(h + 1) * 128], PT[m]) for m in range(2)],
                           [('mvb', 0), ('mvb', 1), ('PT', 0), ('PT', 1)], [pk(5)])
                        mm(bank(1), [(ones_b, PT[m]) for m in range(2)], ['ones_b', ('PT', 0), ('PT', 1)], [pk(1)])
                        P.op('dve', lambda e: e.reciprocal(out=rden, in_=bank(1)), [pk(1)], ['rden'])
                        P.op('dve', lambda e, h=h, g0=g0: e.tensor_tensor(out=moT[:, h, g0:g0 + 512], in0=bank(5), in1=rden, op=ALU.mult),
                             [pk(5), 'rden'], [('moT', h, g0)])
            for tp in range(0, 17, 2):
                caps = []
                for ti in (tp, tp + 1):
                    if ti < 17:
                        P.begin_capture()
                        t3(ti, TT[ti][0], TT[ti][1], ti % 2)
                        caps.append(P.end_capture())
                P.replay(caps)
            if STOP >= 31:
                mkf = [A.alloc([2, 512], F32) for _ in range(2)]
                mvf = [A.alloc([2, 512], F32) for _ in range(2)]
                mvb2 = [A.alloc([2, 512], BF16) for _ in range(2)]
                mkTb = A.alloc([4, 256], BF16)
                PTs = A.alloc([2, 16], BF16)
                rds = A.alloc([16], F32)
                for b in range(NSB):
                    sl = b % 2
                    P.dma('sp', 'mk%d' % sl, mkf[sl], cmk[b].rearrange("(m p) h d -> p m (h d)", p=128), writes=[('mkf', sl)])
                    P.dma('sp', 'mv%d' % sl, mvf[sl], cmv[b].rearrange("(m p) h d -> p m (h d)", p=128), writes=[('mvf', sl)])
                    P.op('pool', lambda e, sl=sl: e.tensor_copy(out=mvb2[sl], in_=mvf[sl]), [('mvf', sl)], [('mvb2', sl)])
                    for m in range(2):
                        P.op('pe', lambda e, sl=sl, m=m: [e.transpose(out=bank(6 + m)[:, h * 128:(h + 1) * 128],
                                                                      in_=mkf[sl][:, m, h * 128:(h + 1) * 128], identity=ident_f)
                                                          for h in range(4)][-1], [('mkf', sl), 'ident_f'], [pk(6 + m)])
                        P.op('act' if m == 0 else 'dve', (lambda e, m=m: e.activation(
                            out=mkTb[:, :, m * 128:(m + 1) * 128], in_=bank(6 + m).rearrange("p (h k) -> p h k", h=4), func=AF.Copy))
                            if m == 0 else (lambda e, m=m: e.tensor_copy(
                                out=mkTb[:, :, m * 128:(m + 1) * 128], in_=bank(6 + m).rearrange("p (h k) -> p h k", h=4))),
                            [pk(6 + m)], [('mkTb', m)])
                    def sc(e, b=b):
                        ins = None
                        for m in range(2):
                            for h in range(4):
                                ins = e.matmul(out=bank(3)[:, m * 16 + h * 4:m * 16 + h * 4 + 4], lhsT=mkTb[:, h, m * 128:(m + 1) * 128],
                                               rhs=mqT[:, h, 4 * b:4 * b + 4], start=True, stop=True)
                        return ins
                    P.op('pe', sc, [('mkTb', 0), ('mkTb', 1), ('mqT', 0)], [pk(3)])
                    P.op('act', lambda e: e.activation(out=PTs.rearrange("p m x -> p (m x)"), in_=bank(3)[:, 0:32], func=AF.Exp, scale=ISQ128),
                         [pk(3)], ['PTs'])

                    def pv(e, sl=sl):
                        ins = None
                        for h in range(4):
                            for m in range(2):
                                ins = e.matmul(out=bank(4)[:, h * 4:h * 4 + 4], lhsT=mvb2[sl][:, m, h * 128:(h + 1) * 128],
                                               rhs=PTs[:, m, h * 4:h * 4 + 4], start=(m == 0), stop=(m == 1))
                        for m in range(2):
                            ins = e.matmul(out=bank(5)[:, 0:16], lhsT=ones_b, rhs=PTs[:, m, :], start=(m == 0), stop=(m == 1))
                        return ins
                    P.op('pe', pv, ['PTs', ('mvb2', sl), 'ones_b'], [pk(4), pk(5)])
                    P.op('dve', lambda e: e.reciprocal(out=rds, in_=bank(5)[:, 0:16]), [pk(5)], ['rds'])
                    P.op('dve', lambda e, b=b: e.tensor_tensor(out=moT[:, :, S + 4 * b:S + 4 * b + 4],
                                                               in0=bank(4)[:, 0:16].rearrange("p (h t) -> p h t", h=4),
                                                               in1=rds.rearrange("p (h t) -> p h t", h=4), op=ALU.mult),
                         [pk(4), 'rds'], [('moTs', b)])
        P.barrier()
        A.off = mark

        aoT = A.alloc([4, NT], BF16)
        knew = A.alloc([4, 128], F32)
        vnew = A.alloc([4, 128], F32)
        vnewb = A.alloc([4, 128], BF16)
        kTnew = A.alloc([4, NS], BF16)
        qs_all = A.alloc([4, NSB, 12], BF16)
        mark = A.off
        if STOP >= 4:
            wq2 = [A.alloc([8, 640], BF16) for _ in range(2)]
            sqt2 = [A.alloc([512], F32) for _ in range(2)]
            ss2 = [A.alloc([8], F32) for _ in range(2)]
            qkf = [A.alloc([4, 128], F32) for _ in range(2)]
            qkb2 = [A.alloc([512], BF16) for _ in range(2)]
            rtmp2 = [A.alloc([4 * 64], F32) for _ in range(2)]
            vfo = [A.alloc([128], F32) for _ in range(2)]
            qkT = A.alloc([4, NT], BF16)
            Vn = A.alloc([17, 128], BF16)
            Vp = [A.alloc([16, 128], BF16) for _ in range(2)]
            acc = A.alloc([S], F32)
            dacc = A.alloc([S], F32)
            PTa = [A.alloc([256], BF16) for _ in range(3)]

            def load_w(h):
                w = wq2[h % 2]
                for g in range(3):
                    P.dma('pool', 'wq%d' % (h % 2), w[:, :, g * 128:(g + 1) * 128],
                          w_in[:, g * 512 + h * 128:g * 512 + (h + 1) * 128].rearrange("(p k) f -> p k f", k=8), writes=[('wq', h % 2, g)])
                P.dma('pool', 'wq%d' % (h % 2), w[:, :, 384:512],
                      w_in[:, 1536 + h * 128:1536 + (h + 1) * 128].rearrange("(p k) f -> p k f", k=8), writes=[('wq', h % 2, 3)])
                P.dma('pool', 'wq%d' % (h % 2), w[:, :, 512:640],
                      w_in[:, 2048 + h * 128:2048 + (h + 1) * 128].rearrange("(p k) f -> p k f", k=8), writes=[('wq', h % 2, 4)])
            load_w(0)
            cnt_pt = 0
            for h in range(4):
                if h + 1 < 4:
                    load_w(h + 1)
                w = wq2[h % 2]
                wks_ = [('wq', h % 2, q_) for q_ in range(5)]
                def tile_ops(ti, t0, rows, sl, h=h, w=w, wks_=wks_):
                    b0, b1, b2 = (0, 1, 2) if sl == 0 else (3, 4, 7)
                    sqt, ss, qkb, rtmp = sqt2[sl], ss2[sl], qkb2[sl], rtmp2[sl]
                    mm(bank(b0)[0:rows, :], [(hT[:, k, t0:t0 + rows], w[:, k, 0:512]) for k in range(8)], wks_, [pk(b0)])
                    mm(bank(b1)[0:rows, 0:128], [(hT[:, k, t0:t0 + rows], w[:, k, 512:640]) for k in range(8)], wks_, [pk(b1)])
                    headnorm(bank(b0)[0:rows, :], [pk(b0)], rows, 4, gqk, 'gqk', qkf[sl][0:rows], ('qkf', sl), 'h4_%d' % sl, sqt, ss,
                             rope_cs=cs[0:rows, ti, :], tmp=rtmp)
                    if ti < 16:
                        P.dma('sp', 'o_k%d' % sl, kwp[t0:t0 + rows, h, :], qkf[sl][0:rows, 3, :], reads=[('qkf', sl)], writes=[('kwp', h, ti)])
                        P.op('act', lambda e, sl=sl, rows=rows: e.activation(out=vfo[sl][0:rows, :], in_=bank(b1)[0:rows, 0:128], func=AF.Copy),
                             [pk(b1)], [('vfo', sl)])
                        P.dma('sp', 'o_v%d' % sl, vwp[t0:t0 + rows, h, :], vfo[sl][0:rows, :], reads=[('vfo', sl)], writes=[('vwp', h, ti)])
                        P.op('dve', lambda e, sl=sl, ti=ti: e.tensor_copy(out=Vn[:, ti, :], in_=vfo[sl]), [('vfo', sl)], [('Vn', ti)])
                    else:
                        P.op('act', lambda e, h=h: e.activation(out=vnew[0:NS, h, :], in_=bank(b1)[0:NS, 0:128], func=AF.Copy),
                             [pk(b1)], [('vnew', h)])
                        P.op('dve', lambda e, h=h: e.tensor_copy(out=vnewb[0:NS, h, :], in_=vnew[0:NS, h, :]), [('vnew', h)], [('vnewb', h)])
                        P.op('dve', lambda e, h=h, sl=sl: e.tensor_copy(out=knew[0:NS, h, :], in_=qkf[sl][0:NS, 3, :]), [('qkf', sl)], [('knew', h)])
                    P.op('act', lambda e, sl=sl, rows=rows: e.activation(out=qkb[0:rows, :], in_=qkf[sl][0:rows].rearrange("p h d -> p (h d)"),
                                                                        func=AF.Copy), [('qkf', sl)], [('qkb', sl)])
                    pbv = bank(b2).bitcast(BF16)
                    P.op('pe', lambda e, pbv=pbv, rows=rows: [e.transpose(out=pbv[:, i * 128:i * 128 + rows], in_=qkb[0:rows, i * 128:(i + 1) * 128],
                                                                         identity=ident_b[0:rows, 0:rows]) for i in range(4)][-1],
                         [('qkb', sl), 'ident_b'], [pk(b2)])
                    P.op('dve', lambda e, pbv=pbv, rows=rows, t0=t0: e.tensor_copy(
                        out=qkT[:, :, t0:t0 + rows], in_=pbv[:, 0:512].rearrange("p (i m) -> p i m", i=4)[:, :, 0:rows]),
                        [pk(b2)], [('qkT', ti)])
                for tp in range(0, 17, 2):
                    caps = []
                    for ti in (tp, tp + 1):
                        if ti < 17:
                            P.begin_capture()
                            tile_ops(ti, TT[ti][0], TT[ti][1], ti % 2)
                            caps.append(P.end_capture())
                    P.replay(caps)
                P.op('dve', lambda e, h=h: e.tensor_copy(out=qs_all[:, h].rearrange("p b (g t) -> p b g t", g=3),
                                                         in_=qkT[:, 0:3, S:NT].rearrange("p g (b t) -> p b g t", t=4)),
                     [('qkT', 16)], [('qs_all', h)])
                P.op('dve', lambda e, h=h: e.tensor_copy(out=kTnew[:, h, :], in_=qkT[:, 3, S:NT]), [('qkT', 16)], [('kTnew', h)])
                for gi, dil in ((0, 4), (1, 16)):
                    for blk in range(16):
                        if dil == 4:
                            r, i = blk // 4, blk % 4
                            c0, step = i * 512 + r, 4
                        else:
                            c0, step = blk, 16
                        mm(bank(1)[:, 0:128], [(hT[:, k, ss_(c0, 128, step)], w[:, k, 512:640]) for k in range(8)], wks_, [pk(1)])
                        P.op('act', lambda e, gi=gi, blk=blk: e.activation(out=Vp[gi][:, blk, :], in_=bank(1)[:, 0:128], func=AF.Copy),
                             [pk(1)], [('Vp', gi, blk)])
                P.op('pool', lambda e: e.memset(acc, 0.0), [], ['acc'])
                P.op('pool', lambda e: e.memset(dacc, 0.0), [], ['dacc'])
                allq = [('qkT', ti) for ti in range(16)]
                for g in range(3):
                    dil = (1, 4, 16)[g]
                    nper = 4
                    for grp in range(4):
                        for s4 in range(4):
                            blk = grp * 4 + s4
                            if g == 0:
                                cq = slice(blk * 128, blk * 128 + 128)
                                ckp = slice((blk - 1) * 128, blk * 128) if blk > 0 else None
                                Vown = Vn[:, blk, :]
                                Vprev = Vn[:, blk - 1, :] if blk > 0 else None
                                vkeys = [('Vn', blk)] + ([('Vn', blk - 1)] if blk > 0 else [])
                            elif g == 1:
                                r, i = blk // 4, blk % 4
                                cq = ss_(i * 512 + r, 128, 4)
                                ckp = ss_((i - 1) * 512 + r, 128, 4) if i > 0 else None
                                Vown = Vp[0][:, blk, :]
                                Vprev = Vp[0][:, blk - 1, :] if i > 0 else None
                                vkeys = [('Vp', 0, blk)] + ([('Vp', 0, blk - 1)] if i > 0 else [])
                            else:
                                cq = ss_(blk, 128, 16)
                                ckp = None
                                Vown = Vp[1][:, blk, :]
                                Vprev = None
                                vkeys = [('Vp', 1, blk)]
                            sb_ = 3 + (cnt_pt % 2)
                            pt = PTa[cnt_pt % 3]
                            ptk = ('PTa', cnt_pt % 3)
                            cnt_pt += 1

                            def sc(e, g=g, cq=cq, ckp=ckp, sb_=sb_):
                                q = qkT[:, g, cq]
                                if ckp is not None:
                                    e.matmul(out=bank(sb_)[:, 0:256], lhsT=ident_b, rhs=maskb, start=True, stop=False)
                                    e.matmul(out=bank(sb_)[:, 0:128], lhsT=qkT[:, 3, ckp], rhs=q, start=False, stop=False)
                                else:
                                    e.matmul(out=bank(sb_)[:, 128:256], lhsT=ident_b, rhs=maskb[:, 128:256], start=True, stop=False)
                                return e.matmul(out=bank(sb_)[:, 128:256], lhsT=qkT[:, 3, cq], rhs=q, start=False, stop=True)
                            P.op('pe', sc, allq + ['ident_b', 'maskb'], [pk(sb_)])
                            lo = 0 if ckp is not None else 128
                            P.op('act', lambda e, sb_=sb_, pt=pt, lo=lo: e.activation(out=pt[:, lo:256], in_=bank(sb_)[:, lo:256],
                                                                                       func=AF.Exp, scale=ISQ128), [pk(sb_)], [ptk])

                            def pvf(e, s4=s4, pt=pt, Vown=Vown, Vprev=Vprev):
                                o = bank(5)[:, s4 * 128:(s4 + 1) * 128]
                                d = bank(6)[:, s4 * 128:(s4 + 1) * 128]
                                if Vprev is not None:
                                    e.matmul(out=o, lhsT=Vprev, rhs=pt[:, 0:128], start=True, stop=False)
                                    e.matmul(out=o, lhsT=Vown, rhs=pt[:, 128:256], start=False, stop=True)
                                    e.matmul(out=d, lhsT=ones_b, rhs=pt[:, 0:128], start=True, stop=False)
                                    return e.matmul(out=d, lhsT=ones_b, rhs=pt[:, 128:256], start=False, stop=True)
                                e.matmul(out=o, lhsT=Vown, rhs=pt[:, 128:256], start=True, stop=True)
                                return e.matmul(out=d, lhsT=ones_b, rhs=pt[:, 128:256], start=True, stop=True)
                            P.op('pe', pvf, [ptk, 'ones_b'] + vkeys, [pk(5), pk(6)])
                        if g == 0:
                            av = acc[:, grp * 512:(grp + 1) * 512]
                            dv = dacc[:, grp * 512:(grp + 1) * 512]
                            sh = None
                        elif g == 1:
                            av = acc[:, ss_(grp, 512, 4)]
                            dv = dacc[:, ss_(grp, 512, 4)]
                            sh = None
                        else:
                            av = acc.rearrange("p (u s) -> p s u", s=16)[:, grp * 4:grp * 4 + 4, :]
                            dv = dacc.rearrange("p (u s) -> p s u", s=16)[:, grp * 4:grp * 4 + 4, :]
                            sh = 4
                        o5 = bank(5) if sh is None else bank(5).rearrange("p (s u) -> p s u", s=4)
                        o6 = bank(6) if sh is None else bank(6).rearrange("p (s u) -> p s u", s=4)
                        P.op('dve', lambda e, av=av, o5=o5: e.tensor_tensor(out=av, in0=o5, in1=av, op=ALU.add), [pk(5), 'acc'], ['acc'])
                        P.op('dve', lambda e, dv=dv, o6=o6: e.tensor_tensor(out=dv, in0=o6, in1=dv, op=ALU.add), [pk(6), 'dacc'], ['dacc'])
                P.op('dve', lambda e: e.reciprocal(out=dacc, in_=dacc), ['dacc'], ['dacc'])
                P.op('dve', lambda e, h=h: e.tensor_tensor(out=aoT[:, h, 0:S], in0=acc, in1=dacc, op=ALU.mult), ['acc', 'dacc'], [('aoT', h)])
        P.barrier()
        A.off = mark

        if STOP >= 5:
            kst = [A.alloc([7, 512], F32) for _ in range(2)]
            vst = [A.alloc([7, 512], F32) for _ in range(2)]
            vsb = [A.alloc([7, 512], BF16) for _ in range(2)]
            kTb = A.alloc([4, 7, 128], BF16)
            PTn = A.alloc([4, 192], BF16)
            PTb = [A.alloc([7, 48], BF16) for _ in range(2)]
            for b in range(NSB):
                P.dma('sp', 'o_kn', kws[b, 2044:2048].rearrange("t h d -> t (h d)"), knew[4 * b:4 * b + 4].rearrange("p h d -> p (h d)"),
                      reads=[('knew', h) for h in range(4)], writes=[('kwsn', b)])
                P.dma('sp', 'o_vn', vws[b, 2044:2048].rearrange("t h d -> t (h d)"), vnew[4 * b:4 * b + 4].rearrange("p h d -> p (h d)"),
                      reads=[('vnew', h) for h in range(4)], writes=[('vwsn', b)])
            for hp in range(2):
                def scn(e, hp=hp):
                    ins = None
                    for hh in range(2):
                        h = hp * 2 + hh
                        o = bank(0 + hp)[0:NS, hh * 192:(hh + 1) * 192]
                        e.matmul(out=o, lhsT=ident_b[0:NS, 0:NS], rhs=nmaskb[0:NS, :], start=True, stop=False)
                        ins = e.matmul(out=o, lhsT=kTnew[:, h, :], rhs=qs_all[:, h].rearrange("p b x -> p (b x)"), start=False, stop=True)
                    return ins
                P.op('pe', scn, ['ident_b', 'nmaskb'] + [('kTnew', h) for h in range(4)] + [('qs_all', h) for h in range(4)], [pk(hp)])
                P.op('act', lambda e, hp=hp: e.activation(out=PTn[0:NS, hp * 2:hp * 2 + 2, :].rearrange("p a x -> p (a x)"),
                                                          in_=bank(hp)[0:NS, 0:384], func=AF.Exp, scale=ISQ128), [pk(hp)], [('PTn', hp)])
            def newpv(e):
                ins = None
                for h in range(4):
                    o = bank(4 + h // 2)[:, (h % 2) * 192:(h % 2 + 1) * 192]
                    ins = e.matmul(out=o, lhsT=vnewb[0:NS, h, :], rhs=PTn[0:NS, h, :], start=(h % 2 == 0), stop=False, skip_group_check=True)
                for h in range(4):
                    for half in range(2):
                        d = bank(6 + half)[:, 0:384].rearrange("p (b hx) -> p b hx", b=8)[:, :, h * 12:(h + 1) * 12]
                        ins = e.matmul(out=d, lhsT=ones_b[0:NS, :], rhs=PTn[0:NS, h, half * 96:(half + 1) * 96].rearrange("p (b x) -> p b x", b=8),
                                       start=(h == 0), stop=False, skip_group_check=True)
                return ins
            P.op('pe', newpv, [('PTn', 0), ('PTn', 1), 'ones_b'] + [('vnewb', h) for h in range(4)], [pk(4), pk(5), pk(6), pk(7)])
            for b in range(NSB):
                sl = b % 2
                P.dma('sp', 'ck%d' % sl, kst[sl][:, 0:4, :], cache_k[b, 1536:2048].rearrange("(j p) h d -> p j (h d)", p=128), writes=[('kst', sl, 4)])
                P.dma('sp', 'cv%d' % sl, vst[sl][:, 0:4, :], cache_v[b, 1536:2048].rearrange("(j p) h d -> p j (h d)", p=128), writes=[('vst', sl, 4)])
                for w_ in range(4):
                    P.dma('sp', 'ck%d' % sl, kst[sl][w_ * 32:(w_ + 1) * 32, 4:7, :],
                          cache_k[b, 0:1536].rearrange("(j gl s) h d -> s gl j (h d)", s=16, gl=32)[w_], writes=[('kst', sl, w_)])
                    P.dma('sp', 'cv%d' % sl, vst[sl][w_ * 32:(w_ + 1) * 32, 4:7, :],
                          cache_v[b, 0:1536].rearrange("(j gl s) h d -> s gl j (h d)", s=16, gl=32)[w_], writes=[('vst', sl, w_)])
                P.op('pool', lambda e, sl=sl: e.tensor_copy(out=vsb[sl], in_=vst[sl]), [('vst', sl, q_) for q_ in range(5)], [('vsb', sl)])
                for j in range(7):
                    tb = 2 + (j % 2)
                    P.op('pe', lambda e, sl=sl, j=j, tb=tb: [e.transpose(out=bank(tb)[:, h * 128:(h + 1) * 128],
                                                                        in_=kst[sl][:, j, h * 128:(h + 1) * 128], identity=ident_f)
                                                            for h in range(4)][-1], [('kst', sl, q_) for q_ in range(5)] + ['ident_f'], [pk(tb)])
                    if j % 2 == 0:
                        P.op('act', lambda e, j=j, tb=tb: e.activation(out=kTb[:, :, j, :], in_=bank(tb).rearrange("p (h k) -> p h k", h=4), func=AF.Copy),
                             [pk(tb)], [('kTb', j)])
                    else:
                        P.op('dve', lambda e, j=j, tb=tb: e.tensor_copy(out=kTb[:, :, j, :], in_=bank(tb).rearrange("p (h k) -> p h k", h=4)),
                             [pk(tb)], [('kTb', j)])
                sbk = b % 2

                def scs(e, b=b, sbk=sbk):
                    ins = None
                    o = bank(sbk)[:, 0:336]
                    e.matmul(out=o, lhsT=ident_b, rhs=smaskb, start=True, stop=False)
                    for j in range(7):
                        for h in range(4):
                            ins = e.matmul(out=bank(sbk)[:, j * 48 + h * 12:j * 48 + (h + 1) * 12], lhsT=kTb[:, h, j, :], rhs=qs_all[:, h, b, :],
                                           start=False, stop=(j == 6 and h == 3))
                    return ins
                P.op('pe', scs, ['ident_b', 'smaskb'] + [('kTb', j) for j in range(7)], [pk(sbk)])
                P.op('act', lambda e, sbk=sbk: e.activation(out=PTb[sbk].rearrange("p j x -> p (j x)"), in_=bank(sbk)[:, 0:336],
                                                            func=AF.Exp, scale=ISQ128), [pk(sbk)], [('PTb', sbk)])

                def pvs(e, b=b, sl=sl, sbk=sbk):
                    ins = None
                    last = (b == NSB - 1)
                    for h in range(4):
                        o = bank(4 + h // 2)[:, (h % 2) * 192 + b * 12:(h % 2) * 192 + (b + 1) * 12]
                        for j in range(7):
                            ins = e.matmul(out=o, lhsT=vsb[sl][:, j, h * 128:(h + 1) * 128], rhs=PTb[sbk][:, j, h * 12:(h + 1) * 12],
                                           start=False, stop=(last and j == 6), skip_group_check=True)
                    d = bank(6 + b // 8)[:, (b % 8) * 48:(b % 8 + 1) * 48]
                    for j in range(7):
                        ins = e.matmul(out=d, lhsT=ones_b, rhs=PTb[sbk][:, j, :], start=False, stop=(last and j == 6), skip_group_check=True)
                    return ins
                P.op('pe', pvs, [('PTb', sbk), ('vsb', sl), 'ones_b'], [pk(4), pk(5), pk(6), pk(7)])
            osum = A.alloc([4, NSB, 4], F32)
            dsum = A.alloc([4, NSB, 4], F32)
            for hp in range(2):
                ov = bank(4 + hp)[:, 0:384].rearrange("p (a b g t) -> p a b g t", a=2, b=NSB, g=3)
                P.op('dve', lambda e, hp=hp, ov=ov: e.tensor_copy(out=osum[:, hp * 2:hp * 2 + 2], in_=ov[:, :, :, 0, :]),
                     [pk(4 + hp)], [('osum', hp)])
                P.op('dve', lambda e, hp=hp, ov=ov: e.tensor_tensor(out=osum[:, hp * 2:hp * 2 + 2], in0=osum[:, hp * 2:hp * 2 + 2], in1=ov[:, :, :, 1, :], op=ALU.add),
                     [pk(4 + hp), ('osum', hp)], [('osum', hp)])
                P.op('dve', lambda e, hp=hp, ov=ov: e.tensor_tensor(out=osum[:, hp * 2:hp * 2 + 2], in0=osum[:, hp * 2:hp * 2 + 2], in1=ov[:, :, :, 2, :], op=ALU.add),
                     [pk(4 + hp), ('osum', hp)], [('osum', hp)])
            for half in range(2):
                dv_ = bank(6 + half)[:, 0:384].rearrange("p (b h g t) -> p h b g t", b=8, h=4, g=3)
                ds_ = dsum[:, :, half * 8:half * 8 + 8, :]
                P.op('dve', lambda e, dv_=dv_, ds_=ds_: e.tensor_copy(out=ds_, in_=dv_[:, :, :, 0, :]),
                     [pk(6 + half)], [('dsum', half)])
                P.op('dve', lambda e, dv_=dv_, ds_=ds_: e.tensor_tensor(out=ds_, in0=ds_, in1=dv_[:, :, :, 1, :], op=ALU.add),
                     [pk(6 + half), ('dsum', half)], [('dsum', half)])
                P.op('dve', lambda e, dv_=dv_, ds_=ds_: e.tensor_tensor(out=ds_, in0=ds_, in1=dv_[:, :, :, 2, :], op=ALU.add),
                     [pk(6 + half), ('dsum', half)], [('dsum', half)])
            P.op('dve', lambda e: e.reciprocal(out=dsum, in_=dsum), [('dsum', 0), ('dsum', 1)], ['dsr'])
            P.op('dve', lambda e: e.tensor_tensor(out=aoT[:, :, S:NT].rearrange("p h (b t) -> p h b t", t=4), in0=osum, in1=dsum, op=ALU.mult),
                 ['dsr', ('osum', 0), ('osum', 1)], ['aoTs'])
        P.barrier()
        A.off = mark

        if DBG:
            for i_, t_ in enumerate((aoT, cT, moT)):
                P.dma('pool', 'dbg', dbg[i_], t_.rearrange("p h t -> p (h t)"), writes=[('dbg', i_)])
            P.barrier()
        if STOP >= 6:
            mgT = A.alloc([8, NT], BF16)
            wo = A.alloc([8, DM], BF16)
            mark5 = A.off
            wj = [A.alloc([8, 384], BF16) for _ in range(2)]
            wpj = [A.alloc([4, 384], BF16) for _ in range(2)]
            sg3 = [A.alloc([512], F32) for _ in range(3)]
            t3 = [A.alloc([512], F32) for _ in range(2)]
            P.dma('pool', 'wo', wo, w_out.rearrange("(k p) f -> p k f", p=128), writes=['wo'])

            def load_j(j):
                sl = j % 2
                for br_ in range(3):
                    P.dma('pool', 'wj%d' % sl, wj[sl][:, :, br_ * 128:(br_ + 1) * 128],
                          w_in[:, 4096 + br_ * 1024 + j * 128:4096 + br_ * 1024 + (j + 1) * 128].rearrange("(p k) f -> p k f", k=8),
                          writes=[('wj', sl, br_)])
                for br_, wp in enumerate((w_attn_proj, w_conv_proj, w_mem_proj)):
                    P.dma('pool', 'wj%d' % sl, wpj[sl][:, :, br_ * 128:(br_ + 1) * 128],
                          wp[:, j * 128:(j + 1) * 128].rearrange("(c p) f -> p c f", p=128), writes=[('wpj', sl, br_)])
            load_j(0)
            brT = (aoT, cT, moT)
            for j in range(8):
                if j + 1 < 8:
                    load_j(j + 1)
                sl = j % 2
                for gi, (t0, n) in enumerate(TG):
                    for br_ in range(3):
                        mm(bank(br_)[:, 0:n], [(wj[sl][:, k, br_ * 128:(br_ + 1) * 128], hT[:, k, t0:t0 + n]) for k in range(8)],
                           [('wj', sl, br_)], [pk(br_)])
                        mm(bank(3 + br_)[:, 0:n], [(wpj[sl][:, c, br_ * 128:(br_ + 1) * 128], brT[br_][:, c, t0:t0 + n]) for c in range(4)],
                           [('wpj', sl, br_)], [pk(3 + br_)])
                        P.op('act', lambda e, br_=br_, n=n: e.activation(out=sg3[br_][:, 0:n], in_=bank(br_)[:, 0:n], func=AF.Sigmoid),
                             [pk(br_)], [('sg3', br_)])
                    P.op('dve', lambda e, n=n: e.tensor_tensor(out=t3[0][:, 0:n], in0=bank(3)[:, 0:n], in1=sg3[0][:, 0:n], op=ALU.mult),
                         [pk(3), ('sg3', 0)], [('t3', 0)])
                    P.op('dve', lambda e, n=n: e.tensor_tensor(out=t3[1][:, 0:n], in0=bank(4)[:, 0:n], in1=sg3[1][:, 0:n], op=ALU.mult),
                         [pk(4), ('sg3', 1)], [('t3', 1)])
                    P.op('dve', lambda e, n=n: e.tensor_tensor(out=t3[0][:, 0:n], in0=t3[0][:, 0:n], in1=t3[1][:, 0:n], op=ALU.add),
                         [('t3', 0), ('t3', 1)], [('t3', 0)])
                    P.op('dve', lambda e, n=n: e.tensor_tensor(out=t3[1][:, 0:n], in0=bank(5)[:, 0:n], in1=sg3[2][:, 0:n], op=ALU.mult),
                         [pk(5), ('sg3', 2)], [('t3', 1)])
                    P.op('dve', lambda e, n=n, j=j, t0=t0: e.tensor_tensor(out=mgT[:, j, t0:t0 + n], in0=t3[0][:, 0:n], in1=t3[1][:, 0:n], op=ALU.add),
                         [('t3', 0), ('t3', 1)], [('mgT', j, gi)])
            P.barrier()
            A.off = mark5
            xt2 = [A.alloc([DM], F32) for _ in range(2)]
            x1t = [A.alloc([DM], F32) for _ in range(2)]
            sqt5 = [A.alloc([DM], F32) for _ in range(2)]
            xn5 = [A.alloc([DM], F32) for _ in range(2)]
            ss5 = [A.alloc([8], F32) for _ in range(2)]

            def t5(ti, t0, rows, sl):
                P.dma('sp', 'x%d' % sl, xt2[sl][0:rows, :], xin[t0:t0 + rows, :], writes=[('xt', sl)])
                for half in range(2):
                    bw = half + 2 * sl
                    mm(bank(bw)[0:rows, :], [(mgT[:, k, t0:t0 + rows], wo[:, k, half * 512:(half + 1) * 512]) for k in range(8)],
                       ['wo'], [pk(bw)])
                    P.op('dve', lambda e, half=half, sl=sl, rows=rows, bw=bw: e.tensor_tensor(
                        out=x1t[sl][0:rows, half * 512:(half + 1) * 512], in0=bank(bw)[0:rows, :],
                        in1=xt2[sl][0:rows, half * 512:(half + 1) * 512], op=ALU.add), [pk(bw), ('xt', sl)], [('x1t', sl, half)])
                P.dma('sp', 'x1o%d' % sl, x1s[t0:t0 + rows, :], x1t[sl][0:rows, :], reads=[('x1t', sl, 0), ('x1t', sl, 1)], writes=[('x1s', ti)])
                norm_transpose(x1t[sl][0:rows, :], rows, g_ffn, 'g_ffn', hT[:, :, t0:t0 + rows], [('hT', ti)],
                               'n5_%d' % sl, [('x1t', sl, 0), ('x1t', sl, 1)], sqt5[sl], ss5[sl], xn5[sl], bb=(6 if sl == 0 else 4))
            for tp in range(0, 17, 2):
                caps = []
                for ti in (tp, tp + 1):
                    if ti < 17:
                        P.begin_capture()
                        t5(ti, TT[ti][0], TT[ti][1], ti % 2)
                        caps.append(P.end_capture())
                P.replay(caps)
        P.barrier()
        A.off = mark0
        h2T = hT

        if STOP >= 7:
            yacc = A.alloc([17, DM], F32)
            gates = A.alloc([17, 32], F32)
            wg = [A.alloc([8, 512], BF16) for _ in range(2)]
            wu = [A.alloc([8, 512], BF16) for _ in range(2)]
            wd = [A.alloc([4, DM], BF16) for _ in range(2)]
            lgt = A.alloc([36], F32)
            rt = A.alloc([16, 8], F32)

            def load_e(ei):
                sl = ei % 2
                P.dma('pool', 'we%d' % sl, wg[sl], w_eg[ei].rearrange("(p k) f -> p k f", k=8), writes=[('wg', sl)])
                P.dma('pool', 'we%d' % sl, wu[sl], w_eu[ei].rearrange("(p k) f -> p k f", k=8), writes=[('wu', sl)])
                P.dma('pool', 'we%d' % sl, wd[sl], w_ed[ei].rearrange("(c p) f -> p c f", p=128), writes=[('wd', sl)])
            load_e(0)
            ssR = [A.alloc([8], F32) for _ in range(2)]
            lgtR = [lgt, A.alloc([36], F32)]
            rtR = [rt, A.alloc([16, 8], F32)]
            mark6 = A.off
            xnR = [A.alloc([DM], F32) for _ in range(2)]
            sqtR = [A.alloc([DM], F32) for _ in range(2)]
            h2fR = [A.alloc([8, 128], F32) for _ in range(2)]
            P.dma('sp', 'ya0', yacc[:, 0:16, :], x1s[0:S, :].rearrange("(t p) d -> p t d", p=128), writes=[('yacc', ti) for ti in range(16)])
            P.dma('sp', 'ya1', yacc[0:NS, 16, :], x1s[S:NT, :], writes=[('yacc', 16)])
            def rtile(ti, t0, rows, sl):
                xn_, sqt_, ss_, h2f_, lgt_, rt_ = xnR[sl], sqtR[sl], ssR[sl], h2fR[sl], lgtR[sl], rtR[sl]
                b5 = 5 if sl == 0 else 2
                rms_rstd(yacc[0:rows, ti, :], rows, DM, sqt_, ss_, 'n6_%d' % sl, [('yacc', ti)])
                P.op('dve', lambda e, rows=rows, ti=ti: e.tensor_scalar(out=xn_[0:rows, :], in0=yacc[0:rows, ti, :], scalar1=ss_[0:rows, 0:1], scalar2=32.0,
                                                                         op0=ALU.mult, op1=ALU.mult), [('yacc', ti), 'n6_%dss' % sl], [('n6xn', sl)])
                xv = xn_[0:rows, :].rearrange("t (p k) -> t k p", k=8)
                for half in range(2):
                    bi = (6 + half) if sl == 0 else (3 + half)
                    P.op('pe', lambda e, half=half, bi=bi, rows=rows, xv=xv: [e.transpose(
                        out=bank(bi)[:, kk * 128:kk * 128 + rows], in_=xv[:, half * 4 + kk, :], identity=ident_f[0:rows, 0:rows])
                        for kk in range(4)][-1], [('n6xn', sl), 'ident_f'], [pk(bi)])
                    P.op('dve', lambda e, half=half, bi=bi, rows=rows: e.tensor_tensor(
                        out=h2f_[:, half * 4:half * 4 + 4, 0:rows], in0=bank(bi).rearrange("p (k t) -> p k t", k=4)[:, :, 0:rows],
                        in1=g_ffn[:, half * 4:half * 4 + 4].unsqueeze(2).to_broadcast([128, 4, rows]), op=ALU.mult),
                        [pk(bi), 'g_ffn'], [('h2f', sl, half)])
                mm(bank(b5)[0:rows, 0:36], [(h2f_[:, k, 0:rows], wr[:, k, :]) for k in range(8)], [('h2f', sl, 0), ('h2f', sl, 1), 'wr'], [pk(b5)])
                def router_tile(ti, rows):
                    R = rows
                    P.op('dve', lambda e, R=R: e.tensor_tensor(out=lgt_[0:R, :], in0=bank(b5)[0:R, 0:36], in1=br[0:R, :], op=ALU.add), [pk(b5), 'br'], [('lgt', sl)])
                    mx = rt_[0:R, 0, 0:1]; gm = rt_[0:R, 1, 0:4]; sme = rt_[0:R, 0, 1:2]; pgt = rt_[0:R, 0, 2:3]
                    ex4 = rt_[0:R, 2, 0:4]; les = rt_[0:R, 3, :]; m1 = rt_[0:R, 0, 3:4]; oh1 = rt_[0:R, 4, :]; le2 = rt_[0:R, 5, :]
                    m2 = rt_[0:R, 0, 4:5]; oh2 = rt_[0:R, 6, :]; dm_ = rt_[0:R, 0, 5:6]; e21 = rt_[0:R, 0, 6:7]; w1 = rt_[0:R, 0, 7:8]
                    w2 = rt_[0:R, 7, 0:1]; g8 = rt_[0:R, 8, :]; tmp8 = rt_[0:R, 9, :]; den = rt_[0:R, 7, 1:2]
                    K = ('rt', sl)
                    seq = [
                        ('dve', lambda e: e.reduce_max(out=mx, in_=lgt_[0:R, 0:4], axis=AX.X)),
                        ('dve', lambda e: e.tensor_scalar(out=gm, in0=lgt_[0:R, 0:4], scalar1=mx, scalar2=None, op0=ALU.is_equal)),
                        ('dve', lambda e: e.tensor_scalar(out=ex4, in0=lgt_[0:R, 0:4], scalar1=mx, scalar2=None, op0=ALU.subtract)),
                        ('act', lambda e: e.activation(out=ex4, in_=ex4, func=AF.Exp)),
                        ('dve', lambda e: e.reduce_sum(out=sme, in_=ex4, axis=AX.X)),
                        ('dve', lambda e: e.reciprocal(out=pgt, in_=sme)),
                        ('dve', lambda e: e.tensor_scalar(out=les, in0=lgt_[0:R, 4:12], scalar1=gm[:, 0:1], scalar2=None, op0=ALU.mult)),
                    ] + [
                        ('dve', (lambda g: (lambda e: e.scalar_tensor_tensor(out=les, in0=lgt_[0:R, 4 + g * 8:12 + g * 8], scalar=gm[:, g:g + 1], in1=les,
                                                                             op0=ALU.mult, op1=ALU.add)))(g)) for g in range(1, 4)
                    ] + [
                        ('dve', lambda e: e.reduce_max(out=m1, in_=les, axis=AX.X)),
                        ('dve', lambda e: e.tensor_scalar(out=oh1, in0=les, scalar1=m1, scalar2=None, op0=ALU.is_equal)),
                        ('dve', lambda e: e.scalar_tensor_tensor(out=le2, in0=oh1, scalar=-1e30, in1=les, op0=ALU.mult, op1=ALU.add)),
                        ('dve', lambda e: e.reduce_max(out=m2, in_=le2, axis=AX.X)),
                        ('dve', lambda e: e.tensor_scalar(out=oh2, in0=le2, scalar1=m2, scalar2=None, op0=ALU.is_equal)),
                        ('dve', lambda e: e.tensor_tensor(out=dm_, in0=m2, in1=m1, op=ALU.subtract)),
                        ('act', lambda e: e.activation(out=e21, in_=dm_, func=AF.Exp)),
                        ('dve', lambda e: e.tensor_scalar(out=den, in0=e21, scalar1=1.0, scalar2=None, op0=ALU.add)),
                        ('dve', lambda e: e.reciprocal(out=den, in_=den)),
                        ('dve', lambda e: e.tensor_tensor(out=w1, in0=pgt, in1=den, op=ALU.mult)),
                        ('dve', lambda e: e.tensor_tensor(out=w2, in0=w1, in1=e21, op=ALU.mult)),
                        ('dve', lambda e: e.tensor_scalar(out=g8, in0=oh1, scalar1=w1, scalar2=None, op0=ALU.mult)),
                        ('dve', lambda e: e.scalar_tensor_tensor(out=g8, in0=oh2, scalar=w2, in1=g8, op0=ALU.mult, op1=ALU.add)),
                    ] + [
                        ('dve', (lambda g, ti=ti: (lambda e: e.tensor_scalar(out=gates[0:R, ti, g * 8:(g + 1) * 8], in0=g8, scalar1=gm[:, g:g + 1], scalar2=None,
                                                                              op0=ALU.mult)))(g)) for g in range(4)
                    ]
                    for eng_, fn_ in seq:
                        P.op(eng_, fn_, [('lgt', sl), K], [K, ('gates', ti)])

                router_tile(ti, rows)
            for tp in range(0, 17, 2):
                caps = []
                for ti in (tp, tp + 1):
                    if ti < 17:
                        P.begin_capture()
                        rtile(ti, TT[ti][0], TT[ti][1], ti % 2)
                        caps.append(P.end_capture())
                P.replay(caps)
            P.barrier()
            A.off = mark6
            hid = A.alloc([4, NT], BF16)
            sgt = [A.alloc([512], F32) for _ in range(2)]
            cnt = 0
            for ei in range(NEXP):
                if ei + 1 < NEXP:
                    load_e(ei + 1)
                sl = ei % 2
                for gi, (t0, n) in enumerate(TG):
                    for c in range(4):
                        bg, bu = (cnt % 2) * 2, (cnt % 2) * 2 + 1
                        sg = sgt[cnt % 2]
                        sgk = ('sgt', cnt % 2)
                        cnt += 1
                        mm(bank(bg)[:, 0:n], [(wg[sl][:, k, c * 128:(c + 1) * 128], h2T[:, k, t0:t0 + n]) for k in range(8)], [('wg', sl)], [pk(bg)])
                        mm(bank(bu)[:, 0:n], [(wu[sl][:, k, c * 128:(c + 1) * 128], h2T[:, k, t0:t0 + n]) for k in range(8)], [('wu', sl)], [pk(bu)])
                        P.op('act', lambda e, bg=bg, n=n, sg=sg: e.activation(out=sg[:, 0:n], in_=bank(bg)[:, 0:n], func=AF.Silu), [pk(bg)], [sgk])
                        P.op('dve', lambda e, bu=bu, n=n, sg=sg, c=c, t0=t0: e.tensor_tensor(out=hid[:, c, t0:t0 + n], in0=bank(bu)[:, 0:n], in1=sg[:, 0:n],
                                                                                         op=ALU.mult), [pk(bu), sgk], [('hid', gi, c)])
                for ti, (t0, rows) in enumerate(TT):
                    gi = min(ti // 4, 4)
                    for half in range(2):
                        bo = 4 + ((ti * 2 + half) % 4)
                        mm(bank(bo)[0:rows, :], [(hid[:, c, t0:t0 + rows], wd[sl][:, c, half * 512:(half + 1) * 512]) for c in range(4)],
                           [('wd', sl)] + [('hid', gi, c) for c in range(4)], [pk(bo)])
                        eng_ = 'dve' if (half == 0 or ti % 2 == 0) else 'pool'
                        eng_ = 'dve'
                        P.op(eng_, lambda e, bo=bo, rows=rows, ti=ti, half=half, ei=ei: e.scalar_tensor_tensor(
                            out=yacc[0:rows, ti, half * 512:(half + 1) * 512], in0=bank(bo)[0:rows, :], scalar=gates[0:rows, ti, ei:ei + 1],
                            in1=yacc[0:rows, ti, half * 512:(half + 1) * 512], op0=ALU.mult, op1=ALU.add),
                            [pk(bo), ('gates', ti), ('yacc', ti)], [('yacc', ti)])
            for ti, (t0, rows) in enumerate(TT):
                P.dma('sp', 'o_y', y[t0:t0 + rows, :], yacc[0:rows, ti, :], reads=[('yacc', ti)], writes=[('y', ti)])
        P.final_wait_all_dma('sp')
        P.emit(nc, st)
    return nc


def _consts():
    half = 16
    inv_freq = np.power(np.float32(500000.0), -np.arange(half, dtype=np.float32) * np.float32(2.0 / 32)).astype(np.float32)
    pos = np.zeros(17 * 128, np.float32)
    pos[:S] = np.arange(S)
    pos[S:S + NS] = 2048 + (np.arange(NS) % 4)
    ang = pos[:, None].astype(np.float32) * inv_freq[None, :]
    cs = np.concatenate([np.cos(ang), np.sin(ang)], axis=1).astype(np.float32)
    kp = np.arange(128)[:, None]
    qf = np.arange(128)[None, :]
    mask = np.full((128, 256), NEG, np.float32)
    mask[:, 0:128][kp >= qf] = 0.0
    mask[:, 128:256][kp <= qf] = 0.0
    sm = np.full((128, 7, 3, 4), NEG, np.float32)
    for j in range(7):
        for p in range(128):
            if j < 4:
                R = 1536 + 128 * j + p
                for t in range(4):
                    if R >= 1920 + t:
                        sm[p, j, 0, t] = 0.0
                    if R % 4 == t:
                        sm[p, j, 1, t] = 0.0
                    if R % 16 == t:
                        sm[p, j, 2, t] = 0.0
            else:
                w = p // 32
                sm[p, j, 2, w] = 0.0
    smask = np.repeat(sm.reshape(128, 7, 1, 12), 4, axis=2).reshape(128, 7 * 48)
    nm = np.full((64, 16, 3, 4), NEG, np.float32)
    for b in range(16):
        for tp in range(4):
            for t in range(4):
                if tp <= t:
                    nm[b * 4 + tp, b, 0, t] = 0.0
                if tp == t:
                    nm[b * 4 + tp, b, 1, t] = 0.0
                    nm[b * 4 + tp, b, 2, t] = 0.0
    return dict(c_ident=np.eye(128, dtype=np.float32), c_cs=cs, c_mask=mask, c_smask=np.ascontiguousarray(smask),
                c_nmask=np.ascontiguousarray(nm.reshape(64, 192)))


_NC = None


def kernel(x_prompt, x_sample, mem_prompt, cache_k, cache_v, state_conv, cache_mem_k, cache_mem_v,
           norm_mix_g, w_in, q_norm_g, k_norm_g, conv_w, conv_b, conv_ln_g, conv_ln_b,
           mem_norm_g, w_mem_kv, mq_norm_g, mk_norm_g, w_attn_proj, w_conv_proj, w_mem_proj, w_out,
           norm_ffn_g, w_router_group, b_router_group, w_router_expert, b_router_expert,
           w_expert_gate, w_expert_up, w_expert_down):
    global _NC
    f = lambda a: np.ascontiguousarray(np.asarray(a, dtype=np.float32))
    x_prompt, x_sample, mem_prompt = f(x_prompt), f(x_sample), f(mem_prompt)
    cache_k, cache_v, state_conv = f(cache_k), f(cache_v), f(state_conv)
    cache_mem_k, cache_mem_v = f(cache_mem_k), f(cache_mem_v)
    wre = np.transpose(f(w_router_expert)[0], (1, 0, 2)).reshape(DM, 32)
    w_router = np.ascontiguousarray(np.concatenate([f(w_router_group)[0], wre], axis=1))
    b_router = np.ascontiguousarray(np.concatenate([f(b_router_group)[0], f(b_router_expert)[0].reshape(32)]))
    shared = dict(
        norm_mix_g=f(norm_mix_g)[0], w_in=f(w_in)[0], q_norm_g=f(q_norm_g)[0], k_norm_g=f(k_norm_g)[0],
        conv_wT=np.ascontiguousarray(f(conv_w)[0].T), conv_b=np.ascontiguousarray(f(conv_b)[0].reshape(4, 128).T), conv_ln_g=np.ascontiguousarray(f(conv_ln_g)[0].reshape(4, 128).T),
        conv_ln_b=np.ascontiguousarray(f(conv_ln_b)[0].reshape(4, 128).T),
        mem_norm_g=f(mem_norm_g)[0], w_mem_kv=f(w_mem_kv)[0], mq_norm_g=f(mq_norm_g)[0], mk_norm_g=f(mk_norm_g)[0],
        w_attn_proj=f(w_attn_proj)[0], w_conv_proj=f(w_conv_proj)[0], w_mem_proj=f(w_mem_proj)[0], w_out=f(w_out)[0],
        norm_ffn_g=f(norm_ffn_g)[0], w_router=w_router, b_router=b_router,
        w_eg=f(w_expert_gate)[0], w_eu=f(w_expert_up)[0], w_ed=f(w_expert_down)[0])
    shared.update(_consts())
    in_maps = []
    for c in range(NCORES):
        m = dict(shared)
        bs = slice(c * NSB, (c + 1) * NSB)
        m["xin"] = np.ascontiguousarray(np.concatenate([x_prompt[c], x_sample[bs].reshape(NS, DM)], axis=0))
        m["mem"] = mem_prompt[c]
        m["cache_k"] = cache_k[0, bs]
        m["cache_v"] = cache_v[0, bs]
        m["state_conv"] = state_conv[0, bs]
        m["cmk"] = cache_mem_k[0, bs]
        m["cmv"] = cache_mem_v[0, bs]
        in_maps.append(m)
    if os.environ.get("MK_ONLY_MAPS"):
        return in_maps
    if _NC is None:
        _NC = build_nc()
    res = run_bass_kernel_spmd(_NC, in_maps, core_ids=list(range(NCORES)))
    R = res.results
    y_prompt = np.stack([R[c]["y"][:S] for c in range(NCORES)], 0)
    y_sample = np.concatenate([R[c]["y"][S:].reshape(NSB, 4, DM) for c in range(NCORES)], 0)
    st = lambda k: np.stack([R[c][k] for c in range(NCORES)], 0)[None]
    ct = lambda k: np.concatenate([R[c][k] for c in range(NCORES)], 0)[None]
    return (y_prompt, y_sample, st("kwp"), st("vwp"), st("convp"), st("mkp"), st("mvp"), ct("kws"), ct("vws"), ct("convs"))
```

```python
import os
import numpy as np
import concourse.bass as bass
import concourse.mybir as mybir
from concourse.bass_utils import run_bass_kernel_spmd
from contextlib import ExitStack

F32 = mybir.dt.float32
BF16 = mybir.dt.bfloat16
ALU = mybir.AluOpType
AF = mybir.ActivationFunctionType
AX = mybir.AxisListType

NCORES = 8
S = 2048
DM = 1024
NSB = 16
NS = 64
NT = S + NS
EPS = 1e-6
NEG = -30000.0
SQ128 = float(np.sqrt(128.0))
ISQ128 = float(1.0 / np.sqrt(128.0))
TT = [(i * 128, 128) for i in range(16)] + [(S, NS)]
TG = [(i * 512, 512) for i in range(4)] + [(S, NS)]
NEXP = 32
STOP = int(os.environ.get("MK_STOP", "99"))


def ss_(start, count, step):
    return slice(start, start + (count - 1) * step + 1, step)


class Prog:
    ENG = ('pe', 'act', 'dve', 'pool', 'sp')

    def __init__(self):
        self.engs = {e: dict(ops=[], n=0, waited={}) for e in self.ENG}
        self.dsem = {}
        self.lastw = {}
        self.readers = {}

    def _deps(self, reads, writes):
        toks = {}

        def add(tok):
            if tok is None:
                return
            s, v = tok
            if s.startswith('d:') and not s.startswith('d:bg'):
                v = 16 * self.dsem[s[2:]]['n']
            if toks.get(s, 0) < v:
                toks[s] = v
        for k in reads:
            add(self.lastw.get(k))
        for k in writes:
            add(self.lastw.get(k))
            for s, v in self.readers.get(k, {}).items():
                add((s, v))
        return toks

    def _commit(self, tok, reads, writes):
        for k in reads:
            d = self.readers.setdefault(k, {})
            if d.get(tok[0], 0) < tok[1]:
                d[tok[0]] = tok[1]
        for k in writes:
            self.lastw[k] = tok
            self.readers[k] = {}

    def _waits(self, eng, toks):
        E = self.engs[eng]
        waits = []
        for s, v in toks.items():
            if eng == 'pe' and s == 'e:pe':
                continue
            if E['waited'].get(s, 0) >= v:
                continue
            E['waited'][s] = v
            waits.append((s, v))
        return waits

    _cap = None

    def begin_capture(self):
        self._cap = []

    def end_capture(self):
        c, self._cap = self._cap, None
        return c

    def replay(self, lists):
        idx = [0] * len(lists)
        while any(idx[i] < len(L) for i, L in enumerate(lists)):
            for i, L in enumerate(lists):
                if idx[i] < len(L):
                    kind, args = L[idx[i]]
                    idx[i] += 1
                    (self.op if kind == 'op' else self.dma)(*args)

    def op(self, eng, fn, reads=(), writes=()):
        if self._cap is not None:
            self._cap.append(('op', (eng, fn, list(reads), list(writes))))
            return
        E = self.engs[eng]
        waits = self._waits(eng, self._deps(reads, writes))
        E['n'] += 1
        tok = ('e:' + eng, E['n'])
        E['ops'].append((waits, fn, tok))
        self._commit(tok, reads, writes)

    def dma(self, queue, semname, out, in_, reads=(), writes=()):
        if self._cap is not None:
            self._cap.append(('dma', (queue, semname, out, in_, list(reads), list(writes))))
            return
        E = self.engs[queue]
        waits = self._waits(queue, self._deps(reads, writes))
        D = self.dsem.setdefault(semname, dict(n=0))
        D['n'] += 1
        tok = ('d:' + semname, 16 * D['n'])
        E['ops'].append((waits, lambda e, o=out, i=in_: e.dma_start(out=o, in_=i), tok))
        self._commit(tok, reads, writes)

    def barrier(self):
        toks = {('e:' + e): self.engs[e]['n'] for e in self.ENG if self.engs[e]['n'] > 0}
        for name, D in self.dsem.items():
            if name.startswith('bg'):
                continue
            toks['d:' + name] = 16 * D['n']
        for e in self.ENG:
            waits = self._waits(e, dict(toks))
            self.engs[e]['ops'].append((waits, None, None))
        keep_w = {k: v for k, v in self.lastw.items() if v[0].startswith('d:bg')}
        self.lastw = keep_w
        self.readers = {}

    def final_wait_all_dma(self, eng='sp'):
        E = self.engs[eng]
        waits = []
        for name, D in self.dsem.items():
            s = 'd:' + name
            v = 16 * D['n']
            if E['waited'].get(s, 0) < v:
                E['waited'][s] = v
                waits.append((s, v))
        E['ops'].append((waits, None, None))

    def emit(self, nc, stack):
        sems = {}
        for e in self.ENG:
            sems['e:' + e] = stack.enter_context(nc.semaphore('s_' + e))
        for name in self.dsem:
            sems['d:' + name] = stack.enter_context(nc.semaphore('d_' + name))
        block = stack.enter_context(nc.Block())

        def run(engname):
            def f(eng):
                for waits, fn, tok in self.engs[engname]['ops']:
                    for s, v in waits:
                        eng.wait_ge(sems[s], v)
                    if fn is None:
                        continue
                    ins = fn(eng)
                    ins.then_inc(sems[tok[0]], 16 if tok[0].startswith('d:') else 1)
            return f
        block.tensor(run('pe'))
        block.scalar(run('act'))
        block.vector(run('dve'))
        block.gpsimd(run('pool'))
        block.sync(run('sp'))


class Arena:
    def __init__(self, t, nelem_bf16):
        self.t = t
        self.n = nelem_bf16
        self.off = 0

    def alloc(self, shape, dt):
        size = 2 if dt == BF16 else 4
        ne = int(np.prod(shape))
        nb = (ne * size + 31) // 32 * 32
        assert self.off + nb // 2 <= self.n, ("arena overflow", self.off * 2, nb, self.n * 2)
        ap = self.t[:, self.off:self.off + ne * size // 2]
        self.off += nb // 2
        if dt != BF16:
            ap = ap.bitcast(dt)
        if len(shape) == 2:
            ap = ap.rearrange("p (a b) -> p a b", a=shape[0], b=shape[1])
        elif len(shape) == 3:
            ap = ap.rearrange("p (a b c) -> p a b c", a=shape[0], b=shape[1], c=shape[2])
        elif len(shape) == 4:
            ap = ap.rearrange("p (a b c d) -> p a b c d", a=shape[0], b=shape[1], c=shape[2], d=shape[3])
        return ap


def build_nc():
    nc = bass.Bass("TRN2", target_bir_lowering=False)

    def din(name, shape):
        return nc.dram_tensor(name, list(shape), F32, kind="ExternalInput").ap()

    def dout(name, shape):
        return nc.dram_tensor(name, list(shape), F32, kind="ExternalOutput").ap()

    xin = din("xin", [NT, DM])
    mem = din("mem", [256, DM])
    cache_k = din("cache_k", [NSB, 2048, 4, 128])
    cache_v = din("cache_v", [NSB, 2048, 4, 128])
    state_conv = din("state_conv", [NSB, 30, 512])
    cmk = din("cmk", [NSB, 256, 4, 128])
    cmv = din("cmv", [NSB, 256, 4, 128])
    norm_mix_g = din("norm_mix_g", [DM])
    w_in = din("w_in", [DM, 7168])
    q_norm_g = din("q_norm_g", [128])
    k_norm_g = din("k_norm_g", [128])
    conv_wT = din("conv_wT", [512, 31])
    conv_b = din("conv_b", [128, 4])
    conv_ln_g = din("conv_ln_g", [128, 4])
    conv_ln_b = din("conv_ln_b", [128, 4])
    mem_norm_g = din("mem_norm_g", [DM])
    w_mem_kv = din("w_mem_kv", [DM, 1024])
    mq_norm_g = din("mq_norm_g", [128])
    mk_norm_g = din("mk_norm_g", [128])
    w_attn_proj = din("w_attn_proj", [512, DM])
    w_conv_proj = din("w_conv_proj", [512, DM])
    w_mem_proj = din("w_mem_proj", [512, DM])
    w_out = din("w_out", [DM, DM])
    norm_ffn_g = din("norm_ffn_g", [DM])
    w_router = din("w_router", [DM, 36])
    b_router = din("b_router", [36])
    w_eg = din("w_eg", [NEXP, DM, 512])
    w_eu = din("w_eu", [NEXP, DM, 512])
    w_ed = din("w_ed", [NEXP, 512, DM])
    c_ident = din("c_ident", [128, 128])
    c_cs = din("c_cs", [17 * 128, 32])
    c_mask = din("c_mask", [128, 256])
    c_smask = din("c_smask", [128, 7 * 48])
    c_nmask = din("c_nmask", [64, 192])

    y = dout("y", [NT, DM])
    kwp = dout("kwp", [S, 4, 128])
    vwp = dout("vwp", [S, 4, 128])
    convp = dout("convp", [30, 512])
    mkp = dout("mkp", [256, 4, 128])
    mvp = dout("mvp", [256, 4, 128])
    kws = dout("kws", [NSB, 2048, 4, 128])
    vws = dout("vws", [NSB, 2048, 4, 128])
    convs = dout("convs", [NSB, 30, 512])
    DBG = bool(int(os.environ.get("MK_DBG", "0")))
    x1s = nc.dram_tensor("x1s", [NT, DM], F32, kind=("ExternalOutput" if DBG else "Internal")).ap()
    dbg = dout("dbg", [3, 128, 4 * NT]) if DBG else None

    P = Prog()
    with ExitStack() as st:
        ARENA_BYTES = 204 * 1024
        arena_t = st.enter_context(nc.sbuf_tensor("arena", [128, ARENA_BYTES // 2], BF16))
        A = Arena(arena_t, ARENA_BYTES // 2)
        psum = st.enter_context(nc.psum_tensor("psum", [128, 4096], F32))

        def bank(i, n=1):
            return psum[:, i * 512:(i + n) * 512]

        def pk(i):
            return ('pb', i)

        ident_f = A.alloc([128], F32)
        ident_b = A.alloc([128], BF16)
        ones_b = A.alloc([128], BF16)
        ones_f = A.alloc([128], F32)
        cs = A.alloc([17, 32], F32)
        maskf = A.alloc([256], F32)
        maskb = A.alloc([256], BF16)
        smaskf = A.alloc([7 * 48], F32)
        smaskb = A.alloc([7 * 48], BF16)
        nmaskf = A.alloc([192], F32)
        nmaskb = A.alloc([192], BF16)
        g_mix = A.alloc([8], F32)
        g_ffn = A.alloc([8], F32)
        g_mem = A.alloc([8], F32)
        gqk = A.alloc([4, 128], F32)
        gmq = A.alloc([4, 128], F32)
        gmk = A.alloc([4, 128], F32)
        convw = A.alloc([4, 31], F32)
        convb = A.alloc([4], F32)
        lng = A.alloc([4], F32)
        lnb = A.alloc([4], F32)
        neghalf = A.alloc([512], F32)
        wr = A.alloc([8, 36], F32)
        br = A.alloc([36], F32)
        zero_c = A.alloc([1], F32)

        P.dma('sp', 'c0', ident_f, c_ident, writes=['ident_f'])
        P.dma('sp', 'c0', cs, c_cs.rearrange("(t p) c -> p t c", p=128), writes=['cs'])
        P.dma('sp', 'c0', maskf, c_mask, writes=['maskf'])
        P.dma('sp', 'c0', smaskf, c_smask, writes=['smaskf'])
        P.dma('sp', 'c0', nmaskf[0:64, :], c_nmask, writes=['nmaskf'])
        P.dma('sp', 'c0', g_mix, norm_mix_g.rearrange("(p k) -> p k", k=8), writes=['g_mix'])
        P.dma('sp', 'c0', g_ffn, norm_ffn_g.rearrange("(p k) -> p k", k=8), writes=['g_ffn'])
        P.dma('sp', 'c0', g_mem, mem_norm_g.rearrange("(p k) -> p k", k=8), writes=['g_mem'])
        for i in range(4):
            P.dma('sp', 'c0', gqk[:, i, :], (q_norm_g if i < 3 else k_norm_g).partition_broadcast(128), writes=['gqk'])
            P.dma('sp', 'c0', gmq[:, i, :], mq_norm_g.partition_broadcast(128), writes=['gmq'])
            P.dma('sp', 'c0', gmk[:, i, :], mk_norm_g.partition_broadcast(128), writes=['gmk'])
        P.dma('sp', 'c0', convw, conv_wT.rearrange("(j p) i -> p j i", p=128), writes=['convw'])
        P.dma('sp', 'c0', convb, conv_b, writes=['convb'])
        P.dma('sp', 'c0', lng, conv_ln_g, writes=['lng'])
        P.dma('sp', 'c0', lnb, conv_ln_b, writes=['lnb'])
        P.dma('sp', 'c0', wr, w_router.rearrange("(p k) f -> p k f", k=8), writes=['wr'])
        P.dma('sp', 'c0', br, b_router.partition_broadcast(128), writes=['br'])
        P.op('dve', lambda e: e.tensor_copy(out=ident_b, in_=ident_f), ['ident_f'], ['ident_b'])
        P.op('dve', lambda e: e.tensor_copy(out=maskb, in_=maskf), ['maskf'], ['maskb'])
        P.op('dve', lambda e: e.tensor_copy(out=smaskb, in_=smaskf), ['smaskf'], ['smaskb'])
        P.op('dve', lambda e: e.tensor_copy(out=nmaskb[0:64, :], in_=nmaskf[0:64, :]), ['nmaskf'], ['nmaskb'])
        P.op('pool', lambda e: e.memset(ones_b, 1.0), [], ['ones_b'])
        P.op('pool', lambda e: e.memset(ones_f, 1.0), [], ['ones_f'])
        P.op('pool', lambda e: e.memset(neghalf, -0.5), [], ['neghalf'])
        P.op('pool', lambda e: e.memset(zero_c, 0.0), [], ['zero_c'])

        P.barrier()
        def issue_bg():
            NB_ = 2044 * 512
            for b in range(NSB):
                for (dst_, src_, nm_) in ((kws, cache_k, 'bgk'), (vws, cache_v, 'bgv')):
                    P.dma('act', nm_, dst_[b].rearrange("t h d -> (t h d)")[0:NB_].rearrange("(p n) -> p n", p=128),
                          src_[b].rearrange("t h d -> (t h d)")[2048:2048 + NB_].rearrange("(p n) -> p n", p=128), writes=[(nm_, b)])
            P.dma('act', 'bgc', convs[:, 0:26, :], state_conv[:, 4:30, :], writes=['convs_bg'])
        if STOP < 2:
            issue_bg()

        hT = A.alloc([8, NT], BF16)

        def mm(out, pairs, reads, writes):
            def f(e):
                ins = None
                n = len(pairs)
                for i, (l, r) in enumerate(pairs):
                    ins = e.matmul(out=out, lhsT=l, rhs=r, start=(i == 0), stop=(i == n - 1))
                return ins
            P.op('pe', f, reads, writes)

        def rms_rstd(src, rows, width, sqt, ss, tag, src_keys):
            P.op('pool', lambda e: e.memset(ss[0:rows, 0:1], 0.0), [], [tag + 'ss'])
            P.op('act', lambda e: e.activation(out=sqt[0:rows, 0:width], in_=src, func=AF.Square,
                                               accum_out=ss[0:rows, 0:1]),
                 src_keys + [tag + 'ss'], [tag + 'sq', tag + 'ss'])
            P.op('dve', lambda e: e.tensor_scalar(out=ss[0:rows, 0:1], in0=ss[0:rows, 0:1], scalar1=width * EPS,
                                                  scalar2=None, op0=ALU.add), [tag + 'ss'], [tag + 'ss'])
            P.op('pool', lambda e: e.tensor_tensor(out=ss[0:rows, 0:1], in0=ss[0:rows, 0:1], in1=neghalf[0:rows, 0:1],
                                                   op=ALU.pow), [tag + 'ss', 'neghalf'], [tag + 'ss'])

        def norm_transpose(xt, rows, gain, gain_key, dst_b, dst_keys, tag, xkeys, sqt, ss, xn, dst_f=None, dst_f_keys=(), bb=6):
            rms_rstd(xt, rows, DM, sqt, ss, tag, xkeys)
            P.op('dve', lambda e: e.tensor_scalar(out=xn[0:rows, :], in0=xt, scalar1=ss[0:rows, 0:1], scalar2=32.0,
                                                  op0=ALU.mult, op1=ALU.mult), xkeys + [tag + 'ss'], [tag + 'xn'])
            xv = xn[0:rows, :].rearrange("t (p k) -> t k p", k=8)
            for half in range(2):
                bi = bb + half

                def tr(e, half=half, bi=bi):
                    ins = None
                    for kk in range(4):
                        k = half * 4 + kk
                        ins = e.transpose(out=bank(bi)[:, kk * 128:kk * 128 + rows], in_=xv[:, k, :],
                                          identity=ident_f[0:rows, 0:rows])
                    return ins
                P.op('pe', tr, [tag + 'xn', 'ident_f'], [pk(bi)])
                pv = bank(bi).rearrange("p (k t) -> p k t", k=4)[:, :, 0:rows]
                gv = gain[:, half * 4:half * 4 + 4].unsqueeze(2).to_broadcast([128, 4, rows])
                P.op('dve', lambda e, pv=pv, gv=gv, half=half: e.tensor_tensor(
                    out=dst_b[:, half * 4:half * 4 + 4, :], in0=pv, in1=gv, op=ALU.mult),
                    [pk(bi), gain_key], list(dst_keys))
                if dst_f is not None:
                    P.op('act', lambda e, half=half, bi=bi: [e.activation(
                        out=dst_f[:, half * 4 + kk, 0:rows], in_=bank(bi)[:, kk * 128:kk * 128 + rows], func=AF.Copy,
                        scale=gain[:, half * 4 + kk:half * 4 + kk + 1]) for kk in range(4)][-1],
                        [pk(bi), gain_key], list(dst_f_keys))

        def headnorm(zps, zkeys, rows, nh, gain, gain_key, out_f, out_key, tag, sqt, ss, rope_cs=None, tmp=None):
            zv = zps.rearrange("t (h d) -> t h d", h=nh)
            P.op('act', lambda e: e.activation(out=sqt[0:rows, 0:nh * 128], in_=zps, func=AF.Square),
                 zkeys, [tag + 'sq'])
            P.op('dve', lambda e: e.reduce_sum(out=ss[0:rows, 0:nh],
                                               in_=sqt[0:rows, 0:nh * 128].rearrange("t (h d) -> t h d", h=nh),
                                               axis=AX.X), [tag + 'sq'], [tag + 'ss'])
            P.op('dve', lambda e: e.tensor_scalar(out=ss[0:rows, 0:nh], in0=ss[0:rows, 0:nh], scalar1=128 * EPS,
                                                  scalar2=None, op0=ALU.add), [tag + 'ss'], [tag + 'ss'])
            P.op('pool', lambda e: e.tensor_tensor(out=ss[0:rows, 0:nh], in0=ss[0:rows, 0:nh], in1=neghalf[0:rows, 0:nh],
                                                   op=ALU.pow), [tag + 'ss', 'neghalf'], [tag + 'ss'])
            rb = ss[0:rows, 0:nh].unsqueeze(2).to_broadcast([rows, nh, 128])
            P.op('dve', lambda e: e.scalar_tensor_tensor(out=out_f, in0=zv, scalar=SQ128, in1=rb,
                                                         op0=ALU.mult, op1=ALU.mult), zkeys + [tag + 'ss'], [out_key])
            P.op('dve', lambda e: e.tensor_tensor(out=out_f, in0=out_f, in1=gain[0:rows], op=ALU.mult),
                 [out_key, gain_key], [out_key])
            if rope_cs is not None:
                cosb = rope_cs[:, 0:16].unsqueeze(1).to_broadcast([rows, nh, 16])
                sinb = rope_cs[:, 16:32].unsqueeze(1).to_broadcast([rows, nh, 16])
                x1 = out_f[:, :, 0:16]
                x2 = out_f[:, :, 16:32]
                t = [tmp[0:rows, i * nh * 16:(i + 1) * nh * 16].rearrange("t (h d) -> t h d", h=nh) for i in range(4)]
                tk = tag + 'rt'
                P.op('dve', lambda e: e.tensor_tensor(out=t[0], in0=x1, in1=cosb, op=ALU.mult), [out_key, 'cs'], [tk + '0'])
                P.op('dve', lambda e: e.tensor_tensor(out=t[1], in0=x2, in1=sinb, op=ALU.mult), [out_key, 'cs'], [tk + '1'])
                P.op('dve', lambda e: e.tensor_tensor(out=t[2], in0=x2, in1=cosb, op=ALU.mult), [out_key, 'cs'], [tk + '2'])
                P.op('dve', lambda e: e.tensor_tensor(out=t[3], in0=x1, in1=sinb, op=ALU.mult), [out_key, 'cs'], [tk + '3'])
                P.op('dve', lambda e: e.tensor_tensor(out=x1, in0=t[0], in1=t[1], op=ALU.subtract),
                     [tk + '0', tk + '1'], [out_key])
                P.op('dve', lambda e: e.tensor_tensor(out=x2, in0=t[2], in1=t[3], op=ALU.add),
                     [tk + '2', tk + '3'], [out_key])

        mark0 = A.off
        xt2 = [A.alloc([DM], F32) for _ in range(2)]
        sqt1 = [A.alloc([DM], F32) for _ in range(2)]
        xn1 = [A.alloc([DM], F32) for _ in range(2)]
        ss1 = [A.alloc([8], F32) for _ in range(2)]

        def t1(ti, t0, rows, sl):
            P.dma('sp', 'x%d' % sl, xt2[sl][0:rows, :], xin[t0:t0 + rows, :], writes=[('xt', sl)])
            norm_transpose(xt2[sl][0:rows, :], rows, g_mix, 'g_mix', hT[:, :, t0:t0 + rows], [('hT', ti)],
                           'n1_%d' % sl, [('xt', sl)], sqt1[sl], ss1[sl], xn1[sl], bb=(6 if sl == 0 else 4))
        for tp in range(0, 17, 2):
            caps = []
            for ti in (tp, tp + 1):
                if ti < 17:
                    P.begin_capture()
                    t1(ti, TT[ti][0], TT[ti][1], ti % 2)
                    caps.append(P.end_capture())
            P.replay(caps)
        P.barrier()
        A.off = mark0

        cT = A.alloc([4, NT], BF16)
        mark = A.off
        if STOP >= 2:
            wconv = A.alloc([8, 1024], BF16)
            uP = A.alloc([4, 30 + S], F32)
            uS = A.alloc([4, NSB, 34], F32)
            cP = A.alloc([4, NT], F32)
            sig = [A.alloc([512], F32) for _ in range(2)]
            P.dma('pool', 'w0', wconv, w_in[:, 2560:3584].rearrange("(p k) f -> p k f", k=8), writes=['wconv'])
            P.op('pool', lambda e: e.memset(uP[:, :, 0:30], 0.0), [], ['uPpad'])
            stc = A.alloc([4, 512], F32)
            for bt in range(4):
                P.dma('sp', 'stc%d' % bt, stc[0:120, bt, :], state_conv[4 * bt:4 * bt + 4].rearrange("b i c -> (b i) c"),
                      writes=[('stc', bt)])
            for bt in range(4):
                P.op('pe', lambda e, bt=bt: [e.transpose(out=bank(0)[:, j * 128:j * 128 + 120],
                                                         in_=stc[0:120, bt, j * 128:(j + 1) * 128],
                                                         identity=ident_f[0:120, 0:120]) for j in range(4)][-1],
                     [('stc', bt), 'ident_f'], [pk(0)])
                P.op('dve', lambda e, bt=bt: e.tensor_copy(
                    out=uS[:, :, 4 * bt:4 * bt + 4, 0:30],
                    in_=bank(0).rearrange("p (j x) -> p j x", j=4)[:, :, 0:120].rearrange("p j (b i) -> p j b i", b=4)),
                    [pk(0)], [('uS', bt)])
            cnt = 0
            for gi, (t0, n) in enumerate(TG):
                for j in range(4):
                    ba, bb = 2 + (cnt % 2) * 2, 3 + (cnt % 2) * 2
                    sgi = cnt % 2
                    sg = sig[sgi]
                    cnt += 1
                    rk = ['wconv'] + [('hT', ti) for ti in range(17)]
                    mm(bank(ba)[:, 0:n], [(wconv[:, k, j * 128:(j + 1) * 128], hT[:, k, t0:t0 + n]) for k in range(8)],
                       rk, [pk(ba)])
                    mm(bank(bb)[:, 0:n], [(wconv[:, k, 512 + j * 128:512 + (j + 1) * 128], hT[:, k, t0:t0 + n]) for k in range(8)],
                       rk, [pk(bb)])
                    P.op('act', lambda e, bb=bb, n=n, sg=sg: e.activation(out=sg[:, 0:n], in_=bank(bb)[:, 0:n], func=AF.Sigmoid),
                         [pk(bb)], [('sig', sgi)])
                    if gi < 4:
                        dst = uP[:, j, 30 + t0:30 + t0 + n]
                        P.op('dve', lambda e, ba=ba, n=n, sg=sg, dst=dst: e.tensor_tensor(out=dst, in0=bank(ba)[:, 0:n], in1=sg[:, 0:n], op=ALU.mult),
                             [pk(ba), ('sig', sgi)], [('uP', j)])
                    else:
                        dst = uS[:, j, :, 30:34]
                        P.op('dve', lambda e, ba=ba, sg=sg, dst=dst: e.tensor_tensor(
                            out=dst, in0=bank(ba)[:, 0:NS].rearrange("p (b t) -> p b t", t=4),
                            in1=sg[:, 0:NS].rearrange("p (b t) -> p b t", t=4), op=ALU.mult),
                            [pk(ba), ('sig', sgi)], [('uSn', j)])
            issue_bg()
            cst = A.alloc([512], F32)
            P.op('pe', lambda e: [e.transpose(out=bank(0)[0:30, j * 128:(j + 1) * 128], in_=uP[:, j, S:S + 30],
                                              identity=ident_f) for j in range(4)][-1],
                 [('uP', j) for j in range(4)] + ['ident_f'], [pk(0)])
            P.op('dve', lambda e: e.tensor_copy(out=cst[0:30, :], in_=bank(0)[0:30, :]), [pk(0)], ['cst'])
            P.dma('sp', 'o_cp', convp, cst[0:30, :], reads=['cst'], writes=['convp'])
            unew = A.alloc([4, NS], F32)
            P.op('dve', lambda e: e.tensor_copy(out=unew.rearrange("p j (b t) -> p j b t", t=4), in_=uS[:, :, :, 30:34]),
                 [('uSn', j) for j in range(4)], ['unew'])
            cst2 = A.alloc([512], F32)
            P.op('pe', lambda e: [e.transpose(out=bank(1)[0:NS, j * 128:(j + 1) * 128], in_=unew[:, j, :],
                                              identity=ident_f) for j in range(4)][-1], ['unew', 'ident_f'], [pk(1)])
            P.op('dve', lambda e: e.tensor_copy(out=cst2[0:NS, :], in_=bank(1)[0:NS, :]), [pk(1)], ['cst2'])
            for b in range(NSB):
                P.dma('sp', 'o_cs', convs[b, 26:30, :], cst2[4 * b:4 * b + 4, :], reads=['cst2'], writes=[('convs', b)])
            for j in range(4):
                eng = 'dve'
                for (src, dst, rk, wk) in (
                        (lambda i, j=j: uP[:, j, i:i + S], cP[:, j, 0:S], [('uP', j), 'uPpad'], ('cP', j)),
                        (lambda i, j=j: uS[:, j, :, i:i + 4], cP[:, j, S:NT].rearrange("p (b t) -> p b t", t=4),
                         [('uSn', j)] + [('uS', bt) for bt in range(4)], ('cS', j))):
                    P.op(eng, lambda e, src=src, dst=dst, j=j: e.tensor_scalar(
                        out=dst, in0=src(0), scalar1=convw[:, j, 0:1], scalar2=convb[:, j:j + 1], op0=ALU.mult, op1=ALU.add),
                        rk + ['convw', 'convb'], [wk])
                    for i in range(1, 31):
                        P.op(eng, lambda e, src=src, dst=dst, j=j, i=i: e.scalar_tensor_tensor(
                            out=dst, in0=src(i), scalar=convw[:, j, i:i + 1], in1=dst, op0=ALU.mult, op1=ALU.add),
                            rk + ['convw', wk], [wk])
            sqc = [A.alloc([512], F32) for _ in range(2)]
            mean = A.alloc([512], F32)
            msq = A.alloc([512], F32)
            var = A.alloc([512], F32)
            nt_ = A.alloc([512], F32)
            tln = [A.alloc([512], F32) for _ in range(2)]
            for gi, (t0, n) in enumerate(TG):
                ck = [('cP', j) if gi < 4 else ('cS', j) for j in range(4)]
                mm(bank(0)[:, 0:n], [(ones_f, cP[:, j, t0:t0 + n]) for j in range(4)], ck + ['ones_f'], [pk(0)])
                for j in range(4):
                    P.op('act', lambda e, j=j, t0=t0, n=n: e.activation(out=sqc[j % 2][:, 0:n], in_=cP[:, j, t0:t0 + n], func=AF.Square),
                         [ck[j]], [('sqc', j % 2)])
                    P.op('pe', lambda e, j=j, n=n: e.matmul(out=bank(1)[:, 0:n], lhsT=ones_f, rhs=sqc[j % 2][:, 0:n],
                                                            start=(j == 0), stop=(j == 3)),
                         [('sqc', j % 2), 'ones_f'], [pk(1)])
                P.op('dve', lambda e, n=n: e.tensor_scalar(out=mean[:, 0:n], in0=bank(0)[:, 0:n], scalar1=1.0 / 512, scalar2=None,
                                                           op0=ALU.mult), [pk(0)], ['mean'])
                P.op('dve', lambda e, n=n: e.tensor_tensor(out=msq[:, 0:n], in0=mean[:, 0:n], in1=mean[:, 0:n], op=ALU.mult),
                     ['mean'], ['msq'])
                P.op('dve', lambda e, n=n: e.scalar_tensor_tensor(out=var[:, 0:n], in0=bank(1)[:, 0:n], scalar=1.0 / 512,
                                                                  in1=msq[:, 0:n], op0=ALU.mult, op1=ALU.subtract),
                     [pk(1), 'msq'], ['var'])
                P.op('dve', lambda e, n=n: e.tensor_scalar(out=var[:, 0:n], in0=var[:, 0:n], scalar1=EPS, scalar2=None, op0=ALU.add),
                     ['var'], ['var'])
                vi = var[:, 0:n].bitcast(mybir.dt.int32)
                ryi = msq[:, 0:n].bitcast(mybir.dt.int32)
                P.op('dve', lambda e, vi=vi, ryi=ryi: e.tensor_single_scalar(out=ryi, in_=vi, scalar=1, op=ALU.arith_shift_right),
                     ['var'], ['msq'])
                P.op('dve', lambda e, ryi=ryi: e.tensor_scalar(out=ryi, in0=ryi, scalar1=-1, scalar2=0x5f3759df, op0=ALU.mult, op1=ALU.add),
                     ['msq'], ['msq'])
                for it_ in range(3):
                    P.op('dve', lambda e, n=n: e.tensor_tensor(out=nt_[:, 0:n], in0=msq[:, 0:n], in1=msq[:, 0:n], op=ALU.mult), ['msq'], ['nt_'])
                    P.op('dve', lambda e, n=n: e.tensor_tensor(out=nt_[:, 0:n], in0=nt_[:, 0:n], in1=var[:, 0:n], op=ALU.mult), ['nt_', 'var'], ['nt_'])
                    P.op('dve', lambda e, n=n: e.tensor_scalar(out=nt_[:, 0:n], in0=nt_[:, 0:n], scalar1=-0.5, scalar2=1.5, op0=ALU.mult, op1=ALU.add),
                         ['nt_'], ['nt_'])
                    P.op('dve', lambda e, n=n: e.tensor_tensor(out=msq[:, 0:n], in0=msq[:, 0:n], in1=nt_[:, 0:n], op=ALU.mult), ['msq', 'nt_'], ['msq'])
                P.op('dve', lambda e, n=n: e.tensor_copy(out=var[:, 0:n], in_=msq[:, 0:n]), ['msq'], ['var'])
                for j in range(4):
                    tl = tln[j % 2]
                    P.op('dve', lambda e, j=j, t0=t0, n=n, tl=tl: e.tensor_tensor(out=tl[:, 0:n], in0=cP[:, j, t0:t0 + n], in1=mean[:, 0:n],
                                                                                 op=ALU.subtract), [ck[j], 'mean'], [('tln', j % 2)])
                    P.op('dve', lambda e, n=n, tl=tl: e.tensor_tensor(out=tl[:, 0:n], in0=tl[:, 0:n], in1=var[:, 0:n], op=ALU.mult),
                         [('tln', j % 2), 'var'], [('tln', j % 2)])
                    P.op('act', lambda e, j=j, t0=t0, n=n, tl=tl: e.activation(out=cT[:, j, t0:t0 + n], in_=tl[:, 0:n], func=AF.Silu,
                                                                              scale=lng[:, j:j + 1], bias=lnb[:, j:j + 1]),
                         [('tln', j % 2), 'lng', 'lnb'], [('cT', gi)])
        P.barrier()
        A.off = mark

        moT = A.alloc([4, NT], BF16)
        mark = A.off
        if STOP >= 3:
            wkv = A.alloc([8, 1024], BF16)
            wmq = A.alloc([8, 512], BF16)
            P.dma('pool', 'w0', wkv, w_mem_kv.rearrange("(p k) f -> p k f", k=8), writes=['wkv'])
            P.dma('pool', 'w1', wmq, w_in[:, 3584:4096].rearrange("(p k) f -> p k f", k=8), writes=['wmq'])
            xt2 = [A.alloc([DM], F32) for _ in range(2)]
            sqt = A.alloc([DM], F32)
            xn = A.alloc([DM], F32)
            ss = A.alloc([8], F32)
            memhT = A.alloc([8, 256], BF16)
            mkT = A.alloc([4, 256], BF16)
            mvb = A.alloc([2, 512], BF16)
            kf = [A.alloc([4, 128], F32) for _ in range(2)]
            vf = [A.alloc([512], F32) for _ in range(2)]
            kb = A.alloc([512], BF16)
            for ti in range(2):
                P.dma('sp', 'x%d' % ti, xt2[ti], mem[ti * 128:(ti + 1) * 128, :], writes=[('xt', ti)])
                norm_transpose(xt2[ti], 128, g_mem, 'g_mem', memhT[:, :, ti * 128:(ti + 1) * 128], [('memhT', ti)],
                               'n3', [('xt', ti)], sqt, ss, xn)
            for ti in range(2):
                mm(bank(0), [(memhT[:, k, ti * 128:(ti + 1) * 128], wkv[:, k, 0:512]) for k in range(8)],
                   [('memhT', ti), 'wkv'], [pk(0)])
                mm(bank(1), [(memhT[:, k, ti * 128:(ti + 1) * 128], wkv[:, k, 512:1024]) for k in range(8)],
                   [('memhT', ti), 'wkv'], [pk(1)])
                headnorm(bank(0), [pk(0)], 128, 4, gmk, 'gmk', kf[ti], ('kf', ti), 'h3', sqt, ss)
                P.dma('sp', 'o_mk', mkp[ti * 128:(ti + 1) * 128].rearrange("m h d -> m (h d)"),
                      kf[ti].rearrange("p h d -> p (h d)"), reads=[('kf', ti)], writes=[('mkp', ti)])
                P.op('act', lambda e, ti=ti: e.activation(out=vf[ti], in_=bank(1), func=AF.Copy), [pk(1)], [('vf', ti)])
                P.dma('sp', 'o_mv', mvp[ti * 128:(ti + 1) * 128].rearrange("m h d -> m (h d)"), vf[ti],
                      reads=[('vf', ti)], writes=[('mvp', ti)])
                P.op('dve', lambda e, ti=ti: e.tensor_copy(out=mvb[:, ti, :], in_=vf[ti]), [('vf', ti)], [('mvb', ti)])
                P.op('dve', lambda e, ti=ti: e.tensor_copy(out=kb, in_=kf[ti].rearrange("p h d -> p (h d)")), [('kf', ti)], ['kb'])
                pbv = bank(2).bitcast(BF16)
                P.op('pe', lambda e, pbv=pbv: [e.transpose(out=pbv[:, h * 128:(h + 1) * 128], in_=kb[:, h * 128:(h + 1) * 128],
                                                           identity=ident_b) for h in range(4)][-1], ['kb', 'ident_b'], [pk(2)])
                P.op('dve', lambda e, ti=ti, pbv=pbv: e.tensor_copy(out=mkT[:, :, ti * 128:(ti + 1) * 128],
                                                                   in_=pbv[:, 0:512].rearrange("p (h m) -> p h m", h=4)),
                     [pk(2)], [('mkT', ti)])
            zf = A.alloc([4, 128], F32)
            zb = A.alloc([512], BF16)
            mqT = A.alloc([4, 512], BF16)
            PT = [A.alloc([512], BF16) for _ in range(4)]
            rden = A.alloc([512], F32)
            zfQ = [zf, A.alloc([4, 128], F32)]
            zbQ = [zb, A.alloc([512], BF16)]
            sqtQ = [sqt, A.alloc([DM], F32)]
            ssQ = [ss, A.alloc([8], F32)]

            def t3(ti, t0, rows, sl):
                bz, bt = (0, 2) if sl == 0 else (6, 7)
                zf_, zb_, sqt_, ss_ = zfQ[sl], zbQ[sl], sqtQ[sl], ssQ[sl]
                mm(bank(bz)[0:rows, :], [(hT[:, k, t0:t0 + rows], wmq[:, k, :]) for k in range(8)], ['wmq'], [pk(bz)])
                headnorm(bank(bz)[0:rows, :], [pk(bz)], rows, 4, gmq, 'gmq', zf_[0:rows], ('zf', sl), 'h3q%d' % sl, sqt_, ss_)
                P.op('act', lambda e, rows=rows: e.activation(out=zb_[0:rows, :], in_=zf_[0:rows].rearrange("p h d -> p (h d)"), func=AF.Copy),
                     [('zf', sl)], [('zb', sl)])
                pbv = bank(bt).bitcast(BF16)
                P.op('pe', lambda e, pbv=pbv, rows=rows: [e.transpose(out=pbv[:, h * 128:h * 128 + rows], in_=zb_[0:rows, h * 128:(h + 1) * 128],
                                                                     identity=ident_b[0:rows, 0:rows]) for h in range(4)][-1],
                     [('zb', sl), 'ident_b'], [pk(bt)])
                c0 = (ti % 4) * 128
                P.op('dve', lambda e, pbv=pbv, rows=rows, c0=c0: e.tensor_copy(
                    out=mqT[:, :, c0:c0 + rows], in_=pbv[:, 0:512].rearrange("p (h m) -> p h m", h=4)[:, :, 0:rows]),
                    [pk(bt)], [('mqT', ti % 4)])
                if ti % 4 == 3 and ti < 16:
                    g0 = (ti // 4) * 512
                    for h in range(4):
                        for m in range(2):
                            mm(bank(3 + m), [(mkT[:, h, m * 128:(m + 1) * 128], mqT[:, h, :])],
                               [('mkT', 0), ('mkT', 1)] + [('mqT', q) for q in range(4)], [pk(3 + m)])
                            P.op('act', lambda e, m=m: e.activation(out=PT[m], in_=bank(3 + m), func=AF.Exp, scale=ISQ128),
                                 [pk(3 + m)], [('PT', m)])
                        mm(bank(5), [(mvb[:, m, h * 128:(h + 1) * 128], PT[m]) for m in range(2)],
                           [('mvb', 0), ('mvb', 1), ('PT', 0), ('PT', 1)], [pk(5)])
                        mm(bank(1), [(ones_b, PT[m]) for m in range(2)], ['ones_b', ('PT', 0), ('PT', 1)], [pk(1)])
                        P.op('dve', lambda e: e.reciprocal(out=rden, in_=bank(1)), [pk(1)], ['rden'])
                        P.op('dve', lambda e, h=h, g0=g0: e.tensor_tensor(out=moT[:, h, g0:g0 + 512], in0=bank(5), in1=rden, op=ALU.mult),
                             [pk(5), 'rden'], [('moT', h, g0)])
            for tp in range(0, 17, 2):
                caps = []
                for ti in (tp, tp + 1):
                    if ti < 17:
                        P.begin_capture()
                        t3(ti, TT[ti][0], TT[ti][1], ti % 2)
                        caps.append(P.end_capture())
                P.replay(caps)
            if STOP >= 31:
                mkf = [A.alloc([2, 512], F32) for _ in range(2)]
                mvf = [A.alloc([2, 512], F32) for _ in range(2)]
                mvb2 = [A.alloc([2, 512], BF16) for _ in range(2)]
                mkTb = A.alloc([4, 256], BF16)
                PTs = A.alloc([2, 16], BF16)
                rds = A.alloc([16], F32)
                for b in range(NSB):
                    sl = b % 2
                    P.dma('sp', 'mk%d' % sl, mkf[sl], cmk[b].rearrange("(m p) h d -> p m (h d)", p=128), writes=[('mkf', sl)])
                    P.dma('sp', 'mv%d' % sl, mvf[sl], cmv[b].rearrange("(m p) h d -> p m (h d)", p=128), writes=[('mvf', sl)])
                    P.op('pool', lambda e, sl=sl: e.tensor_copy(out=mvb2[sl], in_=mvf[sl]), [('mvf', sl)], [('mvb2', sl)])
                    for m in range(2):
                        P.op('pe', lambda e, sl=sl, m=m: [e.transpose(out=bank(6 + m)[:, h * 128:(h + 1) * 128],
                                                                      in_=mkf[sl][:, m, h * 128:(h + 1) * 128], identity=ident_f)
                                                          for h in range(4)][-1], [('mkf', sl), 'ident_f'], [pk(6 + m)])
                        P.op('act' if m == 0 else 'dve', (lambda e, m=m: e.activation(
                            out=mkTb[:, :, m * 128:(m + 1) * 128], in_=bank(6 + m).rearrange("p (h k) -> p h k", h=4), func=AF.Copy))
                            if m == 0 else (lambda e, m=m: e.tensor_copy(
                                out=mkTb[:, :, m * 128:(m + 1) * 128], in_=bank(6 + m).rearrange("p (h k) -> p h k", h=4))),
                            [pk(6 + m)], [('mkTb', m)])
                    def sc(e, b=b):
                        ins = None
                        for m in range(2):
                            for h in range(4):
                                ins = e.matmul(out=bank(3)[:, m * 16 + h * 4:m * 16 + h * 4 + 4], lhsT=mkTb[:, h, m * 128:(m + 1) * 128],
                                               rhs=mqT[:, h, 4 * b:4 * b + 4], start=True, stop=True)
                        return ins
                    P.op('pe', sc, [('mkTb', 0), ('mkTb', 1), ('mqT', 0)], [pk(3)])
                    P.op('act', lambda e: e.activation(out=PTs.rearrange("p m x -> p (m x)"), in_=bank(3)[:, 0:32], func=AF.Exp, scale=ISQ128),
                         [pk(3)], ['PTs'])

                    def pv(e, sl=sl):
                        ins = None
                        for h in range(4):
                            for m in range(2):
                                ins = e.matmul(out=bank(4)[:, h * 4:h * 4 + 4], lhsT=mvb2[sl][:, m, h * 128:(h + 1) * 128],
                                               rhs=PTs[:, m, h * 4:h * 4 + 4], start=(m == 0), stop=(m == 1))
                        for m in range(2):
                            ins = e.matmul(out=bank(5)[:, 0:16], lhsT=ones_b, rhs=PTs[:, m, :], start=(m == 0), stop=(m == 1))
                        return ins
                    P.op('pe', pv, ['PTs', ('mvb2', sl), 'ones_b'], [pk(4), pk(5)])
                    P.op('dve', lambda e: e.reciprocal(out=rds, in_=bank(5)[:, 0:16]), [pk(5)], ['rds'])
                    P.op('dve', lambda e, b=b: e.tensor_tensor(out=moT[:, :, S + 4 * b:S + 4 * b + 4],
                                                               in0=bank(4)[:, 0:16].rearrange("p (h t) -> p h t", h=4),
                                                               in1=rds.rearrange("p (h t) -> p h t", h=4), op=ALU.mult),
                         [pk(4), 'rds'], [('moTs', b)])
        P.barrier()
        A.off = mark

        aoT = A.alloc([4, NT], BF16)
        knew = A.alloc([4, 128], F32)
        vnew = A.alloc([4, 128], F32)
        vnewb = A.alloc([4, 128], BF16)
        kTnew = A.alloc([4, NS], BF16)
        qs_all = A.alloc([4, NSB, 12], BF16)
        mark = A.off
        if STOP >= 4:
            wq2 = [A.alloc([8, 640], BF16) for _ in range(2)]
            sqt2 = [A.alloc([512], F32) for _ in range(2)]
            ss2 = [A.alloc([8], F32) for _ in range(2)]
            qkf = [A.alloc([4, 128], F32) for _ in range(2)]
            qkb2 = [A.alloc([512], BF16) for _ in range(2)]
            rtmp2 = [A.alloc([4 * 64], F32) for _ in range(2)]
            vfo = [A.alloc([128], F32) for _ in range(2)]
            qkT = A.alloc([4, NT], BF16)
            Vn = A.alloc([17, 128], BF16)
            Vp = [A.alloc([16, 128], BF16) for _ in range(2)]
            acc = A.alloc([S], F32)
            dacc = A.alloc([S], F32)
            PTa = [A.alloc([256], BF16) for _ in range(3)]

            def load_w(h):
                w = wq2[h % 2]
                for g in range(3):
                    P.dma('pool', 'wq%d' % (h % 2), w[:, :, g * 128:(g + 1) * 128],
                          w_in[:, g * 512 + h * 128:g * 512 + (h + 1) * 128].rearrange("(p k) f -> p k f", k=8), writes=[('wq', h % 2, g)])
                P.dma('pool', 'wq%d' % (h % 2), w[:, :, 384:512],
                      w_in[:, 1536 + h * 128:1536 + (h + 1) * 128].rearrange("(p k) f -> p k f", k=8), writes=[('wq', h % 2, 3)])
                P.dma('pool', 'wq%d' % (h % 2), w[:, :, 512:640],
                      w_in[:, 2048 + h * 128:2048 + (h + 1) * 128].rearrange("(p k) f -> p k f", k=8), writes=[('wq', h % 2, 4)])
            load_w(0)
            cnt_pt = 0
            for h in range(4):
                if h + 1 < 4:
                    load_w(h + 1)
                w = wq2[h % 2]
                wks_ = [('wq', h % 2, q_) for q_ in range(5)]
                def tile_ops(ti, t0, rows, sl, h=h, w=w, wks_=wks_):
                    b0, b1, b2 = (0, 1, 2) if sl == 0 else (3, 4, 7)
                    sqt, ss, qkb, rtmp = sqt2[sl], ss2[sl], qkb2[sl], rtmp2[sl]
                    mm(bank(b0)[0:rows, :], [(hT[:, k, t0:t0 + rows], w[:, k, 0:512]) for k in range(8)], wks_, [pk(b0)])
                    mm(bank(b1)[0:rows, 0:128], [(hT[:, k, t0:t0 + rows], w[:, k, 512:640]) for k in range(8)], wks_, [pk(b1)])
                    headnorm(bank(b0)[0:rows, :], [pk(b0)], rows, 4, gqk, 'gqk', qkf[sl][0:rows], ('qkf', sl), 'h4_%d' % sl, sqt, ss,
                             rope_cs=cs[0:rows, ti, :], tmp=rtmp)
                    if ti < 16:
                        P.dma('sp', 'o_k%d' % sl, kwp[t0:t0 + rows, h, :], qkf[sl][0:rows, 3, :], reads=[('qkf', sl)], writes=[('kwp', h, ti)])
                        P.op('act', lambda e, sl=sl, rows=rows: e.activation(out=vfo[sl][0:rows, :], in_=bank(b1)[0:rows, 0:128], func=AF.Copy),
                             [pk(b1)], [('vfo', sl)])
                        P.dma('sp', 'o_v%d' % sl, vwp[t0:t0 + rows, h, :], vfo[sl][0:rows, :], reads=[('vfo', sl)], writes=[('vwp', h, ti)])
                        P.op('dve', lambda e, sl=sl, ti=ti: e.tensor_copy(out=Vn[:, ti, :], in_=vfo[sl]), [('vfo', sl)], [('Vn', ti)])
                    else:
                        P.op('act', lambda e, h=h: e.activation(out=vnew[0:NS, h, :], in_=bank(b1)[0:NS, 0:128], func=AF.Copy),
                             [pk(b1)], [('vnew', h)])
                        P.op('dve', lambda e, h=h: e.tensor_copy(out=vnewb[0:NS, h, :], in_=vnew[0:NS, h, :]), [('vnew', h)], [('vnewb', h)])
                        P.op('dve', lambda e, h=h, sl=sl: e.tensor_copy(out=knew[0:NS, h, :], in_=qkf[sl][0:NS, 3, :]), [('qkf', sl)], [('knew', h)])
                    P.op('act', lambda e, sl=sl, rows=rows: e.activation(out=qkb[0:rows, :], in_=qkf[sl][0:rows].rearrange("p h d -> p (h d)"),
                                                                        func=AF.Copy), [('qkf', sl)], [('qkb', sl)])
                    pbv = bank(b2).bitcast(BF16)
                    P.op('pe', lambda e, pbv=pbv, rows=rows: [e.transpose(out=pbv[:, i * 128:i * 128 + rows], in_=qkb[0:rows, i * 128:(i + 1) * 128],
                                                                         identity=ident_b[0:rows, 0:rows]) for i in range(4)][-1],
                         [('qkb', sl), 'ident_b'], [pk(b2)])
                    P.op('dve', lambda e, pbv=pbv, rows=rows, t0=t0: e.tensor_copy(
                        out=qkT[:, :, t0:t0 + rows], in_=pbv[:, 0:512].rearrange("p (i m) -> p i m", i=4)[:, :, 0:rows]),
                        [pk(b2)], [('qkT', ti)])
                for tp in range(0, 17, 2):
                    caps = []
                    for ti in (tp, tp + 1):
                        if ti < 17:
                            P.begin_capture()
                            tile_ops(ti, TT[ti][0], TT[ti][1], ti % 2)
                            caps.append(P.end_capture())
                    P.replay(caps)
                P.op('dve', lambda e, h=h: e.tensor_copy(out=qs_all[:, h].rearrange("p b (g t) -> p b g t", g=3),
                                                         in_=qkT[:, 0:3, S:NT].rearrange("p g (b t) -> p b g t", t=4)),
                     [('qkT', 16)], [('qs_all', h)])
                P.op('dve', lambda e, h=h: e.tensor_copy(out=kTnew[:, h, :], in_=qkT[:, 3, S:NT]), [('qkT', 16)], [('kTnew', h)])
                for gi, dil in ((0, 4), (1, 16)):
                    for blk in range(16):
                        if dil == 4:
                            r, i = blk // 4, blk % 4
                            c0, step = i * 512 + r, 4
                        else:
                            c0, step = blk, 16
                        mm(bank(1)[:, 0:128], [(hT[:, k, ss_(c0, 128, step)], w[:, k, 512:640]) for k in range(8)], wks_, [pk(1)])
                        P.op('act', lambda e, gi=gi, blk=blk: e.activation(out=Vp[gi][:, blk, :], in_=bank(1)[:, 0:128], func=AF.Copy),
                             [pk(1)], [('Vp', gi, blk)])
                P.op('pool', lambda e: e.memset(acc, 0.0), [], ['acc'])
                P.op('pool', lambda e: e.memset(dacc, 0.0), [], ['dacc'])
                allq = [('qkT', ti) for ti in range(16)]
                for g in range(3):
                    dil = (1, 4, 16)[g]
                    nper = 4
                    for grp in range(4):
                        for s4 in range(4):
                            blk = grp * 4 + s4
                            if g == 0:
                                cq = slice(blk * 128, blk * 128 + 128)
                                ckp = slice((blk - 1) * 128, blk * 128) if blk > 0 else None
                                Vown = Vn[:, blk, :]
                                Vprev = Vn[:, blk - 1, :] if blk > 0 else None
                                vkeys = [('Vn', blk)] + ([('Vn', blk - 1)] if blk > 0 else [])
                            elif g == 1:
                                r, i = blk // 4, blk % 4
                                cq = ss_(i * 512 + r, 128, 4)
                                ckp = ss_((i - 1) * 512 + r, 128, 4) if i > 0 else None
                                Vown = Vp[0][:, blk, :]
                                Vprev = Vp[0][:, blk - 1, :] if i > 0 else None
                                vkeys = [('Vp', 0, blk)] + ([('Vp', 0, blk - 1)] if i > 0 else [])
                            else:
                                cq = ss_(blk, 128, 16)
                                ckp = None
                                Vown = Vp[1][:, blk, :]
                                Vprev = None
                                vkeys = [('Vp', 1, blk)]
                            sb_ = 3 + (cnt_pt % 2)
                            pt = PTa[cnt_pt % 3]
                            ptk = ('PTa', cnt_pt % 3)
                            cnt_pt += 1

                            def sc(e, g=g, cq=cq, ckp=ckp, sb_=sb_):
                                q = qkT[:, g, cq]
                                if ckp is not None:
                                    e.matmul(out=bank(sb_)[:, 0:256], lhsT=ident_b, rhs=maskb, start=True, stop=False)
                                    e.matmul(out=bank(sb_)[:, 0:128], lhsT=qkT[:, 3, ckp], rhs=q, start=False, stop=False)
                                else:
                                    e.matmul(out=bank(sb_)[:, 128:256], lhsT=ident_b, rhs=maskb[:, 128:256], start=True, stop=False)
                                return e.matmul(out=bank(sb_)[:, 128:256], lhsT=qkT[:, 3, cq], rhs=q, start=False, stop=True)
                            P.op('pe', sc, allq + ['ident_b', 'maskb'], [pk(sb_)])
                            lo = 0 if ckp is not None else 128
                            P.op('act', lambda e, sb_=sb_, pt=pt, lo=lo: e.activation(out=pt[:, lo:256], in_=bank(sb_)[:, lo:256],
                                                                                       func=AF.Exp, scale=ISQ128), [pk(sb_)], [ptk])

                            def pvf(e, s4=s4, pt=pt, Vown=Vown, Vprev=Vprev):
                                o = bank(5)[:, s4 * 128:(s4 + 1) * 128]
                                d = bank(6)[:, s4 * 128:(s4 + 1) * 128]
                                if Vprev is not None:
                                    e.matmul(out=o, lhsT=Vprev, rhs=pt[:, 0:128], start=True, stop=False)
                                    e.matmul(out=o, lhsT=Vown, rhs=pt[:, 128:256], start=False, stop=True)
                                    e.matmul(out=d, lhsT=ones_b, rhs=pt[:, 0:128], start=True, stop=False)
                                    return e.matmul(out=d, lhsT=ones_b, rhs=pt[:, 128:256], start=False, stop=True)
                                e.matmul(out=o, lhsT=Vown, rhs=pt[:, 128:256], start=True, stop=True)
                                return e.matmul(out=d, lhsT=ones_b, rhs=pt[:, 128:256], start=True, stop=True)
                            P.op('pe', pvf, [ptk, 'ones_b'] + vkeys, [pk(5), pk(6)])
                        if g == 0:
                            av = acc[:, grp * 512:(grp + 1) * 512]
                            dv = dacc[:, grp * 512:(grp + 1) * 512]
                            sh = None
                        elif g == 1:
                            av = acc[:, ss_(grp, 512, 4)]
                            dv = dacc[:, ss_(grp, 512, 4)]
                            sh = None
                        else:
                            av = acc.rearrange("p (u s) -> p s u", s=16)[:, grp * 4:grp * 4 + 4, :]
                            dv = dacc.rearrange("p (u s) -> p s u", s=16)[:, grp * 4:grp * 4 + 4, :]
                            sh = 4
                        o5 = bank(5) if sh is None else bank(5).rearrange("p (s u) -> p s u", s=4)
                        o6 = bank(6) if sh is None else bank(6).rearrange("p (s u) -> p s u", s=4)
                        P.op('dve', lambda e, av=av, o5=o5: e.tensor_tensor(out=av, in0=o5, in1=av, op=ALU.add), [pk(5), 'acc'], ['acc'])
                        P.op('dve', lambda e, dv=dv, o6=o6: e.tensor_tensor(out=dv, in0=o6, in1=dv, op=ALU.add), [pk(6), 'dacc'], ['dacc'])
                P.op('dve', lambda e: e.reciprocal(out=dacc, in_=dacc), ['dacc'], ['dacc'])
                P.op('dve', lambda e, h=h: e.tensor_tensor(out=aoT[:, h, 0:S], in0=acc, in1=dacc, op=ALU.mult), ['acc', 'dacc'], [('aoT', h)])
        P.barrier()
        A.off = mark

        if STOP >= 5:
            kst = [A.alloc([7, 512], F32) for _ in range(2)]
            vst = [A.alloc([7, 512], F32) for _ in range(2)]
            vsb = [A.alloc([7, 512], BF16) for _ in range(2)]
            kTb = A.alloc([4, 7, 128], BF16)
            PTn = A.alloc([4, 192], BF16)
            PTb = [A.alloc([7, 48], BF16) for _ in range(2)]
            for b in range(NSB):
                P.dma('sp', 'o_kn', kws[b, 2044:2048].rearrange("t h d -> t (h d)"), knew[4 * b:4 * b + 4].rearrange("p h d -> p (h d)"),
                      reads=[('knew', h) for h in range(4)], writes=[('kwsn', b)])
                P.dma('sp', 'o_vn', vws[b, 2044:2048].rearrange("t h d -> t (h d)"), vnew[4 * b:4 * b + 4].rearrange("p h d -> p (h d)"),
                      reads=[('vnew', h) for h in range(4)], writes=[('vwsn', b)])
            for hp in range(2):
                def scn(e, hp=hp):
                    ins = None
                    for hh in range(2):
                        h = hp * 2 + hh
                        o = bank(0 + hp)[0:NS, hh * 192:(hh + 1) * 192]
                        e.matmul(out=o, lhsT=ident_b[0:NS, 0:NS], rhs=nmaskb[0:NS, :], start=True, stop=False)
                        ins = e.matmul(out=o, lhsT=kTnew[:, h, :], rhs=qs_all[:, h].rearrange("p b x -> p (b x)"), start=False, stop=True)
                    return ins
                P.op('pe', scn, ['ident_b', 'nmaskb'] + [('kTnew', h) for h in range(4)] + [('qs_all', h) for h in range(4)], [pk(hp)])
                P.op('act', lambda e, hp=hp: e.activation(out=PTn[0:NS, hp * 2:hp * 2 + 2, :].rearrange("p a x -> p (a x)"),
                                                          in_=bank(hp)[0:NS, 0:384], func=AF.Exp, scale=ISQ128), [pk(hp)], [('PTn', hp)])
            def newpv(e):
                ins = None
                for h in range(4):
                    o = bank(4 + h // 2)[:, (h % 2) * 192:(h % 2 + 1) * 192]
                    ins = e.matmul(out=o, lhsT=vnewb[0:NS, h, :], rhs=PTn[0:NS, h, :], start=(h % 2 == 0), stop=False, skip_group_check=True)
                for h in range(4):
                    for half in range(2):
                        d = bank(6 + half)[:, 0:384].rearrange("p (b hx) -> p b hx", b=8)[:, :, h * 12:(h + 1) * 12]
                        ins = e.matmul(out=d, lhsT=ones_b[0:NS, :], rhs=PTn[0:NS, h, half * 96:(half + 1) * 96].rearrange("p (b x) -> p b x", b=8),
                                       start=(h == 0), stop=False, skip_group_check=True)
                return ins
            P.op('pe', newpv, [('PTn', 0), ('PTn', 1), 'ones_b'] + [('vnewb', h) for h in range(4)], [pk(4), pk(5), pk(6), pk(7)])
            for b in range(NSB):
                sl = b % 2
                P.dma('sp', 'ck%d' % sl, kst[sl][:, 0:4, :], cache_k[b, 1536:2048].rearrange("(j p) h d -> p j (h d)", p=128), writes=[('kst', sl, 4)])
                P.dma('sp', 'cv%d' % sl, vst[sl][:, 0:4, :], cache_v[b, 1536:2048].rearrange("(j p) h d -> p j (h d)", p=128), writes=[('vst', sl, 4)])
                for w_ in range(4):
                    P.dma('sp', 'ck%d' % sl, kst[sl][w_ * 32:(w_ + 1) * 32, 4:7, :],
                          cache_k[b, 0:1536].rearrange("(j gl s) h d -> s gl j (h d)", s=16, gl=32)[w_], writes=[('kst', sl, w_)])
                    P.dma('sp', 'cv%d' % sl, vst[sl][w_ * 32:(w_ + 1) * 32, 4:7, :],
                          cache_v[b, 0:1536].rearrange("(j gl s) h d -> s gl j (h d)", s=16, gl=32)[w_], writes=[('vst', sl, w_)])
                P.op('act', lambda e, sl=sl: e.activation(out=vsb[sl], in_=vst[sl], func=AF.Copy), [('vst', sl, q_) for q_ in range(5)], [('vsb', sl)])
                for j in range(7):
                    tb = 2 + (j % 2)
                    P.op('pe', lambda e, sl=sl, j=j, tb=tb: [e.transpose(out=bank(tb)[:, h * 128:(h + 1) * 128],
                                                                        in_=kst[sl][:, j, h * 128:(h + 1) * 128], identity=ident_f)
                                                            for h in range(4)][-1], [('kst', sl, q_) for q_ in range(5)] + ['ident_f'], [pk(tb)])
                    if j % 2 == 0:
                        P.op('act', lambda e, j=j, tb=tb: e.activation(out=kTb[:, :, j, :], in_=bank(tb).rearrange("p (h k) -> p h k", h=4), func=AF.Copy),
                             [pk(tb)], [('kTb', j)])
                    else:
                        P.op('dve', lambda e, j=j, tb=tb: e.tensor_copy(out=kTb[:, :, j, :], in_=bank(tb).rearrange("p (h k) -> p h k", h=4)),
                             [pk(tb)], [('kTb', j)])
                sbk = b % 2

                def scs(e, b=b, sbk=sbk):
                    ins = None
                    o = bank(sbk)[:, 0:336]
                    e.matmul(out=o, lhsT=ident_b, rhs=smaskb, start=True, stop=False)
                    for j in range(7):
                        for h in range(4):
                            ins = e.matmul(out=bank(sbk)[:, j * 48 + h * 12:j * 48 + (h + 1) * 12], lhsT=kTb[:, h, j, :], rhs=qs_all[:, h, b, :],
                                           start=False, stop=(j == 6 and h == 3))
                    return ins
                P.op('pe', scs, ['ident_b', 'smaskb'] + [('kTb', j) for j in range(7)], [pk(sbk)])
                P.op('act', lambda e, sbk=sbk: e.activation(out=PTb[sbk].rearrange("p j x -> p (j x)"), in_=bank(sbk)[:, 0:336],
                                                            func=AF.Exp, scale=ISQ128), [pk(sbk)], [('PTb', sbk)])

                def pvs(e, b=b, sl=sl, sbk=sbk):
                    ins = None
                    last = (b == NSB - 1)
                    for h in range(4):
                        o = bank(4 + h // 2)[:, (h % 2) * 192 + b * 12:(h % 2) * 192 + (b + 1) * 12]
                        for j in range(7):
                            ins = e.matmul(out=o, lhsT=vsb[sl][:, j, h * 128:(h + 1) * 128], rhs=PTb[sbk][:, j, h * 12:(h + 1) * 12],
                                           start=False, stop=(last and j == 6), skip_group_check=True)
                    d = bank(6 + b // 8)[:, (b % 8) * 48:(b % 8 + 1) * 48]
                    for j in range(7):
                        ins = e.matmul(out=d, lhsT=ones_b, rhs=PTb[sbk][:, j, :], start=False, stop=(last and j == 6), skip_group_check=True)
                    return ins
                P.op('pe', pvs, [('PTb', sbk), ('vsb', sl), 'ones_b'], [pk(4), pk(5), pk(6), pk(7)])
            osum = A.alloc([4, NSB, 4], F32)
            dsum = A.alloc([4, NSB, 4], F32)
            for hp in range(2):
                ov = bank(4 + hp)[:, 0:384].rearrange("p (a b g t) -> p a b g t", a=2, b=NSB, g=3)
                P.op('dve', lambda e, hp=hp, ov=ov: e.tensor_copy(out=osum[:, hp * 2:hp * 2 + 2], in_=ov[:, :, :, 0, :]),
                     [pk(4 + hp)], [('osum', hp)])
                P.op('dve', lambda e, hp=hp, ov=ov: e.tensor_tensor(out=osum[:, hp * 2:hp * 2 + 2], in0=osum[:, hp * 2:hp * 2 + 2], in1=ov[:, :, :, 1, :], op=ALU.add),
                     [pk(4 + hp), ('osum', hp)], [('osum', hp)])
                P.op('dve', lambda e, hp=hp, ov=ov: e.tensor_tensor(out=osum[:, hp * 2:hp * 2 + 2], in0=osum[:, hp * 2:hp * 2 + 2], in1=ov[:, :, :, 2, :], op=ALU.add),
                     [pk(4 + hp), ('osum', hp)], [('osum', hp)])
            for half in range(2):
                dv_ = bank(6 + half)[:, 0:384].rearrange("p (b h g t) -> p h b g t", b=8, h=4, g=3)
                ds_ = dsum[:, :, half * 8:half * 8 + 8, :]
                P.op('dve', lambda e, dv_=dv_, ds_=ds_: e.tensor_copy(out=ds_, in_=dv_[:, :, :, 0, :]),
                     [pk(6 + half)], [('dsum', half)])
                P.op('dve', lambda e, dv_=dv_, ds_=ds_: e.tensor_tensor(out=ds_, in0=ds_, in1=dv_[:, :, :, 1, :], op=ALU.add),
                     [pk(6 + half), ('dsum', half)], [('dsum', half)])
                P.op('dve', lambda e, dv_=dv_, ds_=ds_: e.tensor_tensor(out=ds_, in0=ds_, in1=dv_[:, :, :, 2, :], op=ALU.add),
                     [pk(6 + half), ('dsum', half)], [('dsum', half)])
            P.op('dve', lambda e: e.reciprocal(out=dsum, in_=dsum), [('dsum', 0), ('dsum', 1)], ['dsr'])
            P.op('dve', lambda e: e.tensor_tensor(out=aoT[:, :, S:NT].rearrange("p h (b t) -> p h b t", t=4), in0=osum, in1=dsum, op=ALU.mult),
                 ['dsr', ('osum', 0), ('osum', 1)], ['aoTs'])
        P.barrier()
        A.off = mark

        if DBG:
            for i_, t_ in enumerate((aoT, cT, moT)):
                P.dma('pool', 'dbg', dbg[i_], t_.rearrange("p h t -> p (h t)"), writes=[('dbg', i_)])
            P.barrier()
        if STOP >= 6:
            mgT = A.alloc([8, NT], BF16)
            wo = A.alloc([8, DM], BF16)
            mark5 = A.off
            wj = [A.alloc([8, 384], BF16) for _ in range(2)]
            wpj = [A.alloc([4, 384], BF16) for _ in range(2)]
            sg3 = [A.alloc([512], F32) for _ in range(3)]
            t3 = [A.alloc([512], F32) for _ in range(2)]
            P.dma('pool', 'wo', wo, w_out.rearrange("(k p) f -> p k f", p=128), writes=['wo'])

            def load_j(j):
                sl = j % 2
                for br_ in range(3):
                    P.dma('pool', 'wj%d' % sl, wj[sl][:, :, br_ * 128:(br_ + 1) * 128],
                          w_in[:, 4096 + br_ * 1024 + j * 128:4096 + br_ * 1024 + (j + 1) * 128].rearrange("(p k) f -> p k f", k=8),
                          writes=[('wj', sl, br_)])
                for br_, wp in enumerate((w_attn_proj, w_conv_proj, w_mem_proj)):
                    P.dma('pool', 'wj%d' % sl, wpj[sl][:, :, br_ * 128:(br_ + 1) * 128],
                          wp[:, j * 128:(j + 1) * 128].rearrange("(c p) f -> p c f", p=128), writes=[('wpj', sl, br_)])
            load_j(0)
            brT = (aoT, cT, moT)
            for j in range(8):
                if j + 1 < 8:
                    load_j(j + 1)
                sl = j % 2
                for gi, (t0, n) in enumerate(TG):
                    for br_ in range(3):
                        mm(bank(br_)[:, 0:n], [(wj[sl][:, k, br_ * 128:(br_ + 1) * 128], hT[:, k, t0:t0 + n]) for k in range(8)],
                           [('wj', sl, br_)], [pk(br_)])
                        mm(bank(3 + br_)[:, 0:n], [(wpj[sl][:, c, br_ * 128:(br_ + 1) * 128], brT[br_][:, c, t0:t0 + n]) for c in range(4)],
                           [('wpj', sl, br_)], [pk(3 + br_)])
                        P.op('act', lambda e, br_=br_, n=n: e.activation(out=sg3[br_][:, 0:n], in_=bank(br_)[:, 0:n], func=AF.Sigmoid),
                             [pk(br_)], [('sg3', br_)])
                    P.op('dve', lambda e, n=n: e.tensor_tensor(out=t3[0][:, 0:n], in0=bank(3)[:, 0:n], in1=sg3[0][:, 0:n], op=ALU.mult),
                         [pk(3), ('sg3', 0)], [('t3', 0)])
                    P.op('dve', lambda e, n=n: e.tensor_tensor(out=t3[1][:, 0:n], in0=bank(4)[:, 0:n], in1=sg3[1][:, 0:n], op=ALU.mult),
                         [pk(4), ('sg3', 1)], [('t3', 1)])
                    P.op('dve', lambda e, n=n: e.tensor_tensor(out=t3[0][:, 0:n], in0=t3[0][:, 0:n], in1=t3[1][:, 0:n], op=ALU.add),
                         [('t3', 0), ('t3', 1)], [('t3', 0)])
                    P.op('dve', lambda e, n=n: e.tensor_tensor(out=t3[1][:, 0:n], in0=bank(5)[:, 0:n], in1=sg3[2][:, 0:n], op=ALU.mult),
                         [pk(5), ('sg3', 2)], [('t3', 1)])
                    P.op('dve', lambda e, n=n, j=j, t0=t0: e.tensor_tensor(out=mgT[:, j, t0:t0 + n], in0=t3[0][:, 0:n], in1=t3[1][:, 0:n], op=ALU.add),
                         [('t3', 0), ('t3', 1)], [('mgT', j, gi)])
            P.barrier()
            A.off = mark5
            xt2 = [A.alloc([DM], F32) for _ in range(2)]
            x1t = [A.alloc([DM], F32) for _ in range(2)]
            sqt5 = [A.alloc([DM], F32) for _ in range(2)]
            xn5 = [A.alloc([DM], F32) for _ in range(2)]
            ss5 = [A.alloc([8], F32) for _ in range(2)]

            def t5(ti, t0, rows, sl):
                P.dma('sp', 'x%d' % sl, xt2[sl][0:rows, :], xin[t0:t0 + rows, :], writes=[('xt', sl)])
                for half in range(2):
                    bw = half + 2 * sl
                    mm(bank(bw)[0:rows, :], [(mgT[:, k, t0:t0 + rows], wo[:, k, half * 512:(half + 1) * 512]) for k in range(8)],
                       ['wo'], [pk(bw)])
                    P.op('dve', lambda e, half=half, sl=sl, rows=rows, bw=bw: e.tensor_tensor(
                        out=x1t[sl][0:rows, half * 512:(half + 1) * 512], in0=bank(bw)[0:rows, :],
                        in1=xt2[sl][0:rows, half * 512:(half + 1) * 512], op=ALU.add), [pk(bw), ('xt', sl)], [('x1t', sl, half)])
                P.dma('sp', 'x1o%d' % sl, x1s[t0:t0 + rows, :], x1t[sl][0:rows, :], reads=[('x1t', sl, 0), ('x1t', sl, 1)], writes=[('x1s', ti)])
                norm_transpose(x1t[sl][0:rows, :], rows, g_ffn, 'g_ffn', hT[:, :, t0:t0 + rows], [('hT', ti)],
                               'n5_%d' % sl, [('x1t', sl, 0), ('x1t', sl, 1)], sqt5[sl], ss5[sl], xn5[sl], bb=(6 if sl == 0 else 4))
            for tp in range(0, 17, 2):
                caps = []
                for ti in (tp, tp + 1):
                    if ti < 17:
                        P.begin_capture()
                        t5(ti, TT[ti][0], TT[ti][1], ti % 2)
                        caps.append(P.end_capture())
                P.replay(caps)
        P.barrier()
        A.off = mark0
        h2T = hT

        if STOP >= 7:
            yacc = A.alloc([17, DM], F32)
            gates = A.alloc([17, 32], F32)
            wg = [A.alloc([8, 512], BF16) for _ in range(2)]
            wu = [A.alloc([8, 512], BF16) for _ in range(2)]
            wd = [A.alloc([4, DM], BF16) for _ in range(2)]
            lgt = A.alloc([36], F32)
            rt = A.alloc([16, 8], F32)

            def load_e(ei):
                sl = ei % 2
                P.dma('pool', 'we%d' % sl, wg[sl], w_eg[ei].rearrange("(p k) f -> p k f", k=8), writes=[('wg', sl)])
                P.dma('pool', 'we%d' % sl, wu[sl], w_eu[ei].rearrange("(p k) f -> p k f", k=8), writes=[('wu', sl)])
                P.dma('pool', 'we%d' % sl, wd[sl], w_ed[ei].rearrange("(c p) f -> p c f", p=128), writes=[('wd', sl)])
            load_e(0)
            ssR = [A.alloc([8], F32) for _ in range(2)]
            lgtR = [lgt, A.alloc([36], F32)]
            rtR = [rt, A.alloc([16, 8], F32)]
            mark6 = A.off
            xnR = [A.alloc([DM], F32) for _ in range(2)]
            sqtR = [A.alloc([DM], F32) for _ in range(2)]
            h2fR = [A.alloc([8, 128], F32) for _ in range(2)]
            P.dma('sp', 'ya0', yacc[:, 0:16, :], x1s[0:S, :].rearrange("(t p) d -> p t d", p=128), writes=[('yacc', ti) for ti in range(16)])
            P.dma('sp', 'ya1', yacc[0:NS, 16, :], x1s[S:NT, :], writes=[('yacc', 16)])
            def rtile(ti, t0, rows, sl):
                xn_, sqt_, ss_, h2f_, lgt_, rt_ = xnR[sl], sqtR[sl], ssR[sl], h2fR[sl], lgtR[sl], rtR[sl]
                b5 = 5 if sl == 0 else 2
                rms_rstd(yacc[0:rows, ti, :], rows, DM, sqt_, ss_, 'n6_%d' % sl, [('yacc', ti)])
                P.op('dve', lambda e, rows=rows, ti=ti: e.tensor_scalar(out=xn_[0:rows, :], in0=yacc[0:rows, ti, :], scalar1=ss_[0:rows, 0:1], scalar2=32.0,
                                                                         op0=ALU.mult, op1=ALU.mult), [('yacc', ti), 'n6_%dss' % sl], [('n6xn', sl)])
                xv = xn_[0:rows, :].rearrange("t (p k) -> t k p", k=8)
                for half in range(2):
                    bi = (6 + half) if sl == 0 else (3 + half)
                    P.op('pe', lambda e, half=half, bi=bi, rows=rows, xv=xv: [e.transpose(
                        out=bank(bi)[:, kk * 128:kk * 128 + rows], in_=xv[:, half * 4 + kk, :], identity=ident_f[0:rows, 0:rows])
                        for kk in range(4)][-1], [('n6xn', sl), 'ident_f'], [pk(bi)])
                    P.op('dve', lambda e, half=half, bi=bi, rows=rows: e.tensor_tensor(
                        out=h2f_[:, half * 4:half * 4 + 4, 0:rows], in0=bank(bi).rearrange("p (k t) -> p k t", k=4)[:, :, 0:rows],
                        in1=g_ffn[:, half * 4:half * 4 + 4].unsqueeze(2).to_broadcast([128, 4, rows]), op=ALU.mult),
                        [pk(bi), 'g_ffn'], [('h2f', sl, half)])
                mm(bank(b5)[0:rows, 0:36], [(h2f_[:, k, 0:rows], wr[:, k, :]) for k in range(8)], [('h2f', sl, 0), ('h2f', sl, 1), 'wr'], [pk(b5)])
                def router_tile(ti, rows):
                    R = rows
                    P.op('dve', lambda e, R=R: e.tensor_tensor(out=lgt_[0:R, :], in0=bank(b5)[0:R, 0:36], in1=br[0:R, :], op=ALU.add), [pk(b5), 'br'], [('lgt', sl)])
                    mx = rt_[0:R, 0, 0:1]; gm = rt_[0:R, 1, 0:4]; sme = rt_[0:R, 0, 1:2]; pgt = rt_[0:R, 0, 2:3]
                    ex4 = rt_[0:R, 2, 0:4]; les = rt_[0:R, 3, :]; m1 = rt_[0:R, 0, 3:4]; oh1 = rt_[0:R, 4, :]; le2 = rt_[0:R, 5, :]
                    m2 = rt_[0:R, 0, 4:5]; oh2 = rt_[0:R, 6, :]; dm_ = rt_[0:R, 0, 5:6]; e21 = rt_[0:R, 0, 6:7]; w1 = rt_[0:R, 0, 7:8]
                    w2 = rt_[0:R, 7, 0:1]; g8 = rt_[0:R, 8, :]; tmp8 = rt_[0:R, 9, :]; den = rt_[0:R, 7, 1:2]
                    K = ('rt', sl)
                    seq = [
                        ('dve', lambda e: e.reduce_max(out=mx, in_=lgt_[0:R, 0:4], axis=AX.X)),
                        ('dve', lambda e: e.tensor_scalar(out=gm, in0=lgt_[0:R, 0:4], scalar1=mx, scalar2=None, op0=ALU.is_equal)),
                        ('dve', lambda e: e.tensor_scalar(out=ex4, in0=lgt_[0:R, 0:4], scalar1=mx, scalar2=None, op0=ALU.subtract)),
                        ('act', lambda e: e.activation(out=ex4, in_=ex4, func=AF.Exp)),
                        ('dve', lambda e: e.reduce_sum(out=sme, in_=ex4, axis=AX.X)),
                        ('dve', lambda e: e.reciprocal(out=pgt, in_=sme)),
                        ('dve', lambda e: e.tensor_scalar(out=les, in0=lgt_[0:R, 4:12], scalar1=gm[:, 0:1], scalar2=None, op0=ALU.mult)),
                    ] + [
                        ('dve', (lambda g: (lambda e: e.scalar_tensor_tensor(out=les, in0=lgt_[0:R, 4 + g * 8:12 + g * 8], scalar=gm[:, g:g + 1], in1=les,
                                                                             op0=ALU.mult, op1=ALU.add)))(g)) for g in range(1, 4)
                    ] + [
                        ('dve', lambda e: e.reduce_max(out=m1, in_=les, axis=AX.X)),
                        ('dve', lambda e: e.tensor_scalar(out=oh1, in0=les, scalar1=m1, scalar2=None, op0=ALU.is_equal)),
                        ('dve', lambda e: e.scalar_tensor_tensor(out=le2, in0=oh1, scalar=-1e30, in1=les, op0=ALU.mult, op1=ALU.add)),
                        ('dve', lambda e: e.reduce_max(out=m2, in_=le2, axis=AX.X)),
                        ('dve', lambda e: e.tensor_scalar(out=oh2, in0=le2, scalar1=m2, scalar2=None, op0=ALU.is_equal)),
                        ('dve', lambda e: e.tensor_tensor(out=dm_, in0=m2, in1=m1, op=ALU.subtract)),
                        ('act', lambda e: e.activation(out=e21, in_=dm_, func=AF.Exp)),
                        ('dve', lambda e: e.tensor_scalar(out=den, in0=e21, scalar1=1.0, scalar2=None, op0=ALU.add)),
                        ('dve', lambda e: e.reciprocal(out=den, in_=den)),
                        ('dve', lambda e: e.tensor_tensor(out=w1, in0=pgt, in1=den, op=ALU.mult)),
                        ('dve', lambda e: e.tensor_tensor(out=w2, in0=w1, in1=e21, op=ALU.mult)),
                        ('dve', lambda e: e.tensor_scalar(out=g8, in0=oh1, scalar1=w1, scalar2=None, op0=ALU.mult)),
                        ('dve', lambda e: e.scalar_tensor_tensor(out=g8, in0=oh2, scalar=w2, in1=g8, op0=ALU.mult, op1=ALU.add)),
                    ] + [
                        ('dve', (lambda g, ti=ti: (lambda e: e.tensor_scalar(out=gates[0:R, ti, g * 8:(g + 1) * 8], in0=g8, scalar1=gm[:, g:g + 1], scalar2=None,
                                                                              op0=ALU.mult)))(g)) for g in range(4)
                    ]
                    for eng_, fn_ in seq:
                        P.op(eng_, fn_, [('lgt', sl), K], [K, ('gates', ti)])

                router_tile(ti, rows)
            for tp in range(0, 17, 2):
                caps = []
                for ti in (tp, tp + 1):
                    if ti < 17:
                        P.begin_capture()
                        rtile(ti, TT[ti][0], TT[ti][1], ti % 2)
                        caps.append(P.end_capture())
                P.replay(caps)
            P.barrier()
            A.off = mark6
            hid = A.alloc([4, NT], BF16)
            sgt = [A.alloc([512], F32) for _ in range(2)]
            cnt = 0
            for ei in range(NEXP):
                if ei + 1 < NEXP:
                    load_e(ei + 1)
                sl = ei % 2
                for gi, (t0, n) in enumerate(TG):
                    for c in range(4):
                        bg, bu = (cnt % 2) * 2, (cnt % 2) * 2 + 1
                        sg = sgt[cnt % 2]
                        sgk = ('sgt', cnt % 2)
                        cnt += 1
                        mm(bank(bg)[:, 0:n], [(wg[sl][:, k, c * 128:(c + 1) * 128], h2T[:, k, t0:t0 + n]) for k in range(8)], [('wg', sl)], [pk(bg)])
                        mm(bank(bu)[:, 0:n], [(wu[sl][:, k, c * 128:(c + 1) * 128], h2T[:, k, t0:t0 + n]) for k in range(8)], [('wu', sl)], [pk(bu)])
                        P.op('act', lambda e, bg=bg, n=n, sg=sg: e.activation(out=sg[:, 0:n], in_=bank(bg)[:, 0:n], func=AF.Silu), [pk(bg)], [sgk])
                        P.op('dve', lambda e, bu=bu, n=n, sg=sg, c=c, t0=t0: e.tensor_tensor(out=hid[:, c, t0:t0 + n], in0=bank(bu)[:, 0:n], in1=sg[:, 0:n],
                                                                                         op=ALU.mult), [pk(bu), sgk], [('hid', gi, c)])
                for ti, (t0, rows) in enumerate(TT):
                    gi = min(ti // 4, 4)
                    for half in range(2):
                        bo = 4 + ((ti * 2 + half) % 4)
                        mm(bank(bo)[0:rows, :], [(hid[:, c, t0:t0 + rows], wd[sl][:, c, half * 512:(half + 1) * 512]) for c in range(4)],
                           [('wd', sl)] + [('hid', gi, c) for c in range(4)], [pk(bo)])
                        eng_ = 'dve' if (half == 0 or ti % 2 == 0) else 'pool'
                        eng_ = 'dve'
                        P.op(eng_, lambda e, bo=bo, rows=rows, ti=ti, half=half, ei=ei: e.scalar_tensor_tensor(
                            out=yacc[0:rows, ti, half * 512:(half + 1) * 512], in0=bank(bo)[0:rows, :], scalar=gates[0:rows, ti, ei:ei + 1],
                            in1=yacc[0:rows, ti, half * 512:(half + 1) * 512], op0=ALU.mult, op1=ALU.add),
                            [pk(bo), ('gates', ti), ('yacc', ti)], [('yacc', ti)])
            for ti, (t0, rows) in enumerate(TT):
                P.dma('sp', 'o_y', y[t0:t0 + rows, :], yacc[0:rows, ti, :], reads=[('yacc', ti)], writes=[('y', ti)])
        P.final_wait_all_dma('sp')
        P.emit(nc, st)
    return nc


def _consts():
    half = 16
    inv_freq = np.power(np.float32(500000.0), -np.arange(half, dtype=np.float32) * np.float32(2.0 / 32)).astype(np.float32)
    pos = np.zeros(17 * 128, np.float32)
    pos[:S] = np.arange(S)
    pos[S:S + NS] = 2048 + (np.arange(NS) % 4)
    ang = pos[:, None].astype(np.float32) * inv_freq[None, :]
    cs = np.concatenate([np.cos(ang), np.sin(ang)], axis=1).astype(np.float32)
    kp = np.arange(128)[:, None]
    qf = np.arange(128)[None, :]
    mask = np.full((128, 256), NEG, np.float32)
    mask[:, 0:128][kp >= qf] = 0.0
    mask[:, 128:256][kp <= qf] = 0.0
    sm = np.full((128, 7, 3, 4), NEG, np.float32)
    for j in range(7):
        for p in range(128):
            if j < 4:
                R = 1536 + 128 * j + p
                for t in range(4):
                    if R >= 1920 + t:
                        sm[p, j, 0, t] = 0.0
                    if R % 4 == t:
                        sm[p, j, 1, t] = 0.0
                    if R % 16 == t:
                        sm[p, j, 2, t] = 0.0
            else:
                w = p // 32
                sm[p, j, 2, w] = 0.0
    smask = np.repeat(sm.reshape(128, 7, 1, 12), 4, axis=2).reshape(128, 7 * 48)
    nm = np.full((64, 16, 3, 4), NEG, np.float32)
    for b in range(16):
        for tp in range(4):
            for t in range(4):
                if tp <= t:
                    nm[b * 4 + tp, b, 0, t] = 0.0
                if tp == t:
                    nm[b * 4 + tp, b, 1, t] = 0.0
                    nm[b * 4 + tp, b, 2, t] = 0.0
    return dict(c_ident=np.eye(128, dtype=np.float32), c_cs=cs, c_mask=mask, c_smask=np.ascontiguousarray(smask),
                c_nmask=np.ascontiguousarray(nm.reshape(64, 192)))


_NC = None


def kernel(x_prompt, x_sample, mem_prompt, cache_k, cache_v, state_conv, cache_mem_k, cache_mem_v,
           norm_mix_g, w_in, q_norm_g, k_norm_g, conv_w, conv_b, conv_ln_g, conv_ln_b,
           mem_norm_g, w_mem_kv, mq_norm_g, mk_norm_g, w_attn_proj, w_conv_proj, w_mem_proj, w_out,
           norm_ffn_g, w_router_group, b_router_group, w_router_expert, b_router_expert,
           w_expert_gate, w_expert_up, w_expert_down):
    global _NC
    f = lambda a: np.ascontiguousarray(np.asarray(a, dtype=np.float32))
    x_prompt, x_sample, mem_prompt = f(x_prompt), f(x_sample), f(mem_prompt)
    cache_k, cache_v, state_conv = f(cache_k), f(cache_v), f(state_conv)
    cache_mem_k, cache_mem_v = f(cache_mem_k), f(cache_mem_v)
    wre = np.transpose(f(w_router_expert)[0], (1, 0, 2)).reshape(DM, 32)
    w_router = np.ascontiguousarray(np.concatenate([f(w_router_group)[0], wre], axis=1))
    b_router = np.ascontiguousarray(np.concatenate([f(b_router_group)[0], f(b_router_expert)[0].reshape(32)]))
    shared = dict(
        norm_mix_g=f(norm_mix_g)[0], w_in=f(w_in)[0], q_norm_g=f(q_norm_g)[0], k_norm_g=f(k_norm_g)[0],
        conv_wT=np.ascontiguousarray(f(conv_w)[0].T), conv_b=np.ascontiguousarray(f(conv_b)[0].reshape(4, 128).T), conv_ln_g=np.ascontiguousarray(f(conv_ln_g)[0].reshape(4, 128).T),
        conv_ln_b=np.ascontiguousarray(f(conv_ln_b)[0].reshape(4, 128).T),
        mem_norm_g=f(mem_norm_g)[0], w_mem_kv=f(w_mem_kv)[0], mq_norm_g=f(mq_norm_g)[0], mk_norm_g=f(mk_norm_g)[0],
        w_attn_proj=f(w_attn_proj)[0], w_conv_proj=f(w_conv_proj)[0], w_mem_proj=f(w_mem_proj)[0], w_out=f(w_out)[0],
        norm_ffn_g=f(norm_ffn_g)[0], w_router=w_router, b_router=b_router,
        w_eg=f(w_expert_gate)[0], w_eu=f(w_expert_up)[0], w_ed=f(w_expert_down)[0])
    shared.update(_consts())
    in_maps = []
    for c in range(NCORES):
        m = dict(shared)
        bs = slice(c * NSB, (c + 1) * NSB)
        m["xin"] = np.ascontiguousarray(np.concatenate([x_prompt[c], x_sample[bs].reshape(NS, DM)], axis=0))
        m["mem"] = mem_prompt[c]
        m["cache_k"] = cache_k[0, bs]
        m["cache_v"] = cache_v[0, bs]
        m["state_conv"] = state_conv[0, bs]
        m["cmk"] = cache_mem_k[0, bs]
        m["cmv"] = cache_mem_v[0, bs]
        in_maps.append(m)
    if os.environ.get("MK_ONLY_MAPS"):
        return in_maps
    if _NC is None:
        _NC = build_nc()
    res = run_bass_kernel_spmd(_NC, in_maps, core_ids=list(range(NCORES)))
    R = res.results
    y_prompt = np.stack([R[c]["y"][:S] for c in range(NCORES)], 0)
    y_sample = np.concatenate([R[c]["y"][S:].reshape(NSB, 4, DM) for c in range(NCORES)], 0)
    st = lambda k: np.stack([R[c][k] for c in range(NCORES)], 0)[None]
    ct = lambda k: np.concatenate([R[c][k] for c in range(NCORES)], 0)[None]
    return (y_prompt, y_sample, st("kwp"), st("vwp"), st("convp"), st("mkp"), st("mvp"), ct("kws"), ct("vws"), ct("convs"))
```
